# Optimizing a Trainium2 kernel written in Bass

```python
import jax, jax.numpy as jnp
from jax import lax
import numpy as np

D_MODEL = 1024
BATCH = 8
SEQ = 2048
DEPTH = 1

CHUNK = 64
A_WIDTH = D_MODEL // 2
A_HEAD = 64
A_HEADS = A_WIDTH // A_HEAD
D_DECAY_LORA = 64
D_AAA_LORA = 64
D_GATE_LORA = 160
LNX_EPS = 64e-5
B_WIDTH = D_MODEL // 2
B_GROUPS = 4
B_GROUP_CH = B_WIDTH // B_GROUPS
GMLP_BLOCK = 2 * CHUNK
LN_EPS = 1e-5
A_COLS = 3 * A_WIDTH + D_DECAY_LORA + D_AAA_LORA + D_GATE_LORA
B_COLS = 2 * B_WIDTH
GATE_COLS = 2 * D_MODEL
IN_COLS = A_COLS + B_COLS + GATE_COLS
N_GROUPS = 4
EXPERTS_PER_GROUP = 8
N_EXPERTS = N_GROUPS * EXPERTS_PER_GROUP
TOP_K_IN_GROUP = 2
D_EXPERT = 256
NORM_EPS = 1e-6

kernel_name = "hybrid_rwkv7_gmlp_hmoe_block"


def rms_norm(x, g):
    xf = x.astype(jnp.float32)
    y = xf * lax.rsqrt(jnp.mean(xf * xf, axis=-1, keepdims=True) + NORM_EPS)
    return (y * g.astype(jnp.float32)).astype(x.dtype)


def token_shift(p):
    return jnp.pad(p, ((0, 0), (1, 0), (0, 0)))[:, :-1]


def wkv7(r, dec, k, v, a, b):
    bsz, _, nh, n = r.shape

    def step(S, inp):
        r_t, d_t, k_t, v_t, a_t, b_t = inp
        sa = jnp.einsum('bhij,bhj->bhi', S, a_t)
        S = (S * d_t[:, :, None, :]
             + sa[..., :, None] * b_t[..., None, :]
             + v_t[..., :, None] * k_t[..., None, :])
        return S, jnp.einsum('bhij,bhj->bhi', S, r_t)

    xs = (jnp.moveaxis(r, 1, 0), jnp.moveaxis(dec, 1, 0), jnp.moveaxis(k, 1, 0),
          jnp.moveaxis(v, 1, 0), jnp.moveaxis(a, 1, 0), jnp.moveaxis(b, 1, 0))
    S0 = jnp.zeros((bsz, nh, n, n), jnp.float32)
    _, ys = lax.scan(step, S0, xs)
    return jnp.moveaxis(ys, 0, 1)


def rwkv7_branch(p, mu, w0, w2, a0, a2, g2, k_k, k_a, r_k, lnx_g, lnx_b, w_o):
    bsz, t, _ = p.shape
    pf = p.astype(jnp.float32)
    pm = pf + mu * (token_shift(pf) - pf)
    s0 = A_WIDTH
    s1 = 2 * A_WIDTH
    s2 = 3 * A_WIDTH
    s3 = s2 + D_DECAY_LORA
    s4 = s3 + D_AAA_LORA
    r, k, v, xw, xa, xg = jnp.split(pm, [s0, s1, s2, s3, s4], axis=-1)
    w = -jax.nn.softplus(-(w0 + jnp.tanh(xw) @ w2)) - 0.5
    a = jax.nn.sigmoid(a0 + xa @ a2)
    g = jax.nn.sigmoid(xg) @ g2
    hs = (bsz, t, A_HEADS, A_HEAD)
    kk = (k * k_k).reshape(hs)
    kk = kk / jnp.maximum(jnp.sqrt(jnp.sum(kk * kk, axis=-1, keepdims=True)), 1e-12)
    k = k * (1.0 + (a - 1.0) * k_a)
    r_h = r.reshape(hs)
    k_h = k.reshape(hs)
    v_h = v.reshape(hs)
    a_h = a.reshape(hs)
    dec = jnp.exp(-jnp.exp(w)).reshape(hs)
    y = wkv7(r_h, dec, k_h, v_h, -kk, kk * a_h)
    mean = jnp.mean(y, axis=-1, keepdims=True)
    var = jnp.mean(jnp.square(y - mean), axis=-1, keepdims=True)
    y = ((y - mean) * lax.rsqrt(var + LNX_EPS)).reshape(bsz, t, A_WIDTH) * lnx_g + lnx_b
    bonus = jnp.sum(r_h * k_h * r_k, axis=-1, keepdims=True) * v_h
    y = y + bonus.reshape(bsz, t, A_WIDTH)
    return ((y * g).astype(p.dtype)) @ w_o


def gmlp_branch(p, lnv_g, lnv_b, w_s, b_s, w_o):
    bsz, t, _ = p.shape
    z = jax.nn.gelu(p, approximate=False)
    u, v = jnp.split(z, 2, axis=-1)
    vf = v.astype(jnp.float32)
    mean = jnp.mean(vf, axis=-1, keepdims=True)
    var = jnp.mean(jnp.square(vf - mean), axis=-1, keepdims=True)
    vf = (vf - mean) * lax.rsqrt(var + LN_EPS) * lnv_g + lnv_b
    vb = vf.reshape(bsz, t // GMLP_BLOCK, GMLP_BLOCK, B_GROUPS, B_GROUP_CH)
    mask = jnp.tril(jnp.ones((GMLP_BLOCK, GMLP_BLOCK), dtype=bool))
    ws = jnp.where(mask[None], w_s, jnp.zeros_like(w_s)).astype(jnp.float32)
    sv = jnp.einsum('gts,bcsgd->bctgd', ws, vb) + b_s.T.astype(jnp.float32)[:, :, None]
    sv = sv.reshape(bsz, t, B_WIDTH).astype(u.dtype)
    return (u * sv) @ w_o


def hier_moe(h, w_rg, b_rg, w_re, b_re, w_gate, w_up, w_down):
    bsz, t, d = h.shape
    hf = h.reshape(bsz * t, d)
    n_tok = bsz * t
    g_prob = jax.nn.softmax((hf @ w_rg + b_rg).astype(jnp.float32), axis=-1)
    g_p, g_idx = lax.top_k(g_prob, 1)
    e_logits = (jnp.einsum('nd,gde->nge', hf, w_re) + b_re).astype(jnp.float32)
    e_sel = jnp.take_along_axis(e_logits, g_idx[:, :, None], axis=1)[:, 0]
    e_val, e_idx = lax.top_k(e_sel, TOP_K_IN_GROUP)
    e_w = jax.nn.softmax(e_val, axis=-1) * g_p
    w_grp = jnp.sum(jax.nn.one_hot(e_idx, EXPERTS_PER_GROUP, dtype=jnp.float32) * e_w[..., None], axis=1)
    combine = (jax.nn.one_hot(g_idx[:, 0], N_GROUPS, dtype=jnp.float32)[:, :, None]
               * w_grp[:, None, :]).reshape(n_tok, N_EXPERTS).astype(h.dtype)
    y = jnp.zeros_like(hf)
    for e in range(N_EXPERTS):
        act = jax.nn.silu(hf @ w_gate[e]) * (hf @ w_up[e])
        y = y + combine[:, e:e + 1] * (act @ w_down[e])
    return y.reshape(bsz, t, d)


def setup_inputs(seed: int = 0) -> dict:
    key = jax.random.key(seed)
    ks = jax.random.split(key, 32)
    L = DEPTH

    def nrm(k, shape, scale):
        return jax.random.normal(k, shape, jnp.float32) * scale

    return {
        "x": nrm(ks[0], (BATCH, SEQ, D_MODEL), 1.0),
        "norm1_g": 1.0 + nrm(ks[1], (L, D_MODEL), 0.02),
        "w_in": nrm(ks[2], (L, D_MODEL, IN_COLS), D_MODEL ** -0.5),
        "b_gate": nrm(ks[3], (L, GATE_COLS), 0.02),
        "tmix_mu": jax.random.uniform(ks[4], (L, A_COLS), jnp.float32),
        "w0": jax.random.uniform(ks[5], (L, A_WIDTH), jnp.float32, minval=-6.0, maxval=1.0),
        "w2": nrm(ks[6], (L, D_DECAY_LORA, A_WIDTH), 0.1 * D_DECAY_LORA ** -0.5),
        "a0": nrm(ks[7], (L, A_WIDTH), 0.1),
        "a2": nrm(ks[8], (L, D_AAA_LORA, A_WIDTH), 0.1 * D_AAA_LORA ** -0.5),
        "g2": nrm(ks[9], (L, D_GATE_LORA, A_WIDTH), D_GATE_LORA ** -0.5),
        "k_k": 0.85 + nrm(ks[10], (L, A_WIDTH), 0.02),
        "k_a": 1.0 + nrm(ks[11], (L, A_WIDTH), 0.02),
        "r_k": nrm(ks[12], (L, A_HEADS, A_HEAD), 0.1),
        "lnx_g": 1.0 + nrm(ks[13], (L, A_WIDTH), 0.02),
        "lnx_b": nrm(ks[14], (L, A_WIDTH), 0.02),
        "w_oA": nrm(ks[15], (L, A_WIDTH, D_MODEL), A_WIDTH ** -0.5),
        "lnv_g": 1.0 + nrm(ks[16], (L, B_WIDTH), 0.02),
        "lnv_b": nrm(ks[17], (L, B_WIDTH), 0.02),
        "w_s": nrm(ks[18], (L, B_GROUPS, GMLP_BLOCK, GMLP_BLOCK), GMLP_BLOCK ** -0.5),
        "b_s": 1.0 + nrm(ks[19], (L, B_GROUPS, GMLP_BLOCK), 0.02),
        "w_oB": nrm(ks[20], (L, B_WIDTH, D_MODEL), B_WIDTH ** -0.5),
        "w_out": nrm(ks[21], (L, D_MODEL, D_MODEL), D_MODEL ** -0.5),
        "norm2_g": 1.0 + nrm(ks[22], (L, D_MODEL), 0.02),
        "w_rg": nrm(ks[23], (L, D_MODEL, N_GROUPS), D_MODEL ** -0.5),
        "b_rg": nrm(ks[24], (L, N_GROUPS), 0.01),
        "w_re": nrm(ks[25], (L, N_GROUPS, D_MODEL, EXPERTS_PER_GROUP), D_MODEL ** -0.5),
        "b_re": nrm(ks[26], (L, N_GROUPS, EXPERTS_PER_GROUP), 0.01),
        "w_e_gate": nrm(ks[27], (L, N_EXPERTS, D_MODEL, D_EXPERT), D_MODEL ** -0.5),
        "w_e_up": nrm(ks[28], (L, N_EXPERTS, D_MODEL, D_EXPERT), D_MODEL ** -0.5),
        "w_e_down": nrm(ks[29], (L, N_EXPERTS, D_EXPERT, D_MODEL), D_EXPERT ** -0.5),
        "final_g": 1.0 + nrm(ks[30], (D_MODEL,), 0.02),
    }


def reference(x, norm1_g, w_in, b_gate, tmix_mu, w0, w2, a0, a2, g2, k_k, k_a, r_k,
              lnx_g, lnx_b, w_oA, lnv_g, lnv_b, w_s, b_s, w_oB, w_out, norm2_g,
              w_rg, b_rg, w_re, b_re, w_e_gate, w_e_up, w_e_down, final_g):
    for l in range(DEPTH):
        h = rms_norm(x, norm1_g[l])
        p = h @ w_in[l]
        p_a = p[..., :A_COLS]
        p_b = p[..., A_COLS:A_COLS + B_COLS]
        p_g = p[..., A_COLS + B_COLS:]
        y_a = rwkv7_branch(p_a, tmix_mu[l], w0[l], w2[l], a0[l], a2[l], g2[l], k_k[l], k_a[l],
                           r_k[l], lnx_g[l], lnx_b[l], w_oA[l])
        y_b = gmlp_branch(p_b, lnv_g[l], lnv_b[l], w_s[l], b_s[l], w_oB[l])
        gates = jax.nn.sigmoid(p_g + b_gate[l])
        g_a, g_b = jnp.split(gates, 2, axis=-1)
        x = x + (g_a * y_a + g_b * y_b) @ w_out[l]
        x = x + hier_moe(rms_norm(x, norm2_g[l]), w_rg[l], b_rg[l], w_re[l], b_re[l],
                         w_e_gate[l], w_e_up[l], w_e_down[l])
    return rms_norm(x, final_g)
```

```python
import re
import numpy as np
from contextlib import ExitStack
import concourse.bass as bass
import concourse.mybir as mybir
from concourse.bass_utils import run_bass_kernel_spmd

F32 = mybir.dt.float32
BF16 = mybir.dt.bfloat16
AF = mybir.ActivationFunctionType
ALU = mybir.AluOpType
AX = mybir.AxisListType
ENGS = ["pe", "dve", "act", "pool", "sp"]
DMA_RING = {"sp": 6, "pool": 4}

D = 1024
IN_COLS = 4896
A_COLS = 1824
NCHA = 15
LNX_EPS = 64e-5
LN_EPS = 1e-5
NORM_EPS = 1e-6
EXPM05 = float(np.exp(-0.5))
PROFILE_SCOPES = False

C_MU, C_KK, C_KA, C_RK, C_A0, C_LG, C_LB, C_BG, C_G1, C_G2 = 0, 15, 19, 23, 27, 31, 35, 39, 55, 63
NCOL = 71


class Rec:
    def __init__(self):
        self.calls = []

    def __getattr__(self, name):
        def f(*a, **k):
            self.calls.append((name, a, k))
            return self
        return f


def _record(fn):
    r = Rec()
    fn(r)
    assert r.calls
    return r.calls


class Prog:
    def __init__(self, nc, stack):
        self.nc = nc
        self.lists = {e: [] for e in ENGS}
        self.count = {e: 0 for e in ENGS}
        self.sems = {}
        for e in ENGS:
            self.sems[("e", e)] = stack.enter_context(nc.semaphore(f"s_{e}"))
        for q, r in DMA_RING.items():
            for i in range(r):
                self.sems[("d", q, i)] = stack.enter_context(nc.semaphore(f"d_{q}{i}"))
        self.dma_n = {q: 0 for q in DMA_RING}
        self.waited = {e: {} for e in ENGS}
        self.last_w = {}
        self.readers = {}
        self.scope = None

    def _waits(self, eng, reads, writes, extra=()):
        need = {}

        def add(s, v):
            if need.get(s, 0) < v:
                need[s] = v

        for k in reads:
            if k in self.last_w:
                add(*self.last_w[k])
        for k in writes:
            if k in self.last_w:
                add(*self.last_w[k])
            for s, v in self.readers.get(k, {}).items():
                add(s, v)
        for s, v in extra:
            add(s, v)
        out = []
        wd = self.waited[eng]
        for s, v in need.items():
            if wd.get(s, 0) < v:
                wd[s] = v
                out.append((s, v))
        return out

    def _commit(self, tok, reads, writes):
        for k in writes:
            self.last_w[k] = tok
            self.readers[k] = {}
        for k in reads:
            d = self.readers.setdefault(k, {})
            if d.get(tok[0], 0) < tok[1]:
                d[tok[0]] = tok[1]

    LIMIT = None
    NOPS = 0

    def op(self, eng, fn, reads=(), writes=()):
        Prog.NOPS += 1
        if Prog.LIMIT is not None and Prog.NOPS > Prog.LIMIT:
            return None
        writes = list(writes) + [k for k in reads if k.startswith("ps")]
        waits = self._waits(eng, reads, writes)
        self.count[eng] += 1
        tok = (("e", eng), self.count[eng])
        self.lists[eng].append((waits, _record(fn), (("e", eng), 1), self.scope))
        self._commit(tok, reads, writes)
        return tok

    def dma(self, q, fn, reads=(), writes=()):
        Prog.NOPS += 1
        if Prog.LIMIT is not None and Prog.NOPS > Prog.LIMIT:
            return None
        r = DMA_RING[q]
        i = self.dma_n[q]
        self.dma_n[q] += 1
        slot = i % r
        skey = ("d", q, slot)
        extra = [(skey, 16 * (i // r))] if i >= r else []
        waits = self._waits(q, reads, writes, extra)
        tok = (skey, 16 * (i // r + 1))
        self.lists[q].append((waits, _record(fn), (skey, 16), self.scope))
        self._commit(tok, reads, writes)
        return tok

    def _all_tokens(self):
        toks = []
        for q, r in DMA_RING.items():
            n = self.dma_n[q]
            for slot in range(min(r, n)):
                toks.append((("d", q, slot), 16 * ((n - 1 - slot) // r + 1)))
        for e in ENGS:
            if self.count[e]:
                toks.append((("e", e), self.count[e]))
        return toks

    def barrier(self):
        toks = self._all_tokens()
        for e in ENGS:
            waits = self._waits(e, (), (), toks)
            if waits:
                self.lists[e].append((waits, None, None, None))

    def final_wait(self, eng):
        waits = self._waits(eng, (), (), self._all_tokens())
        self.lists[eng].append((waits, None, None, None))

    def emit(self):
        engmap = {"pe": "tensor", "dve": "vector", "act": "scalar", "pool": "gpsimd", "sp": "sync"}
        with self.nc.Block() as block:
            for e in ENGS:
                lst = self.lists[e]
                if not lst:
                    continue

                def body(engine, lst=lst):
                    cur, cm = None, None
                    for waits, fn, inc, scope in lst:
                        if PROFILE_SCOPES and scope != cur:
                            if cm is not None:
                                cm.__exit__(None, None, None)
                                cm = None
                            if scope is not None:
                                cm = self.nc.named_scope(scope)
                                cm.__enter__()
                            cur = scope
                        for s, v in waits:
                            engine.wait_ge(self.sems[s], v)
                        if fn is not None:
                            ins = None
                            for name, a, k in fn:
                                ins = getattr(engine, name)(*a, **k)
                            ins.then_inc(self.sems[inc[0]], inc[1])
                    if cm is not None:
                        cm.__exit__(None, None, None)

                getattr(block, engmap[e])(body)


def build(T, NE=32, dbg=None):
    NT = T // 128
    nc = bass.Bass("TRN2", target_bir_lowering=False)

    def din(name, shape, dt=F32):
        return nc.dram_tensor(name, list(shape), dt, kind="ExternalInput").ap()

    x_d = din("x", [T, D])
    w_in_d = din("w_in", [D, IN_COLS])
    cols_d = din("cols", [128, NCOL])
    w0_d = din("w0", [1, 512])
    w2_d = din("w2", [64, 512])
    a2_d = din("a2", [64, 512])
    g2_d = din("g2", [160, 512])
    woA_d = din("w_oA", [512, D])
    woB_d = din("w_oB", [512, D])
    wout_d = din("w_out", [D, D])
    lnvg_d = din("lnv_g", [1, 512])
    lnvb_d = din("lnv_b", [1, 512])
    wsT_d = din("wsT", [4, 128, 128])
    bs_d = din("b_s", [1, 512])
    wr_d = din("w_r", [D, 36])
    br_d = din("b_r", [1, 36])
    weg_d = din("w_e_gate", [32, D, 256])
    weu_d = din("w_e_up", [32, D, 256])
    wed_d = din("w_e_down", [32, 256, D])
    fg_d = din("final_g", [1, D])
    out_d = nc.dram_tensor("out", [T, D], F32, kind="ExternalOutput").ap()
    x1_d = nc.dram_tensor("x1_scr", [T, D], F32, kind="Internal").ap()
    h2_d = nc.dram_tensor("h2_scr", [128, 8, T], BF16, kind="Internal").ap()
    hT_d = nc.dram_tensor("hT_scr", [T // 128, 128, 1024], BF16, kind="Internal").ap()
    yg_d = nc.dram_tensor("yg_scr", [T // 128, 128, 512], BF16, kind="Internal").ap()
    dbg_out = {}
    if dbg:
        for name, shape in dbg.items():
            dbg_out[name] = nc.dram_tensor("dbg_" + name, list(shape), F32, kind="ExternalOutput").ap()

    with ExitStack() as st0:
        P = Prog(nc, st0)

        def sbuf(st, name, shape, dt):
            return st.enter_context(nc.sbuf_tensor("sb_" + name, list(shape), dt))

        psM = [st0.enter_context(nc.psum_tensor(f"psM{i}", [128, 512], F32)) for i in range(2)]
        psA = st0.enter_context(nc.psum_tensor("psA", [128, 1024], F32))
        psD = st0.enter_context(nc.psum_tensor("psD", [128, 1536], F32))
        psS = st0.enter_context(nc.psum_tensor("psS", [128, 512], F32))

        identf = sbuf(st0, "identf", [128, 128], F32)
        ident = sbuf(st0, "ident", [128, 128], BF16)
        mSU = sbuf(st0, "mSU", [128, 128], F32)
        mIU = sbuf(st0, "mIU", [128, 128], F32)
        maskA = sbuf(st0, "maskA", [128, 2, 512], F32)
        maskL = sbuf(st0, "maskL", [128, 2, 128], F32)
        BDf = sbuf(st0, "BDf", [128, 128], F32)
        BDb = sbuf(st0, "BDb", [128, 128], BF16)
        BD64 = sbuf(st0, "BD64", [128, 128], F32)
        Tri2 = sbuf(st0, "Tri2", [128, 256], F32)
        onesb = sbuf(st0, "onesb", [1, 128], BF16)
        onesf = sbuf(st0, "onesf", [128, 128], F32)
        cols = sbuf(st0, "cols", [128, NCOL], F32)
        omu = sbuf(st0, "omu", [128, NCHA], F32)
        oka = sbuf(st0, "oka", [128, 4], F32)
        g1bc = sbuf(st0, "g1bc", [128, 8, 128], F32)
        g2bc = sbuf(st0, "g2bc", [128, 8, 128], F32)

        def colap(c):
            return cols[:, c:c + 1]

        P.dma("sp", lambda e: e.dma_start(out=cols[:], in_=cols_d), writes=["cols"])
        P.op("pool", lambda e: e.memset(onesf[:], 1.0), writes=["onesf"])
        P.op("pool", lambda e: e.memset(identf[:], 0.0), writes=["identf"])
        P.op("pool", lambda e: e.affine_select(out=identf[:], in_=identf[:], pattern=[[-1, 128]], compare_op=ALU.not_equal,
                                               fill=1.0, base=0, channel_multiplier=1), reads=["identf"], writes=["identf"])
        P.op("dve", lambda e: e.tensor_copy(out=ident[:], in_=identf[:]), reads=["identf"], writes=["ident"])
        P.op("pool", lambda e: e.affine_select(out=mSU[:], in_=onesf[:], pattern=[[1, 128]], compare_op=ALU.is_gt,
                                               fill=0.0, base=0, channel_multiplier=-1), reads=["onesf"], writes=["mSU"])
        P.op("pool", lambda e: e.affine_select(out=mIU[:], in_=onesf[:], pattern=[[1, 128]], compare_op=ALU.is_ge,
                                               fill=0.0, base=0, channel_multiplier=-1), reads=["onesf"], writes=["mIU"])
        for h in range(2):
            for q in range(4):
                src = mSU if q % 2 == 0 else mIU
                P.op("dve", lambda e, h=h, q=q, src=src: e.tensor_copy(out=maskA[:, h, q * 128:(q + 1) * 128], in_=src[:]),
                     reads=["mSU", "mIU"], writes=["maskA"])
            P.op("pool", lambda e, h=h: e.affine_select(out=maskL[:, h, :], in_=onesf[:], pattern=[[-1, 128]], compare_op=ALU.is_gt,
                                                        fill=0.0, base=0, channel_multiplier=1), reads=["onesf"], writes=["maskL"])
        P.op("pool", lambda e: e.memset(BDf[:], 0.0), writes=["BDf"])
        P.op("pool", lambda e: e.memset(BDf[0:64, 0:64], 1.0), reads=["BDf"], writes=["BDf"])
        P.op("pool", lambda e: e.memset(BDf[64:128, 64:128], 1.0), reads=["BDf"], writes=["BDf"])
        P.op("dve", lambda e: e.tensor_copy(out=BDb[:], in_=BDf[:]), reads=["BDf"], writes=["BDb"])
        P.op("dve", lambda e: e.tensor_scalar(out=BD64[:], in0=BDf[:], scalar1=1.0 / 64.0, scalar2=None, op0=ALU.mult),
             reads=["BDf"], writes=["BD64"])
        P.op("dve", lambda e: e.tensor_scalar(out=Tri2[:, 0:128], in0=mIU[:], scalar1=EXPM05, scalar2=None, op0=ALU.mult),
             reads=["mIU"], writes=["Tri2"])
        P.op("dve", lambda e: e.tensor_scalar(out=Tri2[:, 128:256], in0=mSU[:], scalar1=EXPM05, scalar2=None, op0=ALU.mult),
             reads=["mSU", "Tri2"], writes=["Tri2"])
        P.op("dve", lambda e: e.tensor_copy(out=onesb[:], in_=onesf[0:1, :]), reads=["onesf"], writes=["onesb"])
        P.op("dve", lambda e: e.tensor_scalar(out=omu[:], in0=cols[:, C_MU:C_MU + NCHA], scalar1=-1.0, scalar2=1.0,
                                              op0=ALU.mult, op1=ALU.add), reads=["cols"], writes=["omu"])
        P.op("dve", lambda e: e.tensor_scalar(out=oka[:], in0=cols[:, C_KA:C_KA + 4], scalar1=-1.0, scalar2=1.0,
                                              op0=ALU.mult, op1=ALU.add), reads=["cols"], writes=["oka"])
        for k in range(8):
            P.op("dve", lambda e, k=k: e.tensor_scalar(out=g1bc[:, k, :], in0=onesf[:], scalar1=colap(C_G1 + k), scalar2=None,
                                                       op0=ALU.mult), reads=["cols", "onesf"], writes=["g1bc"])
            P.op("dve", lambda e, k=k: e.tensor_scalar(out=g2bc[:, k, :], in0=onesf[:], scalar1=colap(C_G2 + k), scalar2=None,
                                                       op0=ALU.mult), reads=["cols", "onesf"], writes=["g2bc"])

        def rms_to_T(st_keys, xin, xin_key, gbc, gbc_key, dst_ap, dst_key, tmp, tag):
            junk, ss, sd, rs, xsb = tmp
            P.op("act", lambda e: e.activation(out=junk[:], in_=xin, func=AF.Square, accum_out=ss[:]),
                 reads=[xin_key], writes=["junk" + tag, "ss" + tag])
            P.op("act", lambda e: e.activation(out=sd[:], in_=ss[:], func=AF.Sqrt, bias=NORM_EPS, scale=1.0 / D),
                 reads=["ss" + tag], writes=["sd" + tag])
            P.op("dve", lambda e: e.reciprocal(out=rs[:], in_=sd[:]), reads=["sd" + tag], writes=["rs" + tag])
            P.op("dve", lambda e: e.tensor_scalar(out=xsb[:], in0=xin, scalar1=rs[:], scalar2=None, op0=ALU.mult),
                 reads=[xin_key, "rs" + tag], writes=["xsb" + tag])
            pT = psM[0][:].bitcast(BF16)

            def tr(e):
                ins = None
                for k in range(8):
                    ins = e.transpose(out=pT[:, k * 128:(k + 1) * 128], in_=xsb[:, k * 128:(k + 1) * 128], identity=ident[:])
                return ins

            P.op("pe", tr, reads=["xsb" + tag, "ident"], writes=["psM0"])
            P.op("dve", lambda e: e.tensor_tensor(out=dst_ap, in0=pT.rearrange("p (k t) -> p k t", k=8), in1=gbc[:], op=ALU.mult),
                 reads=["psM0", gbc_key], writes=[dst_key])

        P.scope = "M1"
        stP = ExitStack()
        comb = sbuf(st0, "comb", [128, NT, 32], F32)
        hTb = [sbuf(stP, f"hTt{i}", [128, 8, 128], BF16) for i in range(2)]
        ygb = [sbuf(stP, f"ygt{i}", [128, 4, 128], BF16) for i in range(2)]

        with ExitStack() as st:
            w_inA = sbuf(st, "w_inA", [128, 8, A_COLS], BF16)
            w2b = sbuf(st, "w2b", [64, 512], BF16)
            a2b = sbuf(st, "a2b", [128, 512], BF16)
            g2b = sbuf(st, "g2b", [128, 2, 512], BF16)
            w0row = sbuf(st, "w0row", [1, 512], BF16)
            for k in range(8):
                P.dma("pool", lambda e, k=k: e.dma_start(out=w_inA[:, k, :], in_=w_in_d[k * 128:(k + 1) * 128, 0:A_COLS]),
                      writes=["w_inA"])
            P.dma("pool", lambda e: e.dma_start(out=w2b[:], in_=w2_d), writes=["w2b"])
            P.dma("pool", lambda e: e.dma_start(out=a2b[64:128, :], in_=a2_d), writes=["a2b"])
            P.dma("pool", lambda e: e.dma_start(out=g2b[:, 0, :], in_=g2_d[0:128, :]), writes=["g2b"])
            P.dma("pool", lambda e: e.dma_start(out=g2b[0:32, 1, :], in_=g2_d[128:160, :]), reads=["g2b"], writes=["g2b"])
            P.dma("pool", lambda e: e.dma_start(out=w0row[:], in_=w0_d), writes=["w0row"])

            xin = [sbuf(st, f"xin{i}", [128, D], F32) for i in range(2)]
            junk = sbuf(st, "junk", [128, D], BF16)
            ss = sbuf(st, "ss", [128, 1], F32)
            sd = sbuf(st, "sd", [128, 1], F32)
            rs = sbuf(st, "rs", [128, 1], F32)
            xsb = sbuf(st, "xsb", [128, D], BF16)
            carry = sbuf(st, "carry", [128, NCHA], F32)
            ltmp = sbuf(st, "ltmp", [128, 129], F32)
            pmx = sbuf(st, "pmx", [128, 128], F32)
            txw2 = [sbuf(st, f"txw_{i}", [128, 128], BF16) for i in range(2)]
            sxg2 = [sbuf(st, f"sxg_{i}", [128, 2, 128], BF16) for i in range(2)]
            DC2 = [sbuf(st, f"DC_{i}", [128, 4], F32) for i in range(2)]
            etok = sbuf(st, "etok", [128, 512], F32)
            Dt2 = [sbuf(st, f"Dt_{i}", [128, 4, 128], F32) for i in range(2)]
            Dinv2 = [sbuf(st, f"Dinv_{i}", [128, 4, 128], F32) for i in range(2)]
            Dprev2 = [sbuf(st, f"Dprev_{i}", [128, 4, 128], F32) for i in range(2)]
            asig = [sbuf(st, f"asig{p}", [128, 128], F32) for p in range(4)]
            tA = [sbuf(st, f"tA{p}", [128, 128], F32) for p in range(4)]
            tB = [sbuf(st, f"tB{p}", [128, 128], F32) for p in range(4)]
            tC = [sbuf(st, f"tC{p}", [128, 128], F32) for p in range(4)]
            t16 = [sbuf(st, f"t16{p}", [128, 128], BF16) for p in range(4)]
            v16 = [sbuf(st, f"v16{p}", [128, 128], BF16) for p in range(4)]
            pmr = [sbuf(st, f"pmr{p}", [128, 128], F32) for p in range(4)]
            pmk = [sbuf(st, f"pmk{p}", [128, 128], F32) for p in range(4)]
            pmv = [sbuf(st, f"pmv{p}", [128, 128], F32) for p in range(4)]
            ltm = [sbuf(st, f"ltm{p}", [128, 129], F32) for p in range(4)]
            PX = [psM[0], psM[1], psA[:, 0:512], psA[:, 512:1024]]
            PXk = ["psM0", "psM1", "psA", "psA2"]
            PY = [psD[:, 0:512], psD[:, 512:1024], psD[:, 1024:1536], psS]
            PYk = ["psD0", "psD1", "psD2", "psS"]
            AR2 = [[sbuf(st, f"AR{p}_{i}", [128, 2, 128], BF16) for p in range(4)] for i in range(2)]
            BT = [sbuf(st, f"BT{p}", [128, 128], BF16) for p in range(4)]
            KT = [sbuf(st, f"KT{p}", [128, 128], BF16) for p in range(4)]
            TOK2 = [[sbuf(st, f"TOK{p}_{i}", [128, 3, 128], BF16) for p in range(4)] for i in range(2)]
            bon2 = [[sbuf(st, f"bon{p}_{i}", [128, 128], F32) for p in range(4)] for i in range(2)]
            gT2 = [[sbuf(st, f"gT{p}_{i}", [128, 128], F32) for p in range(4)] for i in range(2)]
            AM2 = [[sbuf(st, f"AM{p}_{i}", [128, 2, 512], BF16) for p in range(4)] for i in range(2)]
            L0 = [sbuf(st, f"L0{p}", [128, 2, 128], BF16) for p in range(4)]
            MT2 = [[[sbuf(st, f"MT{p}_{i}_{j}", [128, 2, 256], BF16) for i in range(2)] for p in range(4)] for j in range(2)]
            LK = [[sbuf(st, f"LK{p}_{i}", [128, 2, 128], BF16) for i in range(2)] for p in range(4)]
            Sw = [sbuf(st, f"Sw{p}", [128, 128], F32) for p in range(4)]
            Sb = [sbuf(st, f"Sb{p}", [128, 128], BF16) for p in range(4)]
            Xb = [sbuf(st, f"Xb{p}", [128, 128], BF16) for p in range(4)]
            Ub = [sbuf(st, f"Ub{p}", [128, 128], BF16) for p in range(4)]
            yT = sbuf(st, "yT", [128, 4, 128], F32)
            yc = sbuf(st, "yc", [128, 512], F32)
            ysq = sbuf(st, "ysq", [128, 512], F32)
            yrs = sbuf(st, "yrs", [128, 512], F32)
            y3 = sbuf(st, "y3", [128, 128], F32)
            tS = sbuf(st, "tS", [128, 128], F32)

            P.op("pool", lambda e: e.memset(carry[:], 0.0), writes=[f"carry{c}" for c in range(NCHA)])
            for p in range(4):
                P.op("pool", lambda e, p=p: e.memset(Sw[p][:], 0.0), writes=[f"Sw{p}"])
                P.op("pool", lambda e, p=p: e.memset(Sb[p][:], 0.0), writes=[f"Sb{p}"])

            def LV(p):
                return psD[:, 1024:1280] if p % 2 == 0 else psS[:, 0:256]

            def DK(p):
                return ["psD0", "psD2"] if p % 2 == 0 else ["psD1", "psS"]

            serial_ps = [psS, psM[1], psA[:, 0:512], psA[:, 512:1024]]
            serial_key = ["psS", "psM1", "psA", "psA2"]

            def tile_parts(tl, P):
                par = tl % 2
                AM, AR, TOK, MT, bon, gT = AM2[par], AR2[par], TOK2[par], MT2[par], bon2[par], gT2[par]
                Dt, Dinv, Dprev, txw, sxg, DC = Dt2[par], Dinv2[par], Dprev2[par], txw2[par], sxg2[par], DC2[par]
                tc = slice(tl * 128, (tl + 1) * 128)
                xb_, xk = xin[tl % 2], f"xin{tl % 2}"
                hT, hTk = hTb[tl % 2], f"hTt{tl % 2}"
                ygT, ygk = ygb[tl % 2], f"ygt{tl % 2}"

                def inproj_chunk(c, dst_fn):
                    rows = 128 if c < 14 else 32
                    bank, bkey = (psD[:, 512:1024], "psD1") if c == 13 else (psM[0], "psM0")

                    def mmf(e, c=c, rows=rows, bank=bank):
                        ins = None
                        for k in range(8):
                            ins = e.matmul(bank[0:rows, 0:128], lhsT=w_inA[:, k, c * 128:c * 128 + rows], rhs=hT[:, k, :],
                                           start=(k == 0), stop=(k == 7))
                        return ins

                    P.op("pe", mmf, reads=["w_inA", hTk], writes=[bkey])
                    P.op("act", lambda e: e.activation(out=ltmp[0:rows, 1:129], in_=bank[0:rows, 0:128], func=AF.Copy,
                                                       scale=cols[0:rows, C_MU + c:C_MU + c + 1]),
                         reads=[bkey, "cols"], writes=["ltmp"])
                    P.op("pool", lambda e: e.tensor_copy(out=ltmp[0:rows, 0:1], in_=carry[0:rows, c:c + 1]),
                         reads=[f"carry{c}", "ltmp"], writes=["ltmp"])
                    P.op("pool", lambda e: e.tensor_copy(out=carry[0:rows, c:c + 1], in_=ltmp[0:rows, 128:129]),
                         reads=["ltmp", f"carry{c}"], writes=[f"carry{c}"])
                    dst, dkey = dst_fn
                    P.op("dve", lambda e: e.scalar_tensor_tensor(out=dst[0:rows, :], in0=bank[0:rows, 0:128],
                                                                 scalar=omu[0:rows, c:c + 1], in1=ltmp[0:rows, 0:128],
                                                                 op0=ALU.mult, op1=ALU.add),
                         reads=[bkey, "omu", "ltmp"], writes=[dkey])

                def inproj_chunk_g(c, dst, dkey, bank, bkey, lt, ltk):
                    rows = 128

                    def mmf(e):
                        ins = None
                        for k in range(8):
                            ins = e.matmul(bank[0:rows, 0:128], lhsT=w_inA[:, k, c * 128:c * 128 + rows], rhs=hT[:, k, :],
                                           start=(k == 0), stop=(k == 7))
                        return ins

                    P.op("pe", mmf, reads=["w_inA", hTk], writes=[bkey])
                    yield
                    P.op("act", lambda e: e.activation(out=lt[:, 1:129], in_=bank[:, 0:128], func=AF.Copy, scale=cols[:, C_MU + c:C_MU + c + 1]),
                         reads=[bkey, "cols"], writes=[ltk])
                    yield
                    P.op("pool", lambda e: e.tensor_copy(out=lt[:, 0:1], in_=carry[:, c:c + 1]), reads=[f"carry{c}", ltk], writes=[ltk])
                    P.op("pool", lambda e: e.tensor_copy(out=carry[:, c:c + 1], in_=lt[:, 128:129]), reads=[ltk, f"carry{c}"], writes=[f"carry{c}"])
                    yield
                    P.op("dve", lambda e: e.scalar_tensor_tensor(out=dst[:], in0=bank[:, 0:128], scalar=omu[:, c:c + 1], in1=lt[:, 0:128],
                                                                 op0=ALU.mult, op1=ALU.add), reads=[bkey, "omu", ltk], writes=[dkey])
                    yield


                def front():
                    xb_ = xin[tl % 2]
                    xk = f"xin{tl % 2}"
                    P.dma("sp", lambda e, xb_=xb_, tl=tl: e.dma_start(out=xb_[:], in_=x_d[tl * 128:(tl + 1) * 128, :]), writes=[xk])
                    yield
                    hT, hTk = hTb[tl % 2], f"hTt{tl % 2}"
                    ygT, ygk = ygb[tl % 2], f"ygt{tl % 2}"
                    rms_to_T(None, xb_[:], xk, g1bc, "g1bc", hT[:], hTk, (junk, ss, sd, rs, xsb), "1")
                    yield
                    P.dma("sp", lambda e, tl=tl, hT=hT: e.dma_start(out=hT_d[tl], in_=hT[:].rearrange("p k t -> p (k t)")), reads=[hTk], writes=["hT_d"])
                    yield

                    inproj_chunk(12, (pmx, "pmx"))
                    yield
                    P.op("act", lambda e: e.activation(out=txw[0:64, :], in_=pmx[0:64, :], func=AF.Tanh), reads=["pmx"], writes=["txw"])
                    yield
                    P.op("dve", lambda e: e.tensor_copy(out=txw[64:128, :], in_=pmx[64:128, :]), reads=["pmx", "txw"], writes=["txw"])
                    yield
                    inproj_chunk(13, (pmx, "pmx"))
                    yield
                    P.op("act", lambda e: e.activation(out=sxg[:, 0, :], in_=pmx[:], func=AF.Sigmoid), reads=["pmx"], writes=["sxg"])
                    yield
                    inproj_chunk(14, (pmx, "pmx"))
                    yield
                    P.op("act", lambda e: e.activation(out=sxg[0:32, 1, :], in_=pmx[0:32, :], func=AF.Sigmoid),
                         reads=["pmx", "sxg"], writes=["sxg"])
                    yield

                    def zmm(e):
                        e.matmul(psM[0][:, 0:512], lhsT=txw[0:64, :], rhs=w2b[0:64, :], start=True, stop=False)
                        return e.matmul(psM[0][:, 0:512], lhsT=onesb[0:1, :], rhs=w0row[0:1, :], start=False, stop=True)

                    P.op("pe", zmm, reads=["txw", "w2b", "onesb", "w0row"], writes=["psM0"])
                    yield
                    P.op("act", lambda e: e.activation(out=etok[:], in_=psM[0][:, 0:512], func=AF.Sigmoid), reads=["psM0"], writes=["etok"])
                    yield

                    def cmm(e):
                        ins = None
                        for p in range(4):
                            ins = e.matmul(psD[:, 512 + p * 256:512 + (p + 1) * 256], lhsT=etok[:, p * 128:(p + 1) * 128], rhs=Tri2[:],
                                           start=True, stop=True)
                        return ins

                    P.op("pe", cmm, reads=["etok", "Tri2"], writes=["psD1", "psD2"])
                    yield
                    cum = psD[:, 512:1536].rearrange("p (a c) -> p a c", a=4)
                    P.op("act", lambda e: e.activation(out=Dt[:], in_=cum[:, :, 0:128], func=AF.Exp, scale=-1.0),
                         reads=["psD1", "psD2"], writes=["Dt"])
                    yield
                    P.op("act", lambda e: e.activation(out=Dinv[:], in_=cum[:, :, 0:128], func=AF.Exp, scale=1.0),
                         reads=["psD1", "psD2"], writes=["Dinv"])
                    yield
                    P.op("act", lambda e: e.activation(out=Dprev[:], in_=cum[:, :, 128:256], func=AF.Exp, scale=-1.0),
                         reads=["psD1", "psD2"], writes=["Dprev"])
                    yield


                def pair_gen(p):
                    X, Xk, Y, Yk = PX[p], PXk[p], PY[p], PYk[p]
                    rf, kf, vf = pmr[p], pmk[p], pmv[p]
                    rk_, kk_, vk_ = f"pmr{p}", f"pmk{p}", f"pmv{p}"
                    as_, tA_, tB_, tC_, t16_, v16_ = asig[p], tA[p], tB[p], tC[p], t16[p], v16[p]
                    ak, tAk, tBk, tCk, t16k, v16k = f"asig{p}", f"tA{p}", f"tB{p}", f"tC{p}", f"t16{p}", f"v16{p}"
                    cs = slice(p * 128, (p + 1) * 128)
                    for (c, dst, dkey, bank, bkey) in ((p, rf, rk_, X, Xk), (4 + p, kf, kk_, Y, Yk), (8 + p, vf, vk_, X, Xk)):
                        yield from inproj_chunk_g(c, dst, dkey, bank, bkey, ltm[p], f"ltm{p}")
                    P.op("pe", lambda e: e.matmul(Y[:, 0:128], lhsT=a2b[64:128, cs], rhs=txw[64:128, :], start=True, stop=True),
                         reads=["a2b", "txw"], writes=[Yk])
                    yield
                    P.op("act", lambda e: e.activation(out=as_[:], in_=Y[:, 0:128], func=AF.Sigmoid, bias=colap(C_A0 + p)),
                         reads=[Yk, "cols"], writes=[ak])
                    yield

                    def gmm(e):
                        e.matmul(X[:, 0:128], lhsT=g2b[:, 0, cs], rhs=sxg[:, 0, :], start=True, stop=False)
                        return e.matmul(X[:, 0:128], lhsT=g2b[0:32, 1, cs], rhs=sxg[0:32, 1, :], start=False, stop=True)

                    P.op("pe", gmm, reads=["g2b", "sxg"], writes=[Xk])
                    yield
                    P.op("act", lambda e: e.activation(out=gT[p][:], in_=X[:, 0:128], func=AF.Copy), reads=[Xk], writes=[f"gT{p}"])
                    yield
                    P.op("dve", lambda e: e.tensor_scalar(out=tA_[:], in0=kf[:], scalar1=colap(C_KK + p), scalar2=None, op0=ALU.mult),
                         reads=[kk_, "cols"], writes=[tAk])
                    yield
                    P.op("pool", lambda e: e.tensor_tensor(out=t16_[:], in0=tA_[:], in1=tA_[:], op=ALU.mult), reads=[tAk], writes=[t16k])
                    yield
                    P.op("pe", lambda e: e.matmul(Y[:, 0:128], lhsT=BDb[:], rhs=t16_[:], start=True, stop=True), reads=["BDb", t16k], writes=[Yk])
                    yield
                    P.op("act", lambda e: e.activation(out=tB_[:], in_=Y[:, 0:128], func=AF.Sqrt), reads=[Yk], writes=[tBk])
                    yield
                    P.op("dve", lambda e: e.tensor_scalar(out=tB_[:], in0=tB_[:], scalar1=1e-12, scalar2=None, op0=ALU.max), reads=[tBk], writes=[tBk])
                    P.op("dve", lambda e: e.reciprocal(out=tB_[:], in_=tB_[:]), reads=[tBk], writes=[tBk])
                    yield
                    P.op("pool", lambda e: e.tensor_tensor(out=tA_[:], in0=tA_[:], in1=tB_[:], op=ALU.mult), reads=[tAk, tBk], writes=[tAk])
                    yield
                    P.op("dve", lambda e: e.tensor_scalar(out=tC_[:], in0=as_[:], scalar1=colap(C_KA + p), scalar2=oka[:, p:p + 1],
                                                          op0=ALU.mult, op1=ALU.add), reads=[ak, "cols", "oka"], writes=[tCk])
                    yield
                    P.op("pool", lambda e: e.tensor_tensor(out=kf[:], in0=kf[:], in1=tC_[:], op=ALU.mult), reads=[kk_, tCk], writes=[kk_])
                    yield
                    P.op("dve", lambda e: e.scalar_tensor_tensor(out=AR[p][:, 0, :], in0=tA_[:], scalar=-1.0, in1=Dprev[:, p, :],
                                                                 op0=ALU.mult, op1=ALU.mult), reads=[tAk, "Dprev"], writes=[f"AR{p}"])
                    P.op("pool", lambda e: e.tensor_tensor(out=AR[p][:, 1, :], in0=rf[:], in1=Dt[:, p, :], op=ALU.mult),
                         reads=[rk_, "Dt", f"AR{p}"], writes=[f"AR{p}"])
                    yield
                    P.op("pool", lambda e: e.tensor_tensor(out=tB_[:], in0=tA_[:], in1=as_[:], op=ALU.mult), reads=[tAk, ak, tBk], writes=[tBk])
                    yield
                    P.op("dve", lambda e: e.tensor_tensor(out=BT[p][:], in0=tB_[:], in1=Dinv[:, p, :], op=ALU.mult), reads=[tBk, "Dinv"], writes=[f"BT{p}"])
                    P.op("dve", lambda e: e.tensor_tensor(out=KT[p][:], in0=kf[:], in1=Dinv[:, p, :], op=ALU.mult), reads=[kk_, "Dinv"], writes=[f"KT{p}"])
                    yield
                    P.op("dve", lambda e: e.scalar_tensor_tensor(out=t16_[:], in0=rf[:], scalar=colap(C_RK + p), in1=kf[:],
                                                                 op0=ALU.mult, op1=ALU.mult), reads=[rk_, kk_, "cols", t16k], writes=[t16k])
                    yield
                    P.op("pe", lambda e: e.matmul(Y[:, 0:128], lhsT=BDb[:], rhs=t16_[:], start=True, stop=True), reads=["BDb", t16k], writes=[Yk])
                    yield
                    P.op("dve", lambda e: e.tensor_tensor(out=bon[p][:], in0=Y[:, 0:128], in1=vf[:], op=ALU.mult), reads=[Yk, vk_], writes=[f"bon{p}"])
                    P.op("dve", lambda e: e.tensor_scalar(out=bon[p][:], in0=bon[p][:], scalar1=colap(C_LB + p), scalar2=None, op0=ALU.add),
                         reads=[f"bon{p}", "cols"], writes=[f"bon{p}"])
                    yield
                    P.op("act", lambda e: e.activation(out=v16_[:], in_=vf[:], func=AF.Copy), reads=[vk_], writes=[v16k])
                    yield
                    pT = X[:, 0:256].bitcast(BF16)

                    def tr3(e):
                        e.transpose(out=pT[:, 0:128], in_=v16_[:], identity=ident[:])
                        e.transpose(out=pT[:, 128:256], in_=BT[p][:], identity=ident[:])
                        return e.transpose(out=pT[:, 256:384], in_=KT[p][:], identity=ident[:])

                    P.op("pe", tr3, reads=[v16k, f"BT{p}", f"KT{p}", "ident"], writes=[Xk])
                    yield
                    P.op("act", lambda e: e.activation(out=TOK[p][:].rearrange("p a t -> p (a t)"), in_=pT[:, 0:384], func=AF.Copy),
                         reads=[Xk], writes=[f"TOK{p}"])
                    yield

                    def amm(e):
                        ins = None
                        for h, bank in ((0, X), (1, Y)):
                            hr = slice(64 * h, 64 * h + 64)
                            e.matmul(bank[:, 0:256], lhsT=BT[p][hr, :], rhs=AR[p][hr, :, :].rearrange("p a t -> p (a t)"), start=True, stop=True)
                            ins = e.matmul(bank[:, 256:512], lhsT=KT[p][hr, :], rhs=AR[p][hr, :, :].rearrange("p a t -> p (a t)"),
                                           start=True, stop=True)
                        return ins

                    P.op("pe", amm, reads=[f"BT{p}", f"KT{p}", f"AR{p}"], writes=[Xk, Yk])
                    yield
                    P.op("dve", lambda e: e.tensor_tensor(out=AM[p][:, 0, :], in0=X[:, 0:512], in1=maskA[:, 0, :], op=ALU.mult),
                         reads=[Xk, "maskA"], writes=[f"AM{p}"])
                    P.op("dve", lambda e: e.tensor_tensor(out=AM[p][:, 1, :], in0=Y[:, 0:512], in1=maskA[:, 1, :], op=ALU.mult),
                         reads=[Yk, "maskA", f"AM{p}"], writes=[f"AM{p}"])
                    yield
                    pTL = Y[:, 0:128].bitcast(BF16)

                    def lmm(e):
                        e.transpose(out=pTL[:, 0:128], in_=AM[p][:, 0, 0:128], identity=ident[:])
                        return e.transpose(out=pTL[:, 128:256], in_=AM[p][:, 1, 0:128], identity=ident[:])

                    P.op("pe", lmm, reads=[f"AM{p}", "ident"], writes=[Yk])
                    for h in range(2):
                        P.op("pool", lambda e, h=h: e.tensor_tensor(out=MT[p][0][:, h, 128:256], in0=AM[p][:, h, 0:128], in1=ident[:], op=ALU.add),
                             reads=[f"AM{p}", "ident", f"MT{p}_0"], writes=[f"MT{p}_0"])
                    yield
                    P.op("dve", lambda e: e.tensor_copy(out=L0[p][:].rearrange("p h c -> p (h c)"), in_=pTL), reads=[Yk], writes=[f"L0{p}"])
                    yield
                    Xm = X[:, 0:512].rearrange("p (h c) -> p h c", h=2)
                    Yl = Y[:, 0:256].rearrange("p (h c) -> p h c", h=2)

                    def r1(e):
                        ins = None
                        for h in range(2):
                            e.matmul(X[:, h * 256:h * 256 + 128], lhsT=L0[p][:, h, :], rhs=AM[p][:, h, 0:128], start=True, stop=True)
                            ins = e.matmul(Y[:, h * 128:(h + 1) * 128], lhsT=AM[p][:, h, 0:128], rhs=L0[p][:, h, :], start=True, stop=True)
                        return ins

                    P.op("pe", r1, reads=[f"L0{p}", f"AM{p}"], writes=[Xk, Yk])
                    yield
                    P.op("act", lambda e: e.activation(out=MT[p][0][:, :, 0:128], in_=Xm[:, :, 0:128], func=AF.Copy),
                         reads=[Xk, f"MT{p}_0"], writes=[f"MT{p}_0"])
                    P.op("dve", lambda e: e.tensor_copy(out=LK[p][0][:], in_=Yl), reads=[Yk], writes=[f"LK{p}_0"])
                    yield
                    for rnd in range(2, 8):
                        src = rnd % 2
                        dst = 1 - src
                        last = (rnd == 7)
                        mts, mtd, lks, lkd = MT[p][src], MT[p][dst], LK[p][src], LK[p][dst]

                        def rk(e, mts=mts, lks=lks, last=last):
                            ins = None
                            for h in range(2):
                                if last:
                                    e.matmul(X[:, h * 256 + 128:h * 256 + 256], lhsT=lks[:, h, :], rhs=mts[:, h, 128:256], start=True, stop=False)
                                    ins = e.matmul(X[:, h * 256 + 128:h * 256 + 256], lhsT=ident[:], rhs=mts[:, h, 128:256], start=False, stop=True)
                                else:
                                    e.matmul(X[:, h * 256:h * 256 + 256], lhsT=lks[:, h, :], rhs=mts[:, h, :], start=True, stop=False)
                                    e.matmul(X[:, h * 256 + 128:h * 256 + 256], lhsT=ident[:], rhs=mts[:, h, 128:256], start=False, stop=True)
                            if not last:
                                for h in range(2):
                                    ins = e.matmul(Y[:, h * 128:(h + 1) * 128], lhsT=mts[:, h, 0:128], rhs=lks[:, h, :], start=True, stop=True)
                            return ins

                        P.op("pe", rk, reads=[f"MT{p}_{src}", f"LK{p}_{src}", "ident"], writes=[Xk] if last else [Xk, Yk])
                        yield
                        if last:
                            P.op("act", lambda e, mtd=mtd: e.activation(out=mtd[:, :, 128:256], in_=Xm[:, :, 128:256], func=AF.Copy),
                                 reads=[Xk, f"MT{p}_{dst}"], writes=[f"MT{p}_{dst}"])
                        else:
                            P.op("act", lambda e, mtd=mtd: e.activation(out=mtd[:], in_=Xm, func=AF.Copy), reads=[Xk], writes=[f"MT{p}_{dst}"])
                            P.op("dve", lambda e, lkd=lkd: e.tensor_copy(out=lkd[:], in_=Yl), reads=[Yk], writes=[f"LK{p}_{dst}"])
                        yield
                    P.op("pool", lambda e: e.tensor_copy(out=DC[:, p:p + 1], in_=Dt[:, p, 127:128]), reads=["Dt", "DC"], writes=["DC"])
                    yield


                def tail():
                    for p in range(4):
                        bank, bk = serial_ps[p], serial_key[p]

                        def xmm(e, p=p, bank=bank):
                            e.matmul(bank[:, 0:64], lhsT=AM[p][:, 0, 256:384], rhs=TOK[p][:, 0, 0:64], start=True, stop=False)
                            e.matmul(bank[:, 64:128], lhsT=AM[p][:, 1, 256:384], rhs=TOK[p][:, 0, 64:128], start=False, stop=False)
                            return e.matmul(bank[:, 0:128], lhsT=AR[p][:, 0, :], rhs=Sb[p][:], start=False, stop=True)

                        P.op("pe", xmm, reads=[f"AM{p}", f"TOK{p}", f"AR{p}", f"Sb{p}"], writes=[bk])
                    for p in range(4):
                        bank, bk = serial_ps[p], serial_key[p]
                        P.op("act", lambda e, p=p, bank=bank: e.activation(out=Xb[p][:], in_=bank[:, 0:128], func=AF.Copy),
                             reads=[bk], writes=[f"Xb{p}"])
                    for p in range(4):
                        bank, bk = serial_ps[p], serial_key[p]

                        def umm(e, p=p, bank=bank):
                            e.matmul(bank[:, 128:192], lhsT=MT[p][0][:, 0, 128:256], rhs=Xb[p][:, 0:64], start=True, stop=True)
                            return e.matmul(bank[:, 192:256], lhsT=MT[p][0][:, 1, 128:256], rhs=Xb[p][:, 64:128], start=True, stop=True)

                        P.op("pe", umm, reads=[f"MT{p}_0", f"Xb{p}"], writes=[bk])
                    for p in range(4):
                        bank, bk = serial_ps[p], serial_key[p]
                        P.op("dve", lambda e, p=p, bank=bank: e.tensor_copy(out=Ub[p][:], in_=bank[:, 128:256]), reads=[bk], writes=[f"Ub{p}"])
                    for p in range(4):
                        bank, bk = serial_ps[p], serial_key[p]

                        def ymm(e, p=p, bank=bank):
                            e.matmul(bank[:, 256:384], lhsT=Sb[p][:], rhs=AR[p][:, 1, :], start=True, stop=False)
                            ins = None
                            for h in range(2):
                                hs = slice(64 * h, 64 * h + 64)
                                e.matmul(bank[hs, 256:384], lhsT=Ub[p][:, hs], rhs=AM[p][:, h, 128:256], start=False, stop=False)
                                ins = e.matmul(bank[hs, 256:384], lhsT=TOK[p][:, 0, hs], rhs=AM[p][:, h, 384:512], start=False, stop=True)
                            return ins

                        P.op("pe", ymm, reads=[f"Sb{p}", f"AR{p}", f"Ub{p}", f"AM{p}", f"TOK{p}"], writes=[bk])

                        def smm(e, p=p, bank=bank):
                            e.matmul(bank[:, 384:512], lhsT=TOK[p][:, 2, :], rhs=TOK[p][:, 0, :], start=True, stop=False)
                            return e.matmul(bank[:, 384:512], lhsT=TOK[p][:, 1, :], rhs=Ub[p][:], start=False, stop=True)

                        P.op("pe", smm, reads=[f"TOK{p}", f"Ub{p}"], writes=[bk])
                    for p in range(4):
                        bank, bk = serial_ps[p], serial_key[p]
                        P.op("act", lambda e, p=p, bank=bank: e.activation(out=yT[:, p, :], in_=bank[:, 256:384], func=AF.Copy),
                             reads=[bk, "yT"], writes=["yT"])
                        P.op("dve", lambda e, p=p, bank=bank: e.tensor_tensor(out=tS[:], in0=bank[:, 384:512], in1=Sw[p][:], op=ALU.add),
                             reads=[bk, f"Sw{p}", "tS"], writes=["tS"])
                        P.op("dve", lambda e, p=p: e.scalar_tensor_tensor(out=Sw[p][:], in0=tS[:], scalar=DC[:, p:p + 1], in1=BDf[:],
                                                                          op0=ALU.mult, op1=ALU.mult),
                             reads=["tS", f"Sw{p}", "DC", "BDf"], writes=[f"Sw{p}"])
                        P.op("act", lambda e, p=p: e.activation(out=Sb[p][:], in_=Sw[p][:], func=AF.Copy), reads=[f"Sw{p}"], writes=[f"Sb{p}"])

                    yTf = yT[:].rearrange("p a t -> p (a t)")
                    P.op("pe", lambda e: e.matmul(psD[:, 0:512], lhsT=BD64[:], rhs=yTf, start=True, stop=True), reads=["BD64", "yT"], writes=["psD0"])
                    yield
                    P.op("dve", lambda e: e.tensor_tensor(out=yc[:], in0=yTf, in1=psD[:, 0:512], op=ALU.subtract), reads=["yT", "psD0"], writes=["yc"])
                    yield
                    P.op("act", lambda e: e.activation(out=ysq[:], in_=yc[:], func=AF.Square), reads=["yc"], writes=["ysq"])
                    yield
                    P.op("pe", lambda e: e.matmul(psD[:, 0:512], lhsT=BD64[:], rhs=ysq[:], start=True, stop=True), reads=["BD64", "ysq"], writes=["psD0"])
                    yield
                    P.op("act", lambda e: e.activation(out=yrs[:], in_=psD[:, 0:512], func=AF.Sqrt, bias=LNX_EPS), reads=["psD0"], writes=["yrs"])
                    yield
                    P.op("dve", lambda e: e.reciprocal(out=yrs[:], in_=yrs[:]), reads=["yrs"], writes=["yrs"])
                    yield
                    P.op("pool", lambda e: e.tensor_tensor(out=yc[:], in0=yc[:], in1=yrs[:], op=ALU.mult), reads=["yc", "yrs"], writes=["yc"])
                    yield
                    for p in range(4):
                        P.op("dve", lambda e, p=p: e.scalar_tensor_tensor(out=y3[:], in0=yc[:, p * 128:(p + 1) * 128], scalar=colap(C_LG + p),
                                                                          in1=bon[p][:], op0=ALU.mult, op1=ALU.add),
                             reads=["yc", "cols", f"bon{p}"], writes=["y3"])
                        P.op("pool", lambda e, p=p: e.tensor_tensor(out=ygT[:, p, :], in0=y3[:], in1=gT[p][:], op=ALU.mult),
                             reads=["y3", f"gT{p}", ygk], writes=[ygk])
                    P.dma("sp", lambda e, tl=tl, ygT=ygT: e.dma_start(out=yg_d[tl], in_=ygT[:].rearrange("p a t -> p (a t)")), reads=[ygk], writes=["yg_d"])
                    yield


                return front, pair_gen, tail

            class KeyProxy:
                PAT = re.compile(r'^(AM|AR|TOK|MT|bon|gT)\d')

                def __init__(self, par):
                    self.par = par

                def km(self, k):
                    if KeyProxy.PAT.match(k) or k in ('Dt', 'Dinv', 'Dprev', 'txw', 'sxg', 'DC'):
                        return f'{k}@{self.par}'
                    return k

                def op(self, eng, fn, reads=(), writes=()):
                    return P.op(eng, fn, [self.km(k) for k in reads], [self.km(k) for k in writes])

                def dma(self, q, fn, reads=(), writes=()):
                    return P.dma(q, fn, [self.km(k) for k in reads], [self.km(k) for k in writes])

            def run_rr(gens):
                gens = list(gens)
                while gens:
                    for g in list(gens):
                        try:
                            next(g)
                        except StopIteration:
                            gens.remove(g)

            parts = [tile_parts(tl, KeyProxy(tl % 2)) for tl in range(NT)]
            run_rr([parts[0][0]()])
            for tl in range(NT):
                run_rr([parts[tl][1](p) for p in range(4)])
                gens = [parts[tl][2]()]
                if tl + 1 < NT:
                    gens.append(parts[tl + 1][0]())
                run_rr(gens)

            P.barrier()

        with ExitStack() as st:
            P.scope = "M2"
            w_inB = sbuf(st, "w_inB", [128, 8, 3072], BF16)
            woA = sbuf(st, "woA", [128, 4, D], BF16)
            woB = sbuf(st, "woB", [128, 4, D], BF16)
            wout = sbuf(st, "wout", [128, 8, D], BF16)
            wsTf = sbuf(st, "wsTf", [128, 4, 128], F32)
            wsTb = sbuf(st, "wsTb", [128, 4, 128], BF16)
            bsrow = sbuf(st, "bsrow", [1, 512], BF16)
            lrow = sbuf(st, "lrow", [1, 2, 512], F32)
            lnvg = sbuf(st, "lnvg", [128, 512], F32)
            lnvb = sbuf(st, "lnvb", [128, 512], F32)
            for k in range(8):
                for j in range(3):
                    P.dma("pool", lambda e, k=k, j=j: e.dma_start(out=w_inB[:, k, j * 1024:(j + 1) * 1024],
                                                                  in_=w_in_d[k * 128:(k + 1) * 128, A_COLS + j * 1024:A_COLS + (j + 1) * 1024]),
                          writes=["w_inB"])
            P.dma("pool", lambda e: e.dma_start(out=woA[:], in_=woA_d.rearrange("(k p) n -> p k n", p=128)), writes=["woA"])
            P.dma("pool", lambda e: e.dma_start(out=woB[:], in_=woB_d.rearrange("(k p) n -> p k n", p=128)), writes=["woB"])
            P.dma("pool", lambda e: e.dma_start(out=wout[:], in_=wout_d.rearrange("(k p) n -> p k n", p=128)), writes=["wout"])
            P.dma("sp", lambda e: e.dma_start(out=wsTf[:], in_=wsT_d.rearrange("g s t -> s g t")), writes=["wsTf"])
            P.dma("pool", lambda e: e.dma_start(out=bsrow[:], in_=bs_d), writes=["bsrow"])
            P.dma("sp", lambda e: e.dma_start(out=lrow[:, 0, :], in_=lnvg_d), writes=["lrow"])
            P.dma("sp", lambda e: e.dma_start(out=lrow[:, 1, :], in_=lnvb_d), reads=["lrow"], writes=["lrow"])
            for g in range(4):
                P.op("dve", lambda e, g=g: e.tensor_tensor(out=wsTb[:, g, :], in0=wsTf[:, g, :], in1=mIU[:], op=ALU.mult),
                     reads=["wsTf", "mIU", "wsTb"], writes=["wsTb"])
            for i, (dst, dk) in enumerate([(lnvg, "lnvg"), (lnvb, "lnvb")]):
                P.op("pe", lambda e, i=i: e.matmul(psM[0][:, 0:512], lhsT=onesf[0:1, :], rhs=lrow[0:1, i, :], start=True, stop=True),
                     reads=["onesf", "lrow"], writes=["psM0"])
                P.op("act", lambda e, dst=dst: e.activation(out=dst[:], in_=psM[0][:, 0:512], func=AF.Copy), reads=["psM0"], writes=[dk])

            NIF = 3
            def mk(name, shape, dt):
                return [sbuf(st, f"{name}_{i}", shape, dt) for i in range(NIF)]
            xinL, hTL, ygL = mk("xinb", [128, D], F32), mk("hTm", [128, 8, 128], BF16), mk("ygm", [128, 4, 128], BF16)
            uTL, vgL, bstL, mvL = mk("uT", [128, 4, 128], BF16), mk("vg", [128, 512], F32), mk("bst", [128, 6], F32), mk("mv", [128, 2], F32)
            lsdL, lrsL, lnmL = mk("lsd", [128, 1], F32), mk("lrs", [128, 1], F32), mk("lnm", [128, 1], F32)
            vlnL, mixL = mk("vln", [128, 512], BF16), mk("mixT", [128, 4, 128], BF16)
            gsaL, gsbL = mk("gsa", [128, 4, 128], BF16), mk("gsb", [128, 4, 128], BF16)
            t1L, t2L, zTL = mk("t1", [128, 512], BF16), mk("t2", [128, 512], BF16), mk("zT", [128, 8, 128], BF16)
            x1L, ssL, sdL, rsL = mk("x1", [128, D], F32), mk("ss2", [128, 1], F32), mk("sd2", [128, 1], F32), mk("rs2", [128, 1], F32)
            xsbL, h2tL = mk("xsb2", [128, D], BF16), mk("h2t", [128, 8, 128], BF16)
            UB = [(psM[0], "psM0"), (psM[1], "psM1"), (psS, "psS")]
            VB = [(psM[1], "psM1"), (psS, "psS"), (psM[0], "psM0")]

            wrb = sbuf(st, "wrb", [128, 8, 36], BF16)
            brrow = sbuf(st, "brrow", [1, 36], BF16)
            P.dma("pool", lambda e: e.dma_start(out=wrb[:], in_=wr_d.rearrange("(k p) n -> p k n", p=128)), writes=["wrb"])
            P.dma("pool", lambda e: e.dma_start(out=brrow[:], in_=br_d), writes=["brrow"])
            NRS = 6
            rsc = []
            for i in range(NRS):
                d = {"Lg": sbuf(st, f"Lg{i}", [128, 36], F32), "c": sbuf(st, f"rc{i}", [128, 10], F32), "ohg": sbuf(st, f"ohg{i}", [128, 4], F32),
                     "ex4": sbuf(st, f"ex4{i}", [128, 4], F32), "esel": sbuf(st, f"esel{i}", [128, 8], F32), "e2": sbuf(st, f"e2{i}", [128, 8], F32),
                     "mk1": sbuf(st, f"mk1{i}", [128, 8], F32), "mk2": sbuf(st, f"mk2{i}", [128, 8], F32), "wg8": sbuf(st, f"wg8{i}", [128, 8], F32)}
                rsc.append(d)

            def router_tile(tl, h2t, h2k):
                sl = tl % NRS
                d = rsc[sl]
                Lg, ohg, ex4, esel, e2, mk1, mk2, wg8 = d["Lg"], d["ohg"], d["ex4"], d["esel"], d["e2"], d["mk1"], d["mk2"], d["wg8"]
                cc = d["c"]
                gmax, ngmax, se, gp, m1, m2, dd, w1, w2 = [cc[:, i:i + 1] for i in range(9)]
                bank, bk = psD[:, 1024:1536], "psD2"
                rk = f"rt{sl}"
                tc = slice(tl * 128, (tl + 1) * 128)

                def rmm(e):
                    for k in range(8):
                        e.matmul(bank[:, 0:36], lhsT=h2t[:, k, :], rhs=wrb[:, k, :], start=(k == 0), stop=False)
                    return e.matmul(bank[:, 0:36], lhsT=onesb[0:1, :], rhs=brrow[0:1, :], start=False, stop=True)

                yield ("acq", [bk, f"rslot{sl}"])
                P.op("pe", rmm, reads=[h2k, "wrb", "onesb", "brrow"], writes=[bk])
                yield
                P.op("dve", lambda e: e.tensor_copy(out=Lg[:], in_=bank[:, 0:36]), reads=[bk, rk], writes=[rk])
                yield ("rel", [bk])
                yield ("spawn", router_rest(tl))

            def router_rest(tl):
                sl = tl % NRS
                d = rsc[sl]
                Lg, ohg, ex4, esel, e2, mk1, mk2, wg8 = d["Lg"], d["ohg"], d["ex4"], d["esel"], d["e2"], d["mk1"], d["mk2"], d["wg8"]
                cc = d["c"]
                gmax, ngmax, se, gp, m1, m2, dd, w1, w2 = [cc[:, i:i + 1] for i in range(9)]
                rk = f"rt{sl}"
                steps = [
                    ("dve", lambda e: e.tensor_reduce(out=gmax, in_=Lg[:, 0:4], axis=AX.X, op=ALU.max)),
                    ("dve", lambda e: e.tensor_scalar(out=ohg[:], in0=Lg[:, 0:4], scalar1=gmax, scalar2=None, op0=ALU.is_ge)),
                    ("dve", lambda e: e.tensor_scalar(out=ngmax, in0=gmax, scalar1=-1.0, scalar2=None, op0=ALU.mult)),
                    ("act", lambda e: e.activation(out=ex4[:], in_=Lg[:, 0:4], func=AF.Exp, bias=ngmax)),
                    ("dve", lambda e: e.tensor_reduce(out=se, in_=ex4[:], axis=AX.X, op=ALU.add)),
                    ("dve", lambda e: e.reciprocal(out=gp, in_=se)),
                    ("dve", lambda e: e.tensor_scalar(out=esel[:], in0=Lg[:, 4:12], scalar1=ohg[:, 0:1], scalar2=None, op0=ALU.mult)),
                ]
                for g in range(1, 4):
                    steps.append(("dve", lambda e, g=g: e.scalar_tensor_tensor(out=esel[:], in0=Lg[:, 4 + 8 * g:12 + 8 * g], scalar=ohg[:, g:g + 1],
                                                                               in1=esel[:], op0=ALU.mult, op1=ALU.add)))
                steps += [
                    ("dve", lambda e: e.tensor_reduce(out=m1, in_=esel[:], axis=AX.X, op=ALU.max)),
                    ("dve", lambda e: e.tensor_scalar(out=mk1[:], in0=esel[:], scalar1=m1, scalar2=None, op0=ALU.is_ge)),
                    ("dve", lambda e: e.scalar_tensor_tensor(out=e2[:], in0=mk1[:], scalar=-1e30, in1=esel[:], op0=ALU.mult, op1=ALU.add)),
                    ("dve", lambda e: e.tensor_reduce(out=m2, in_=e2[:], axis=AX.X, op=ALU.max)),
                    ("dve", lambda e: e.tensor_scalar(out=mk2[:], in0=e2[:], scalar1=m2, scalar2=None, op0=ALU.is_ge)),
                    ("dve", lambda e: e.tensor_tensor(out=dd, in0=m2, in1=m1, op=ALU.subtract)),
                    ("act", lambda e: e.activation(out=w2, in_=dd, func=AF.Sigmoid)),
                    ("act", lambda e: e.activation(out=w1, in_=dd, func=AF.Sigmoid, scale=-1.0)),
                    ("dve", lambda e: e.tensor_tensor(out=w1, in0=w1, in1=gp, op=ALU.mult)),
                    ("dve", lambda e: e.tensor_tensor(out=w2, in0=w2, in1=gp, op=ALU.mult)),
                    ("dve", lambda e: e.tensor_scalar(out=wg8[:], in0=mk1[:], scalar1=w1, scalar2=None, op0=ALU.mult)),
                    ("dve", lambda e: e.scalar_tensor_tensor(out=wg8[:], in0=mk2[:], scalar=w2, in1=wg8[:], op0=ALU.mult, op1=ALU.add)),
                ]
                for eng, fn in steps:
                    P.op(eng, fn, reads=[rk], writes=[rk])
                    yield
                for g in range(4):
                    P.op("dve", lambda e, g=g: e.tensor_scalar(out=comb[:, tl, g * 8:(g + 1) * 8], in0=wg8[:], scalar1=ohg[:, g:g + 1],
                                                               scalar2=None, op0=ALU.mult), reads=[rk, f"comb{tl}"], writes=[f"comb{tl}"])
                yield ("rel", [f"rslot{sl}"])


            def m2_tile(tl):
                sl = tl % NIF
                K_ = lambda n: f"{n}_{sl}"
                xb_, hT, ygT = xinL[sl], hTL[sl], ygL[sl]
                uT, vg, bst, mv, lsd, lrs, lnm = uTL[sl], vgL[sl], bstL[sl], mvL[sl], lsdL[sl], lrsL[sl], lnmL[sl]
                vln, mixT, gsa, gsb, t1, t2, zT = vlnL[sl], mixL[sl], gsaL[sl], gsbL[sl], t1L[sl], t2L[sl], zTL[sl]
                x1, ss, sd, rs, xsb, h2t = x1L[sl], ssL[sl], sdL[sl], rsL[sl], xsbL[sl], h2tL[sl]
                (ub, ubk), (vb, vbk) = UB[sl], VB[sl]
                tc = slice(tl * 128, (tl + 1) * 128)
                P.dma("sp", lambda e: e.dma_start(out=xb_[:], in_=x_d[tl * 128:(tl + 1) * 128, :]), writes=[K_("xinb")])
                P.dma("sp", lambda e: e.dma_start(out=hT[:].rearrange("p k t -> p (k t)"), in_=hT_d[tl]), reads=["hT_d"], writes=[K_("hTm")])
                P.dma("sp", lambda e: e.dma_start(out=ygT[:].rearrange("p a t -> p (a t)"), in_=yg_d[tl]), reads=["yg_d"], writes=[K_("ygm")])
                yield

                def umm2(e):
                    ins = None
                    for c in range(4):
                        for k in range(8):
                            ins = e.matmul(ub[:, c * 128:(c + 1) * 128], lhsT=w_inB[:, k, c * 128:(c + 1) * 128], rhs=hT[:, k, :],
                                           start=(k == 0), stop=(k == 7))
                    return ins

                yield ("acq", [ubk])
                P.op("pe", umm2, reads=["w_inB", K_("hTm")], writes=[ubk])
                yield
                P.op("act", lambda e: e.activation(out=uT[:].rearrange("p a t -> p (a t)"), in_=ub[:, 0:512], func=AF.Gelu), reads=[ubk], writes=[K_("uT")])
                yield ("rel", [ubk])

                def vmm(e):
                    ins = None
                    for k in range(8):
                        ins = e.matmul(vb[:, 0:512], lhsT=hT[:, k, :], rhs=w_inB[:, k, 512:1024], start=(k == 0), stop=(k == 7))
                    return ins

                yield ("acq", [vbk])
                P.op("pe", vmm, reads=["w_inB", K_("hTm")], writes=[vbk])
                yield
                P.op("act", lambda e: e.activation(out=vg[:], in_=vb[:, 0:512], func=AF.Gelu), reads=[vbk], writes=[K_("vg")])
                yield ("rel", [vbk])
                P.op("dve", lambda e: e.bn_stats(out=bst[:], in_=vg[:]), reads=[K_("vg")], writes=[K_("bst")])
                yield
                P.op("dve", lambda e: e.bn_aggr(out=mv[:], in_=bst[:]), reads=[K_("bst")], writes=[K_("mv")])
                yield
                P.op("act", lambda e: e.activation(out=lsd[:], in_=mv[:, 1:2], func=AF.Sqrt, bias=LN_EPS), reads=[K_("mv")], writes=[K_("lsd")])
                yield
                P.op("dve", lambda e: e.reciprocal(out=lrs[:], in_=lsd[:]), reads=[K_("lsd")], writes=[K_("lrs")])
                P.op("dve", lambda e: e.tensor_scalar(out=lnm[:], in0=mv[:, 0:1], scalar1=-1.0, scalar2=lrs[:], op0=ALU.mult, op1=ALU.mult),
                     reads=[K_("mv"), K_("lrs")], writes=[K_("lnm")])
                P.op("dve", lambda e: e.tensor_scalar(out=vg[:], in0=vg[:], scalar1=lrs[:], scalar2=lnm[:], op0=ALU.mult, op1=ALU.add),
                     reads=[K_("vg"), K_("lrs"), K_("lnm")], writes=[K_("vg")])
                yield
                P.op("pool", lambda e: e.tensor_tensor(out=vg[:], in0=vg[:], in1=lnvg[:], op=ALU.mult), reads=[K_("vg"), "lnvg"], writes=[K_("vg")])
                P.op("pool", lambda e: e.tensor_tensor(out=vln[:], in0=vg[:], in1=lnvb[:], op=ALU.add), reads=[K_("vg"), "lnvb"], writes=[K_("vln")])
                yield

                def svmm(e):
                    ins = None
                    for g in range(4):
                        e.matmul(vb[:, g * 128:(g + 1) * 128], lhsT=vln[:, g * 128:(g + 1) * 128], rhs=wsTb[:, g, :], start=True, stop=False)
                        ins = e.matmul(vb[:, g * 128:(g + 1) * 128], lhsT=onesb[0:1, :], rhs=bsrow[0:1, g * 128:(g + 1) * 128],
                                       start=False, stop=True)
                    return ins

                yield ("acq", [vbk])
                P.op("pe", svmm, reads=[K_("vln"), "wsTb", "onesb", "bsrow"], writes=[vbk])
                yield
                P.op("dve", lambda e: e.tensor_tensor(out=mixT[:].rearrange("p a t -> p (a t)"), in0=vb[:, 0:512],
                                                      in1=uT[:].rearrange("p a t -> p (a t)"), op=ALU.mult),
                     reads=[vbk, K_("uT")], writes=[K_("mixT")])
                yield ("rel", [vbk])
                for hf in range(2):
                    def yamm(e, hf=hf):
                        ins = None
                        for mi in range(4):
                            m = hf * 4 + mi
                            for c in range(4):
                                ins = e.matmul(psA[:, mi * 128:(mi + 1) * 128], lhsT=woA[:, c, m * 128:(m + 1) * 128], rhs=ygT[:, c, :],
                                               start=(c == 0), stop=(c == 3))
                        for mi in range(4):
                            m = hf * 4 + mi
                            for c in range(4):
                                ins = e.matmul(psA[:, 512 + mi * 128:512 + (mi + 1) * 128], lhsT=woB[:, c, m * 128:(m + 1) * 128], rhs=mixT[:, c, :],
                                               start=(c == 0), stop=(c == 3))
                        return ins

                    def gmm2(e, hf=hf):
                        ins = None
                        for ab in range(2):
                            for mi in range(4):
                                m = hf * 4 + mi
                                c0 = 1024 + ab * 1024 + m * 128
                                for k in range(8):
                                    ins = e.matmul(psD[:, ab * 512 + mi * 128:ab * 512 + (mi + 1) * 128], lhsT=w_inB[:, k, c0:c0 + 128],
                                                   rhs=hT[:, k, :], start=(k == 0), stop=(k == 7))
                        return ins

                    yield ("acq", ["psD0", "psD1"])
                    P.op("pe", gmm2, reads=["w_inB", K_("hTm")], writes=["psD0", "psD1"])
                    yield
                    for mi in range(4):
                        m = hf * 4 + mi
                        P.op("act", lambda e, mi=mi, m=m: e.activation(out=gsa[:, mi, :], in_=psD[:, mi * 128:(mi + 1) * 128], func=AF.Sigmoid,
                                                                       bias=colap(C_BG + m)), reads=["psD0", "cols", K_("gsa")], writes=[K_("gsa")])
                    yield
                    for mi in range(4):
                        m = hf * 4 + mi
                        P.op("act", lambda e, mi=mi, m=m: e.activation(out=gsb[:, mi, :], in_=psD[:, 512 + mi * 128:512 + (mi + 1) * 128],
                                                                       func=AF.Sigmoid, bias=colap(C_BG + 8 + m)),
                             reads=["psD1", "cols", K_("gsb")], writes=[K_("gsb")])
                    yield ("rel", ["psD0", "psD1"])
                    yield ("acq", ["psA", "psA2"])
                    P.op("pe", yamm, reads=["woA", "woB", K_("ygm"), K_("mixT")], writes=["psA", "psA2"])
                    yield
                    P.op("dve", lambda e: e.tensor_tensor(out=t1[:], in0=psA[:, 0:512], in1=gsa[:].rearrange("p a t -> p (a t)"), op=ALU.mult),
                         reads=["psA", K_("gsa")], writes=[K_("t1")])
                    P.op("dve", lambda e: e.tensor_tensor(out=t2[:], in0=psA[:, 512:1024], in1=gsb[:].rearrange("p a t -> p (a t)"), op=ALU.mult),
                         reads=["psA2", K_("gsb")], writes=[K_("t2")])
                    yield ("rel", ["psA", "psA2"])
                    P.op("pool", lambda e, hf=hf: e.tensor_tensor(out=zT[:, hf * 4:(hf + 1) * 4, :].rearrange("p a t -> p (a t)"), in0=t1[:], in1=t2[:],
                                                                  op=ALU.add), reads=[K_("t1"), K_("t2"), K_("zT")], writes=[K_("zT")])
                    yield

                def omm(e):
                    ins = None
                    for hf in range(2):
                        for m in range(8):
                            ins = e.matmul(psA[:, hf * 512:(hf + 1) * 512], lhsT=zT[:, m, :], rhs=wout[:, m, hf * 512:(hf + 1) * 512],
                                           start=(m == 0), stop=(m == 7))
                    return ins

                yield ("acq", ["psA", "psA2"])
                P.op("pe", omm, reads=[K_("zT"), "wout"], writes=["psA", "psA2"])
                yield
                P.op("dve", lambda e: e.tensor_tensor(out=x1[:], in0=psA[:], in1=xb_[:], op=ALU.add), reads=["psA", "psA2", K_("xinb")], writes=[K_("x1")])
                yield ("rel", ["psA", "psA2"])
                P.dma("sp", lambda e: e.dma_start(out=x1_d[tl * 128:(tl + 1) * 128, :], in_=x1[:]), reads=[K_("x1")], writes=["x1_d"])
                P.op("act", lambda e: e.activation(out=xsb[:], in_=x1[:], func=AF.Square, accum_out=ss[:]), reads=[K_("x1")], writes=[K_("xsb2"), K_("ss2")])
                yield
                P.op("act", lambda e: e.activation(out=sd[:], in_=ss[:], func=AF.Sqrt, bias=NORM_EPS, scale=1.0 / D), reads=[K_("ss2")], writes=[K_("sd2")])
                yield
                P.op("dve", lambda e: e.reciprocal(out=rs[:], in_=sd[:]), reads=[K_("sd2")], writes=[K_("rs2")])
                P.op("dve", lambda e: e.tensor_scalar(out=xsb[:], in0=x1[:], scalar1=rs[:], scalar2=None, op0=ALU.mult),
                     reads=[K_("x1"), K_("rs2"), K_("xsb2")], writes=[K_("xsb2")])
                yield
                pT = ub[:].bitcast(BF16)

                def tr(e):
                    ins = None
                    for k in range(8):
                        ins = e.transpose(out=pT[:, k * 128:(k + 1) * 128], in_=xsb[:, k * 128:(k + 1) * 128], identity=ident[:])
                    return ins

                yield ("acq", [ubk])
                P.op("pe", tr, reads=[K_("xsb2"), "ident"], writes=[ubk])
                yield
                P.op("dve", lambda e: e.tensor_tensor(out=h2t[:], in0=pT.rearrange("p (k t) -> p k t", k=8), in1=g2bc[:], op=ALU.mult),
                     reads=[ubk, "g2bc"], writes=[K_("h2t")])
                yield ("rel", [ubk])
                P.dma("sp", lambda e: e.dma_start(out=h2_d[:, :, tc], in_=h2t[:]), reads=[K_("h2t")], writes=["h2_d"])
                yield from router_tile(tl, h2t, K_("h2t"))

            STAG = 12
            active, nxt, tick = [], 0, 0
            held, blocked, extra = {}, {}, []
            ntile = 0
            while nxt < NT or active:
                ntile = sum(1 for g in active if g not in extra)
                if nxt < NT and ntile < NIF and (tick % STAG == 0 or ntile == 0):
                    active.append(m2_tile(nxt))
                    nxt += 1
                for g in extra:
                    if g not in active:
                        active.append(g)
                for g in list(active):
                    if g in blocked:
                        if any(k in held for k in blocked[g]):
                            continue
                        for k in blocked.pop(g):
                            held[k] = g
                    try:
                        r = next(g)
                    except StopIteration:
                        active.remove(g)
                        if g in extra:
                            extra.remove(g)
                        continue
                    if r is not None:
                        kind, keys = r
                        if kind == "acq":
                            if any(k in held for k in keys):
                                blocked[g] = keys
                            else:
                                for k in keys:
                                    held[k] = g
                        elif kind == "spawn":
                            extra.append(keys)
                        else:
                            for k in keys:
                                held.pop(k)
                tick += 1

            P.barrier()
        stP.close()

        with ExitStack() as st:
            P.scope = "router"
            h2 = sbuf(st, "h2", [128, 8, T], BF16)
            yacc = sbuf(st, "yacc", [128, NT, D], F32)
            fgrow = sbuf(st, "fgrow", [1, D], F32)
            fgbc = sbuf(st, "fgbc", [128, D], F32)
            P.dma("sp", lambda e: e.dma_start(out=h2[:], in_=h2_d), reads=["h2_d"], writes=["h2"])
            for tl in range(NT):
                P.dma("sp", lambda e, tl=tl: e.dma_start(out=yacc[:, tl, :], in_=x1_d[tl * 128:(tl + 1) * 128, :]), reads=["x1_d"], writes=[f"yacc{tl}"])
            P.dma("sp", lambda e: e.dma_start(out=fgrow[:], in_=fg_d), writes=["fgrow"])
            for hf in range(2):
                P.op("pe", lambda e, hf=hf: e.matmul(psM[0][:, 0:512], lhsT=onesf[0:1, :], rhs=fgrow[0:1, hf * 512:(hf + 1) * 512], start=True, stop=True),
                     reads=["onesf", "fgrow"], writes=["psM0"])
                P.op("act", lambda e, hf=hf: e.activation(out=fgbc[:, hf * 512:(hf + 1) * 512], in_=psM[0][:, 0:512], func=AF.Copy),
                     reads=["psM0", "fgbc"], writes=["fgbc"])

            P.scope = "experts"
            Wg = [sbuf(st, f"Wg{i}", [128, 8, 256], BF16) for i in range(2)]
            Wu = [sbuf(st, f"Wu{i}", [128, 8, 256], BF16) for i in range(2)]
            Wd = [sbuf(st, f"Wd{i}", [128, 2, D], BF16) for i in range(2)]
            GN_ = min(512, T)
            NG = T // GN_
            sg = [sbuf(st, f"sg{i}", [128, GN_], F32) for i in range(2)]
            actT = sbuf(st, "actT", [128, 2, GN_], BF16)
            gu_ps = [psM[0], psM[1], psS, psD[:, 0:512]]
            gu_k = ["psM0", "psM1", "psS", "psD0"]
            d_ps = [psA, psD[:, 512:1536]]
            d_k = [["psA", "psA2"], ["psD1", "psD2"]]
            actT2 = [actT, sbuf(st, "actTb", [128, 2, GN_], BF16)]
            TPG = GN_ // 128

            def load_expert(ex):
                b = ex % 2
                P.dma("pool", lambda e: e.dma_start(out=Wg[b][:], in_=weg_d[ex].rearrange("(k p) n -> p k n", p=128)), writes=[f"Wg{b}"])
                P.dma("pool", lambda e: e.dma_start(out=Wu[b][:], in_=weu_d[ex].rearrange("(k p) n -> p k n", p=128)), writes=[f"Wu{b}"])
                P.dma("pool", lambda e: e.dma_start(out=Wd[b][:], in_=wed_d[ex].rearrange("(k p) n -> p k n", p=128)), writes=[f"Wd{b}"])

            def G(u, f):
                ex, gi = divmod(u, NG)
                b = ex % 2
                gc = slice(gi * GN_, (gi + 1) * GN_)
                aT = actT2[u % 2]
                ak = f"actT{u % 2}_{f}"

                def gumm(e):
                    ins = None
                    for k in range(8):
                        e.matmul(gu_ps[2 * f][:, 0:GN_], lhsT=Wg[b][:, k, f * 128:(f + 1) * 128], rhs=h2[:, k, gc], start=(k == 0), stop=(k == 7))
                    for k in range(8):
                        ins = e.matmul(gu_ps[2 * f + 1][:, 0:GN_], lhsT=Wu[b][:, k, f * 128:(f + 1) * 128], rhs=h2[:, k, gc], start=(k == 0), stop=(k == 7))
                    return ins

                P.op("pe", gumm, reads=[f"Wg{b}", f"Wu{b}", "h2"], writes=[gu_k[2 * f], gu_k[2 * f + 1]])
                P.op("act", lambda e: e.activation(out=sg[f][:], in_=gu_ps[2 * f][:, 0:GN_], func=AF.Silu), reads=[gu_k[2 * f]], writes=[f"sg{f}"])
                P.op("dve", lambda e: e.tensor_tensor(out=aT[:, f, :], in0=gu_ps[2 * f + 1][:, 0:GN_], in1=sg[f][:], op=ALU.mult),
                     reads=[gu_k[2 * f + 1], f"sg{f}"], writes=[ak])

            dstate = [0]

            def Dn(u, ti):
                ex, gi = divmod(u, NG)
                b = ex % 2
                tl = gi * TPG + ti
                aT = actT2[u % 2]
                dps = d_ps[dstate[0] % 2]
                dk = d_k[dstate[0] % 2]
                dstate[0] += 1

                def dmm(e):
                    ins = None
                    for hf in range(2):
                        for f in range(2):
                            ins = e.matmul(dps[:, hf * 512:(hf + 1) * 512], lhsT=aT[:, f, ti * 128:(ti + 1) * 128],
                                           rhs=Wd[b][:, f, hf * 512:(hf + 1) * 512], start=(f == 0), stop=(f == 1))
                    return ins

                P.op("pe", dmm, reads=[f"actT{u % 2}_0", f"actT{u % 2}_1", f"Wd{b}"], writes=dk)
                P.op("dve", lambda e: e.scalar_tensor_tensor(out=yacc[:, tl, :], in0=dps[:, 0:1024], scalar=comb[:, tl, ex:ex + 1],
                                                             in1=yacc[:, tl, :], op0=ALU.mult, op1=ALU.add),
                     reads=dk + [f"comb{tl}", f"yacc{tl}"], writes=[f"yacc{tl}"])

            junk = sbuf(st, "junk3", [128, D], BF16)
            ss = sbuf(st, "ss3", [128, 1], F32)
            sd = sbuf(st, "sd3", [128, 1], F32)
            rs = sbuf(st, "rs3", [128, 1], F32)
            ob = [sbuf(st, f"ob{i}", [128, D], F32) for i in range(2)]

            def final_tile(tl):
                o = ob[tl % 2]
                ok = f"ob{tl % 2}"
                P.op("act", lambda e: e.activation(out=junk[:], in_=yacc[:, tl, :], func=AF.Square, accum_out=ss[:]),
                     reads=[f"yacc{tl}"], writes=["junk3", "ss3"])
                P.op("act", lambda e: e.activation(out=sd[:], in_=ss[:], func=AF.Sqrt, bias=NORM_EPS, scale=1.0 / D), reads=["ss3"], writes=["sd3"])
                P.op("dve", lambda e: e.reciprocal(out=rs[:], in_=sd[:]), reads=["sd3"], writes=["rs3"])
                P.op("dve", lambda e: e.scalar_tensor_tensor(out=o[:], in0=yacc[:, tl, :], scalar=rs[:], in1=fgbc[:], op0=ALU.mult, op1=ALU.mult),
                     reads=[f"yacc{tl}", "rs3", "fgbc"], writes=[ok])
                P.dma("sp", lambda e: e.dma_start(out=out_d[tl * 128:(tl + 1) * 128, :], in_=o[:]), reads=[ok], writes=["out"])

            NU = NE * NG
            load_expert(0)
            if NE > 1:
                load_expert(1)
            G(0, 0)
            G(0, 1)
            for u in range(NU):
                ex, gi = divmod(u, NG)
                P.scope = "experts" if ex != 5 else "ex5"
                half = (TPG + 1) // 2
                if u + 1 < NU:
                    G(u + 1, 0)
                for ti in range(0, half):
                    Dn(u, ti)
                    if ex == NE - 1:
                        final_tile(gi * TPG + ti)
                if u + 1 < NU:
                    G(u + 1, 1)
                for ti in range(half, TPG):
                    Dn(u, ti)
                    if ex == NE - 1:
                        final_tile(gi * TPG + ti)
                if gi == NG - 1 and ex + 2 < NE:
                    load_expert(ex + 2)

            P.final_wait("sp")
            P.emit()
    return nc


def host_layout(inp, b, T):
    f = lambda a: np.ascontiguousarray(np.asarray(a, dtype=np.float32))

    def colpack(v, n):
        v = np.asarray(v, np.float32).reshape(-1)
        pad = np.zeros(n * 128, np.float32)
        pad[:v.size] = v
        return pad.reshape(n, 128).T

    cols = np.concatenate([
        colpack(inp["tmix_mu"][0], 15), colpack(inp["k_k"][0], 4), colpack(inp["k_a"][0], 4), colpack(inp["r_k"][0], 4),
        colpack(inp["a0"][0], 4), colpack(inp["lnx_g"][0], 4), colpack(inp["lnx_b"][0], 4), colpack(inp["b_gate"][0], 16),
        colpack(inp["norm1_g"][0], 8), colpack(inp["norm2_g"][0], 8)], axis=1)
    w_re = np.asarray(inp["w_re"][0], np.float32)
    w_r = np.concatenate([np.asarray(inp["w_rg"][0], np.float32), w_re.transpose(1, 0, 2).reshape(D, 32)], axis=1)
    b_r = np.concatenate([np.asarray(inp["b_rg"][0], np.float32).reshape(-1), np.asarray(inp["b_re"][0], np.float32).reshape(-1)])
    return {
        "x": f(inp["x"][b, :T]), "w_in": f(inp["w_in"][0]), "cols": f(cols), "w0": f(inp["w0"][0]).reshape(1, 512),
        "w2": f(inp["w2"][0]), "a2": f(inp["a2"][0]), "g2": f(inp["g2"][0]), "w_oA": f(inp["w_oA"][0]), "w_oB": f(inp["w_oB"][0]),
        "w_out": f(inp["w_out"][0]), "lnv_g": f(inp["lnv_g"][0]).reshape(1, 512), "lnv_b": f(inp["lnv_b"][0]).reshape(1, 512),
        "wsT": f(np.asarray(inp["w_s"][0], np.float32).transpose(0, 2, 1)), "b_s": f(inp["b_s"][0]).reshape(1, 512),
        "w_r": f(w_r), "b_r": f(b_r).reshape(1, 36), "w_e_gate": f(inp["w_e_gate"][0]), "w_e_up": f(inp["w_e_up"][0]),
        "w_e_down": f(inp["w_e_down"][0]), "final_g": f(inp["final_g"]).reshape(1, D),
    }


def kernel(**inputs):
    T = 2048
    nc = build(T)
    in_maps = [host_layout(inputs, b, T) for b in range(8)]
    res = run_bass_kernel_spmd(nc, in_maps, core_ids=list(range(8)))
    return np.stack([np.asarray(r["out"], dtype=np.float32) for r in res.results], axis=0)
```

```python
import re
import numpy as np
from contextlib import ExitStack
import concourse.bass as bass
import concourse.mybir as mybir
from concourse.bass_utils import run_bass_kernel_spmd

F32 = mybir.dt.float32
BF16 = mybir.dt.bfloat16
AF = mybir.ActivationFunctionType
ALU = mybir.AluOpType
AX = mybir.AxisListType
ENGS = ["pe", "dve", "act", "pool", "sp"]
DMA_RING = {"sp": 6, "pool": 4}

D = 1024
IN_COLS = 4896
A_COLS = 1824
NCHA = 15
LNX_EPS = 64e-5
LN_EPS = 1e-5
NORM_EPS = 1e-6
EXPM05 = float(np.exp(-0.5))
PROFILE_SCOPES = False

C_MU, C_KK, C_KA, C_RK, C_A0, C_LG, C_LB, C_BG, C_G1, C_G2 = 0, 15, 19, 23, 27, 31, 35, 39, 55, 63
NCOL = 71


class Rec:
    def __init__(self):
        self.calls = []

    def __getattr__(self, name):
        def f(*a, **k):
            self.calls.append((name, a, k))
            return self
        return f


def _record(fn):
    r = Rec()
    fn(r)
    assert r.calls
    return r.calls


class Prog:
    def __init__(self, nc, stack):
        self.nc = nc
        self.lists = {e: [] for e in ENGS}
        self.count = {e: 0 for e in ENGS}
        self.sems = {}
        for e in ENGS:
            self.sems[("e", e)] = stack.enter_context(nc.semaphore(f"s_{e}"))
        for q, r in DMA_RING.items():
            for i in range(r):
                self.sems[("d", q, i)] = stack.enter_context(nc.semaphore(f"d_{q}{i}"))
        self.dma_n = {q: 0 for q in DMA_RING}
        self.waited = {e: {} for e in ENGS}
        self.last_w = {}
        self.readers = {}
        self.scope = None

    def _waits(self, eng, reads, writes, extra=()):
        need = {}

        def add(s, v):
            if need.get(s, 0) < v:
                need[s] = v

        for k in reads:
            if k in self.last_w:
                add(*self.last_w[k])
        for k in writes:
            if k in self.last_w:
                add(*self.last_w[k])
            for s, v in self.readers.get(k, {}).items():
                add(s, v)
        for s, v in extra:
            add(s, v)
        out = []
        wd = self.waited[eng]
        for s, v in need.items():
            if wd.get(s, 0) < v:
                wd[s] = v
                out.append((s, v))
        return out

    def _commit(self, tok, reads, writes):
        for k in writes:
            self.last_w[k] = tok
            self.readers[k] = {}
        for k in reads:
            d = self.readers.setdefault(k, {})
            if d.get(tok[0], 0) < tok[1]:
                d[tok[0]] = tok[1]

    LIMIT = None
    NOPS = 0

    def op(self, eng, fn, reads=(), writes=()):
        Prog.NOPS += 1
        if Prog.LIMIT is not None and Prog.NOPS > Prog.LIMIT:
            return None
        writes = list(writes) + [k for k in reads if k.startswith("ps")]
        waits = self._waits(eng, reads, writes)
        self.count[eng] += 1
        tok = (("e", eng), self.count[eng])
        self.lists[eng].append((waits, _record(fn), (("e", eng), 1), self.scope))
        self._commit(tok, reads, writes)
        return tok

    def dma(self, q, fn, reads=(), writes=()):
        Prog.NOPS += 1
        if Prog.LIMIT is not None and Prog.NOPS > Prog.LIMIT:
            return None
        r = DMA_RING[q]
        i = self.dma_n[q]
        self.dma_n[q] += 1
        slot = i % r
        skey = ("d", q, slot)
        extra = [(skey, 16 * (i // r))] if i >= r else []
        waits = self._waits(q, reads, writes, extra)
        tok = (skey, 16 * (i // r + 1))
        self.lists[q].append((waits, _record(fn), (skey, 16), self.scope))
        self._commit(tok, reads, writes)
        return tok

    def _all_tokens(self):
        toks = []
        for q, r in DMA_RING.items():
            n = self.dma_n[q]
            for slot in range(min(r, n)):
                toks.append((("d", q, slot), 16 * ((n - 1 - slot) // r + 1)))
        for e in ENGS:
            if self.count[e]:
                toks.append((("e", e), self.count[e]))
        return toks

    def barrier(self):
        toks = self._all_tokens()
        for e in ENGS:
            waits = self._waits(e, (), (), toks)
            if waits:
                self.lists[e].append((waits, None, None, None))

    def final_wait(self, eng):
        waits = self._waits(eng, (), (), self._all_tokens())
        self.lists[eng].append((waits, None, None, None))

    def emit(self):
        engmap = {"pe": "tensor", "dve": "vector", "act": "scalar", "pool": "gpsimd", "sp": "sync"}
        with self.nc.Block() as block:
            for e in ENGS:
                lst = self.lists[e]
                if not lst:
                    continue

                def body(engine, lst=lst):
                    cur, cm = None, None
                    for waits, fn, inc, scope in lst:
                        if PROFILE_SCOPES and scope != cur:
                            if cm is not None:
                                cm.__exit__(None, None, None)
                                cm = None
                            if scope is not None:
                                cm = self.nc.named_scope(scope)
                                cm.__enter__()
                            cur = scope
                        for s, v in waits:
                            engine.wait_ge(self.sems[s], v)
                        if fn is not None:
                            ins = None
                            for name, a, k in fn:
                                ins = getattr(engine, name)(*a, **k)
                            ins.then_inc(self.sems[inc[0]], inc[1])
                    if cm is not None:
                        cm.__exit__(None, None, None)

                getattr(block, engmap[e])(body)


def build(T, NE=32, dbg=None):
    NT = T // 128
    nc = bass.Bass("TRN2", target_bir_lowering=False)

    def din(name, shape, dt=F32):
        return nc.dram_tensor(name, list(shape), dt, kind="ExternalInput").ap()

    x_d = din("x", [T, D])
    w_in_d = din("w_in", [D, IN_COLS])
    cols_d = din("cols", [128, NCOL])
    w0_d = din("w0", [1, 512])
    w2_d = din("w2", [64, 512])
    a2_d = din("a2", [64, 512])
    g2_d = din("g2", [160, 512])
    woA_d = din("w_oA", [512, D])
    woB_d = din("w_oB", [512, D])
    wout_d = din("w_out", [D, D])
    lnvg_d = din("lnv_g", [1, 512])
    lnvb_d = din("lnv_b", [1, 512])
    wsT_d = din("wsT", [4, 128, 128])
    bs_d = din("b_s", [1, 512])
    wr_d = din("w_r", [D, 36])
    br_d = din("b_r", [1, 36])
    weg_d = din("w_e_gate", [32, D, 256])
    weu_d = din("w_e_up", [32, D, 256])
    wed_d = din("w_e_down", [32, 256, D])
    fg_d = din("final_g", [1, D])
    out_d = nc.dram_tensor("out", [T, D], F32, kind="ExternalOutput").ap()
    x1_d = nc.dram_tensor("x1_scr", [T, D], F32, kind="Internal").ap()
    h2_d = nc.dram_tensor("h2_scr", [128, 8, T], BF16, kind="Internal").ap()
    hT_d = nc.dram_tensor("hT_scr", [T // 128, 128, 1024], BF16, kind="Internal").ap()
    yg_d = nc.dram_tensor("yg_scr", [T // 128, 128, 512], BF16, kind="Internal").ap()
    dbg_out = {}
    if dbg:
        for name, shape in dbg.items():
            dbg_out[name] = nc.dram_tensor("dbg_" + name, list(shape), F32, kind="ExternalOutput").ap()

    with ExitStack() as st0:
        P = Prog(nc, st0)

        def sbuf(st, name, shape, dt):
            return st.enter_context(nc.sbuf_tensor("sb_" + name, list(shape), dt))

        psM = [st0.enter_context(nc.psum_tensor(f"psM{i}", [128, 512], F32)) for i in range(2)]
        psA = st0.enter_context(nc.psum_tensor("psA", [128, 1024], F32))
        psD = st0.enter_context(nc.psum_tensor("psD", [128, 1536], F32))
        psS = st0.enter_context(nc.psum_tensor("psS", [128, 512], F32))

        identf = sbuf(st0, "identf", [128, 128], F32)
        ident = sbuf(st0, "ident", [128, 128], BF16)
        mSU = sbuf(st0, "mSU", [128, 128], F32)
        mIU = sbuf(st0, "mIU", [128, 128], F32)
        maskA = sbuf(st0, "maskA", [128, 2, 512], F32)
        maskL = sbuf(st0, "maskL", [128, 2, 128], F32)
        BDf = sbuf(st0, "BDf", [128, 128], F32)
        BDb = sbuf(st0, "BDb", [128, 128], BF16)
        BD64 = sbuf(st0, "BD64", [128, 128], F32)
        Tri2 = sbuf(st0, "Tri2", [128, 256], F32)
        onesb = sbuf(st0, "onesb", [1, 128], BF16)
        onesf = sbuf(st0, "onesf", [128, 128], F32)
        cols = sbuf(st0, "cols", [128, NCOL], F32)
        omu = sbuf(st0, "omu", [128, NCHA], F32)
        oka = sbuf(st0, "oka", [128, 4], F32)
        g1bc = sbuf(st0, "g1bc", [128, 8, 128], F32)
        g2bc = sbuf(st0, "g2bc", [128, 8, 128], F32)

        def colap(c):
            return cols[:, c:c + 1]

        P.dma("sp", lambda e: e.dma_start(out=cols[:], in_=cols_d), writes=["cols"])
        P.op("pool", lambda e: e.memset(onesf[:], 1.0), writes=["onesf"])
        P.op("pool", lambda e: e.memset(identf[:], 0.0), writes=["identf"])
        P.op("pool", lambda e: e.affine_select(out=identf[:], in_=identf[:], pattern=[[-1, 128]], compare_op=ALU.not_equal,
                                               fill=1.0, base=0, channel_multiplier=1), reads=["identf"], writes=["identf"])
        P.op("dve", lambda e: e.tensor_copy(out=ident[:], in_=identf[:]), reads=["identf"], writes=["ident"])
        P.op("pool", lambda e: e.affine_select(out=mSU[:], in_=onesf[:], pattern=[[1, 128]], compare_op=ALU.is_gt,
                                               fill=0.0, base=0, channel_multiplier=-1), reads=["onesf"], writes=["mSU"])
        P.op("pool", lambda e: e.affine_select(out=mIU[:], in_=onesf[:], pattern=[[1, 128]], compare_op=ALU.is_ge,
                                               fill=0.0, base=0, channel_multiplier=-1), reads=["onesf"], writes=["mIU"])
        for h in range(2):
            for q in range(4):
                src = mSU if q % 2 == 0 else mIU
                P.op("dve", lambda e, h=h, q=q, src=src: e.tensor_copy(out=maskA[:, h, q * 128:(q + 1) * 128], in_=src[:]),
                     reads=["mSU", "mIU"], writes=["maskA"])
            P.op("pool", lambda e, h=h: e.affine_select(out=maskL[:, h, :], in_=onesf[:], pattern=[[-1, 128]], compare_op=ALU.is_gt,
                                                        fill=0.0, base=0, channel_multiplier=1), reads=["onesf"], writes=["maskL"])
        P.op("pool", lambda e: e.memset(BDf[:], 0.0), writes=["BDf"])
        P.op("pool", lambda e: e.memset(BDf[0:64, 0:64], 1.0), reads=["BDf"], writes=["BDf"])
        P.op("pool", lambda e: e.memset(BDf[64:128, 64:128], 1.0), reads=["BDf"], writes=["BDf"])
        P.op("dve", lambda e: e.tensor_copy(out=BDb[:], in_=BDf[:]), reads=["BDf"], writes=["BDb"])
        P.op("dve", lambda e: e.tensor_scalar(out=BD64[:], in0=BDf[:], scalar1=1.0 / 64.0, scalar2=None, op0=ALU.mult),
             reads=["BDf"], writes=["BD64"])
        P.op("dve", lambda e: e.tensor_scalar(out=Tri2[:, 0:128], in0=mIU[:], scalar1=EXPM05, scalar2=None, op0=ALU.mult),
             reads=["mIU"], writes=["Tri2"])
        P.op("dve", lambda e: e.tensor_scalar(out=Tri2[:, 128:256], in0=mSU[:], scalar1=EXPM05, scalar2=None, op0=ALU.mult),
             reads=["mSU", "Tri2"], writes=["Tri2"])
        P.op("dve", lambda e: e.tensor_copy(out=onesb[:], in_=onesf[0:1, :]), reads=["onesf"], writes=["onesb"])
        P.op("dve", lambda e: e.tensor_scalar(out=omu[:], in0=cols[:, C_MU:C_MU + NCHA], scalar1=-1.0, scalar2=1.0,
                                              op0=ALU.mult, op1=ALU.add), reads=["cols"], writes=["omu"])
        P.op("dve", lambda e: e.tensor_scalar(out=oka[:], in0=cols[:, C_KA:C_KA + 4], scalar1=-1.0, scalar2=1.0,
                                              op0=ALU.mult, op1=ALU.add), reads=["cols"], writes=["oka"])
        for k in range(8):
            P.op("dve", lambda e, k=k: e.tensor_scalar(out=g1bc[:, k, :], in0=onesf[:], scalar1=colap(C_G1 + k), scalar2=None,
                                                       op0=ALU.mult), reads=["cols", "onesf"], writes=["g1bc"])
            P.op("dve", lambda e, k=k: e.tensor_scalar(out=g2bc[:, k, :], in0=onesf[:], scalar1=colap(C_G2 + k), scalar2=None,
                                                       op0=ALU.mult), reads=["cols", "onesf"], writes=["g2bc"])

        def rms_to_T(st_keys, xin, xin_key, gbc, gbc_key, dst_ap, dst_key, tmp, tag):
            junk, ss, sd, rs, xsb = tmp
            P.op("act", lambda e: e.activation(out=junk[:], in_=xin, func=AF.Square, accum_out=ss[:]),
                 reads=[xin_key], writes=["junk" + tag, "ss" + tag])
            P.op("act", lambda e: e.activation(out=sd[:], in_=ss[:], func=AF.Sqrt, bias=NORM_EPS, scale=1.0 / D),
                 reads=["ss" + tag], writes=["sd" + tag])
            P.op("dve", lambda e: e.reciprocal(out=rs[:], in_=sd[:]), reads=["sd" + tag], writes=["rs" + tag])
            P.op("dve", lambda e: e.tensor_scalar(out=xsb[:], in0=xin, scalar1=rs[:], scalar2=None, op0=ALU.mult),
                 reads=[xin_key, "rs" + tag], writes=["xsb" + tag])
            pT = psM[0][:].bitcast(BF16)

            def tr(e):
                ins = None
                for k in range(8):
                    ins = e.transpose(out=pT[:, k * 128:(k + 1) * 128], in_=xsb[:, k * 128:(k + 1) * 128], identity=ident[:])
                return ins

            P.op("pe", tr, reads=["xsb" + tag, "ident"], writes=["psM0"])
            P.op("dve", lambda e: e.tensor_tensor(out=dst_ap, in0=pT.rearrange("p (k t) -> p k t", k=8), in1=gbc[:], op=ALU.mult),
                 reads=["psM0", gbc_key], writes=[dst_key])

        P.scope = "M1"
        stP = ExitStack()
        comb = sbuf(st0, "comb", [128, NT, 32], F32)
        woA = sbuf(stP, "woA", [128, 4, D], BF16)
        woB = sbuf(stP, "woB", [128, 4, D], BF16)
        wout = sbuf(stP, "wout", [128, 8, D], BF16)

        with ExitStack() as st:
            hTb = [sbuf(st, f"hTt{i}", [128, 8, 128], BF16) for i in range(2)]
            ygb = [sbuf(st, f"ygt{i}", [128, 4, 128], BF16) for i in range(2)]
            w_inA = sbuf(st, "w_inA", [128, 8, A_COLS], BF16)
            w2b = sbuf(st, "w2b", [64, 512], BF16)
            a2b = sbuf(st, "a2b", [128, 512], BF16)
            g2b = sbuf(st, "g2b", [128, 2, 512], BF16)
            w0row = sbuf(st, "w0row", [1, 512], BF16)
            for k in range(8):
                P.dma("pool", lambda e, k=k: e.dma_start(out=w_inA[:, k, :], in_=w_in_d[k * 128:(k + 1) * 128, 0:A_COLS]),
                      writes=["w_inA"])
            P.dma("pool", lambda e: e.dma_start(out=w2b[:], in_=w2_d), writes=["w2b"])
            P.dma("pool", lambda e: e.dma_start(out=a2b[64:128, :], in_=a2_d), writes=["a2b"])
            P.dma("pool", lambda e: e.dma_start(out=g2b[:, 0, :], in_=g2_d[0:128, :]), writes=["g2b"])
            P.dma("pool", lambda e: e.dma_start(out=g2b[0:32, 1, :], in_=g2_d[128:160, :]), reads=["g2b"], writes=["g2b"])
            P.dma("pool", lambda e: e.dma_start(out=w0row[:], in_=w0_d), writes=["w0row"])
            P.dma("pool", lambda e: e.dma_start(out=woA[:], in_=woA_d.rearrange("(k p) n -> p k n", p=128)), writes=["woA"])
            P.dma("pool", lambda e: e.dma_start(out=woB[:], in_=woB_d.rearrange("(k p) n -> p k n", p=128)), writes=["woB"])
            P.dma("pool", lambda e: e.dma_start(out=wout[:], in_=wout_d.rearrange("(k p) n -> p k n", p=128)), writes=["wout"])

            xin = [sbuf(st, f"xin{i}", [128, D], F32) for i in range(2)]
            junk = sbuf(st, "junk", [128, D], BF16)
            ss = sbuf(st, "ss", [128, 1], F32)
            sd = sbuf(st, "sd", [128, 1], F32)
            rs = sbuf(st, "rs", [128, 1], F32)
            xsb = sbuf(st, "xsb", [128, D], BF16)
            carry = sbuf(st, "carry", [128, NCHA], F32)
            ltmp = sbuf(st, "ltmp", [128, 129], F32)
            pmx = sbuf(st, "pmx", [128, 128], F32)
            txw2 = [sbuf(st, f"txw_{i}", [128, 128], BF16) for i in range(2)]
            sxg2 = [sbuf(st, f"sxg_{i}", [128, 2, 128], BF16) for i in range(2)]
            DC2 = [sbuf(st, f"DC_{i}", [128, 4], F32) for i in range(2)]
            etok = sbuf(st, "etok", [128, 512], F32)
            Dt2 = [sbuf(st, f"Dt_{i}", [128, 4, 128], F32) for i in range(2)]
            Dinv2 = [sbuf(st, f"Dinv_{i}", [128, 4, 128], F32) for i in range(2)]
            Dprev2 = [sbuf(st, f"Dprev_{i}", [128, 4, 128], F32) for i in range(2)]
            asig = [sbuf(st, f"asig{p}", [128, 128], F32) for p in range(4)]
            tA = [sbuf(st, f"tA{p}", [128, 128], F32) for p in range(4)]
            tB = [sbuf(st, f"tB{p}", [128, 128], F32) for p in range(4)]
            tC = [sbuf(st, f"tC{p}", [128, 128], F32) for p in range(4)]
            t16 = [sbuf(st, f"t16{p}", [128, 128], BF16) for p in range(4)]
            v16 = [sbuf(st, f"v16{p}", [128, 128], BF16) for p in range(4)]
            pmr = [sbuf(st, f"pmr{p}", [128, 128], F32) for p in range(4)]
            pmk = [sbuf(st, f"pmk{p}", [128, 128], F32) for p in range(4)]
            pmv = [sbuf(st, f"pmv{p}", [128, 128], F32) for p in range(4)]
            ltm = [sbuf(st, f"ltm{p}", [128, 129], F32) for p in range(4)]
            PX = [psM[0], psM[1], psA[:, 0:512], psA[:, 512:1024]]
            PXk = ["psM0", "psM1", "psA", "psA2"]
            PY = [psD[:, 0:512], psD[:, 512:1024], psD[:, 1024:1536], psS]
            PYk = ["psD0", "psD1", "psD2", "psS"]
            AR2 = [[sbuf(st, f"AR{p}_{i}", [128, 2, 128], BF16) for p in range(4)] for i in range(1)] * 2
            BT = [sbuf(st, f"BT{p}", [128, 128], BF16) for p in range(4)]
            KT = [sbuf(st, f"KT{p}", [128, 128], BF16) for p in range(4)]
            TOK2 = [[sbuf(st, f"TOK{p}_{i}", [128, 3, 128], BF16) for p in range(4)] for i in range(1)] * 2
            bon2 = [[sbuf(st, f"bon{p}_{i}", [128, 128], F32) for p in range(4)] for i in range(1)] * 2
            gT2 = [[sbuf(st, f"gT{p}_{i}", [128, 128], F32) for p in range(4)] for i in range(1)] * 2
            AM2 = [[sbuf(st, f"AM{p}_{i}", [128, 2, 512], BF16) for p in range(4)] for i in range(1)] * 2
            L0 = [sbuf(st, f"L0{p}", [128, 2, 128], BF16) for p in range(4)]
            MT2 = [[[sbuf(st, f"MT{p}_{i}_{j}", [128, 2, 256], BF16) for i in range(2)] for p in range(4)] for j in range(1)] * 2
            LK = [[sbuf(st, f"LK{p}_{i}", [128, 2, 128], BF16) for i in range(2)] for p in range(4)]
            Sw = [sbuf(st, f"Sw{p}", [128, 128], F32) for p in range(4)]
            Sb = [sbuf(st, f"Sb{p}", [128, 128], BF16) for p in range(4)]
            Xb = [sbuf(st, f"Xb{p}", [128, 128], BF16) for p in range(4)]
            Ub = [sbuf(st, f"Ub{p}", [128, 128], BF16) for p in range(4)]
            yT = sbuf(st, "yT", [128, 4, 128], F32)
            yc = sbuf(st, "yc", [128, 512], F32)
            ysq = sbuf(st, "ysq", [128, 512], F32)
            yrs = sbuf(st, "yrs", [128, 512], F32)
            y3 = sbuf(st, "y3", [128, 128], F32)
            tS = sbuf(st, "tS", [128, 128], F32)

            P.op("pool", lambda e: e.memset(carry[:], 0.0), writes=[f"carry{c}" for c in range(NCHA)])
            for p in range(4):
                P.op("pool", lambda e, p=p: e.memset(Sw[p][:], 0.0), writes=[f"Sw{p}"])
                P.op("pool", lambda e, p=p: e.memset(Sb[p][:], 0.0), writes=[f"Sb{p}"])

            def LV(p):
                return psD[:, 1024:1280] if p % 2 == 0 else psS[:, 0:256]

            def DK(p):
                return ["psD0", "psD2"] if p % 2 == 0 else ["psD1", "psS"]

            serial_ps = [psS, psM[1], psA[:, 0:512], psA[:, 512:1024]]
            serial_key = ["psS", "psM1", "psA", "psA2"]

            def tile_parts(tl, P):
                par = tl % 2
                AM, AR, TOK, MT, bon, gT = AM2[par], AR2[par], TOK2[par], MT2[par], bon2[par], gT2[par]
                Dt, Dinv, Dprev, txw, sxg, DC = Dt2[par], Dinv2[par], Dprev2[par], txw2[par], sxg2[par], DC2[par]
                tc = slice(tl * 128, (tl + 1) * 128)
                xb_, xk = xin[tl % 2], f"xin{tl % 2}"
                hT, hTk = hTb[tl % 2], f"hTt{tl % 2}"
                ygT, ygk = ygb[tl % 2], f"ygt{tl % 2}"

                def inproj_chunk(c, dst_fn):
                    rows = 128 if c < 14 else 32
                    bank, bkey = (psD[:, 512:1024], "psD1") if c == 13 else (psM[0], "psM0")

                    def mmf(e, c=c, rows=rows, bank=bank):
                        ins = None
                        for k in range(8):
                            ins = e.matmul(bank[0:rows, 0:128], lhsT=w_inA[:, k, c * 128:c * 128 + rows], rhs=hT[:, k, :],
                                           start=(k == 0), stop=(k == 7))
                        return ins

                    P.op("pe", mmf, reads=["w_inA", hTk], writes=[bkey])
                    P.op("act", lambda e: e.activation(out=ltmp[0:rows, 1:129], in_=bank[0:rows, 0:128], func=AF.Copy,
                                                       scale=cols[0:rows, C_MU + c:C_MU + c + 1]),
                         reads=[bkey, "cols"], writes=["ltmp"])
                    P.op("pool", lambda e: e.tensor_copy(out=ltmp[0:rows, 0:1], in_=carry[0:rows, c:c + 1]),
                         reads=[f"carry{c}", "ltmp"], writes=["ltmp"])
                    P.op("pool", lambda e: e.tensor_copy(out=carry[0:rows, c:c + 1], in_=ltmp[0:rows, 128:129]),
                         reads=["ltmp", f"carry{c}"], writes=[f"carry{c}"])
                    dst, dkey = dst_fn
                    P.op("dve", lambda e: e.scalar_tensor_tensor(out=dst[0:rows, :], in0=bank[0:rows, 0:128],
                                                                 scalar=omu[0:rows, c:c + 1], in1=ltmp[0:rows, 0:128],
                                                                 op0=ALU.mult, op1=ALU.add),
                         reads=[bkey, "omu", "ltmp"], writes=[dkey])

                def inproj_chunk_g(c, dst, dkey, bank, bkey, lt, ltk):
                    rows = 128

                    def mmf(e):
                        ins = None
                        for k in range(8):
                            ins = e.matmul(bank[0:rows, 0:128], lhsT=w_inA[:, k, c * 128:c * 128 + rows], rhs=hT[:, k, :],
                                           start=(k == 0), stop=(k == 7))
                        return ins

                    P.op("pe", mmf, reads=["w_inA", hTk], writes=[bkey])
                    yield
                    P.op("act", lambda e: e.activation(out=lt[:, 1:129], in_=bank[:, 0:128], func=AF.Copy, scale=cols[:, C_MU + c:C_MU + c + 1]),
                         reads=[bkey, "cols"], writes=[ltk])
                    yield
                    P.op("pool", lambda e: e.tensor_copy(out=lt[:, 0:1], in_=carry[:, c:c + 1]), reads=[f"carry{c}", ltk], writes=[ltk])
                    P.op("pool", lambda e: e.tensor_copy(out=carry[:, c:c + 1], in_=lt[:, 128:129]), reads=[ltk, f"carry{c}"], writes=[f"carry{c}"])
                    yield
                    P.op("dve", lambda e: e.scalar_tensor_tensor(out=dst[:], in0=bank[:, 0:128], scalar=omu[:, c:c + 1], in1=lt[:, 0:128],
                                                                 op0=ALU.mult, op1=ALU.add), reads=[bkey, "omu", ltk], writes=[dkey])
                    yield


                def front():
                    xb_ = xin[tl % 2]
                    xk = f"xin{tl % 2}"
                    P.dma("sp", lambda e, xb_=xb_, tl=tl: e.dma_start(out=xb_[:], in_=x_d[tl * 128:(tl + 1) * 128, :]), writes=[xk])
                    yield
                    hT, hTk = hTb[tl % 2], f"hTt{tl % 2}"
                    ygT, ygk = ygb[tl % 2], f"ygt{tl % 2}"
                    rms_to_T(None, xb_[:], xk, g1bc, "g1bc", hT[:], hTk, (junk, ss, sd, rs, xsb), "1")
                    yield
                    P.dma("sp", lambda e, tl=tl, hT=hT: e.dma_start(out=hT_d[tl], in_=hT[:].rearrange("p k t -> p (k t)")), reads=[hTk], writes=["hT_d"])
                    yield

                    inproj_chunk(12, (pmx, "pmx"))
                    yield
                    P.op("act", lambda e: e.activation(out=txw[0:64, :], in_=pmx[0:64, :], func=AF.Tanh), reads=["pmx"], writes=["txw"])
                    yield
                    P.op("dve", lambda e: e.tensor_copy(out=txw[64:128, :], in_=pmx[64:128, :]), reads=["pmx", "txw"], writes=["txw"])
                    yield
                    inproj_chunk(13, (pmx, "pmx"))
                    yield
                    P.op("act", lambda e: e.activation(out=sxg[:, 0, :], in_=pmx[:], func=AF.Sigmoid), reads=["pmx"], writes=["sxg"])
                    yield
                    inproj_chunk(14, (pmx, "pmx"))
                    yield
                    P.op("act", lambda e: e.activation(out=sxg[0:32, 1, :], in_=pmx[0:32, :], func=AF.Sigmoid),
                         reads=["pmx", "sxg"], writes=["sxg"])
                    yield

                    def zmm(e):
                        e.matmul(psM[0][:, 0:512], lhsT=txw[0:64, :], rhs=w2b[0:64, :], start=True, stop=False)
                        return e.matmul(psM[0][:, 0:512], lhsT=onesb[0:1, :], rhs=w0row[0:1, :], start=False, stop=True)

                    P.op("pe", zmm, reads=["txw", "w2b", "onesb", "w0row"], writes=["psM0"])
                    yield
                    P.op("act", lambda e: e.activation(out=etok[:], in_=psM[0][:, 0:512], func=AF.Sigmoid), reads=["psM0"], writes=["etok"])
                    yield

                    def cmm(e):
                        ins = None
                        for p in range(4):
                            ins = e.matmul(psD[:, 512 + p * 256:512 + (p + 1) * 256], lhsT=etok[:, p * 128:(p + 1) * 128], rhs=Tri2[:],
                                           start=True, stop=True)
                        return ins

                    P.op("pe", cmm, reads=["etok", "Tri2"], writes=["psD1", "psD2"])
                    yield
                    cum = psD[:, 512:1536].rearrange("p (a c) -> p a c", a=4)
                    P.op("act", lambda e: e.activation(out=Dt[:], in_=cum[:, :, 0:128], func=AF.Exp, scale=-1.0),
                         reads=["psD1", "psD2"], writes=["Dt"])
                    yield
                    P.op("act", lambda e: e.activation(out=Dinv[:], in_=cum[:, :, 0:128], func=AF.Exp, scale=1.0),
                         reads=["psD1", "psD2"], writes=["Dinv"])
                    yield
                    P.op("act", lambda e: e.activation(out=Dprev[:], in_=cum[:, :, 128:256], func=AF.Exp, scale=-1.0),
                         reads=["psD1", "psD2"], writes=["Dprev"])
                    yield


                def pair_gen(p):
                    X, Xk, Y, Yk = PX[p], PXk[p], PY[p], PYk[p]
                    rf, kf, vf = pmr[p], pmk[p], pmv[p]
                    rk_, kk_, vk_ = f"pmr{p}", f"pmk{p}", f"pmv{p}"
                    as_, tA_, tB_, tC_, t16_, v16_ = asig[p], tA[p], tB[p], tC[p], t16[p], v16[p]
                    ak, tAk, tBk, tCk, t16k, v16k = f"asig{p}", f"tA{p}", f"tB{p}", f"tC{p}", f"t16{p}", f"v16{p}"
                    cs = slice(p * 128, (p + 1) * 128)
                    for (c, dst, dkey, bank, bkey) in ((p, rf, rk_, X, Xk), (4 + p, kf, kk_, Y, Yk), (8 + p, vf, vk_, X, Xk)):
                        yield from inproj_chunk_g(c, dst, dkey, bank, bkey, ltm[p], f"ltm{p}")
                    P.op("pe", lambda e: e.matmul(Y[:, 0:128], lhsT=a2b[64:128, cs], rhs=txw[64:128, :], start=True, stop=True),
                         reads=["a2b", "txw"], writes=[Yk])
                    yield
                    P.op("act", lambda e: e.activation(out=as_[:], in_=Y[:, 0:128], func=AF.Sigmoid, bias=colap(C_A0 + p)),
                         reads=[Yk, "cols"], writes=[ak])
                    yield

                    def gmm(e):
                        e.matmul(X[:, 0:128], lhsT=g2b[:, 0, cs], rhs=sxg[:, 0, :], start=True, stop=False)
                        return e.matmul(X[:, 0:128], lhsT=g2b[0:32, 1, cs], rhs=sxg[0:32, 1, :], start=False, stop=True)

                    P.op("pe", gmm, reads=["g2b", "sxg"], writes=[Xk])
                    yield
                    P.op("act", lambda e: e.activation(out=gT[p][:], in_=X[:, 0:128], func=AF.Copy), reads=[Xk], writes=[f"gT{p}"])
                    yield
                    P.op("dve", lambda e: e.tensor_scalar(out=tA_[:], in0=kf[:], scalar1=colap(C_KK + p), scalar2=None, op0=ALU.mult),
                         reads=[kk_, "cols"], writes=[tAk])
                    yield
                    P.op("pool", lambda e: e.tensor_tensor(out=t16_[:], in0=tA_[:], in1=tA_[:], op=ALU.mult), reads=[tAk], writes=[t16k])
                    yield
                    P.op("pe", lambda e: e.matmul(Y[:, 0:128], lhsT=BDb[:], rhs=t16_[:], start=True, stop=True), reads=["BDb", t16k], writes=[Yk])
                    yield
                    P.op("act", lambda e: e.activation(out=tB_[:], in_=Y[:, 0:128], func=AF.Sqrt), reads=[Yk], writes=[tBk])
                    yield
                    P.op("dve", lambda e: e.tensor_scalar(out=tB_[:], in0=tB_[:], scalar1=1e-12, scalar2=None, op0=ALU.max), reads=[tBk], writes=[tBk])
                    P.op("dve", lambda e: e.reciprocal(out=tB_[:], in_=tB_[:]), reads=[tBk], writes=[tBk])
                    yield
                    P.op("pool", lambda e: e.tensor_tensor(out=tA_[:], in0=tA_[:], in1=tB_[:], op=ALU.mult), reads=[tAk, tBk], writes=[tAk])
                    yield
                    P.op("dve", lambda e: e.tensor_scalar(out=tC_[:], in0=as_[:], scalar1=colap(C_KA + p), scalar2=oka[:, p:p + 1],
                                                          op0=ALU.mult, op1=ALU.add), reads=[ak, "cols", "oka"], writes=[tCk])
                    yield
                    P.op("pool", lambda e: e.tensor_tensor(out=kf[:], in0=kf[:], in1=tC_[:], op=ALU.mult), reads=[kk_, tCk], writes=[kk_])
                    yield
                    P.op("dve", lambda e: e.scalar_tensor_tensor(out=AR[p][:, 0, :], in0=tA_[:], scalar=-1.0, in1=Dprev[:, p, :],
                                                                 op0=ALU.mult, op1=ALU.mult), reads=[tAk, "Dprev"], writes=[f"AR{p}"])
                    P.op("pool", lambda e: e.tensor_tensor(out=AR[p][:, 1, :], in0=rf[:], in1=Dt[:, p, :], op=ALU.mult),
                         reads=[rk_, "Dt", f"AR{p}"], writes=[f"AR{p}"])
                    yield
                    P.op("pool", lambda e: e.tensor_tensor(out=tB_[:], in0=tA_[:], in1=as_[:], op=ALU.mult), reads=[tAk, ak, tBk], writes=[tBk])
                    yield
                    P.op("dve", lambda e: e.tensor_tensor(out=BT[p][:], in0=tB_[:], in1=Dinv[:, p, :], op=ALU.mult), reads=[tBk, "Dinv"], writes=[f"BT{p}"])
                    P.op("dve", lambda e: e.tensor_tensor(out=KT[p][:], in0=kf[:], in1=Dinv[:, p, :], op=ALU.mult), reads=[kk_, "Dinv"], writes=[f"KT{p}"])
                    yield
                    P.op("dve", lambda e: e.scalar_tensor_tensor(out=t16_[:], in0=rf[:], scalar=colap(C_RK + p), in1=kf[:],
                                                                 op0=ALU.mult, op1=ALU.mult), reads=[rk_, kk_, "cols", t16k], writes=[t16k])
                    yield
                    P.op("pe", lambda e: e.matmul(Y[:, 0:128], lhsT=BDb[:], rhs=t16_[:], start=True, stop=True), reads=["BDb", t16k], writes=[Yk])
                    yield
                    P.op("dve", lambda e: e.tensor_tensor(out=bon[p][:], in0=Y[:, 0:128], in1=vf[:], op=ALU.mult), reads=[Yk, vk_], writes=[f"bon{p}"])
                    P.op("dve", lambda e: e.tensor_scalar(out=bon[p][:], in0=bon[p][:], scalar1=colap(C_LB + p), scalar2=None, op0=ALU.add),
                         reads=[f"bon{p}", "cols"], writes=[f"bon{p}"])
                    yield
                    P.op("act", lambda e: e.activation(out=v16_[:], in_=vf[:], func=AF.Copy), reads=[vk_], writes=[v16k])
                    yield
                    pT = X[:, 0:256].bitcast(BF16)

                    def tr3(e):
                        e.transpose(out=pT[:, 0:128], in_=v16_[:], identity=ident[:])
                        e.transpose(out=pT[:, 128:256], in_=BT[p][:], identity=ident[:])
                        return e.transpose(out=pT[:, 256:384], in_=KT[p][:], identity=ident[:])

                    P.op("pe", tr3, reads=[v16k, f"BT{p}", f"KT{p}", "ident"], writes=[Xk])
                    yield
                    P.op("act", lambda e: e.activation(out=TOK[p][:].rearrange("p a t -> p (a t)"), in_=pT[:, 0:384], func=AF.Copy),
                         reads=[Xk], writes=[f"TOK{p}"])
                    yield

                    def amm(e):
                        ins = None
                        for h, bank in ((0, X), (1, Y)):
                            hr = slice(64 * h, 64 * h + 64)
                            e.matmul(bank[:, 0:256], lhsT=BT[p][hr, :], rhs=AR[p][hr, :, :].rearrange("p a t -> p (a t)"), start=True, stop=True)
                            ins = e.matmul(bank[:, 256:512], lhsT=KT[p][hr, :], rhs=AR[p][hr, :, :].rearrange("p a t -> p (a t)"),
                                           start=True, stop=True)
                        return ins

                    P.op("pe", amm, reads=[f"BT{p}", f"KT{p}", f"AR{p}"], writes=[Xk, Yk])
                    yield
                    P.op("dve", lambda e: e.tensor_tensor(out=AM[p][:, 0, :], in0=X[:, 0:512], in1=maskA[:, 0, :], op=ALU.mult),
                         reads=[Xk, "maskA"], writes=[f"AM{p}"])
                    P.op("dve", lambda e: e.tensor_tensor(out=AM[p][:, 1, :], in0=Y[:, 0:512], in1=maskA[:, 1, :], op=ALU.mult),
                         reads=[Yk, "maskA", f"AM{p}"], writes=[f"AM{p}"])
                    yield
                    pTL = Y[:, 0:128].bitcast(BF16)

                    def lmm(e):
                        e.transpose(out=pTL[:, 0:128], in_=AM[p][:, 0, 0:128], identity=ident[:])
                        return e.transpose(out=pTL[:, 128:256], in_=AM[p][:, 1, 0:128], identity=ident[:])

                    P.op("pe", lmm, reads=[f"AM{p}", "ident"], writes=[Yk])
                    for h in range(2):
                        P.op("pool", lambda e, h=h: e.tensor_tensor(out=MT[p][0][:, h, 128:256], in0=AM[p][:, h, 0:128], in1=ident[:], op=ALU.add),
                             reads=[f"AM{p}", "ident", f"MT{p}_0"], writes=[f"MT{p}_0"])
                    yield
                    P.op("dve", lambda e: e.tensor_copy(out=L0[p][:].rearrange("p h c -> p (h c)"), in_=pTL), reads=[Yk], writes=[f"L0{p}"])
                    yield
                    Xm = X[:, 0:512].rearrange("p (h c) -> p h c", h=2)
                    Yl = Y[:, 0:256].rearrange("p (h c) -> p h c", h=2)

                    def r1(e):
                        ins = None
                        for h in range(2):
                            e.matmul(X[:, h * 256:h * 256 + 128], lhsT=L0[p][:, h, :], rhs=AM[p][:, h, 0:128], start=True, stop=True)
                            ins = e.matmul(Y[:, h * 128:(h + 1) * 128], lhsT=AM[p][:, h, 0:128], rhs=L0[p][:, h, :], start=True, stop=True)
                        return ins

                    P.op("pe", r1, reads=[f"L0{p}", f"AM{p}"], writes=[Xk, Yk])
                    yield
                    P.op("act", lambda e: e.activation(out=MT[p][0][:, :, 0:128], in_=Xm[:, :, 0:128], func=AF.Copy),
                         reads=[Xk, f"MT{p}_0"], writes=[f"MT{p}_0"])
                    P.op("dve", lambda e: e.tensor_copy(out=LK[p][0][:], in_=Yl), reads=[Yk], writes=[f"LK{p}_0"])
                    yield
                    for rnd in range(2, 8):
                        src = rnd % 2
                        dst = 1 - src
                        last = (rnd == 7)
                        mts, mtd, lks, lkd = MT[p][src], MT[p][dst], LK[p][src], LK[p][dst]

                        def rk(e, mts=mts, lks=lks, last=last):
                            ins = None
                            for h in range(2):
                                if last:
                                    e.matmul(X[:, h * 256 + 128:h * 256 + 256], lhsT=lks[:, h, :], rhs=mts[:, h, 128:256], start=True, stop=False)
                                    ins = e.matmul(X[:, h * 256 + 128:h * 256 + 256], lhsT=ident[:], rhs=mts[:, h, 128:256], start=False, stop=True)
                                else:
                                    e.matmul(X[:, h * 256:h * 256 + 256], lhsT=lks[:, h, :], rhs=mts[:, h, :], start=True, stop=False)
                                    e.matmul(X[:, h * 256 + 128:h * 256 + 256], lhsT=ident[:], rhs=mts[:, h, 128:256], start=False, stop=True)
                            if not last:
                                for h in range(2):
                                    ins = e.matmul(Y[:, h * 128:(h + 1) * 128], lhsT=mts[:, h, 0:128], rhs=lks[:, h, :], start=True, stop=True)
                            return ins

                        P.op("pe", rk, reads=[f"MT{p}_{src}", f"LK{p}_{src}", "ident"], writes=[Xk] if last else [Xk, Yk])
                        yield
                        if last:
                            P.op("act", lambda e, mtd=mtd: e.activation(out=mtd[:, :, 128:256], in_=Xm[:, :, 128:256], func=AF.Copy),
                                 reads=[Xk, f"MT{p}_{dst}"], writes=[f"MT{p}_{dst}"])
                        else:
                            P.op("act", lambda e, mtd=mtd: e.activation(out=mtd[:], in_=Xm, func=AF.Copy), reads=[Xk], writes=[f"MT{p}_{dst}"])
                            P.op("dve", lambda e, lkd=lkd: e.tensor_copy(out=lkd[:], in_=Yl), reads=[Yk], writes=[f"LK{p}_{dst}"])
                        yield
                    P.op("pool", lambda e: e.tensor_copy(out=DC[:, p:p + 1], in_=Dt[:, p, 127:128]), reads=["Dt", "DC"], writes=["DC"])
                    yield


                def tail():
                    for p in range(4):
                        bank, bk = serial_ps[p], serial_key[p]

                        def xmm(e, p=p, bank=bank):
                            e.matmul(bank[:, 0:64], lhsT=AM[p][:, 0, 256:384], rhs=TOK[p][:, 0, 0:64], start=True, stop=False)
                            e.matmul(bank[:, 64:128], lhsT=AM[p][:, 1, 256:384], rhs=TOK[p][:, 0, 64:128], start=False, stop=False)
                            return e.matmul(bank[:, 0:128], lhsT=AR[p][:, 0, :], rhs=Sb[p][:], start=False, stop=True)

                        P.op("pe", xmm, reads=[f"AM{p}", f"TOK{p}", f"AR{p}", f"Sb{p}"], writes=[bk])
                    for p in range(4):
                        bank, bk = serial_ps[p], serial_key[p]
                        P.op("act", lambda e, p=p, bank=bank: e.activation(out=Xb[p][:], in_=bank[:, 0:128], func=AF.Copy),
                             reads=[bk], writes=[f"Xb{p}"])
                    for p in range(4):
                        bank, bk = serial_ps[p], serial_key[p]

                        def umm(e, p=p, bank=bank):
                            e.matmul(bank[:, 128:192], lhsT=MT[p][0][:, 0, 128:256], rhs=Xb[p][:, 0:64], start=True, stop=True)
                            return e.matmul(bank[:, 192:256], lhsT=MT[p][0][:, 1, 128:256], rhs=Xb[p][:, 64:128], start=True, stop=True)

                        P.op("pe", umm, reads=[f"MT{p}_0", f"Xb{p}"], writes=[bk])
                    for p in range(4):
                        bank, bk = serial_ps[p], serial_key[p]
                        P.op("dve", lambda e, p=p, bank=bank: e.tensor_copy(out=Ub[p][:], in_=bank[:, 128:256]), reads=[bk], writes=[f"Ub{p}"])
                    for p in range(4):
                        bank, bk = serial_ps[p], serial_key[p]

                        def ymm(e, p=p, bank=bank):
                            e.matmul(bank[:, 256:384], lhsT=Sb[p][:], rhs=AR[p][:, 1, :], start=True, stop=False)
                            ins = None
                            for h in range(2):
                                hs = slice(64 * h, 64 * h + 64)
                                e.matmul(bank[hs, 256:384], lhsT=Ub[p][:, hs], rhs=AM[p][:, h, 128:256], start=False, stop=False)
                                ins = e.matmul(bank[hs, 256:384], lhsT=TOK[p][:, 0, hs], rhs=AM[p][:, h, 384:512], start=False, stop=True)
                            return ins

                        P.op("pe", ymm, reads=[f"Sb{p}", f"AR{p}", f"Ub{p}", f"AM{p}", f"TOK{p}"], writes=[bk])

                        def smm(e, p=p, bank=bank):
                            e.matmul(bank[:, 384:512], lhsT=TOK[p][:, 2, :], rhs=TOK[p][:, 0, :], start=True, stop=False)
                            return e.matmul(bank[:, 384:512], lhsT=TOK[p][:, 1, :], rhs=Ub[p][:], start=False, stop=True)

                        P.op("pe", smm, reads=[f"TOK{p}", f"Ub{p}"], writes=[bk])
                    for p in range(4):
                        bank, bk = serial_ps[p], serial_key[p]
                        P.op("act", lambda e, p=p, bank=bank: e.activation(out=yT[:, p, :], in_=bank[:, 256:384], func=AF.Copy),
                             reads=[bk, "yT"], writes=["yT"])
                        P.op("dve", lambda e, p=p, bank=bank: e.tensor_tensor(out=tS[:], in0=bank[:, 384:512], in1=Sw[p][:], op=ALU.add),
                             reads=[bk, f"Sw{p}", "tS"], writes=["tS"])
                        P.op("dve", lambda e, p=p: e.scalar_tensor_tensor(out=Sw[p][:], in0=tS[:], scalar=DC[:, p:p + 1], in1=BDf[:],
                                                                          op0=ALU.mult, op1=ALU.mult),
                             reads=["tS", f"Sw{p}", "DC", "BDf"], writes=[f"Sw{p}"])
                        P.op("act", lambda e, p=p: e.activation(out=Sb[p][:], in_=Sw[p][:], func=AF.Copy), reads=[f"Sw{p}"], writes=[f"Sb{p}"])

                    yTf = yT[:].rearrange("p a t -> p (a t)")
                    P.op("pe", lambda e: e.matmul(psD[:, 0:512], lhsT=BD64[:], rhs=yTf, start=True, stop=True), reads=["BD64", "yT"], writes=["psD0"])
                    yield
                    P.op("dve", lambda e: e.tensor_tensor(out=yc[:], in0=yTf, in1=psD[:, 0:512], op=ALU.subtract), reads=["yT", "psD0"], writes=["yc"])
                    yield
                    P.op("act", lambda e: e.activation(out=ysq[:], in_=yc[:], func=AF.Square), reads=["yc"], writes=["ysq"])
                    yield
                    P.op("pe", lambda e: e.matmul(psD[:, 0:512], lhsT=BD64[:], rhs=ysq[:], start=True, stop=True), reads=["BD64", "ysq"], writes=["psD0"])
                    yield
                    P.op("act", lambda e: e.activation(out=yrs[:], in_=psD[:, 0:512], func=AF.Sqrt, bias=LNX_EPS), reads=["psD0"], writes=["yrs"])
                    yield
                    P.op("dve", lambda e: e.reciprocal(out=yrs[:], in_=yrs[:]), reads=["yrs"], writes=["yrs"])
                    yield
                    P.op("pool", lambda e: e.tensor_tensor(out=yc[:], in0=yc[:], in1=yrs[:], op=ALU.mult), reads=["yc", "yrs"], writes=["yc"])
                    yield
                    for p in range(4):
                        P.op("dve", lambda e, p=p: e.scalar_tensor_tensor(out=y3[:], in0=yc[:, p * 128:(p + 1) * 128], scalar=colap(C_LG + p),
                                                                          in1=bon[p][:], op0=ALU.mult, op1=ALU.add),
                             reads=["yc", "cols", f"bon{p}"], writes=["y3"])
                        P.op("pool", lambda e, p=p: e.tensor_tensor(out=ygT[:, p, :], in0=y3[:], in1=gT[p][:], op=ALU.mult),
                             reads=["y3", f"gT{p}", ygk], writes=[ygk])
                    P.dma("sp", lambda e, tl=tl, ygT=ygT: e.dma_start(out=yg_d[tl], in_=ygT[:].rearrange("p a t -> p (a t)")), reads=[ygk], writes=["yg_d"])
                    yield


                return front, pair_gen, tail

            class KeyProxy:
                PAT = re.compile(r'^(AM|AR|TOK|MT|bon|gT)\d')

                def __init__(self, par):
                    self.par = par

                def km(self, k):
                    if k in ('Dt', 'Dinv', 'Dprev', 'txw', 'sxg', 'DC'):
                        return f'{k}@{self.par}'
                    return k

                def op(self, eng, fn, reads=(), writes=()):
                    return P.op(eng, fn, [self.km(k) for k in reads], [self.km(k) for k in writes])

                def dma(self, q, fn, reads=(), writes=()):
                    return P.dma(q, fn, [self.km(k) for k in reads], [self.km(k) for k in writes])

            def run_rr(gens):
                gens = list(gens)
                while gens:
                    for g in list(gens):
                        try:
                            next(g)
                        except StopIteration:
                            gens.remove(g)

            parts = [tile_parts(tl, KeyProxy(tl % 2)) for tl in range(NT)]
            run_rr([parts[0][0]()])
            for tl in range(NT):
                run_rr([parts[tl][1](p) for p in range(4)])
                gens = [parts[tl][2]()]
                if tl + 1 < NT:
                    gens.append(parts[tl + 1][0]())
                run_rr(gens)

            P.barrier()

        with ExitStack() as st:
            P.scope = "M2"
            w_inB = sbuf(st, "w_inB", [128, 8, 3072], BF16)
            wsTf = sbuf(st, "wsTf", [128, 4, 128], F32)
            wsTb = sbuf(st, "wsTb", [128, 4, 128], BF16)
            bsrow = sbuf(st, "bsrow", [1, 512], BF16)
            lrow = sbuf(st, "lrow", [1, 2, 512], F32)
            lnvg = sbuf(st, "lnvg", [128, 512], F32)
            lnvb = sbuf(st, "lnvb", [128, 512], F32)
            for k in range(8):
                for j in range(3):
                    P.dma("pool", lambda e, k=k, j=j: e.dma_start(out=w_inB[:, k, j * 1024:(j + 1) * 1024],
                                                                  in_=w_in_d[k * 128:(k + 1) * 128, A_COLS + j * 1024:A_COLS + (j + 1) * 1024]),
                          writes=["w_inB"])
            P.dma("sp", lambda e: e.dma_start(out=wsTf[:], in_=wsT_d.rearrange("g s t -> s g t")), writes=["wsTf"])
            P.dma("pool", lambda e: e.dma_start(out=bsrow[:], in_=bs_d), writes=["bsrow"])
            P.dma("sp", lambda e: e.dma_start(out=lrow[:, 0, :], in_=lnvg_d), writes=["lrow"])
            P.dma("sp", lambda e: e.dma_start(out=lrow[:, 1, :], in_=lnvb_d), reads=["lrow"], writes=["lrow"])
            for g in range(4):
                P.op("dve", lambda e, g=g: e.tensor_tensor(out=wsTb[:, g, :], in0=wsTf[:, g, :], in1=mIU[:], op=ALU.mult),
                     reads=["wsTf", "mIU", "wsTb"], writes=["wsTb"])
            for i, (dst, dk) in enumerate([(lnvg, "lnvg"), (lnvb, "lnvb")]):
                P.op("pe", lambda e, i=i: e.matmul(psM[0][:, 0:512], lhsT=onesf[0:1, :], rhs=lrow[0:1, i, :], start=True, stop=True),
                     reads=["onesf", "lrow"], writes=["psM0"])
                P.op("act", lambda e, dst=dst: e.activation(out=dst[:], in_=psM[0][:, 0:512], func=AF.Copy), reads=["psM0"], writes=[dk])

            TB = min(4, NT)
            BN = TB * 128
            NBLK = NT // TB
            hTB = [sbuf(st, f"hTB{i}", [128, 8, BN], BF16) for i in range(2)]
            ygB = [sbuf(st, f"ygB{i}", [128, 4, BN], BF16) for i in range(2)]
            uT = sbuf(st, "uTB", [128, 4, BN], BF16)
            mixT = sbuf(st, "mixTB", [128, 4, BN], BF16)
            t1 = sbuf(st, "t1B", [128, 8, BN], BF16)
            zT = sbuf(st, "zTB", [128, 8, BN], BF16)
            gsT = [sbuf(st, f"gsT{i}", [128, BN], BF16) for i in range(2)]
            t2T = [sbuf(st, f"t2T{i}", [128, BN], BF16) for i in range(2)]
            xinL = [sbuf(st, f"xinb{i}", [128, D], F32) for i in range(2)]
            vgL = [sbuf(st, f"vg{i}", [128, 512], F32) for i in range(2)]
            vlnL = [sbuf(st, f"vln{i}", [128, 512], BF16) for i in range(2)]
            xsbL = [sbuf(st, f"xsb2{i}", [128, D], BF16) for i in range(2)]
            h2tL = [sbuf(st, f"h2t{i}", [128, 8, 128], BF16) for i in range(4)]
            sm = [{n: sbuf(st, f"{n}{i}", [128, w], F32) for n, w in (("bst", 6), ("mv", 2), ("lsd", 1), ("lrs", 1), ("lnm", 1), ("ss2", 1), ("sd2", 1), ("rs2", 1))}
                  for i in range(2)]
            RING = [(psM[0], "psM0"), (psM[1], "psM1"), (psS, "psS"), (psD[:, 0:512], "psD0"), (psD[:, 512:1024], "psD1"), (psD[:, 1024:1536], "psD2"),
                    (psA[:, 0:512], "psA"), (psA[:, 512:1024], "psA2")]
            ring_i = [0]

            def ring():
                r = RING[ring_i[0] % len(RING)]
                ring_i[0] += 1
                return r

            wrb = sbuf(st, "wrb", [128, 8, 36], BF16)
            brrow = sbuf(st, "brrow", [1, 36], BF16)
            P.dma("pool", lambda e: e.dma_start(out=wrb[:], in_=wr_d.rearrange("(k p) n -> p k n", p=128)), writes=["wrb"])
            P.dma("pool", lambda e: e.dma_start(out=brrow[:], in_=br_d), writes=["brrow"])
            NRS = 4
            rsc = []
            for i in range(NRS):
                d = {"Lg": sbuf(st, f"Lg{i}", [128, 36], F32), "c": sbuf(st, f"rc{i}", [128, 10], F32), "ohg": sbuf(st, f"ohg{i}", [128, 4], F32),
                     "ex4": sbuf(st, f"ex4{i}", [128, 4], F32), "esel": sbuf(st, f"esel{i}", [128, 8], F32), "e2": sbuf(st, f"e2{i}", [128, 8], F32),
                     "mk1": sbuf(st, f"mk1{i}", [128, 8], F32), "mk2": sbuf(st, f"mk2{i}", [128, 8], F32), "wg8": sbuf(st, f"wg8{i}", [128, 8], F32)}
                rsc.append(d)

            def router_rest(tl):
                sl = tl % NRS
                d = rsc[sl]
                Lg, ohg, ex4, esel, e2, mk1, mk2, wg8 = d["Lg"], d["ohg"], d["ex4"], d["esel"], d["e2"], d["mk1"], d["mk2"], d["wg8"]
                cc = d["c"]
                gmax, ngmax, se, gp, m1, m2, dd, w1, w2 = [cc[:, i:i + 1] for i in range(9)]
                rk = f"rt{sl}"
                steps = [
                    ("dve", lambda e: e.tensor_reduce(out=gmax, in_=Lg[:, 0:4], axis=AX.X, op=ALU.max)),
                    ("dve", lambda e: e.tensor_scalar(out=ohg[:], in0=Lg[:, 0:4], scalar1=gmax, scalar2=None, op0=ALU.is_ge)),
                    ("dve", lambda e: e.tensor_scalar(out=ngmax, in0=gmax, scalar1=-1.0, scalar2=None, op0=ALU.mult)),
                    ("act", lambda e: e.activation(out=ex4[:], in_=Lg[:, 0:4], func=AF.Exp, bias=ngmax)),
                    ("dve", lambda e: e.tensor_reduce(out=se, in_=ex4[:], axis=AX.X, op=ALU.add)),
                    ("dve", lambda e: e.reciprocal(out=gp, in_=se)),
                    ("dve", lambda e: e.tensor_scalar(out=esel[:], in0=Lg[:, 4:12], scalar1=ohg[:, 0:1], scalar2=None, op0=ALU.mult)),
                ]
                for g in range(1, 4):
                    steps.append(("dve", lambda e, g=g: e.scalar_tensor_tensor(out=esel[:], in0=Lg[:, 4 + 8 * g:12 + 8 * g], scalar=ohg[:, g:g + 1],
                                                                               in1=esel[:], op0=ALU.mult, op1=ALU.add)))
                steps += [
                    ("dve", lambda e: e.tensor_reduce(out=m1, in_=esel[:], axis=AX.X, op=ALU.max)),
                    ("dve", lambda e: e.tensor_scalar(out=mk1[:], in0=esel[:], scalar1=m1, scalar2=None, op0=ALU.is_ge)),
                    ("dve", lambda e: e.scalar_tensor_tensor(out=e2[:], in0=mk1[:], scalar=-1e30, in1=esel[:], op0=ALU.mult, op1=ALU.add)),
                    ("dve", lambda e: e.tensor_reduce(out=m2, in_=e2[:], axis=AX.X, op=ALU.max)),
                    ("dve", lambda e: e.tensor_scalar(out=mk2[:], in0=e2[:], scalar1=m2, scalar2=None, op0=ALU.is_ge)),
                    ("dve", lambda e: e.tensor_tensor(out=dd, in0=m2, in1=m1, op=ALU.subtract)),
                    ("act", lambda e: e.activation(out=w2, in_=dd, func=AF.Sigmoid)),
                    ("act", lambda e: e.activation(out=w1, in_=dd, func=AF.Sigmoid, scale=-1.0)),
                    ("dve", lambda e: e.tensor_tensor(out=w1, in0=w1, in1=gp, op=ALU.mult)),
                    ("dve", lambda e: e.tensor_tensor(out=w2, in0=w2, in1=gp, op=ALU.mult)),
                    ("dve", lambda e: e.tensor_scalar(out=wg8[:], in0=mk1[:], scalar1=w1, scalar2=None, op0=ALU.mult)),
                    ("dve", lambda e: e.scalar_tensor_tensor(out=wg8[:], in0=mk2[:], scalar=w2, in1=wg8[:], op0=ALU.mult, op1=ALU.add)),
                ]
                for eng, fn in steps:
                    P.op(eng, fn, reads=[rk], writes=[rk])
                    yield
                for g in range(4):
                    P.op("dve", lambda e, g=g: e.tensor_scalar(out=comb[:, tl, g * 8:(g + 1) * 8], in0=wg8[:], scalar1=ohg[:, g:g + 1],
                                                               scalar2=None, op0=ALU.mult), reads=[rk, f"comb{tl}"], writes=[f"comb{tl}"])
                yield


            def run_rr2(gens):
                gens = list(gens)
                while gens:
                    for g in list(gens):
                        try:
                            next(g)
                        except StopIteration:
                            gens.remove(g)

            def load_block(b):
                hB, yB = hTB[b % 2], ygB[b % 2]
                for i in range(TB):
                    tl = b * TB + i
                    P.dma("sp", lambda e, tl=tl, i=i: e.dma_start(out=hB[:, :, i * 128:(i + 1) * 128], in_=hT_d[tl].rearrange("p (k t) -> p k t", k=8)),
                          reads=["hT_d"], writes=[f"hTB{b % 2}"])
                    P.dma("sp", lambda e, tl=tl, i=i: e.dma_start(out=yB[:, :, i * 128:(i + 1) * 128], in_=yg_d[tl].rearrange("p (a t) -> p a t", a=4)),
                          reads=["yg_d"], writes=[f"ygB{b % 2}"])

            load_block(0)
            for b in range(NBLK):
                hB, yB, hk, yk = hTB[b % 2], ygB[b % 2], f"hTB{b % 2}", f"ygB{b % 2}"
                if b + 1 < NBLK:
                    load_block(b + 1)

                for c in range(4):
                    bank, bk = ring()

                    def umm2(e, c=c, bank=bank):
                        ins = None
                        for k in range(8):
                            ins = e.matmul(bank[:, 0:BN], lhsT=w_inB[:, k, c * 128:(c + 1) * 128], rhs=hB[:, k, :], start=(k == 0), stop=(k == 7))
                        return ins

                    P.op("pe", umm2, reads=["w_inB", hk], writes=[bk])
                    P.op("act", lambda e, c=c, bank=bank: e.activation(out=uT[:, c, :], in_=bank[:, 0:BN], func=AF.Gelu), reads=[bk, "uTB"], writes=["uTB"])

                def g1(m):
                    bank, bk = ring()
                    c0 = 1024 + m * 128

                    def gm(e):
                        ins = None
                        for k in range(8):
                            ins = e.matmul(bank[:, 0:BN], lhsT=w_inB[:, k, c0:c0 + 128], rhs=hB[:, k, :], start=(k == 0), stop=(k == 7))
                        return ins

                    gs = gsT[m % 2]
                    P.op("pe", gm, reads=["w_inB", hk], writes=[bk])
                    P.op("act", lambda e: e.activation(out=gs[:], in_=bank[:, 0:BN], func=AF.Sigmoid, bias=colap(C_BG + m)), reads=[bk, "cols"], writes=[f"gsT{m % 2}"])
                    bank2, bk2 = ring()

                    def ym(e):
                        ins = None
                        for c in range(4):
                            ins = e.matmul(bank2[:, 0:BN], lhsT=woA[:, c, m * 128:(m + 1) * 128], rhs=yB[:, c, :], start=(c == 0), stop=(c == 3))
                        return ins

                    P.op("pe", ym, reads=["woA", yk], writes=[bk2])
                    P.op("dve", lambda e: e.tensor_tensor(out=t1[:, m, :], in0=bank2[:, 0:BN], in1=gs[:], op=ALU.mult), reads=[bk2, f"gsT{m % 2}", "t1B"], writes=["t1B"])

                for i in range(TB):
                    tl = b * TB + i
                    ts_ = slice(i * 128, (i + 1) * 128)
                    vg, vln, d_ = vgL[i % 2], vlnL[i % 2], sm[i % 2]
                    q = i % 2
                    bank, bk = ring()

                    def vmm(e, bank=bank, ts_=ts_):
                        ins = None
                        for k in range(8):
                            ins = e.matmul(bank[:, 0:512], lhsT=hB[:, k, ts_], rhs=w_inB[:, k, 512:1024], start=(k == 0), stop=(k == 7))
                        return ins

                    P.op("pe", vmm, reads=["w_inB", hk], writes=[bk])
                    P.op("act", lambda e, bank=bank, vg=vg: e.activation(out=vg[:], in_=bank[:, 0:512], func=AF.Gelu), reads=[bk], writes=[f"vg{q}"])
                    g1(2 * i)
                    P.op("dve", lambda e, vg=vg, d_=d_: e.bn_stats(out=d_["bst"][:], in_=vg[:]), reads=[f"vg{q}"], writes=[f"sm{q}"])
                    P.op("dve", lambda e, d_=d_: e.bn_aggr(out=d_["mv"][:], in_=d_["bst"][:]), reads=[f"sm{q}"], writes=[f"sm{q}"])
                    P.op("act", lambda e, d_=d_: e.activation(out=d_["lsd"][:], in_=d_["mv"][:, 1:2], func=AF.Sqrt, bias=LN_EPS), reads=[f"sm{q}"], writes=[f"sm{q}"])
                    P.op("dve", lambda e, d_=d_: e.reciprocal(out=d_["lrs"][:], in_=d_["lsd"][:]), reads=[f"sm{q}"], writes=[f"sm{q}"])
                    P.op("dve", lambda e, d_=d_: e.tensor_scalar(out=d_["lnm"][:], in0=d_["mv"][:, 0:1], scalar1=-1.0, scalar2=d_["lrs"][:], op0=ALU.mult, op1=ALU.mult),
                         reads=[f"sm{q}"], writes=[f"sm{q}"])
                    P.op("dve", lambda e, vg=vg, d_=d_: e.tensor_scalar(out=vg[:], in0=vg[:], scalar1=d_["lrs"][:], scalar2=d_["lnm"][:], op0=ALU.mult, op1=ALU.add),
                         reads=[f"vg{q}", f"sm{q}"], writes=[f"vg{q}"])
                    P.op("pool", lambda e, vg=vg: e.tensor_tensor(out=vg[:], in0=vg[:], in1=lnvg[:], op=ALU.mult), reads=[f"vg{q}", "lnvg"], writes=[f"vg{q}"])
                    P.op("pool", lambda e, vg=vg, vln=vln: e.tensor_tensor(out=vln[:], in0=vg[:], in1=lnvb[:], op=ALU.add), reads=[f"vg{q}", "lnvb"], writes=[f"vln{q}"])
                    g1(2 * i + 1)
                    bank, bk = ring()

                    def svmm(e, bank=bank, vln=vln):
                        ins = None
                        for g in range(4):
                            e.matmul(bank[:, g * 128:(g + 1) * 128], lhsT=vln[:, g * 128:(g + 1) * 128], rhs=wsTb[:, g, :], start=True, stop=False)
                            ins = e.matmul(bank[:, g * 128:(g + 1) * 128], lhsT=onesb[0:1, :], rhs=bsrow[0:1, g * 128:(g + 1) * 128], start=False, stop=True)
                        return ins

                    P.op("pe", svmm, reads=[f"vln{q}", "wsTb", "onesb", "bsrow"], writes=[bk])
                    P.op("dve", lambda e, bank=bank, ts_=ts_: e.tensor_tensor(out=mixT[:, :, ts_], in0=bank[:, 0:512].rearrange("p (g t) -> p g t", g=4),
                                                                           in1=uT[:, :, ts_], op=ALU.mult), reads=[bk, "uTB", "mixTB"], writes=["mixTB"])
                for m in range(2 * TB, 8):
                    g1(m)

                for m in range(8):
                    bank, bk = ring()
                    c0 = 2048 + m * 128

                    def gm(e, bank=bank, c0=c0):
                        ins = None
                        for k in range(8):
                            ins = e.matmul(bank[:, 0:BN], lhsT=w_inB[:, k, c0:c0 + 128], rhs=hB[:, k, :], start=(k == 0), stop=(k == 7))
                        return ins

                    gs, t2 = gsT[m % 2], t2T[m % 2]
                    P.op("pe", gm, reads=["w_inB", hk], writes=[bk])
                    P.op("act", lambda e, bank=bank, gs=gs, m=m: e.activation(out=gs[:], in_=bank[:, 0:BN], func=AF.Sigmoid, bias=colap(C_BG + 8 + m)),
                         reads=[bk, "cols"], writes=[f"gsT{m % 2}"])
                    bank2, bk2 = ring()

                    def ym(e, bank2=bank2, m=m):
                        ins = None
                        for c in range(4):
                            ins = e.matmul(bank2[:, 0:BN], lhsT=woB[:, c, m * 128:(m + 1) * 128], rhs=mixT[:, c, :], start=(c == 0), stop=(c == 3))
                        return ins

                    P.op("pe", ym, reads=["woB", "mixTB"], writes=[bk2])
                    P.op("dve", lambda e, bank2=bank2, gs=gs, t2=t2: e.tensor_tensor(out=t2[:], in0=bank2[:, 0:BN], in1=gs[:], op=ALU.mult),
                         reads=[bk2, f"gsT{m % 2}"], writes=[f"t2T{m % 2}"])
                    P.op("pool", lambda e, m=m, t2=t2: e.tensor_tensor(out=zT[:, m, :], in0=t1[:, m, :], in1=t2[:], op=ALU.add),
                         reads=["t1B", f"t2T{m % 2}", "zTB"], writes=["zTB"])

                for i in range(TB):
                    tl = b * TB + i
                    ts_ = slice(i * 128, (i + 1) * 128)
                    tc = slice(tl * 128, (tl + 1) * 128)
                    q = i % 2
                    xb_, xsb, d_, h2t = xinL[q], xsbL[q], sm[q], h2tL[i]
                    P.dma("sp", lambda e, xb_=xb_, tl=tl: e.dma_start(out=xb_[:], in_=x_d[tl * 128:(tl + 1) * 128, :]), writes=[f"xinb{q}"])
                    for hf in range(2):
                        bank, bk = ring()

                        def omm(e, bank=bank, hf=hf, ts_=ts_):
                            ins = None
                            for m in range(8):
                                ins = e.matmul(bank[:, 0:512], lhsT=zT[:, m, ts_], rhs=wout[:, m, hf * 512:(hf + 1) * 512], start=(m == 0), stop=(m == 7))
                            return ins

                        P.op("pe", omm, reads=["zTB", "wout"], writes=[bk])
                        P.op("dve", lambda e, bank=bank, hf=hf, xb_=xb_: e.tensor_tensor(out=xb_[:, hf * 512:(hf + 1) * 512], in0=bank[:, 0:512],
                                                                                     in1=xb_[:, hf * 512:(hf + 1) * 512], op=ALU.add),
                             reads=[bk, f"xinb{q}"], writes=[f"xinb{q}"])
                    P.dma("sp", lambda e, xb_=xb_, tl=tl: e.dma_start(out=x1_d[tl * 128:(tl + 1) * 128, :], in_=xb_[:]), reads=[f"xinb{q}"], writes=["x1_d"])
                    P.op("act", lambda e, xb_=xb_, xsb=xsb, d_=d_: e.activation(out=xsb[:], in_=xb_[:], func=AF.Square, accum_out=d_["ss2"][:]),
                         reads=[f"xinb{q}"], writes=[f"xsb2{q}", f"sm{q}"])
                    P.op("act", lambda e, d_=d_: e.activation(out=d_["sd2"][:], in_=d_["ss2"][:], func=AF.Sqrt, bias=NORM_EPS, scale=1.0 / D), reads=[f"sm{q}"], writes=[f"sm{q}"])
                    P.op("dve", lambda e, d_=d_: e.reciprocal(out=d_["rs2"][:], in_=d_["sd2"][:]), reads=[f"sm{q}"], writes=[f"sm{q}"])
                    P.op("dve", lambda e, xb_=xb_, xsb=xsb, d_=d_: e.tensor_scalar(out=xsb[:], in0=xb_[:], scalar1=d_["rs2"][:], scalar2=None, op0=ALU.mult),
                         reads=[f"xinb{q}", f"sm{q}", f"xsb2{q}"], writes=[f"xsb2{q}"])
                    bank, bk = ring()
                    pT = bank[:].bitcast(BF16)

                    def tr(e, pT=pT, xsb=xsb):
                        ins = None
                        for k in range(8):
                            ins = e.transpose(out=pT[:, k * 128:(k + 1) * 128], in_=xsb[:, k * 128:(k + 1) * 128], identity=ident[:])
                        return ins

                    P.op("pe", tr, reads=[f"xsb2{q}", "ident"], writes=[bk])
                    P.op("dve", lambda e, pT=pT, h2t=h2t: e.tensor_tensor(out=h2t[:], in0=pT.rearrange("p (k t) -> p k t", k=8), in1=g2bc[:], op=ALU.mult),
                         reads=[bk, "g2bc"], writes=[f"h2t{i}"])
                    P.dma("sp", lambda e, h2t=h2t, tc=tc: e.dma_start(out=h2_d[:, :, tc], in_=h2t[:]), reads=[f"h2t{i}"], writes=["h2_d"])
                    bank, bk = ring()
                    Lg = rsc[i % NRS]["Lg"]

                    def rmm(e, bank=bank, h2t=h2t):
                        for k in range(8):
                            e.matmul(bank[:, 0:36], lhsT=h2t[:, k, :], rhs=wrb[:, k, :], start=(k == 0), stop=False)
                        return e.matmul(bank[:, 0:36], lhsT=onesb[0:1, :], rhs=brrow[0:1, :], start=False, stop=True)

                    P.op("pe", rmm, reads=[f"h2t{i}", "wrb", "onesb", "brrow"], writes=[bk])
                    P.op("dve", lambda e, bank=bank, Lg=Lg: e.tensor_copy(out=Lg[:], in_=bank[:, 0:36]), reads=[bk, f"rt{i % NRS}"], writes=[f"rt{i % NRS}"])
                run_rr2([router_rest(b * TB + i) for i in range(TB)])

            P.barrier()
        stP.close()

        with ExitStack() as st:
            P.scope = "router"
            h2 = sbuf(st, "h2", [128, 8, T], BF16)
            yacc = sbuf(st, "yacc", [128, NT, D], F32)
            fgrow = sbuf(st, "fgrow", [1, D], F32)
            fgbc = sbuf(st, "fgbc", [128, D], F32)
            P.dma("sp", lambda e: e.dma_start(out=h2[:], in_=h2_d), reads=["h2_d"], writes=["h2"])
            for tl in range(NT):
                P.dma("sp", lambda e, tl=tl: e.dma_start(out=yacc[:, tl, :], in_=x1_d[tl * 128:(tl + 1) * 128, :]), reads=["x1_d"], writes=[f"yacc{tl}"])
            P.dma("sp", lambda e: e.dma_start(out=fgrow[:], in_=fg_d), writes=["fgrow"])
            for hf in range(2):
                P.op("pe", lambda e, hf=hf: e.matmul(psM[0][:, 0:512], lhsT=onesf[0:1, :], rhs=fgrow[0:1, hf * 512:(hf + 1) * 512], start=True, stop=True),
                     reads=["onesf", "fgrow"], writes=["psM0"])
                P.op("act", lambda e, hf=hf: e.activation(out=fgbc[:, hf * 512:(hf + 1) * 512], in_=psM[0][:, 0:512], func=AF.Copy),
                     reads=["psM0", "fgbc"], writes=["fgbc"])

            P.scope = "experts"
            Wg = [sbuf(st, f"Wg{i}", [128, 8, 256], BF16) for i in range(2)]
            Wu = [sbuf(st, f"Wu{i}", [128, 8, 256], BF16) for i in range(2)]
            Wd = [sbuf(st, f"Wd{i}", [128, 2, D], BF16) for i in range(2)]
            GN_ = min(512, T)
            NG = T // GN_
            sg = [sbuf(st, f"sg{i}", [128, GN_], F32) for i in range(2)]
            actT = sbuf(st, "actT", [128, 2, GN_], BF16)
            gu_ps = [psM[0], psM[1], psS, psD[:, 0:512]]
            gu_k = ["psM0", "psM1", "psS", "psD0"]
            d_ps = [psA, psD[:, 512:1536]]
            d_k = [["psA", "psA2"], ["psD1", "psD2"]]
            actT2 = [actT, sbuf(st, "actTb", [128, 2, GN_], BF16)]
            TPG = GN_ // 128

            def load_expert(ex):
                b = ex % 2
                P.dma("pool", lambda e: e.dma_start(out=Wg[b][:], in_=weg_d[ex].rearrange("(k p) n -> p k n", p=128)), writes=[f"Wg{b}"])
                P.dma("pool", lambda e: e.dma_start(out=Wu[b][:], in_=weu_d[ex].rearrange("(k p) n -> p k n", p=128)), writes=[f"Wu{b}"])
                P.dma("pool", lambda e: e.dma_start(out=Wd[b][:], in_=wed_d[ex].rearrange("(k p) n -> p k n", p=128)), writes=[f"Wd{b}"])

            def G(u, f):
                ex, gi = divmod(u, NG)
                b = ex % 2
                gc = slice(gi * GN_, (gi + 1) * GN_)
                aT = actT2[u % 2]
                ak = f"actT{u % 2}_{f}"

                def gumm(e):
                    ins = None
                    for k in range(8):
                        e.matmul(gu_ps[2 * f][:, 0:GN_], lhsT=Wg[b][:, k, f * 128:(f + 1) * 128], rhs=h2[:, k, gc], start=(k == 0), stop=(k == 7))
                    for k in range(8):
                        ins = e.matmul(gu_ps[2 * f + 1][:, 0:GN_], lhsT=Wu[b][:, k, f * 128:(f + 1) * 128], rhs=h2[:, k, gc], start=(k == 0), stop=(k == 7))
                    return ins

                P.op("pe", gumm, reads=[f"Wg{b}", f"Wu{b}", "h2"], writes=[gu_k[2 * f], gu_k[2 * f + 1]])
                P.op("act", lambda e: e.activation(out=sg[f][:], in_=gu_ps[2 * f][:, 0:GN_], func=AF.Silu), reads=[gu_k[2 * f]], writes=[f"sg{f}"])
                P.op("dve", lambda e: e.tensor_tensor(out=aT[:, f, :], in0=gu_ps[2 * f + 1][:, 0:GN_], in1=sg[f][:], op=ALU.mult),
                     reads=[gu_k[2 * f + 1], f"sg{f}"], writes=[ak])

            dstate = [0]

            def Dn(u, ti):
                ex, gi = divmod(u, NG)
                b = ex % 2
                tl = gi * TPG + ti
                aT = actT2[u % 2]
                dps = d_ps[dstate[0] % 2]
                dk = d_k[dstate[0] % 2]
                dstate[0] += 1

                def dmm(e):
                    ins = None
                    for hf in range(2):
                        for f in range(2):
                            ins = e.matmul(dps[:, hf * 512:(hf + 1) * 512], lhsT=aT[:, f, ti * 128:(ti + 1) * 128],
                                           rhs=Wd[b][:, f, hf * 512:(hf + 1) * 512], start=(f == 0), stop=(f == 1))
                    return ins

                P.op("pe", dmm, reads=[f"actT{u % 2}_0", f"actT{u % 2}_1", f"Wd{b}"], writes=dk)
                P.op("dve", lambda e: e.scalar_tensor_tensor(out=yacc[:, tl, :], in0=dps[:, 0:1024], scalar=comb[:, tl, ex:ex + 1],
                                                             in1=yacc[:, tl, :], op0=ALU.mult, op1=ALU.add),
                     reads=dk + [f"comb{tl}", f"yacc{tl}"], writes=[f"yacc{tl}"])

            junk = sbuf(st, "junk3", [128, D], BF16)
            ss = sbuf(st, "ss3", [128, 1], F32)
            sd = sbuf(st, "sd3", [128, 1], F32)
            rs = sbuf(st, "rs3", [128, 1], F32)
            ob = [sbuf(st, f"ob{i}", [128, D], F32) for i in range(2)]

            def final_tile(tl):
                o = ob[tl % 2]
                ok = f"ob{tl % 2}"
                P.op("act", lambda e: e.activation(out=junk[:], in_=yacc[:, tl, :], func=AF.Square, accum_out=ss[:]),
                     reads=[f"yacc{tl}"], writes=["junk3", "ss3"])
                P.op("act", lambda e: e.activation(out=sd[:], in_=ss[:], func=AF.Sqrt, bias=NORM_EPS, scale=1.0 / D), reads=["ss3"], writes=["sd3"])
                P.op("dve", lambda e: e.reciprocal(out=rs[:], in_=sd[:]), reads=["sd3"], writes=["rs3"])
                P.op("dve", lambda e: e.scalar_tensor_tensor(out=o[:], in0=yacc[:, tl, :], scalar=rs[:], in1=fgbc[:], op0=ALU.mult, op1=ALU.mult),
                     reads=[f"yacc{tl}", "rs3", "fgbc"], writes=[ok])
                P.dma("sp", lambda e: e.dma_start(out=out_d[tl * 128:(tl + 1) * 128, :], in_=o[:]), reads=[ok], writes=["out"])

            NU = NE * NG
            load_expert(0)
            if NE > 1:
                load_expert(1)
            G(0, 0)
            G(0, 1)
            for u in range(NU):
                ex, gi = divmod(u, NG)
                P.scope = "experts" if ex != 5 else "ex5"
                half = (TPG + 1) // 2
                if u + 1 < NU:
                    G(u + 1, 0)
                for ti in range(0, half):
                    Dn(u, ti)
                    if ex == NE - 1:
                        final_tile(gi * TPG + ti)
                if u + 1 < NU:
                    G(u + 1, 1)
                for ti in range(half, TPG):
                    Dn(u, ti)
                    if ex == NE - 1:
                        final_tile(gi * TPG + ti)
                if gi == NG - 1 and ex + 2 < NE:
                    load_expert(ex + 2)

            P.final_wait("sp")
            P.emit()
    return nc


def host_layout(inp, b, T):
    f = lambda a: np.ascontiguousarray(np.asarray(a, dtype=np.float32))

    def colpack(v, n):
        v = np.asarray(v, np.float32).reshape(-1)
        pad = np.zeros(n * 128, np.float32)
        pad[:v.size] = v
        return pad.reshape(n, 128).T

    cols = np.concatenate([
        colpack(inp["tmix_mu"][0], 15), colpack(inp["k_k"][0], 4), colpack(inp["k_a"][0], 4), colpack(inp["r_k"][0], 4),
        colpack(inp["a0"][0], 4), colpack(inp["lnx_g"][0], 4), colpack(inp["lnx_b"][0], 4), colpack(inp["b_gate"][0], 16),
        colpack(inp["norm1_g"][0], 8), colpack(inp["norm2_g"][0], 8)], axis=1)
    w_re = np.asarray(inp["w_re"][0], np.float32)
    w_r = np.concatenate([np.asarray(inp["w_rg"][0], np.float32), w_re.transpose(1, 0, 2).reshape(D, 32)], axis=1)
    b_r = np.concatenate([np.asarray(inp["b_rg"][0], np.float32).reshape(-1), np.asarray(inp["b_re"][0], np.float32).reshape(-1)])
    return {
        "x": f(inp["x"][b, :T]), "w_in": f(inp["w_in"][0]), "cols": f(cols), "w0": f(inp["w0"][0]).reshape(1, 512),
        "w2": f(inp["w2"][0]), "a2": f(inp["a2"][0]), "g2": f(inp["g2"][0]), "w_oA": f(inp["w_oA"][0]), "w_oB": f(inp["w_oB"][0]),
        "w_out": f(inp["w_out"][0]), "lnv_g": f(inp["lnv_g"][0]).reshape(1, 512), "lnv_b": f(inp["lnv_b"][0]).reshape(1, 512),
        "wsT": f(np.asarray(inp["w_s"][0], np.float32).transpose(0, 2, 1)), "b_s": f(inp["b_s"][0]).reshape(1, 512),
        "w_r": f(w_r), "b_r": f(b_r).reshape(1, 36), "w_e_gate": f(inp["w_e_gate"][0]), "w_e_up": f(inp["w_e_up"][0]),
        "w_e_down": f(inp["w_e_down"][0]), "final_g": f(inp["final_g"]).reshape(1, D),
    }


def kernel(**inputs):
    T = 2048
    nc = build(T)
    in_maps = [host_layout(inputs, b, T) for b in range(8)]
    res = run_bass_kernel_spmd(nc, in_maps, core_ids=list(range(8)))
    return np.stack([np.asarray(r["out"], dtype=np.float32) for r in res.results], axis=0)
```

```python
import re
import numpy as np
from contextlib import ExitStack
import concourse.bass as bass
import concourse.mybir as mybir
from concourse.bass_utils import run_bass_kernel_spmd

F32 = mybir.dt.float32
BF16 = mybir.dt.bfloat16
AF = mybir.ActivationFunctionType
ALU = mybir.AluOpType
AX = mybir.AxisListType
ENGS = ["pe", "dve", "act", "pool", "sp"]
DMA_RING = {"sp": 6, "pool": 4}

D = 1024
IN_COLS = 4896
A_COLS = 1824
NCHA = 15
LNX_EPS = 64e-5
LN_EPS = 1e-5
NORM_EPS = 1e-6
EXPM05 = float(np.exp(-0.5))
PROFILE_SCOPES = False

C_MU, C_KK, C_KA, C_RK, C_A0, C_LG, C_LB, C_BG, C_G1, C_G2 = 0, 15, 19, 23, 27, 31, 35, 39, 55, 63
NCOL = 71


class Rec:
    def __init__(self):
        self.calls = []

    def __getattr__(self, name):
        def f(*a, **k):
            self.calls.append((name, a, k))
            return self
        return f


def _record(fn):
    r = Rec()
    fn(r)
    assert r.calls
    return r.calls


class Prog:
    def __init__(self, nc, stack):
        self.nc = nc
        self.lists = {e: [] for e in ENGS}
        self.count = {e: 0 for e in ENGS}
        self.sems = {}
        for e in ENGS:
            self.sems[("e", e)] = stack.enter_context(nc.semaphore(f"s_{e}"))
        for q, r in DMA_RING.items():
            for i in range(r):
                self.sems[("d", q, i)] = stack.enter_context(nc.semaphore(f"d_{q}{i}"))
        self.dma_n = {q: 0 for q in DMA_RING}
        self.waited = {e: {} for e in ENGS}
        self.last_w = {}
        self.readers = {}
        self.scope = None

    def _waits(self, eng, reads, writes, extra=()):
        need = {}

        def add(s, v):
            if need.get(s, 0) < v:
                need[s] = v

        for k in reads:
            if k in self.last_w:
                add(*self.last_w[k])
        for k in writes:
            if k in self.last_w:
                add(*self.last_w[k])
            for s, v in self.readers.get(k, {}).items():
                add(s, v)
        for s, v in extra:
            add(s, v)
        out = []
        wd = self.waited[eng]
        for s, v in need.items():
            if wd.get(s, 0) < v:
                wd[s] = v
                out.append((s, v))
        return out

    def _commit(self, tok, reads, writes):
        for k in writes:
            self.last_w[k] = tok
            self.readers[k] = {}
        for k in reads:
            d = self.readers.setdefault(k, {})
            if d.get(tok[0], 0) < tok[1]:
                d[tok[0]] = tok[1]

    LIMIT = None
    NOPS = 0

    def op(self, eng, fn, reads=(), writes=()):
        Prog.NOPS += 1
        if Prog.LIMIT is not None and Prog.NOPS > Prog.LIMIT:
            return None
        writes = list(writes) + [k for k in reads if k.startswith("ps")]
        waits = self._waits(eng, reads, writes)
        self.count[eng] += 1
        tok = (("e", eng), self.count[eng])
        self.lists[eng].append((waits, _record(fn), (("e", eng), 1), self.scope))
        self._commit(tok, reads, writes)
        return tok

    def dma(self, q, fn, reads=(), writes=()):
        Prog.NOPS += 1
        if Prog.LIMIT is not None and Prog.NOPS > Prog.LIMIT:
            return None
        r = DMA_RING[q]
        i = self.dma_n[q]
        self.dma_n[q] += 1
        slot = i % r
        skey = ("d", q, slot)
        extra = [(skey, 16 * (i // r))] if i >= r else []
        waits = self._waits(q, reads, writes, extra)
        tok = (skey, 16 * (i // r + 1))
        self.lists[q].append((waits, _record(fn), (skey, 16), self.scope))
        self._commit(tok, reads, writes)
        return tok

    def _all_tokens(self):
        toks = []
        for q, r in DMA_RING.items():
            n = self.dma_n[q]
            for slot in range(min(r, n)):
                toks.append((("d", q, slot), 16 * ((n - 1 - slot) // r + 1)))
        for e in ENGS:
            if self.count[e]:
                toks.append((("e", e), self.count[e]))
        return toks

    def barrier(self):
        toks = self._all_tokens()
        for e in ENGS:
            waits = self._waits(e, (), (), toks)
            if waits:
                self.lists[e].append((waits, None, None, None))

    def final_wait(self, eng):
        waits = self._waits(eng, (), (), self._all_tokens())
        self.lists[eng].append((waits, None, None, None))

    def emit(self):
        engmap = {"pe": "tensor", "dve": "vector", "act": "scalar", "pool": "gpsimd", "sp": "sync"}
        with self.nc.Block() as block:
            for e in ENGS:
                lst = self.lists[e]
                if not lst:
                    continue

                def body(engine, lst=lst):
                    cur, cm = None, None
                    for waits, fn, inc, scope in lst:
                        if PROFILE_SCOPES and scope != cur:
                            if cm is not None:
                                cm.__exit__(None, None, None)
                                cm = None
                            if scope is not None:
                                cm = self.nc.named_scope(scope)
                                cm.__enter__()
                            cur = scope
                        for s, v in waits:
                            engine.wait_ge(self.sems[s], v)
                        if fn is not None:
                            ins = None
                            for name, a, k in fn:
                                ins = getattr(engine, name)(*a, **k)
                            ins.then_inc(self.sems[inc[0]], inc[1])
                    if cm is not None:
                        cm.__exit__(None, None, None)

                getattr(block, engmap[e])(body)


def build(T, NE=32, dbg=None):
    NT = T // 128
    nc = bass.Bass("TRN2", target_bir_lowering=False)

    def din(name, shape, dt=F32):
        return nc.dram_tensor(name, list(shape), dt, kind="ExternalInput").ap()

    x_d = din("x", [T, D])
    w_in_d = din("w_in", [D, IN_COLS])
    cols_d = din("cols", [128, NCOL])
    w0_d = din("w0", [1, 512])
    w2_d = din("w2", [64, 512])
    a2_d = din("a2", [64, 512])
    g2_d = din("g2", [160, 512])
    woA_d = din("w_oA", [512, D])
    woB_d = din("w_oB", [512, D])
    wout_d = din("w_out", [D, D])
    lnvg_d = din("lnv_g", [1, 512])
    lnvb_d = din("lnv_b", [1, 512])
    wsT_d = din("wsT", [4, 128, 128])
    bs_d = din("b_s", [1, 512])
    wr_d = din("w_r", [D, 36])
    br_d = din("b_r", [1, 36])
    weg_d = din("w_e_gate", [32, D, 256])
    weu_d = din("w_e_up", [32, D, 256])
    wed_d = din("w_e_down", [32, 256, D])
    fg_d = din("final_g", [1, D])
    out_d = nc.dram_tensor("out", [T, D], F32, kind="ExternalOutput").ap()
    x1_d = nc.dram_tensor("x1_scr", [T, D], F32, kind="Internal").ap()
    h2_d = nc.dram_tensor("h2_scr", [128, 8, T], BF16, kind="Internal").ap()
    hT_d = nc.dram_tensor("hT_scr", [T // 128, 128, 1024], BF16, kind="Internal").ap()
    yg_d = nc.dram_tensor("yg_scr", [T // 128, 128, 512], BF16, kind="Internal").ap()
    dbg_out = {}
    if dbg:
        for name, shape in dbg.items():
            dbg_out[name] = nc.dram_tensor("dbg_" + name, list(shape), F32, kind="ExternalOutput").ap()

    with ExitStack() as st0:
        P = Prog(nc, st0)

        def sbuf(st, name, shape, dt):
            return st.enter_context(nc.sbuf_tensor("sb_" + name, list(shape), dt))

        psM = [st0.enter_context(nc.psum_tensor(f"psM{i}", [128, 512], F32)) for i in range(2)]
        psA = st0.enter_context(nc.psum_tensor("psA", [128, 1024], F32))
        psD = st0.enter_context(nc.psum_tensor("psD", [128, 1536], F32))
        psS = st0.enter_context(nc.psum_tensor("psS", [128, 512], F32))

        identf = sbuf(st0, "identf", [128, 128], F32)
        ident = sbuf(st0, "ident", [128, 128], BF16)
        mSU = sbuf(st0, "mSU", [128, 128], F32)
        mIU = sbuf(st0, "mIU", [128, 128], F32)
        maskA = sbuf(st0, "maskA", [128, 2, 512], F32)
        maskL = sbuf(st0, "maskL", [128, 2, 128], F32)
        BDf = sbuf(st0, "BDf", [128, 128], F32)
        BDb = sbuf(st0, "BDb", [128, 128], BF16)
        BD64 = sbuf(st0, "BD64", [128, 128], F32)
        Tri2 = sbuf(st0, "Tri2", [128, 256], F32)
        onesb = sbuf(st0, "onesb", [1, 128], BF16)
        onesf = sbuf(st0, "onesf", [128, 128], F32)
        cols = sbuf(st0, "cols", [128, NCOL], F32)
        omu = sbuf(st0, "omu", [128, NCHA], F32)
        oka = sbuf(st0, "oka", [128, 4], F32)
        g1bc = sbuf(st0, "g1bc", [128, 8, 128], F32)
        g2bc = sbuf(st0, "g2bc", [128, 8, 128], F32)

        def colap(c):
            return cols[:, c:c + 1]

        P.dma("sp", lambda e: e.dma_start(out=cols[:], in_=cols_d), writes=["cols"])
        P.op("pool", lambda e: e.memset(onesf[:], 1.0), writes=["onesf"])
        P.op("pool", lambda e: e.memset(identf[:], 0.0), writes=["identf"])
        P.op("pool", lambda e: e.affine_select(out=identf[:], in_=identf[:], pattern=[[-1, 128]], compare_op=ALU.not_equal,
                                               fill=1.0, base=0, channel_multiplier=1), reads=["identf"], writes=["identf"])
        P.op("dve", lambda e: e.tensor_copy(out=ident[:], in_=identf[:]), reads=["identf"], writes=["ident"])
        P.op("pool", lambda e: e.affine_select(out=mSU[:], in_=onesf[:], pattern=[[1, 128]], compare_op=ALU.is_gt,
                                               fill=0.0, base=0, channel_multiplier=-1), reads=["onesf"], writes=["mSU"])
        P.op("pool", lambda e: e.affine_select(out=mIU[:], in_=onesf[:], pattern=[[1, 128]], compare_op=ALU.is_ge,
                                               fill=0.0, base=0, channel_multiplier=-1), reads=["onesf"], writes=["mIU"])
        for h in range(2):
            for q in range(4):
                src = mSU if q % 2 == 0 else mIU
                P.op("dve", lambda e, h=h, q=q, src=src: e.tensor_copy(out=maskA[:, h, q * 128:(q + 1) * 128], in_=src[:]),
                     reads=["mSU", "mIU"], writes=["maskA"])
            P.op("pool", lambda e, h=h: e.affine_select(out=maskL[:, h, :], in_=onesf[:], pattern=[[-1, 128]], compare_op=ALU.is_gt,
                                                        fill=0.0, base=0, channel_multiplier=1), reads=["onesf"], writes=["maskL"])
        P.op("pool", lambda e: e.memset(BDf[:], 0.0), writes=["BDf"])
        P.op("pool", lambda e: e.memset(BDf[0:64, 0:64], 1.0), reads=["BDf"], writes=["BDf"])
        P.op("pool", lambda e: e.memset(BDf[64:128, 64:128], 1.0), reads=["BDf"], writes=["BDf"])
        P.op("dve", lambda e: e.tensor_copy(out=BDb[:], in_=BDf[:]), reads=["BDf"], writes=["BDb"])
        P.op("dve", lambda e: e.tensor_scalar(out=BD64[:], in0=BDf[:], scalar1=1.0 / 64.0, scalar2=None, op0=ALU.mult),
             reads=["BDf"], writes=["BD64"])
        P.op("dve", lambda e: e.tensor_scalar(out=Tri2[:, 0:128], in0=mIU[:], scalar1=EXPM05, scalar2=None, op0=ALU.mult),
             reads=["mIU"], writes=["Tri2"])
        P.op("dve", lambda e: e.tensor_scalar(out=Tri2[:, 128:256], in0=mSU[:], scalar1=EXPM05, scalar2=None, op0=ALU.mult),
             reads=["mSU", "Tri2"], writes=["Tri2"])
        P.op("dve", lambda e: e.tensor_copy(out=onesb[:], in_=onesf[0:1, :]), reads=["onesf"], writes=["onesb"])
        P.op("dve", lambda e: e.tensor_scalar(out=omu[:], in0=cols[:, C_MU:C_MU + NCHA], scalar1=-1.0, scalar2=1.0,
                                              op0=ALU.mult, op1=ALU.add), reads=["cols"], writes=["omu"])
        P.op("dve", lambda e: e.tensor_scalar(out=oka[:], in0=cols[:, C_KA:C_KA + 4], scalar1=-1.0, scalar2=1.0,
                                              op0=ALU.mult, op1=ALU.add), reads=["cols"], writes=["oka"])
        for k in range(8):
            P.op("dve", lambda e, k=k: e.tensor_scalar(out=g1bc[:, k, :], in0=onesf[:], scalar1=colap(C_G1 + k), scalar2=None,
                                                       op0=ALU.mult), reads=["cols", "onesf"], writes=["g1bc"])
            P.op("dve", lambda e, k=k: e.tensor_scalar(out=g2bc[:, k, :], in0=onesf[:], scalar1=colap(C_G2 + k), scalar2=None,
                                                       op0=ALU.mult), reads=["cols", "onesf"], writes=["g2bc"])

        def rms_to_T(st_keys, xin, xin_key, gbc, gbc_key, dst_ap, dst_key, tmp, tag):
            junk, ss, sd, rs, xsb = tmp
            P.op("act", lambda e: e.activation(out=junk[:], in_=xin, func=AF.Square, accum_out=ss[:]),
                 reads=[xin_key], writes=["junk" + tag, "ss" + tag])
            P.op("act", lambda e: e.activation(out=sd[:], in_=ss[:], func=AF.Sqrt, bias=NORM_EPS, scale=1.0 / D),
                 reads=["ss" + tag], writes=["sd" + tag])
            P.op("dve", lambda e: e.reciprocal(out=rs[:], in_=sd[:]), reads=["sd" + tag], writes=["rs" + tag])
            P.op("dve", lambda e: e.tensor_scalar(out=xsb[:], in0=xin, scalar1=rs[:], scalar2=None, op0=ALU.mult),
                 reads=[xin_key, "rs" + tag], writes=["xsb" + tag])
            pT = psM[0][:].bitcast(BF16)

            def tr(e):
                ins = None
                for k in range(8):
                    ins = e.transpose(out=pT[:, k * 128:(k + 1) * 128], in_=xsb[:, k * 128:(k + 1) * 128], identity=ident[:])
                return ins

            P.op("pe", tr, reads=["xsb" + tag, "ident"], writes=["psM0"])
            P.op("dve", lambda e: e.tensor_tensor(out=dst_ap, in0=pT.rearrange("p (k t) -> p k t", k=8), in1=gbc[:], op=ALU.mult),
                 reads=["psM0", gbc_key], writes=[dst_key])

        P.scope = "M1"
        stP = ExitStack()
        comb = sbuf(st0, "comb", [128, NT, 32], F32)
        woA = sbuf(stP, "woA", [128, 4, D], BF16)
        woB = sbuf(stP, "woB", [128, 4, D], BF16)
        wout = sbuf(stP, "wout", [128, 8, D], BF16)

        with ExitStack() as st:
            hTb = [sbuf(st, f"hTt{i}", [128, 8, 128], BF16) for i in range(2)]
            ygb = [sbuf(st, f"ygt{i}", [128, 4, 128], BF16) for i in range(2)]
            w_inA = sbuf(st, "w_inA", [128, 8, A_COLS], BF16)
            w2b = sbuf(st, "w2b", [64, 512], BF16)
            a2b = sbuf(st, "a2b", [128, 512], BF16)
            g2b = sbuf(st, "g2b", [128, 2, 512], BF16)
            w0row = sbuf(st, "w0row", [1, 512], BF16)
            for k in range(8):
                P.dma("pool", lambda e, k=k: e.dma_start(out=w_inA[:, k, :], in_=w_in_d[k * 128:(k + 1) * 128, 0:A_COLS]),
                      writes=["w_inA"])
            P.dma("pool", lambda e: e.dma_start(out=w2b[:], in_=w2_d), writes=["w2b"])
            P.dma("pool", lambda e: e.dma_start(out=a2b[64:128, :], in_=a2_d), writes=["a2b"])
            P.dma("pool", lambda e: e.dma_start(out=g2b[:, 0, :], in_=g2_d[0:128, :]), writes=["g2b"])
            P.dma("pool", lambda e: e.dma_start(out=g2b[0:32, 1, :], in_=g2_d[128:160, :]), reads=["g2b"], writes=["g2b"])
            P.dma("pool", lambda e: e.dma_start(out=w0row[:], in_=w0_d), writes=["w0row"])
            P.dma("pool", lambda e: e.dma_start(out=woA[:], in_=woA_d.rearrange("(k p) n -> p k n", p=128)), writes=["woA"])
            P.dma("pool", lambda e: e.dma_start(out=woB[:], in_=woB_d.rearrange("(k p) n -> p k n", p=128)), writes=["woB"])
            P.dma("pool", lambda e: e.dma_start(out=wout[:], in_=wout_d.rearrange("(k p) n -> p k n", p=128)), writes=["wout"])

            xin = [sbuf(st, f"xin{i}", [128, D], F32) for i in range(2)]
            junk = sbuf(st, "junk", [128, D], BF16)
            ss = sbuf(st, "ss", [128, 1], F32)
            sd = sbuf(st, "sd", [128, 1], F32)
            rs = sbuf(st, "rs", [128, 1], F32)
            xsb = sbuf(st, "xsb", [128, D], BF16)
            carry = sbuf(st, "carry", [128, NCHA], F32)
            ltmp = sbuf(st, "ltmp", [128, 129], F32)
            pmx = sbuf(st, "pmx", [128, 128], F32)
            txw2 = [sbuf(st, f"txw_{i}", [128, 128], BF16) for i in range(2)]
            sxg2 = [sbuf(st, f"sxg_{i}", [128, 2, 128], BF16) for i in range(2)]
            DC2 = [sbuf(st, f"DC_{i}", [128, 4], F32) for i in range(2)]
            etok = sbuf(st, "etok", [128, 512], F32)
            Dt2 = [sbuf(st, f"Dt_{i}", [128, 4, 128], F32) for i in range(2)]
            Dinv2 = [sbuf(st, f"Dinv_{i}", [128, 4, 128], F32) for i in range(2)]
            Dprev2 = [sbuf(st, f"Dprev_{i}", [128, 4, 128], F32) for i in range(2)]
            asig = [sbuf(st, f"asig{p}", [128, 128], F32) for p in range(4)]
            tA = [sbuf(st, f"tA{p}", [128, 128], F32) for p in range(4)]
            tB = [sbuf(st, f"tB{p}", [128, 128], F32) for p in range(4)]
            tC = [sbuf(st, f"tC{p}", [128, 128], F32) for p in range(4)]
            t16 = [sbuf(st, f"t16{p}", [128, 128], BF16) for p in range(4)]
            v16 = [sbuf(st, f"v16{p}", [128, 128], BF16) for p in range(4)]
            pmr = [sbuf(st, f"pmr{p}", [128, 128], F32) for p in range(4)]
            pmk = [sbuf(st, f"pmk{p}", [128, 128], F32) for p in range(4)]
            pmv = [sbuf(st, f"pmv{p}", [128, 128], F32) for p in range(4)]
            ltm = [sbuf(st, f"ltm{p}", [128, 129], F32) for p in range(4)]
            PX = [psM[0], psM[1], psA[:, 0:512], psA[:, 512:1024]]
            PXk = ["psM0", "psM1", "psA", "psA2"]
            PY = [psD[:, 0:512], psD[:, 512:1024], psD[:, 1024:1536], psS]
            PYk = ["psD0", "psD1", "psD2", "psS"]
            AR2 = [[sbuf(st, f"AR{p}_{i}", [128, 2, 128], BF16) for p in range(4)] for i in range(1)] * 2
            BT = [sbuf(st, f"BT{p}", [128, 128], BF16) for p in range(4)]
            KT = [sbuf(st, f"KT{p}", [128, 128], BF16) for p in range(4)]
            TOK2 = [[sbuf(st, f"TOK{p}_{i}", [128, 3, 128], BF16) for p in range(4)] for i in range(1)] * 2
            bon2 = [[sbuf(st, f"bon{p}_{i}", [128, 128], F32) for p in range(4)] for i in range(1)] * 2
            gT2 = [[sbuf(st, f"gT{p}_{i}", [128, 128], F32) for p in range(4)] for i in range(1)] * 2
            AM2 = [[sbuf(st, f"AM{p}_{i}", [128, 2, 512], BF16) for p in range(4)] for i in range(1)] * 2
            L0 = [sbuf(st, f"L0{p}", [128, 2, 128], BF16) for p in range(4)]
            MT2 = [[[sbuf(st, f"MT{p}_{i}_{j}", [128, 2, 256], BF16) for i in range(2)] for p in range(4)] for j in range(1)] * 2
            LK = [[sbuf(st, f"LK{p}_{i}", [128, 2, 128], BF16) for i in range(2)] for p in range(4)]
            Sw = [sbuf(st, f"Sw{p}", [128, 128], F32) for p in range(4)]
            Sb = [sbuf(st, f"Sb{p}", [128, 128], BF16) for p in range(4)]
            Xb = [sbuf(st, f"Xb{p}", [128, 128], BF16) for p in range(4)]
            Ub = [sbuf(st, f"Ub{p}", [128, 128], BF16) for p in range(4)]
            yT = sbuf(st, "yT", [128, 4, 128], F32)
            yc = sbuf(st, "yc", [128, 512], F32)
            ysq = sbuf(st, "ysq", [128, 512], F32)
            yrs = sbuf(st, "yrs", [128, 512], F32)
            y3 = sbuf(st, "y3", [128, 128], F32)
            tS = sbuf(st, "tS", [128, 128], F32)

            P.op("pool", lambda e: e.memset(carry[:], 0.0), writes=[f"carry{c}" for c in range(NCHA)])
            for p in range(4):
                P.op("pool", lambda e, p=p: e.memset(Sw[p][:], 0.0), writes=[f"Sw{p}"])
                P.op("pool", lambda e, p=p: e.memset(Sb[p][:], 0.0), writes=[f"Sb{p}"])

            def LV(p):
                return psD[:, 1024:1280] if p % 2 == 0 else psS[:, 0:256]

            def DK(p):
                return ["psD0", "psD2"] if p % 2 == 0 else ["psD1", "psS"]

            serial_ps = [psS, psM[1], psA[:, 0:512], psA[:, 512:1024]]
            serial_key = ["psS", "psM1", "psA", "psA2"]

            def tile_parts(tl, P):
                par = tl % 2
                AM, AR, TOK, MT, bon, gT = AM2[par], AR2[par], TOK2[par], MT2[par], bon2[par], gT2[par]
                Dt, Dinv, Dprev, txw, sxg, DC = Dt2[par], Dinv2[par], Dprev2[par], txw2[par], sxg2[par], DC2[par]
                tc = slice(tl * 128, (tl + 1) * 128)
                xb_, xk = xin[tl % 2], f"xin{tl % 2}"
                hT, hTk = hTb[tl % 2], f"hTt{tl % 2}"
                ygT, ygk = ygb[tl % 2], f"ygt{tl % 2}"

                def inproj_chunk(c, dst_fn):
                    rows = 128 if c < 14 else 32
                    bank, bkey = (psD[:, 512:1024], "psD1") if c == 13 else (psM[0], "psM0")

                    def mmf(e, c=c, rows=rows, bank=bank):
                        ins = None
                        for k in range(8):
                            ins = e.matmul(bank[0:rows, 0:128], lhsT=w_inA[:, k, c * 128:c * 128 + rows], rhs=hT[:, k, :],
                                           start=(k == 0), stop=(k == 7))
                        return ins

                    P.op("pe", mmf, reads=["w_inA", hTk], writes=[bkey])
                    P.op("act", lambda e: e.activation(out=ltmp[0:rows, 1:129], in_=bank[0:rows, 0:128], func=AF.Copy,
                                                       scale=cols[0:rows, C_MU + c:C_MU + c + 1]),
                         reads=[bkey, "cols"], writes=["ltmp"])
                    P.op("pool", lambda e: e.tensor_copy(out=ltmp[0:rows, 0:1], in_=carry[0:rows, c:c + 1]),
                         reads=[f"carry{c}", "ltmp"], writes=["ltmp"])
                    P.op("pool", lambda e: e.tensor_copy(out=carry[0:rows, c:c + 1], in_=ltmp[0:rows, 128:129]),
                         reads=["ltmp", f"carry{c}"], writes=[f"carry{c}"])
                    dst, dkey = dst_fn
                    P.op("dve", lambda e: e.scalar_tensor_tensor(out=dst[0:rows, :], in0=bank[0:rows, 0:128],
                                                                 scalar=omu[0:rows, c:c + 1], in1=ltmp[0:rows, 0:128],
                                                                 op0=ALU.mult, op1=ALU.add),
                         reads=[bkey, "omu", "ltmp"], writes=[dkey])

                def inproj_chunk_g(c, dst, dkey, bank, bkey, lt, ltk):
                    rows = 128

                    def mmf(e):
                        ins = None
                        for k in range(8):
                            ins = e.matmul(bank[0:rows, 0:128], lhsT=w_inA[:, k, c * 128:c * 128 + rows], rhs=hT[:, k, :],
                                           start=(k == 0), stop=(k == 7))
                        return ins

                    P.op("pe", mmf, reads=["w_inA", hTk], writes=[bkey])
                    yield
                    P.op("act", lambda e: e.activation(out=lt[:, 1:129], in_=bank[:, 0:128], func=AF.Copy, scale=cols[:, C_MU + c:C_MU + c + 1]),
                         reads=[bkey, "cols"], writes=[ltk])
                    yield
                    P.op("pool", lambda e: e.tensor_copy(out=lt[:, 0:1], in_=carry[:, c:c + 1]), reads=[f"carry{c}", ltk], writes=[ltk])
                    P.op("pool", lambda e: e.tensor_copy(out=carry[:, c:c + 1], in_=lt[:, 128:129]), reads=[ltk, f"carry{c}"], writes=[f"carry{c}"])
                    yield
                    P.op("dve", lambda e: e.scalar_tensor_tensor(out=dst[:], in0=bank[:, 0:128], scalar=omu[:, c:c + 1], in1=lt[:, 0:128],
                                                                 op0=ALU.mult, op1=ALU.add), reads=[bkey, "omu", ltk], writes=[dkey])
                    yield


                def front():
                    xb_ = xin[tl % 2]
                    xk = f"xin{tl % 2}"
                    P.dma("sp", lambda e, xb_=xb_, tl=tl: e.dma_start(out=xb_[:], in_=x_d[tl * 128:(tl + 1) * 128, :]), writes=[xk])
                    yield
                    hT, hTk = hTb[tl % 2], f"hTt{tl % 2}"
                    ygT, ygk = ygb[tl % 2], f"ygt{tl % 2}"
                    rms_to_T(None, xb_[:], xk, g1bc, "g1bc", hT[:], hTk, (junk, ss, sd, rs, xsb), "1")
                    yield
                    P.dma("sp", lambda e, tl=tl, hT=hT: e.dma_start(out=hT_d[tl], in_=hT[:].rearrange("p k t -> p (k t)")), reads=[hTk], writes=["hT_d"])
                    yield

                    inproj_chunk(12, (pmx, "pmx"))
                    yield
                    P.op("act", lambda e: e.activation(out=txw[0:64, :], in_=pmx[0:64, :], func=AF.Tanh), reads=["pmx"], writes=["txw"])
                    yield
                    P.op("dve", lambda e: e.tensor_copy(out=txw[64:128, :], in_=pmx[64:128, :]), reads=["pmx", "txw"], writes=["txw"])
                    yield
                    inproj_chunk(13, (pmx, "pmx"))
                    yield
                    P.op("act", lambda e: e.activation(out=sxg[:, 0, :], in_=pmx[:], func=AF.Sigmoid), reads=["pmx"], writes=["sxg"])
                    yield
                    inproj_chunk(14, (pmx, "pmx"))
                    yield
                    P.op("act", lambda e: e.activation(out=sxg[0:32, 1, :], in_=pmx[0:32, :], func=AF.Sigmoid),
                         reads=["pmx", "sxg"], writes=["sxg"])
                    yield

                    def zmm(e):
                        e.matmul(psM[0][:, 0:512], lhsT=txw[0:64, :], rhs=w2b[0:64, :], start=True, stop=False)
                        return e.matmul(psM[0][:, 0:512], lhsT=onesb[0:1, :], rhs=w0row[0:1, :], start=False, stop=True)

                    P.op("pe", zmm, reads=["txw", "w2b", "onesb", "w0row"], writes=["psM0"])
                    yield
                    P.op("act", lambda e: e.activation(out=etok[:], in_=psM[0][:, 0:512], func=AF.Sigmoid), reads=["psM0"], writes=["etok"])
                    yield

                    def cmm(e):
                        ins = None
                        for p in range(4):
                            ins = e.matmul(psD[:, 512 + p * 256:512 + (p + 1) * 256], lhsT=etok[:, p * 128:(p + 1) * 128], rhs=Tri2[:],
                                           start=True, stop=True)
                        return ins

                    P.op("pe", cmm, reads=["etok", "Tri2"], writes=["psD1", "psD2"])
                    yield
                    cum = psD[:, 512:1536].rearrange("p (a c) -> p a c", a=4)
                    P.op("act", lambda e: e.activation(out=Dt[:], in_=cum[:, :, 0:128], func=AF.Exp, scale=-1.0),
                         reads=["psD1", "psD2"], writes=["Dt"])
                    yield
                    P.op("act", lambda e: e.activation(out=Dinv[:], in_=cum[:, :, 0:128], func=AF.Exp, scale=1.0),
                         reads=["psD1", "psD2"], writes=["Dinv"])
                    yield
                    P.op("act", lambda e: e.activation(out=Dprev[:], in_=cum[:, :, 128:256], func=AF.Exp, scale=-1.0),
                         reads=["psD1", "psD2"], writes=["Dprev"])
                    yield


                def pair_gen(p):
                    X, Xk, Y, Yk = PX[p], PXk[p], PY[p], PYk[p]
                    rf, kf, vf = pmr[p], pmk[p], pmv[p]
                    rk_, kk_, vk_ = f"pmr{p}", f"pmk{p}", f"pmv{p}"
                    as_, tA_, tB_, tC_, t16_, v16_ = asig[p], tA[p], tB[p], tC[p], t16[p], v16[p]
                    ak, tAk, tBk, tCk, t16k, v16k = f"asig{p}", f"tA{p}", f"tB{p}", f"tC{p}", f"t16{p}", f"v16{p}"
                    cs = slice(p * 128, (p + 1) * 128)
                    for (c, dst, dkey, bank, bkey) in ((p, rf, rk_, X, Xk), (4 + p, kf, kk_, Y, Yk), (8 + p, vf, vk_, X, Xk)):
                        yield from inproj_chunk_g(c, dst, dkey, bank, bkey, ltm[p], f"ltm{p}")
                    P.op("pe", lambda e: e.matmul(Y[:, 0:128], lhsT=a2b[64:128, cs], rhs=txw[64:128, :], start=True, stop=True),
                         reads=["a2b", "txw"], writes=[Yk])
                    yield
                    P.op("act", lambda e: e.activation(out=as_[:], in_=Y[:, 0:128], func=AF.Sigmoid, bias=colap(C_A0 + p)),
                         reads=[Yk, "cols"], writes=[ak])
                    yield

                    def gmm(e):
                        e.matmul(X[:, 0:128], lhsT=g2b[:, 0, cs], rhs=sxg[:, 0, :], start=True, stop=False)
                        return e.matmul(X[:, 0:128], lhsT=g2b[0:32, 1, cs], rhs=sxg[0:32, 1, :], start=False, stop=True)

                    P.op("pe", gmm, reads=["g2b", "sxg"], writes=[Xk])
                    yield
                    P.op("act", lambda e: e.activation(out=gT[p][:], in_=X[:, 0:128], func=AF.Copy), reads=[Xk], writes=[f"gT{p}"])
                    yield
                    P.op("dve", lambda e: e.tensor_scalar(out=tA_[:], in0=kf[:], scalar1=colap(C_KK + p), scalar2=None, op0=ALU.mult),
                         reads=[kk_, "cols"], writes=[tAk])
                    yield
                    P.op("pool", lambda e: e.tensor_tensor(out=t16_[:], in0=tA_[:], in1=tA_[:], op=ALU.mult), reads=[tAk], writes=[t16k])
                    yield
                    P.op("pe", lambda e: e.matmul(Y[:, 0:128], lhsT=BDb[:], rhs=t16_[:], start=True, stop=True), reads=["BDb", t16k], writes=[Yk])
                    yield
                    P.op("act", lambda e: e.activation(out=tB_[:], in_=Y[:, 0:128], func=AF.Sqrt), reads=[Yk], writes=[tBk])
                    yield
                    P.op("dve", lambda e: e.tensor_scalar(out=tB_[:], in0=tB_[:], scalar1=1e-12, scalar2=None, op0=ALU.max), reads=[tBk], writes=[tBk])
                    P.op("dve", lambda e: e.reciprocal(out=tB_[:], in_=tB_[:]), reads=[tBk], writes=[tBk])
                    yield
                    P.op("pool", lambda e: e.tensor_tensor(out=tA_[:], in0=tA_[:], in1=tB_[:], op=ALU.mult), reads=[tAk, tBk], writes=[tAk])
                    yield
                    P.op("dve", lambda e: e.tensor_scalar(out=tC_[:], in0=as_[:], scalar1=colap(C_KA + p), scalar2=oka[:, p:p + 1],
                                                          op0=ALU.mult, op1=ALU.add), reads=[ak, "cols", "oka"], writes=[tCk])
                    yield
                    P.op("pool", lambda e: e.tensor_tensor(out=kf[:], in0=kf[:], in1=tC_[:], op=ALU.mult), reads=[kk_, tCk], writes=[kk_])
                    yield
                    P.op("dve", lambda e: e.scalar_tensor_tensor(out=AR[p][:, 0, :], in0=tA_[:], scalar=-1.0, in1=Dprev[:, p, :],
                                                                 op0=ALU.mult, op1=ALU.mult), reads=[tAk, "Dprev"], writes=[f"AR{p}"])
                    P.op("pool", lambda e: e.tensor_tensor(out=AR[p][:, 1, :], in0=rf[:], in1=Dt[:, p, :], op=ALU.mult),
                         reads=[rk_, "Dt", f"AR{p}"], writes=[f"AR{p}"])
                    yield
                    P.op("pool", lambda e: e.tensor_tensor(out=tB_[:], in0=tA_[:], in1=as_[:], op=ALU.mult), reads=[tAk, ak, tBk], writes=[tBk])
                    yield
                    P.op("dve", lambda e: e.tensor_tensor(out=BT[p][:], in0=tB_[:], in1=Dinv[:, p, :], op=ALU.mult), reads=[tBk, "Dinv"], writes=[f"BT{p}"])
                    P.op("dve", lambda e: e.tensor_tensor(out=KT[p][:], in0=kf[:], in1=Dinv[:, p, :], op=ALU.mult), reads=[kk_, "Dinv"], writes=[f"KT{p}"])
                    yield
                    P.op("dve", lambda e: e.scalar_tensor_tensor(out=t16_[:], in0=rf[:], scalar=colap(C_RK + p), in1=kf[:],
                                                                 op0=ALU.mult, op1=ALU.mult), reads=[rk_, kk_, "cols", t16k], writes=[t16k])
                    yield
                    P.op("pe", lambda e: e.matmul(Y[:, 0:128], lhsT=BDb[:], rhs=t16_[:], start=True, stop=True), reads=["BDb", t16k], writes=[Yk])
                    yield
                    P.op("dve", lambda e: e.tensor_tensor(out=bon[p][:], in0=Y[:, 0:128], in1=vf[:], op=ALU.mult), reads=[Yk, vk_], writes=[f"bon{p}"])
                    P.op("dve", lambda e: e.tensor_scalar(out=bon[p][:], in0=bon[p][:], scalar1=colap(C_LB + p), scalar2=None, op0=ALU.add),
                         reads=[f"bon{p}", "cols"], writes=[f"bon{p}"])
                    yield
                    P.op("act", lambda e: e.activation(out=v16_[:], in_=vf[:], func=AF.Copy), reads=[vk_], writes=[v16k])
                    yield
                    pT = X[:, 0:256].bitcast(BF16)

                    def tr3(e):
                        e.transpose(out=pT[:, 0:128], in_=v16_[:], identity=ident[:])
                        e.transpose(out=pT[:, 128:256], in_=BT[p][:], identity=ident[:])
                        return e.transpose(out=pT[:, 256:384], in_=KT[p][:], identity=ident[:])

                    P.op("pe", tr3, reads=[v16k, f"BT{p}", f"KT{p}", "ident"], writes=[Xk])
                    yield
                    P.op("act", lambda e: e.activation(out=TOK[p][:].rearrange("p a t -> p (a t)"), in_=pT[:, 0:384], func=AF.Copy),
                         reads=[Xk], writes=[f"TOK{p}"])
                    yield

                    def amm(e):
                        ins = None
                        for h, bank in ((0, X), (1, Y)):
                            hr = slice(64 * h, 64 * h + 64)
                            e.matmul(bank[:, 0:256], lhsT=BT[p][hr, :], rhs=AR[p][hr, :, :].rearrange("p a t -> p (a t)"), start=True, stop=True)
                            ins = e.matmul(bank[:, 256:512], lhsT=KT[p][hr, :], rhs=AR[p][hr, :, :].rearrange("p a t -> p (a t)"),
                                           start=True, stop=True)
                        return ins

                    P.op("pe", amm, reads=[f"BT{p}", f"KT{p}", f"AR{p}"], writes=[Xk, Yk])
                    yield
                    P.op("dve", lambda e: e.tensor_tensor(out=AM[p][:, 0, :], in0=X[:, 0:512], in1=maskA[:, 0, :], op=ALU.mult),
                         reads=[Xk, "maskA"], writes=[f"AM{p}"])
                    P.op("dve", lambda e: e.tensor_tensor(out=AM[p][:, 1, :], in0=Y[:, 0:512], in1=maskA[:, 1, :], op=ALU.mult),
                         reads=[Yk, "maskA", f"AM{p}"], writes=[f"AM{p}"])
                    yield
                    pTL = Y[:, 0:128].bitcast(BF16)

                    def lmm(e):
                        e.transpose(out=pTL[:, 0:128], in_=AM[p][:, 0, 0:128], identity=ident[:])
                        return e.transpose(out=pTL[:, 128:256], in_=AM[p][:, 1, 0:128], identity=ident[:])

                    P.op("pe", lmm, reads=[f"AM{p}", "ident"], writes=[Yk])
                    for h in range(2):
                        P.op("pool", lambda e, h=h: e.tensor_tensor(out=MT[p][0][:, h, 128:256], in0=AM[p][:, h, 0:128], in1=ident[:], op=ALU.add),
                             reads=[f"AM{p}", "ident", f"MT{p}_0"], writes=[f"MT{p}_0"])
                    yield
                    P.op("dve", lambda e: e.tensor_copy(out=L0[p][:].rearrange("p h c -> p (h c)"), in_=pTL), reads=[Yk], writes=[f"L0{p}"])
                    yield
                    Xm = X[:, 0:512].rearrange("p (h c) -> p h c", h=2)
                    Yl = Y[:, 0:256].rearrange("p (h c) -> p h c", h=2)

                    def r1(e):
                        ins = None
                        for h in range(2):
                            e.matmul(X[:, h * 256:h * 256 + 128], lhsT=L0[p][:, h, :], rhs=AM[p][:, h, 0:128], start=True, stop=True)
                            ins = e.matmul(Y[:, h * 128:(h + 1) * 128], lhsT=AM[p][:, h, 0:128], rhs=L0[p][:, h, :], start=True, stop=True)
                        return ins

                    P.op("pe", r1, reads=[f"L0{p}", f"AM{p}"], writes=[Xk, Yk])
                    yield
                    P.op("act", lambda e: e.activation(out=MT[p][0][:, :, 0:128], in_=Xm[:, :, 0:128], func=AF.Copy),
                         reads=[Xk, f"MT{p}_0"], writes=[f"MT{p}_0"])
                    P.op("dve", lambda e: e.tensor_copy(out=LK[p][0][:], in_=Yl), reads=[Yk], writes=[f"LK{p}_0"])
                    yield
                    for rnd in range(2, 8):
                        src = rnd % 2
                        dst = 1 - src
                        last = (rnd == 7)
                        mts, mtd, lks, lkd = MT[p][src], MT[p][dst], LK[p][src], LK[p][dst]

                        def rk(e, mts=mts, lks=lks, last=last):
                            ins = None
                            for h in range(2):
                                if last:
                                    e.matmul(X[:, h * 256 + 128:h * 256 + 256], lhsT=lks[:, h, :], rhs=mts[:, h, 128:256], start=True, stop=False)
                                    ins = e.matmul(X[:, h * 256 + 128:h * 256 + 256], lhsT=ident[:], rhs=mts[:, h, 128:256], start=False, stop=True)
                                else:
                                    e.matmul(X[:, h * 256:h * 256 + 256], lhsT=lks[:, h, :], rhs=mts[:, h, :], start=True, stop=False)
                                    e.matmul(X[:, h * 256 + 128:h * 256 + 256], lhsT=ident[:], rhs=mts[:, h, 128:256], start=False, stop=True)
                            if not last:
                                for h in range(2):
                                    ins = e.matmul(Y[:, h * 128:(h + 1) * 128], lhsT=mts[:, h, 0:128], rhs=lks[:, h, :], start=True, stop=True)
                            return ins

                        P.op("pe", rk, reads=[f"MT{p}_{src}", f"LK{p}_{src}", "ident"], writes=[Xk] if last else [Xk, Yk])
                        yield
                        if last:
                            P.op("act", lambda e, mtd=mtd: e.activation(out=mtd[:, :, 128:256], in_=Xm[:, :, 128:256], func=AF.Copy),
                                 reads=[Xk, f"MT{p}_{dst}"], writes=[f"MT{p}_{dst}"])
                        else:
                            P.op("act", lambda e, mtd=mtd: e.activation(out=mtd[:], in_=Xm, func=AF.Copy), reads=[Xk], writes=[f"MT{p}_{dst}"])
                            P.op("dve", lambda e, lkd=lkd: e.tensor_copy(out=lkd[:], in_=Yl), reads=[Yk], writes=[f"LK{p}_{dst}"])
                        yield
                    P.op("pool", lambda e: e.tensor_copy(out=DC[:, p:p + 1], in_=Dt[:, p, 127:128]), reads=["Dt", "DC"], writes=["DC"])
                    yield


                def tail():
                    for p in range(4):
                        bank, bk = serial_ps[p], serial_key[p]

                        def xmm(e, p=p, bank=bank):
                            e.matmul(bank[:, 0:64], lhsT=AM[p][:, 0, 256:384], rhs=TOK[p][:, 0, 0:64], start=True, stop=False)
                            e.matmul(bank[:, 64:128], lhsT=AM[p][:, 1, 256:384], rhs=TOK[p][:, 0, 64:128], start=False, stop=False)
                            return e.matmul(bank[:, 0:128], lhsT=AR[p][:, 0, :], rhs=Sb[p][:], start=False, stop=True)

                        P.op("pe", xmm, reads=[f"AM{p}", f"TOK{p}", f"AR{p}", f"Sb{p}"], writes=[bk])
                    for p in range(4):
                        bank, bk = serial_ps[p], serial_key[p]
                        P.op("act", lambda e, p=p, bank=bank: e.activation(out=Xb[p][:], in_=bank[:, 0:128], func=AF.Copy),
                             reads=[bk], writes=[f"Xb{p}"])
                    for p in range(4):
                        bank, bk = serial_ps[p], serial_key[p]

                        def umm(e, p=p, bank=bank):
                            e.matmul(bank[:, 128:192], lhsT=MT[p][0][:, 0, 128:256], rhs=Xb[p][:, 0:64], start=True, stop=True)
                            return e.matmul(bank[:, 192:256], lhsT=MT[p][0][:, 1, 128:256], rhs=Xb[p][:, 64:128], start=True, stop=True)

                        P.op("pe", umm, reads=[f"MT{p}_0", f"Xb{p}"], writes=[bk])
                    for p in range(4):
                        bank, bk = serial_ps[p], serial_key[p]
                        P.op("dve", lambda e, p=p, bank=bank: e.tensor_copy(out=Ub[p][:], in_=bank[:, 128:256]), reads=[bk], writes=[f"Ub{p}"])
                    for p in range(4):
                        bank, bk = serial_ps[p], serial_key[p]

                        def ymm(e, p=p, bank=bank):
                            e.matmul(bank[:, 256:384], lhsT=Sb[p][:], rhs=AR[p][:, 1, :], start=True, stop=False)
                            ins = None
                            for h in range(2):
                                hs = slice(64 * h, 64 * h + 64)
                                e.matmul(bank[hs, 256:384], lhsT=Ub[p][:, hs], rhs=AM[p][:, h, 128:256], start=False, stop=False)
                                ins = e.matmul(bank[hs, 256:384], lhsT=TOK[p][:, 0, hs], rhs=AM[p][:, h, 384:512], start=False, stop=True)
                            return ins

                        P.op("pe", ymm, reads=[f"Sb{p}", f"AR{p}", f"Ub{p}", f"AM{p}", f"TOK{p}"], writes=[bk])

                        def smm(e, p=p, bank=bank):
                            e.matmul(bank[:, 384:512], lhsT=TOK[p][:, 2, :], rhs=TOK[p][:, 0, :], start=True, stop=False)
                            return e.matmul(bank[:, 384:512], lhsT=TOK[p][:, 1, :], rhs=Ub[p][:], start=False, stop=True)

                        P.op("pe", smm, reads=[f"TOK{p}", f"Ub{p}"], writes=[bk])
                    for p in range(4):
                        bank, bk = serial_ps[p], serial_key[p]
                        P.op("act", lambda e, p=p, bank=bank: e.activation(out=yT[:, p, :], in_=bank[:, 256:384], func=AF.Copy),
                             reads=[bk, "yT"], writes=["yT"])
                        P.op("dve", lambda e, p=p, bank=bank: e.tensor_tensor(out=tS[:], in0=bank[:, 384:512], in1=Sw[p][:], op=ALU.add),
                             reads=[bk, f"Sw{p}", "tS"], writes=["tS"])
                        P.op("dve", lambda e, p=p: e.scalar_tensor_tensor(out=Sw[p][:], in0=tS[:], scalar=DC[:, p:p + 1], in1=BDf[:],
                                                                          op0=ALU.mult, op1=ALU.mult),
                             reads=["tS", f"Sw{p}", "DC", "BDf"], writes=[f"Sw{p}"])
                        P.op("act", lambda e, p=p: e.activation(out=Sb[p][:], in_=Sw[p][:], func=AF.Copy), reads=[f"Sw{p}"], writes=[f"Sb{p}"])

                    yTf = yT[:].rearrange("p a t -> p (a t)")
                    P.op("pe", lambda e: e.matmul(psD[:, 0:512], lhsT=BD64[:], rhs=yTf, start=True, stop=True), reads=["BD64", "yT"], writes=["psD0"])
                    yield
                    P.op("dve", lambda e: e.tensor_tensor(out=yc[:], in0=yTf, in1=psD[:, 0:512], op=ALU.subtract), reads=["yT", "psD0"], writes=["yc"])
                    yield
                    P.op("act", lambda e: e.activation(out=ysq[:], in_=yc[:], func=AF.Square), reads=["yc"], writes=["ysq"])
                    yield
                    P.op("pe", lambda e: e.matmul(psD[:, 0:512], lhsT=BD64[:], rhs=ysq[:], start=True, stop=True), reads=["BD64", "ysq"], writes=["psD0"])
                    yield
                    P.op("act", lambda e: e.activation(out=yrs[:], in_=psD[:, 0:512], func=AF.Sqrt, bias=LNX_EPS), reads=["psD0"], writes=["yrs"])
                    yield
                    P.op("dve", lambda e: e.reciprocal(out=yrs[:], in_=yrs[:]), reads=["yrs"], writes=["yrs"])
                    yield
                    P.op("pool", lambda e: e.tensor_tensor(out=yc[:], in0=yc[:], in1=yrs[:], op=ALU.mult), reads=["yc", "yrs"], writes=["yc"])
                    yield
                    for p in range(4):
                        P.op("dve", lambda e, p=p: e.scalar_tensor_tensor(out=y3[:], in0=yc[:, p * 128:(p + 1) * 128], scalar=colap(C_LG + p),
                                                                          in1=bon[p][:], op0=ALU.mult, op1=ALU.add),
                             reads=["yc", "cols", f"bon{p}"], writes=["y3"])
                        P.op("pool", lambda e, p=p: e.tensor_tensor(out=ygT[:, p, :], in0=y3[:], in1=gT[p][:], op=ALU.mult),
                             reads=["y3", f"gT{p}", ygk], writes=[ygk])
                    P.dma("sp", lambda e, tl=tl, ygT=ygT: e.dma_start(out=yg_d[tl], in_=ygT[:].rearrange("p a t -> p (a t)")), reads=[ygk], writes=["yg_d"])
                    yield


                return front, pair_gen, tail

            class KeyProxy:
                PAT = re.compile(r'^(AM|AR|TOK|MT|bon|gT)\d')

                def __init__(self, par):
                    self.par = par

                def km(self, k):
                    if k in ('Dt', 'Dinv', 'Dprev', 'txw', 'sxg', 'DC'):
                        return f'{k}@{self.par}'
                    return k

                def op(self, eng, fn, reads=(), writes=()):
                    return P.op(eng, fn, [self.km(k) for k in reads], [self.km(k) for k in writes])

                def dma(self, q, fn, reads=(), writes=()):
                    return P.dma(q, fn, [self.km(k) for k in reads], [self.km(k) for k in writes])

            def run_rr(gens):
                gens = list(gens)
                while gens:
                    for g in list(gens):
                        try:
                            next(g)
                        except StopIteration:
                            gens.remove(g)

            parts = [tile_parts(tl, KeyProxy(tl % 2)) for tl in range(NT)]
            run_rr([parts[0][0]()])
            for tl in range(NT):
                run_rr([parts[tl][1](p) for p in range(4)])
                gens = [parts[tl][2]()]
                if tl + 1 < NT:
                    gens.append(parts[tl + 1][0]())
                run_rr(gens)

            P.barrier()

        with ExitStack() as st:
            P.scope = "M2"
            w_inB = sbuf(st, "w_inB", [128, 8, 3072], BF16)
            wsTf = sbuf(st, "wsTf", [128, 4, 128], F32)
            wsTb = sbuf(st, "wsTb", [128, 4, 128], BF16)
            bsrow = sbuf(st, "bsrow", [1, 512], BF16)
            lrow = sbuf(st, "lrow", [1, 2, 512], F32)
            lnvg = sbuf(st, "lnvg", [128, 512], F32)
            lnvb = sbuf(st, "lnvb", [128, 512], F32)
            for k in range(8):
                for j in range(3):
                    P.dma("pool", lambda e, k=k, j=j: e.dma_start(out=w_inB[:, k, j * 1024:(j + 1) * 1024],
                                                                  in_=w_in_d[k * 128:(k + 1) * 128, A_COLS + j * 1024:A_COLS + (j + 1) * 1024]),
                          writes=["w_inB"])
            P.dma("sp", lambda e: e.dma_start(out=wsTf[:], in_=wsT_d.rearrange("g s t -> s g t")), writes=["wsTf"])
            P.dma("pool", lambda e: e.dma_start(out=bsrow[:], in_=bs_d), writes=["bsrow"])
            P.dma("sp", lambda e: e.dma_start(out=lrow[:, 0, :], in_=lnvg_d), writes=["lrow"])
            P.dma("sp", lambda e: e.dma_start(out=lrow[:, 1, :], in_=lnvb_d), reads=["lrow"], writes=["lrow"])
            for g in range(4):
                P.op("dve", lambda e, g=g: e.tensor_tensor(out=wsTb[:, g, :], in0=wsTf[:, g, :], in1=mIU[:], op=ALU.mult),
                     reads=["wsTf", "mIU", "wsTb"], writes=["wsTb"])
            for i, (dst, dk) in enumerate([(lnvg, "lnvg"), (lnvb, "lnvb")]):
                P.op("pe", lambda e, i=i: e.matmul(psM[0][:, 0:512], lhsT=onesf[0:1, :], rhs=lrow[0:1, i, :], start=True, stop=True),
                     reads=["onesf", "lrow"], writes=["psM0"])
                P.op("act", lambda e, dst=dst: e.activation(out=dst[:], in_=psM[0][:, 0:512], func=AF.Copy), reads=["psM0"], writes=[dk])

            TB = min(4, NT)
            BN = TB * 128
            NBLK = NT // TB
            hTB = [sbuf(st, f"hTB{i}", [128, 8, BN], BF16) for i in range(2)]
            ygB = [sbuf(st, f"ygB{i}", [128, 4, BN], BF16) for i in range(2)]
            uT = sbuf(st, "uTB", [128, 4, BN], BF16)
            mixT = sbuf(st, "mixTB", [128, 4, BN], BF16)
            t1 = sbuf(st, "t1B", [128, 8, BN], BF16)
            zT = sbuf(st, "zTB", [128, 8, BN], BF16)
            gsT = [sbuf(st, f"gsT{i}", [128, BN], BF16) for i in range(2)]
            t2T = [sbuf(st, f"t2T{i}", [128, BN], BF16) for i in range(2)]
            xinL = [sbuf(st, f"xinb{i}", [128, D], F32) for i in range(2)]
            vgL = [sbuf(st, f"vg{i}", [128, 512], F32) for i in range(2)]
            vlnL = [sbuf(st, f"vln{i}", [128, 512], BF16) for i in range(2)]
            xsbL = [sbuf(st, f"xsb2{i}", [128, D], BF16) for i in range(2)]
            h2tL = [sbuf(st, f"h2t{i}", [128, 8, 128], BF16) for i in range(4)]
            sm = [{n: sbuf(st, f"{n}{i}", [128, w], F32) for n, w in (("bst", 6), ("mv", 2), ("lsd", 1), ("lrs", 1), ("lnm", 1), ("ss2", 1), ("sd2", 1), ("rs2", 1))}
                  for i in range(2)]
            RING = [(psM[0], "psM0"), (psM[1], "psM1"), (psS, "psS"), (psD[:, 0:512], "psD0"), (psD[:, 512:1024], "psD1"), (psD[:, 1024:1536], "psD2"),
                    (psA[:, 0:512], "psA"), (psA[:, 512:1024], "psA2")]
            ring_i = [0]

            def ring():
                r = RING[ring_i[0] % len(RING)]
                ring_i[0] += 1
                return r

            wrb = sbuf(st, "wrb", [128, 8, 36], BF16)
            brrow = sbuf(st, "brrow", [1, 36], BF16)
            P.dma("pool", lambda e: e.dma_start(out=wrb[:], in_=wr_d.rearrange("(k p) n -> p k n", p=128)), writes=["wrb"])
            P.dma("pool", lambda e: e.dma_start(out=brrow[:], in_=br_d), writes=["brrow"])
            NRS = 4
            rsc = []
            for i in range(NRS):
                d = {"Lg": sbuf(st, f"Lg{i}", [128, 36], F32), "c": sbuf(st, f"rc{i}", [128, 10], F32), "ohg": sbuf(st, f"ohg{i}", [128, 4], F32),
                     "ex4": sbuf(st, f"ex4{i}", [128, 4], F32), "esel": sbuf(st, f"esel{i}", [128, 8], F32), "e2": sbuf(st, f"e2{i}", [128, 8], F32),
                     "mk1": sbuf(st, f"mk1{i}", [128, 8], F32), "mk2": sbuf(st, f"mk2{i}", [128, 8], F32), "wg8": sbuf(st, f"wg8{i}", [128, 8], F32)}
                rsc.append(d)

            def router_rest(tl):
                sl = tl % NRS
                d = rsc[sl]
                Lg, ohg, ex4, esel, e2, mk1, mk2, wg8 = d["Lg"], d["ohg"], d["ex4"], d["esel"], d["e2"], d["mk1"], d["mk2"], d["wg8"]
                cc = d["c"]
                gmax, ngmax, se, gp, m1, m2, dd, w1, w2 = [cc[:, i:i + 1] for i in range(9)]
                rk = f"rt{sl}"
                steps = [
                    ("dve", lambda e: e.tensor_reduce(out=gmax, in_=Lg[:, 0:4], axis=AX.X, op=ALU.max)),
                    ("dve", lambda e: e.tensor_scalar(out=ohg[:], in0=Lg[:, 0:4], scalar1=gmax, scalar2=None, op0=ALU.is_ge)),
                    ("dve", lambda e: e.tensor_scalar(out=ngmax, in0=gmax, scalar1=-1.0, scalar2=None, op0=ALU.mult)),
                    ("act", lambda e: e.activation(out=ex4[:], in_=Lg[:, 0:4], func=AF.Exp, bias=ngmax)),
                    ("dve", lambda e: e.tensor_reduce(out=se, in_=ex4[:], axis=AX.X, op=ALU.add)),
                    ("dve", lambda e: e.reciprocal(out=gp, in_=se)),
                    ("dve", lambda e: e.tensor_scalar(out=esel[:], in0=Lg[:, 4:12], scalar1=ohg[:, 0:1], scalar2=None, op0=ALU.mult)),
                ]
                for g in range(1, 4):
                    steps.append(("dve", lambda e, g=g: e.scalar_tensor_tensor(out=esel[:], in0=Lg[:, 4 + 8 * g:12 + 8 * g], scalar=ohg[:, g:g + 1],
                                                                               in1=esel[:], op0=ALU.mult, op1=ALU.add)))
                steps += [
                    ("dve", lambda e: e.tensor_reduce(out=m1, in_=esel[:], axis=AX.X, op=ALU.max)),
                    ("dve", lambda e: e.tensor_scalar(out=mk1[:], in0=esel[:], scalar1=m1, scalar2=None, op0=ALU.is_ge)),
                    ("dve", lambda e: e.scalar_tensor_tensor(out=e2[:], in0=mk1[:], scalar=-1e30, in1=esel[:], op0=ALU.mult, op1=ALU.add)),
                    ("dve", lambda e: e.tensor_reduce(out=m2, in_=e2[:], axis=AX.X, op=ALU.max)),
                    ("dve", lambda e: e.tensor_scalar(out=mk2[:], in0=e2[:], scalar1=m2, scalar2=None, op0=ALU.is_ge)),
                    ("dve", lambda e: e.tensor_tensor(out=dd, in0=m2, in1=m1, op=ALU.subtract)),
                    ("act", lambda e: e.activation(out=w2, in_=dd, func=AF.Sigmoid)),
                    ("act", lambda e: e.activation(out=w1, in_=dd, func=AF.Sigmoid, scale=-1.0)),
                    ("dve", lambda e: e.tensor_tensor(out=w1, in0=w1, in1=gp, op=ALU.mult)),
                    ("dve", lambda e: e.tensor_tensor(out=w2, in0=w2, in1=gp, op=ALU.mult)),
                    ("dve", lambda e: e.tensor_scalar(out=wg8[:], in0=mk1[:], scalar1=w1, scalar2=None, op0=ALU.mult)),
                    ("dve", lambda e: e.scalar_tensor_tensor(out=wg8[:], in0=mk2[:], scalar=w2, in1=wg8[:], op0=ALU.mult, op1=ALU.add)),
                ]
                for eng, fn in steps:
                    P.op(eng, fn, reads=[rk], writes=[rk])
                    yield
                for g in range(4):
                    P.op("dve", lambda e, g=g: e.tensor_scalar(out=comb[:, tl, g * 8:(g + 1) * 8], in0=wg8[:], scalar1=ohg[:, g:g + 1],
                                                               scalar2=None, op0=ALU.mult), reads=[rk, f"comb{tl}"], writes=[f"comb{tl}"])
                yield


            def run_rr2(gens):
                gens = list(gens)
                while gens:
                    for g in list(gens):
                        try:
                            next(g)
                        except StopIteration:
                            gens.remove(g)

            def load_block(b):
                hB, yB = hTB[b % 2], ygB[b % 2]
                for i in range(TB):
                    tl = b * TB + i
                    P.dma("sp", lambda e, tl=tl, i=i: e.dma_start(out=hB[:, :, i * 128:(i + 1) * 128], in_=hT_d[tl].rearrange("p (k t) -> p k t", k=8)),
                          reads=["hT_d"], writes=[f"hTB{b % 2}"])
                    P.dma("sp", lambda e, tl=tl, i=i: e.dma_start(out=yB[:, :, i * 128:(i + 1) * 128], in_=yg_d[tl].rearrange("p (a t) -> p a t", a=4)),
                          reads=["yg_d"], writes=[f"ygB{b % 2}"])

            load_block(0)
            pending = []
            for b in range(NBLK):
                hB, yB, hk, yk = hTB[b % 2], ygB[b % 2], f"hTB{b % 2}", f"ygB{b % 2}"
                if b + 1 < NBLK:
                    load_block(b + 1)

                def U(c):
                    bank, bk = ring()

                    def umm2(e, c=c, bank=bank):
                        ins = None
                        for k in range(8):
                            ins = e.matmul(bank[:, 0:BN], lhsT=w_inB[:, k, c * 128:(c + 1) * 128], rhs=hB[:, k, :], start=(k == 0), stop=(k == 7))
                        return ins

                    P.op("pe", umm2, reads=["w_inB", hk], writes=[bk])
                    P.op("act", lambda e, c=c, bank=bank: e.activation(out=uT[:, c, :], in_=bank[:, 0:BN], func=AF.Gelu), reads=[bk, "uTB"], writes=["uTB"])

                def g1(m):
                    bank, bk = ring()
                    c0 = 1024 + m * 128

                    def gm(e):
                        ins = None
                        for k in range(8):
                            ins = e.matmul(bank[:, 0:BN], lhsT=w_inB[:, k, c0:c0 + 128], rhs=hB[:, k, :], start=(k == 0), stop=(k == 7))
                        return ins

                    gs = gsT[m % 2]
                    P.op("pe", gm, reads=["w_inB", hk], writes=[bk])
                    P.op("act", lambda e: e.activation(out=gs[:], in_=bank[:, 0:BN], func=AF.Sigmoid, bias=colap(C_BG + m)), reads=[bk, "cols"], writes=[f"gsT{m % 2}"])
                    bank2, bk2 = ring()

                    def ym(e):
                        ins = None
                        for c in range(4):
                            ins = e.matmul(bank2[:, 0:BN], lhsT=woA[:, c, m * 128:(m + 1) * 128], rhs=yB[:, c, :], start=(c == 0), stop=(c == 3))
                        return ins

                    P.op("pe", ym, reads=["woA", yk], writes=[bk2])
                    P.op("dve", lambda e: e.tensor_tensor(out=t1[:, m, :], in0=bank2[:, 0:BN], in1=gs[:], op=ALU.mult), reads=[bk2, f"gsT{m % 2}", "t1B"], writes=["t1B"])

                def V(i):
                    tl = b * TB + i
                    ts_ = slice(i * 128, (i + 1) * 128)
                    vg, vln, d_ = vgL[i % 2], vlnL[i % 2], sm[i % 2]
                    q = i % 2
                    bank, bk = ring()

                    def vmm(e, bank=bank, ts_=ts_):
                        ins = None
                        for k in range(8):
                            ins = e.matmul(bank[:, 0:512], lhsT=hB[:, k, ts_], rhs=w_inB[:, k, 512:1024], start=(k == 0), stop=(k == 7))
                        return ins

                    P.op("pe", vmm, reads=["w_inB", hk], writes=[bk])
                    P.op("act", lambda e, bank=bank, vg=vg: e.activation(out=vg[:], in_=bank[:, 0:512], func=AF.Gelu), reads=[bk], writes=[f"vg{q}"])
                    P.op("dve", lambda e, vg=vg, d_=d_: e.bn_stats(out=d_["bst"][:], in_=vg[:]), reads=[f"vg{q}"], writes=[f"sm{q}"])
                    P.op("dve", lambda e, d_=d_: e.bn_aggr(out=d_["mv"][:], in_=d_["bst"][:]), reads=[f"sm{q}"], writes=[f"sm{q}"])
                    P.op("act", lambda e, d_=d_: e.activation(out=d_["lsd"][:], in_=d_["mv"][:, 1:2], func=AF.Sqrt, bias=LN_EPS), reads=[f"sm{q}"], writes=[f"sm{q}"])
                    P.op("dve", lambda e, d_=d_: e.reciprocal(out=d_["lrs"][:], in_=d_["lsd"][:]), reads=[f"sm{q}"], writes=[f"sm{q}"])
                    P.op("dve", lambda e, d_=d_: e.tensor_scalar(out=d_["lnm"][:], in0=d_["mv"][:, 0:1], scalar1=-1.0, scalar2=d_["lrs"][:], op0=ALU.mult, op1=ALU.mult),
                         reads=[f"sm{q}"], writes=[f"sm{q}"])
                    P.op("dve", lambda e, vg=vg, d_=d_: e.tensor_scalar(out=vg[:], in0=vg[:], scalar1=d_["lrs"][:], scalar2=d_["lnm"][:], op0=ALU.mult, op1=ALU.add),
                         reads=[f"vg{q}", f"sm{q}"], writes=[f"vg{q}"])
                    P.op("pool", lambda e, vg=vg: e.tensor_tensor(out=vg[:], in0=vg[:], in1=lnvg[:], op=ALU.mult), reads=[f"vg{q}", "lnvg"], writes=[f"vg{q}"])
                    P.op("pool", lambda e, vg=vg, vln=vln: e.tensor_tensor(out=vln[:], in0=vg[:], in1=lnvb[:], op=ALU.add), reads=[f"vg{q}", "lnvb"], writes=[f"vln{q}"])

                def SV(i):
                    ts_ = slice(i * 128, (i + 1) * 128)
                    vln = vlnL[i % 2]
                    q = i % 2
                    bank, bk = ring()

                    def svmm(e, bank=bank, vln=vln):
                        ins = None
                        for g in range(4):
                            e.matmul(bank[:, g * 128:(g + 1) * 128], lhsT=vln[:, g * 128:(g + 1) * 128], rhs=wsTb[:, g, :], start=True, stop=False)
                            ins = e.matmul(bank[:, g * 128:(g + 1) * 128], lhsT=onesb[0:1, :], rhs=bsrow[0:1, g * 128:(g + 1) * 128], start=False, stop=True)
                        return ins

                    P.op("pe", svmm, reads=[f"vln{q}", "wsTb", "onesb", "bsrow"], writes=[bk])
                    P.op("dve", lambda e, bank=bank, ts_=ts_: e.tensor_tensor(out=mixT[:, :, ts_], in0=bank[:, 0:512].rearrange("p (g t) -> p g t", g=4),
                                                                           in1=uT[:, :, ts_], op=ALU.mult), reads=[bk, "uTB", "mixTB"], writes=["mixTB"])
                def G2(m):
                    bank, bk = ring()
                    c0 = 2048 + m * 128

                    def gm(e, bank=bank, c0=c0):
                        ins = None
                        for k in range(8):
                            ins = e.matmul(bank[:, 0:BN], lhsT=w_inB[:, k, c0:c0 + 128], rhs=hB[:, k, :], start=(k == 0), stop=(k == 7))
                        return ins

                    gs, t2 = gsT[m % 2], t2T[m % 2]
                    P.op("pe", gm, reads=["w_inB", hk], writes=[bk])
                    P.op("act", lambda e, bank=bank, gs=gs, m=m: e.activation(out=gs[:], in_=bank[:, 0:BN], func=AF.Sigmoid, bias=colap(C_BG + 8 + m)),
                         reads=[bk, "cols"], writes=[f"gsT{m % 2}"])
                    bank2, bk2 = ring()

                    def ym(e, bank2=bank2, m=m):
                        ins = None
                        for c in range(4):
                            ins = e.matmul(bank2[:, 0:BN], lhsT=woB[:, c, m * 128:(m + 1) * 128], rhs=mixT[:, c, :], start=(c == 0), stop=(c == 3))
                        return ins

                    P.op("pe", ym, reads=["woB", "mixTB"], writes=[bk2])
                    P.op("dve", lambda e, bank2=bank2, gs=gs, t2=t2: e.tensor_tensor(out=t2[:], in0=bank2[:, 0:BN], in1=gs[:], op=ALU.mult),
                         reads=[bk2, f"gsT{m % 2}"], writes=[f"t2T{m % 2}"])
                    P.op("pool", lambda e, m=m, t2=t2: e.tensor_tensor(out=zT[:, m, :], in0=t1[:, m, :], in1=t2[:], op=ALU.add),
                         reads=["t1B", f"t2T{m % 2}", "zTB"], writes=["zTB"])

                def O(i):
                    tl = b * TB + i
                    ts_ = slice(i * 128, (i + 1) * 128)
                    q = i % 2
                    xb_, xsb, d_ = xinL[q], xsbL[q], sm[q]
                    P.dma("sp", lambda e, xb_=xb_, tl=tl: e.dma_start(out=xb_[:], in_=x_d[tl * 128:(tl + 1) * 128, :]), writes=[f"xinb{q}"])
                    for hf in range(2):
                        bank, bk = ring()

                        def omm(e, bank=bank, hf=hf, ts_=ts_):
                            ins = None
                            for m in range(8):
                                ins = e.matmul(bank[:, 0:512], lhsT=zT[:, m, ts_], rhs=wout[:, m, hf * 512:(hf + 1) * 512], start=(m == 0), stop=(m == 7))
                            return ins

                        P.op("pe", omm, reads=["zTB", "wout"], writes=[bk])
                        P.op("dve", lambda e, bank=bank, hf=hf, xb_=xb_: e.tensor_tensor(out=xb_[:, hf * 512:(hf + 1) * 512], in0=bank[:, 0:512],
                                                                                     in1=xb_[:, hf * 512:(hf + 1) * 512], op=ALU.add),
                             reads=[bk, f"xinb{q}"], writes=[f"xinb{q}"])
                    P.dma("sp", lambda e, xb_=xb_, tl=tl: e.dma_start(out=x1_d[tl * 128:(tl + 1) * 128, :], in_=xb_[:]), reads=[f"xinb{q}"], writes=["x1_d"])
                    P.op("act", lambda e, xb_=xb_, xsb=xsb, d_=d_: e.activation(out=xsb[:], in_=xb_[:], func=AF.Square, accum_out=d_["ss2"][:]),
                         reads=[f"xinb{q}"], writes=[f"xsb2{q}", f"sm{q}"])
                    P.op("act", lambda e, d_=d_: e.activation(out=d_["sd2"][:], in_=d_["ss2"][:], func=AF.Sqrt, bias=NORM_EPS, scale=1.0 / D), reads=[f"sm{q}"], writes=[f"sm{q}"])
                    P.op("dve", lambda e, d_=d_: e.reciprocal(out=d_["rs2"][:], in_=d_["sd2"][:]), reads=[f"sm{q}"], writes=[f"sm{q}"])
                    P.op("dve", lambda e, xb_=xb_, xsb=xsb, d_=d_: e.tensor_scalar(out=xsb[:], in0=xb_[:], scalar1=d_["rs2"][:], scalar2=None, op0=ALU.mult),
                         reads=[f"xinb{q}", f"sm{q}", f"xsb2{q}"], writes=[f"xsb2{q}"])

                def TR(i):
                    tl = b * TB + i
                    tc = slice(tl * 128, (tl + 1) * 128)
                    q = i % 2
                    xsb, h2t = xsbL[q], h2tL[i]
                    bank, bk = ring()
                    pT = bank[:].bitcast(BF16)

                    def tr(e, pT=pT, xsb=xsb):
                        ins = None
                        for k in range(8):
                            ins = e.transpose(out=pT[:, k * 128:(k + 1) * 128], in_=xsb[:, k * 128:(k + 1) * 128], identity=ident[:])
                        return ins

                    P.op("pe", tr, reads=[f"xsb2{q}", "ident"], writes=[bk])
                    P.op("dve", lambda e, pT=pT, h2t=h2t: e.tensor_tensor(out=h2t[:], in0=pT.rearrange("p (k t) -> p k t", k=8), in1=g2bc[:], op=ALU.mult),
                         reads=[bk, "g2bc"], writes=[f"h2t{i}"])
                    P.dma("sp", lambda e, h2t=h2t, tc=tc: e.dma_start(out=h2_d[:, :, tc], in_=h2t[:]), reads=[f"h2t{i}"], writes=["h2_d"])

                def RM(i):
                    h2t = h2tL[i]
                    bank, bk = ring()
                    Lg = rsc[i % NRS]["Lg"]

                    def rmm(e, bank=bank, h2t=h2t):
                        for k in range(8):
                            e.matmul(bank[:, 0:36], lhsT=h2t[:, k, :], rhs=wrb[:, k, :], start=(k == 0), stop=False)
                        return e.matmul(bank[:, 0:36], lhsT=onesb[0:1, :], rhs=brrow[0:1, :], start=False, stop=True)

                    P.op("pe", rmm, reads=[f"h2t{i}", "wrb", "onesb", "brrow"], writes=[bk])
                    P.op("dve", lambda e, bank=bank, Lg=Lg: e.tensor_copy(out=Lg[:], in_=bank[:, 0:36]), reads=[bk, f"rt{i % NRS}"], writes=[f"rt{i % NRS}"])
                def drain(nsteps):
                    for _ in range(nsteps):
                        for g in list(pending):
                            try:
                                next(g)
                            except StopIteration:
                                pending.remove(g)

                for c in range(4):
                    U(c)
                for i in range(TB):
                    V(i)
                    for m in (2 * i, 2 * i + 1):
                        if m < 8:
                            g1(m)
                    if i >= 1:
                        SV(i - 1)
                for m in range(2 * TB, 8):
                    g1(m)
                SV(TB - 1)
                for m in range(8):
                    G2(m)
                    drain(4)
                for i in range(TB):
                    O(i)
                    if i >= 1:
                        TR(i - 1)
                    if i >= 2:
                        RM(i - 2)
                TR(TB - 1)
                if TB >= 2:
                    RM(TB - 2)
                RM(TB - 1)
                drain(1000)
                pending.extend(router_rest(b * TB + i) for i in range(TB))
            drain(1000)

            P.barrier()
        stP.close()

        with ExitStack() as st:
            P.scope = "router"
            h2 = sbuf(st, "h2", [128, 8, T], BF16)
            yacc = sbuf(st, "yacc", [128, NT, D], F32)
            fgrow = sbuf(st, "fgrow", [1, D], F32)
            fgbc = sbuf(st, "fgbc", [128, D], F32)
            P.dma("sp", lambda e: e.dma_start(out=h2[:], in_=h2_d), reads=["h2_d"], writes=["h2"])
            for tl in range(NT):
                P.dma("sp", lambda e, tl=tl: e.dma_start(out=yacc[:, tl, :], in_=x1_d[tl * 128:(tl + 1) * 128, :]), reads=["x1_d"], writes=[f"yacc{tl}"])
            P.dma("sp", lambda e: e.dma_start(out=fgrow[:], in_=fg_d), writes=["fgrow"])
            for hf in range(2):
                P.op("pe", lambda e, hf=hf: e.matmul(psM[0][:, 0:512], lhsT=onesf[0:1, :], rhs=fgrow[0:1, hf * 512:(hf + 1) * 512], start=True, stop=True),
                     reads=["onesf", "fgrow"], writes=["psM0"])
                P.op("act", lambda e, hf=hf: e.activation(out=fgbc[:, hf * 512:(hf + 1) * 512], in_=psM[0][:, 0:512], func=AF.Copy),
                     reads=["psM0", "fgbc"], writes=["fgbc"])

            P.scope = "experts"
            Wg = [sbuf(st, f"Wg{i}", [128, 8, 256], BF16) for i in range(2)]
            Wu = [sbuf(st, f"Wu{i}", [128, 8, 256], BF16) for i in range(2)]
            Wd = [sbuf(st, f"Wd{i}", [128, 2, D], BF16) for i in range(2)]
            GN_ = min(512, T)
            NG = T // GN_
            sg = [sbuf(st, f"sg{i}", [128, GN_], F32) for i in range(2)]
            actT = sbuf(st, "actT", [128, 2, GN_], BF16)
            gu_ps = [psM[0], psM[1], psS, psD[:, 0:512]]
            gu_k = ["psM0", "psM1", "psS", "psD0"]
            d_ps = [psA, psD[:, 512:1536]]
            d_k = [["psA", "psA2"], ["psD1", "psD2"]]
            actT2 = [actT, sbuf(st, "actTb", [128, 2, GN_], BF16)]
            TPG = GN_ // 128

            def load_expert(ex):
                b = ex % 2
                P.dma("pool", lambda e: e.dma_start(out=Wg[b][:], in_=weg_d[ex].rearrange("(k p) n -> p k n", p=128)), writes=[f"Wg{b}"])
                P.dma("pool", lambda e: e.dma_start(out=Wu[b][:], in_=weu_d[ex].rearrange("(k p) n -> p k n", p=128)), writes=[f"Wu{b}"])
                P.dma("pool", lambda e: e.dma_start(out=Wd[b][:], in_=wed_d[ex].rearrange("(k p) n -> p k n", p=128)), writes=[f"Wd{b}"])

            def G(u, f):
                ex, gi = divmod(u, NG)
                b = ex % 2
                gc = slice(gi * GN_, (gi + 1) * GN_)
                aT = actT2[u % 2]
                ak = f"actT{u % 2}_{f}"

                def gumm(e):
                    ins = None
                    for k in range(8):
                        e.matmul(gu_ps[2 * f][:, 0:GN_], lhsT=Wg[b][:, k, f * 128:(f + 1) * 128], rhs=h2[:, k, gc], start=(k == 0), stop=(k == 7))
                    for k in range(8):
                        ins = e.matmul(gu_ps[2 * f + 1][:, 0:GN_], lhsT=Wu[b][:, k, f * 128:(f + 1) * 128], rhs=h2[:, k, gc], start=(k == 0), stop=(k == 7))
                    return ins

                P.op("pe", gumm, reads=[f"Wg{b}", f"Wu{b}", "h2"], writes=[gu_k[2 * f], gu_k[2 * f + 1]])
                P.op("act", lambda e: e.activation(out=sg[f][:], in_=gu_ps[2 * f][:, 0:GN_], func=AF.Silu), reads=[gu_k[2 * f]], writes=[f"sg{f}"])
                P.op("dve", lambda e: e.tensor_tensor(out=aT[:, f, :], in0=gu_ps[2 * f + 1][:, 0:GN_], in1=sg[f][:], op=ALU.mult),
                     reads=[gu_k[2 * f + 1], f"sg{f}"], writes=[ak])

            dstate = [0]

            def Dn(u, ti):
                ex, gi = divmod(u, NG)
                b = ex % 2
                tl = gi * TPG + ti
                aT = actT2[u % 2]
                dps = d_ps[dstate[0] % 2]
                dk = d_k[dstate[0] % 2]
                dstate[0] += 1

                def dmm(e):
                    ins = None
                    for hf in range(2):
                        for f in range(2):
                            ins = e.matmul(dps[:, hf * 512:(hf + 1) * 512], lhsT=aT[:, f, ti * 128:(ti + 1) * 128],
                                           rhs=Wd[b][:, f, hf * 512:(hf + 1) * 512], start=(f == 0), stop=(f == 1))
                    return ins

                P.op("pe", dmm, reads=[f"actT{u % 2}_0", f"actT{u % 2}_1", f"Wd{b}"], writes=dk)
                P.op("dve", lambda e: e.scalar_tensor_tensor(out=yacc[:, tl, :], in0=dps[:, 0:1024], scalar=comb[:, tl, ex:ex + 1],
                                                             in1=yacc[:, tl, :], op0=ALU.mult, op1=ALU.add),
                     reads=dk + [f"comb{tl}", f"yacc{tl}"], writes=[f"yacc{tl}"])

            junk = sbuf(st, "junk3", [128, D], BF16)
            ss = sbuf(st, "ss3", [128, 1], F32)
            sd = sbuf(st, "sd3", [128, 1], F32)
            rs = sbuf(st, "rs3", [128, 1], F32)
            ob = [sbuf(st, f"ob{i}", [128, D], F32) for i in range(2)]

            def final_tile(tl):
                o = ob[tl % 2]
                ok = f"ob{tl % 2}"
                P.op("act", lambda e: e.activation(out=junk[:], in_=yacc[:, tl, :], func=AF.Square, accum_out=ss[:]),
                     reads=[f"yacc{tl}"], writes=["junk3", "ss3"])
                P.op("act", lambda e: e.activation(out=sd[:], in_=ss[:], func=AF.Sqrt, bias=NORM_EPS, scale=1.0 / D), reads=["ss3"], writes=["sd3"])
                P.op("dve", lambda e: e.reciprocal(out=rs[:], in_=sd[:]), reads=["sd3"], writes=["rs3"])
                P.op("dve", lambda e: e.scalar_tensor_tensor(out=o[:], in0=yacc[:, tl, :], scalar=rs[:], in1=fgbc[:], op0=ALU.mult, op1=ALU.mult),
                     reads=[f"yacc{tl}", "rs3", "fgbc"], writes=[ok])
                P.dma("sp", lambda e: e.dma_start(out=out_d[tl * 128:(tl + 1) * 128, :], in_=o[:]), reads=[ok], writes=["out"])

            NU = NE * NG
            load_expert(0)
            if NE > 1:
                load_expert(1)
            G(0, 0)
            G(0, 1)
            for u in range(NU):
                ex, gi = divmod(u, NG)
                P.scope = "experts" if ex != 5 else "ex5"
                half = (TPG + 1) // 2
                if u + 1 < NU:
                    G(u + 1, 0)
                for ti in range(0, half):
                    Dn(u, ti)
                    if ex == NE - 1:
                        final_tile(gi * TPG + ti)
                if u + 1 < NU:
                    G(u + 1, 1)
                for ti in range(half, TPG):
                    Dn(u, ti)
                    if ex == NE - 1:
                        final_tile(gi * TPG + ti)
                if gi == NG - 1 and ex + 2 < NE:
                    load_expert(ex + 2)

            P.final_wait("sp")
            P.emit()
    return nc


def host_layout(inp, b, T):
    f = lambda a: np.ascontiguousarray(np.asarray(a, dtype=np.float32))

    def colpack(v, n):
        v = np.asarray(v, np.float32).reshape(-1)
        pad = np.zeros(n * 128, np.float32)
        pad[:v.size] = v
        return pad.reshape(n, 128).T

    cols = np.concatenate([
        colpack(inp["tmix_mu"][0], 15), colpack(inp["k_k"][0], 4), colpack(inp["k_a"][0], 4), colpack(inp["r_k"][0], 4),
        colpack(inp["a0"][0], 4), colpack(inp["lnx_g"][0], 4), colpack(inp["lnx_b"][0], 4), colpack(inp["b_gate"][0], 16),
        colpack(inp["norm1_g"][0], 8), colpack(inp["norm2_g"][0], 8)], axis=1)
    w_re = np.asarray(inp["w_re"][0], np.float32)
    w_r = np.concatenate([np.asarray(inp["w_rg"][0], np.float32), w_re.transpose(1, 0, 2).reshape(D, 32)], axis=1)
    b_r = np.concatenate([np.asarray(inp["b_rg"][0], np.float32).reshape(-1), np.asarray(inp["b_re"][0], np.float32).reshape(-1)])
    return {
        "x": f(inp["x"][b, :T]), "w_in": f(inp["w_in"][0]), "cols": f(cols), "w0": f(inp["w0"][0]).reshape(1, 512),
        "w2": f(inp["w2"][0]), "a2": f(inp["a2"][0]), "g2": f(inp["g2"][0]), "w_oA": f(inp["w_oA"][0]), "w_oB": f(inp["w_oB"][0]),
        "w_out": f(inp["w_out"][0]), "lnv_g": f(inp["lnv_g"][0]).reshape(1, 512), "lnv_b": f(inp["lnv_b"][0]).reshape(1, 512),
        "wsT": f(np.asarray(inp["w_s"][0], np.float32).transpose(0, 2, 1)), "b_s": f(inp["b_s"][0]).reshape(1, 512),
        "w_r": f(w_r), "b_r": f(b_r).reshape(1, 36), "w_e_gate": f(inp["w_e_gate"][0]), "w_e_up": f(inp["w_e_up"][0]),
        "w_e_down": f(inp["w_e_down"][0]), "final_g": f(inp["final_g"]).reshape(1, D),
    }


def kernel(**inputs):
    T = 2048
    nc = build(T)
    in_maps = [host_layout(inputs, b, T) for b in range(8)]
    res = run_bass_kernel_spmd(nc, in_maps, core_ids=list(range(8)))
    return np.stack([np.asarray(r["out"], dtype=np.float32) for r in res.results], axis=0)
```

```python
import re
import numpy as np
from contextlib import ExitStack
import concourse.bass as bass
import concourse.mybir as mybir
from concourse.bass_utils import run_bass_kernel_spmd

F32 = mybir.dt.float32
BF16 = mybir.dt.bfloat16
AF = mybir.ActivationFunctionType
ALU = mybir.AluOpType
AX = mybir.AxisListType
ENGS = ["pe", "dve", "act", "pool", "sp"]
DMA_RING = {"sp": 6, "pool": 4}

D = 1024
IN_COLS = 4896
A_COLS = 1824
NCHA = 15
LNX_EPS = 64e-5
LN_EPS = 1e-5
NORM_EPS = 1e-6
EXPM05 = float(np.exp(-0.5))
PROFILE_SCOPES = False

C_MU, C_KK, C_KA, C_RK, C_A0, C_LG, C_LB, C_BG, C_G1, C_G2 = 0, 15, 19, 23, 27, 31, 35, 39, 55, 63
NCOL = 71


class Rec:
    def __init__(self):
        self.calls = []

    def __getattr__(self, name):
        def f(*a, **k):
            self.calls.append((name, a, k))
            return self
        return f


def _record(fn):
    r = Rec()
    fn(r)
    assert r.calls
    return r.calls


class Prog:
    def __init__(self, nc, stack):
        self.nc = nc
        self.lists = {e: [] for e in ENGS}
        self.count = {e: 0 for e in ENGS}
        self.sems = {}
        for e in ENGS:
            self.sems[("e", e)] = stack.enter_context(nc.semaphore(f"s_{e}"))
        for q, r in DMA_RING.items():
            for i in range(r):
                self.sems[("d", q, i)] = stack.enter_context(nc.semaphore(f"d_{q}{i}"))
        self.dma_n = {q: 0 for q in DMA_RING}
        self.waited = {e: {} for e in ENGS}
        self.last_w = {}
        self.readers = {}
        self.scope = None

    def _waits(self, eng, reads, writes, extra=()):
        need = {}

        def add(s, v):
            if need.get(s, 0) < v:
                need[s] = v

        for k in reads:
            if k in self.last_w:
                add(*self.last_w[k])
        for k in writes:
            if k in self.last_w:
                add(*self.last_w[k])
            for s, v in self.readers.get(k, {}).items():
                add(s, v)
        for s, v in extra:
            add(s, v)
        out = []
        wd = self.waited[eng]
        for s, v in need.items():
            if wd.get(s, 0) < v:
                wd[s] = v
                out.append((s, v))
        return out

    def _commit(self, tok, reads, writes):
        for k in writes:
            self.last_w[k] = tok
            self.readers[k] = {}
        for k in reads:
            d = self.readers.setdefault(k, {})
            if d.get(tok[0], 0) < tok[1]:
                d[tok[0]] = tok[1]

    LIMIT = None
    NOPS = 0

    def op(self, eng, fn, reads=(), writes=()):
        Prog.NOPS += 1
        if Prog.LIMIT is not None and Prog.NOPS > Prog.LIMIT:
            return None
        writes = list(writes) + [k for k in reads if k.startswith("ps")]
        waits = self._waits(eng, reads, writes)
        self.count[eng] += 1
        tok = (("e", eng), self.count[eng])
        self.lists[eng].append((waits, _record(fn), (("e", eng), 1), self.scope))
        self._commit(tok, reads, writes)
        return tok

    def dma(self, q, fn, reads=(), writes=()):
        Prog.NOPS += 1
        if Prog.LIMIT is not None and Prog.NOPS > Prog.LIMIT:
            return None
        r = DMA_RING[q]
        i = self.dma_n[q]
        self.dma_n[q] += 1
        slot = i % r
        skey = ("d", q, slot)
        extra = [(skey, 16 * (i // r))] if i >= r else []
        waits = self._waits(q, reads, writes, extra)
        tok = (skey, 16 * (i // r + 1))
        self.lists[q].append((waits, _record(fn), (skey, 16), self.scope))
        self._commit(tok, reads, writes)
        return tok

    def _all_tokens(self):
        toks = []
        for q, r in DMA_RING.items():
            n = self.dma_n[q]
            for slot in range(min(r, n)):
                toks.append((("d", q, slot), 16 * ((n - 1 - slot) // r + 1)))
        for e in ENGS:
            if self.count[e]:
                toks.append((("e", e), self.count[e]))
        return toks

    def barrier(self):
        toks = self._all_tokens()
        for e in ENGS:
            waits = self._waits(e, (), (), toks)
            if waits:
                self.lists[e].append((waits, None, None, None))

    def final_wait(self, eng):
        waits = self._waits(eng, (), (), self._all_tokens())
        self.lists[eng].append((waits, None, None, None))

    def emit(self):
        engmap = {"pe": "tensor", "dve": "vector", "act": "scalar", "pool": "gpsimd", "sp": "sync"}
        with self.nc.Block() as block:
            for e in ENGS:
                lst = self.lists[e]
                if not lst:
                    continue

                def body(engine, lst=lst):
                    cur, cm = None, None
                    for waits, fn, inc, scope in lst:
                        if PROFILE_SCOPES and scope != cur:
                            if cm is not None:
                                cm.__exit__(None, None, None)
                                cm = None
                            if scope is not None:
                                cm = self.nc.named_scope(scope)
                                cm.__enter__()
                            cur = scope
                        for s, v in waits:
                            engine.wait_ge(self.sems[s], v)
                        if fn is not None:
                            ins = None
                            for name, a, k in fn:
                                ins = getattr(engine, name)(*a, **k)
                            ins.then_inc(self.sems[inc[0]], inc[1])
                    if cm is not None:
                        cm.__exit__(None, None, None)

                getattr(block, engmap[e])(body)


def build(T, NE=32, dbg=None):
    NT = T // 128
    nc = bass.Bass("TRN2", target_bir_lowering=False)

    def din(name, shape, dt=F32):
        return nc.dram_tensor(name, list(shape), dt, kind="ExternalInput").ap()

    x_d = din("x", [T, D])
    w_in_d = din("w_in", [D, IN_COLS])
    cols_d = din("cols", [128, NCOL])
    w0_d = din("w0", [1, 512])
    w2_d = din("w2", [64, 512])
    a2_d = din("a2", [64, 512])
    g2_d = din("g2", [160, 512])
    woA_d = din("w_oA", [512, D])
    woB_d = din("w_oB", [512, D])
    wout_d = din("w_out", [D, D])
    lnvg_d = din("lnv_g", [1, 512])
    lnvb_d = din("lnv_b", [1, 512])
    wsT_d = din("wsT", [4, 128, 128])
    bs_d = din("b_s", [1, 512])
    wr_d = din("w_r", [D, 36])
    br_d = din("b_r", [1, 36])
    weg_d = din("w_e_gate", [32, D, 256])
    weu_d = din("w_e_up", [32, D, 256])
    wed_d = din("w_e_down", [32, 256, D])
    fg_d = din("final_g", [1, D])
    out_d = nc.dram_tensor("out", [T, D], F32, kind="ExternalOutput").ap()
    x1_d = nc.dram_tensor("x1_scr", [T, D], F32, kind="Internal").ap()
    h2_d = nc.dram_tensor("h2_scr", [128, 8, T], BF16, kind="Internal").ap()
    hT_d = nc.dram_tensor("hT_scr", [T // 128, 128, 1024], BF16, kind="Internal").ap()
    yg_d = nc.dram_tensor("yg_scr", [T // 128, 128, 512], BF16, kind="Internal").ap()
    dbg_out = {}
    if dbg:
        for name, shape in dbg.items():
            dbg_out[name] = nc.dram_tensor("dbg_" + name, list(shape), F32, kind="ExternalOutput").ap()

    with ExitStack() as st0:
        P = Prog(nc, st0)

        def sbuf(st, name, shape, dt):
            return st.enter_context(nc.sbuf_tensor("sb_" + name, list(shape), dt))

        psM = [st0.enter_context(nc.psum_tensor(f"psM{i}", [128, 512], F32)) for i in range(2)]
        psA = st0.enter_context(nc.psum_tensor("psA", [128, 1024], F32))
        psD = st0.enter_context(nc.psum_tensor("psD", [128, 1536], F32))
        psS = st0.enter_context(nc.psum_tensor("psS", [128, 512], F32))

        identf = sbuf(st0, "identf", [128, 128], F32)
        ident = sbuf(st0, "ident", [128, 128], BF16)
        mSU = sbuf(st0, "mSU", [128, 128], F32)
        mIU = sbuf(st0, "mIU", [128, 128], F32)
        maskA = sbuf(st0, "maskA", [128, 2, 512], F32)
        maskL = sbuf(st0, "maskL", [128, 2, 128], F32)
        BDf = sbuf(st0, "BDf", [128, 128], F32)
        BDb = sbuf(st0, "BDb", [128, 128], BF16)
        BD64 = sbuf(st0, "BD64", [128, 128], F32)
        Tri2 = sbuf(st0, "Tri2", [128, 256], F32)
        onesb = sbuf(st0, "onesb", [1, 128], BF16)
        onesf = sbuf(st0, "onesf", [128, 128], F32)
        cols = sbuf(st0, "cols", [128, NCOL], F32)
        omu = sbuf(st0, "omu", [128, NCHA], F32)
        oka = sbuf(st0, "oka", [128, 4], F32)
        g1bc = sbuf(st0, "g1bc", [128, 8, 128], F32)
        g2bc = sbuf(st0, "g2bc", [128, 8, 128], F32)

        def colap(c):
            return cols[:, c:c + 1]

        P.dma("sp", lambda e: e.dma_start(out=cols[:], in_=cols_d), writes=["cols"])
        P.op("pool", lambda e: e.memset(onesf[:], 1.0), writes=["onesf"])
        P.op("pool", lambda e: e.memset(identf[:], 0.0), writes=["identf"])
        P.op("pool", lambda e: e.affine_select(out=identf[:], in_=identf[:], pattern=[[-1, 128]], compare_op=ALU.not_equal,
                                               fill=1.0, base=0, channel_multiplier=1), reads=["identf"], writes=["identf"])
        P.op("dve", lambda e: e.tensor_copy(out=ident[:], in_=identf[:]), reads=["identf"], writes=["ident"])
        P.op("pool", lambda e: e.affine_select(out=mSU[:], in_=onesf[:], pattern=[[1, 128]], compare_op=ALU.is_gt,
                                               fill=0.0, base=0, channel_multiplier=-1), reads=["onesf"], writes=["mSU"])
        P.op("pool", lambda e: e.affine_select(out=mIU[:], in_=onesf[:], pattern=[[1, 128]], compare_op=ALU.is_ge,
                                               fill=0.0, base=0, channel_multiplier=-1), reads=["onesf"], writes=["mIU"])
        for h in range(2):
            for q in range(4):
                src = mSU if q % 2 == 0 else mIU
                P.op("dve", lambda e, h=h, q=q, src=src: e.tensor_copy(out=maskA[:, h, q * 128:(q + 1) * 128], in_=src[:]),
                     reads=["mSU", "mIU"], writes=["maskA"])
            P.op("pool", lambda e, h=h: e.affine_select(out=maskL[:, h, :], in_=onesf[:], pattern=[[-1, 128]], compare_op=ALU.is_gt,
                                                        fill=0.0, base=0, channel_multiplier=1), reads=["onesf"], writes=["maskL"])
        P.op("pool", lambda e: e.memset(BDf[:], 0.0), writes=["BDf"])
        P.op("pool", lambda e: e.memset(BDf[0:64, 0:64], 1.0), reads=["BDf"], writes=["BDf"])
        P.op("pool", lambda e: e.memset(BDf[64:128, 64:128], 1.0), reads=["BDf"], writes=["BDf"])
        P.op("dve", lambda e: e.tensor_copy(out=BDb[:], in_=BDf[:]), reads=["BDf"], writes=["BDb"])
        P.op("dve", lambda e: e.tensor_scalar(out=BD64[:], in0=BDf[:], scalar1=1.0 / 64.0, scalar2=None, op0=ALU.mult),
             reads=["BDf"], writes=["BD64"])
        P.op("dve", lambda e: e.tensor_scalar(out=Tri2[:, 0:128], in0=mIU[:], scalar1=EXPM05, scalar2=None, op0=ALU.mult),
             reads=["mIU"], writes=["Tri2"])
        P.op("dve", lambda e: e.tensor_scalar(out=Tri2[:, 128:256], in0=mSU[:], scalar1=EXPM05, scalar2=None, op0=ALU.mult),
             reads=["mSU", "Tri2"], writes=["Tri2"])
        P.op("dve", lambda e: e.tensor_copy(out=onesb[:], in_=onesf[0:1, :]), reads=["onesf"], writes=["onesb"])
        P.op("dve", lambda e: e.tensor_scalar(out=omu[:], in0=cols[:, C_MU:C_MU + NCHA], scalar1=-1.0, scalar2=1.0,
                                              op0=ALU.mult, op1=ALU.add), reads=["cols"], writes=["omu"])
        P.op("dve", lambda e: e.tensor_scalar(out=oka[:], in0=cols[:, C_KA:C_KA + 4], scalar1=-1.0, scalar2=1.0,
                                              op0=ALU.mult, op1=ALU.add), reads=["cols"], writes=["oka"])
        for k in range(8):
            P.op("dve", lambda e, k=k: e.tensor_scalar(out=g1bc[:, k, :], in0=onesf[:], scalar1=colap(C_G1 + k), scalar2=None,
                                                       op0=ALU.mult), reads=["cols", "onesf"], writes=["g1bc"])
            P.op("dve", lambda e, k=k: e.tensor_scalar(out=g2bc[:, k, :], in0=onesf[:], scalar1=colap(C_G2 + k), scalar2=None,
                                                       op0=ALU.mult), reads=["cols", "onesf"], writes=["g2bc"])

        def rms_to_T(st_keys, xin, xin_key, gbc, gbc_key, dst_ap, dst_key, tmp, tag):
            junk, ss, sd, rs, xsb = tmp
            P.op("act", lambda e: e.activation(out=junk[:], in_=xin, func=AF.Square, accum_out=ss[:]),
                 reads=[xin_key], writes=["junk" + tag, "ss" + tag])
            P.op("act", lambda e: e.activation(out=sd[:], in_=ss[:], func=AF.Sqrt, bias=NORM_EPS, scale=1.0 / D),
                 reads=["ss" + tag], writes=["sd" + tag])
            P.op("dve", lambda e: e.reciprocal(out=rs[:], in_=sd[:]), reads=["sd" + tag], writes=["rs" + tag])
            P.op("dve", lambda e: e.tensor_scalar(out=xsb[:], in0=xin, scalar1=rs[:], scalar2=None, op0=ALU.mult),
                 reads=[xin_key, "rs" + tag], writes=["xsb" + tag])
            pT = psM[0][:].bitcast(BF16)

            def tr(e):
                ins = None
                for k in range(8):
                    ins = e.transpose(out=pT[:, k * 128:(k + 1) * 128], in_=xsb[:, k * 128:(k + 1) * 128], identity=ident[:])
                return ins

            P.op("pe", tr, reads=["xsb" + tag, "ident"], writes=["psM0"])
            P.op("dve", lambda e: e.tensor_tensor(out=dst_ap, in0=pT.rearrange("p (k t) -> p k t", k=8), in1=gbc[:], op=ALU.mult),
                 reads=["psM0", gbc_key], writes=[dst_key])

        P.scope = "M1"
        stP = ExitStack()
        comb = sbuf(st0, "comb", [128, NT, 32], F32)
        woA = sbuf(stP, "woA", [128, 4, D], BF16)
        woB = sbuf(stP, "woB", [128, 4, D], BF16)
        wout = sbuf(stP, "wout", [128, 8, D], BF16)

        with ExitStack() as st:
            hTb = [sbuf(st, f"hTt{i}", [128, 8, 128], BF16) for i in range(2)]
            ygb = [sbuf(st, f"ygt{i}", [128, 4, 128], BF16) for i in range(2)]
            w_inA = sbuf(st, "w_inA", [128, 8, A_COLS], BF16)
            w2b = sbuf(st, "w2b", [64, 512], BF16)
            a2b = sbuf(st, "a2b", [128, 512], BF16)
            g2b = sbuf(st, "g2b", [128, 2, 512], BF16)
            w0row = sbuf(st, "w0row", [1, 512], BF16)
            for k in range(8):
                P.dma("pool", lambda e, k=k: e.dma_start(out=w_inA[:, k, :], in_=w_in_d[k * 128:(k + 1) * 128, 0:A_COLS]),
                      writes=["w_inA"])
            P.dma("pool", lambda e: e.dma_start(out=w2b[:], in_=w2_d), writes=["w2b"])
            P.dma("pool", lambda e: e.dma_start(out=a2b[64:128, :], in_=a2_d), writes=["a2b"])
            P.dma("pool", lambda e: e.dma_start(out=g2b[:, 0, :], in_=g2_d[0:128, :]), writes=["g2b"])
            P.dma("pool", lambda e: e.dma_start(out=g2b[0:32, 1, :], in_=g2_d[128:160, :]), reads=["g2b"], writes=["g2b"])
            P.dma("pool", lambda e: e.dma_start(out=w0row[:], in_=w0_d), writes=["w0row"])
            P.dma("pool", lambda e: e.dma_start(out=woA[:], in_=woA_d.rearrange("(k p) n -> p k n", p=128)), writes=["woA"])
            P.dma("pool", lambda e: e.dma_start(out=woB[:], in_=woB_d.rearrange("(k p) n -> p k n", p=128)), writes=["woB"])
            P.dma("pool", lambda e: e.dma_start(out=wout[:], in_=wout_d.rearrange("(k p) n -> p k n", p=128)), writes=["wout"])

            xin = [sbuf(st, f"xin{i}", [128, D], F32) for i in range(2)]
            junk = sbuf(st, "junk", [128, D], BF16)
            ss = sbuf(st, "ss", [128, 1], F32)
            sd = sbuf(st, "sd", [128, 1], F32)
            rs = sbuf(st, "rs", [128, 1], F32)
            xsb = sbuf(st, "xsb", [128, D], BF16)
            carry = sbuf(st, "carry", [128, NCHA], F32)
            ltmp = sbuf(st, "ltmp", [128, 129], F32)
            pmx = sbuf(st, "pmx", [128, 128], F32)
            txw2 = [sbuf(st, f"txw_{i}", [128, 128], BF16) for i in range(2)]
            sxg2 = [sbuf(st, f"sxg_{i}", [128, 2, 128], BF16) for i in range(2)]
            DC2 = [sbuf(st, f"DC_{i}", [128, 4], F32) for i in range(2)]
            etok = sbuf(st, "etok", [128, 512], F32)
            Dt2 = [sbuf(st, f"Dt_{i}", [128, 4, 128], F32) for i in range(2)]
            Dinv2 = [sbuf(st, f"Dinv_{i}", [128, 4, 128], F32) for i in range(2)]
            Dprev2 = [sbuf(st, f"Dprev_{i}", [128, 4, 128], F32) for i in range(2)]
            asig = [sbuf(st, f"asig{p}", [128, 128], F32) for p in range(4)]
            tA = [sbuf(st, f"tA{p}", [128, 128], F32) for p in range(4)]
            tB = [sbuf(st, f"tB{p}", [128, 128], F32) for p in range(4)]
            tC = [sbuf(st, f"tC{p}", [128, 128], F32) for p in range(4)]
            t16 = [sbuf(st, f"t16{p}", [128, 128], BF16) for p in range(4)]
            v16 = [sbuf(st, f"v16{p}", [128, 128], BF16) for p in range(4)]
            pmr = [sbuf(st, f"pmr{p}", [128, 128], F32) for p in range(4)]
            pmk = [sbuf(st, f"pmk{p}", [128, 128], F32) for p in range(4)]
            pmv = [sbuf(st, f"pmv{p}", [128, 128], F32) for p in range(4)]
            ltm = [sbuf(st, f"ltm{p}", [128, 129], F32) for p in range(4)]
            PX = [psM[0], psM[1], psA[:, 0:512], psA[:, 512:1024]]
            PXk = ["psM0", "psM1", "psA", "psA2"]
            PY = [psD[:, 0:512], psD[:, 512:1024], psD[:, 1024:1536], psS]
            PYk = ["psD0", "psD1", "psD2", "psS"]
            AR2 = [[sbuf(st, f"AR{p}_{i}", [128, 2, 128], BF16) for p in range(4)] for i in range(1)] * 2
            BT = [sbuf(st, f"BT{p}", [128, 128], BF16) for p in range(4)]
            KT = [sbuf(st, f"KT{p}", [128, 128], BF16) for p in range(4)]
            TOK2 = [[sbuf(st, f"TOK{p}_{i}", [128, 3, 128], BF16) for p in range(4)] for i in range(1)] * 2
            bon2 = [[sbuf(st, f"bon{p}_{i}", [128, 128], F32) for p in range(4)] for i in range(1)] * 2
            gT2 = [[sbuf(st, f"gT{p}_{i}", [128, 128], F32) for p in range(4)] for i in range(1)] * 2
            AM2 = [[sbuf(st, f"AM{p}_{i}", [128, 2, 512], BF16) for p in range(4)] for i in range(1)] * 2
            L0 = [sbuf(st, f"L0{p}", [128, 2, 128], BF16) for p in range(4)]
            MT2 = [[[sbuf(st, f"MT{p}_{i}_{j}", [128, 2, 256], BF16) for i in range(2)] for p in range(4)] for j in range(1)] * 2
            LK = [[sbuf(st, f"LK{p}_{i}", [128, 2, 128], BF16) for i in range(2)] for p in range(4)]
            Sw = [sbuf(st, f"Sw{p}", [128, 128], F32) for p in range(4)]
            Sb = [sbuf(st, f"Sb{p}", [128, 128], BF16) for p in range(4)]
            Xb = [sbuf(st, f"Xb{p}", [128, 128], BF16) for p in range(4)]
            Ub = [sbuf(st, f"Ub{p}", [128, 128], BF16) for p in range(4)]
            yT = sbuf(st, "yT", [128, 4, 128], F32)
            yc = sbuf(st, "yc", [128, 512], F32)
            ysq = sbuf(st, "ysq", [128, 512], F32)
            yrs = sbuf(st, "yrs", [128, 512], F32)
            y3 = sbuf(st, "y3", [128, 128], F32)
            tS = sbuf(st, "tS", [128, 128], F32)

            P.op("pool", lambda e: e.memset(carry[:], 0.0), writes=[f"carry{c}" for c in range(NCHA)])
            for p in range(4):
                P.op("pool", lambda e, p=p: e.memset(Sw[p][:], 0.0), writes=[f"Sw{p}"])
                P.op("pool", lambda e, p=p: e.memset(Sb[p][:], 0.0), writes=[f"Sb{p}"])

            def LV(p):
                return psD[:, 1024:1280] if p % 2 == 0 else psS[:, 0:256]

            def DK(p):
                return ["psD0", "psD2"] if p % 2 == 0 else ["psD1", "psS"]

            serial_ps = [psS, psM[1], psA[:, 0:512], psA[:, 512:1024]]
            serial_key = ["psS", "psM1", "psA", "psA2"]

            def tile_parts(tl, P):
                par = tl % 2
                AM, AR, TOK, MT, bon, gT = AM2[par], AR2[par], TOK2[par], MT2[par], bon2[par], gT2[par]
                Dt, Dinv, Dprev, txw, sxg, DC = Dt2[par], Dinv2[par], Dprev2[par], txw2[par], sxg2[par], DC2[par]
                tc = slice(tl * 128, (tl + 1) * 128)
                xb_, xk = xin[tl % 2], f"xin{tl % 2}"
                hT, hTk = hTb[tl % 2], f"hTt{tl % 2}"
                ygT, ygk = ygb[tl % 2], f"ygt{tl % 2}"

                def inproj_chunk(c, dst_fn):
                    rows = 128 if c < 14 else 32
                    bank, bkey = (psD[:, 512:1024], "psD1") if c == 13 else (psM[0], "psM0")

                    def mmf(e, c=c, rows=rows, bank=bank):
                        ins = None
                        for k in range(8):
                            ins = e.matmul(bank[0:rows, 0:128], lhsT=w_inA[:, k, c * 128:c * 128 + rows], rhs=hT[:, k, :],
                                           start=(k == 0), stop=(k == 7))
                        return ins

                    P.op("pe", mmf, reads=["w_inA", hTk], writes=[bkey])
                    P.op("act", lambda e: e.activation(out=ltmp[0:rows, 1:129], in_=bank[0:rows, 0:128], func=AF.Copy,
                                                       scale=cols[0:rows, C_MU + c:C_MU + c + 1]),
                         reads=[bkey, "cols"], writes=["ltmp"])
                    P.op("pool", lambda e: e.tensor_copy(out=ltmp[0:rows, 0:1], in_=carry[0:rows, c:c + 1]),
                         reads=[f"carry{c}", "ltmp"], writes=["ltmp"])
                    P.op("pool", lambda e: e.tensor_copy(out=carry[0:rows, c:c + 1], in_=ltmp[0:rows, 128:129]),
                         reads=["ltmp", f"carry{c}"], writes=[f"carry{c}"])
                    dst, dkey = dst_fn
                    P.op("dve", lambda e: e.scalar_tensor_tensor(out=dst[0:rows, :], in0=bank[0:rows, 0:128],
                                                                 scalar=omu[0:rows, c:c + 1], in1=ltmp[0:rows, 0:128],
                                                                 op0=ALU.mult, op1=ALU.add),
                         reads=[bkey, "omu", "ltmp"], writes=[dkey])

                def inproj_chunk_g(c, dst, dkey, bank, bkey, lt, ltk):
                    rows = 128

                    def mmf(e):
                        ins = None
                        for k in range(8):
                            ins = e.matmul(bank[0:rows, 0:128], lhsT=w_inA[:, k, c * 128:c * 128 + rows], rhs=hT[:, k, :],
                                           start=(k == 0), stop=(k == 7))
                        return ins

                    P.op("pe", mmf, reads=["w_inA", hTk], writes=[bkey])
                    yield
                    P.op("act", lambda e: e.activation(out=lt[:, 1:129], in_=bank[:, 0:128], func=AF.Copy, scale=cols[:, C_MU + c:C_MU + c + 1]),
                         reads=[bkey, "cols"], writes=[ltk])
                    yield
                    P.op("pool", lambda e: e.tensor_copy(out=lt[:, 0:1], in_=carry[:, c:c + 1]), reads=[f"carry{c}", ltk], writes=[ltk])
                    P.op("pool", lambda e: e.tensor_copy(out=carry[:, c:c + 1], in_=lt[:, 128:129]), reads=[ltk, f"carry{c}"], writes=[f"carry{c}"])
                    yield
                    P.op("dve", lambda e: e.scalar_tensor_tensor(out=dst[:], in0=bank[:, 0:128], scalar=omu[:, c:c + 1], in1=lt[:, 0:128],
                                                                 op0=ALU.mult, op1=ALU.add), reads=[bkey, "omu", ltk], writes=[dkey])
                    yield


                def front():
                    xb_ = xin[tl % 2]
                    xk = f"xin{tl % 2}"
                    P.dma("sp", lambda e, xb_=xb_, tl=tl: e.dma_start(out=xb_[:], in_=x_d[tl * 128:(tl + 1) * 128, :]), writes=[xk])
                    yield
                    hT, hTk = hTb[tl % 2], f"hTt{tl % 2}"
                    ygT, ygk = ygb[tl % 2], f"ygt{tl % 2}"
                    rms_to_T(None, xb_[:], xk, g1bc, "g1bc", hT[:], hTk, (junk, ss, sd, rs, xsb), "1")
                    yield
                    P.dma("sp", lambda e, tl=tl, hT=hT: e.dma_start(out=hT_d[tl], in_=hT[:].rearrange("p k t -> p (k t)")), reads=[hTk], writes=["hT_d"])
                    yield

                    inproj_chunk(12, (pmx, "pmx"))
                    yield
                    P.op("act", lambda e: e.activation(out=txw[0:64, :], in_=pmx[0:64, :], func=AF.Tanh), reads=["pmx"], writes=["txw"])
                    yield
                    P.op("dve", lambda e: e.tensor_copy(out=txw[64:128, :], in_=pmx[64:128, :]), reads=["pmx", "txw"], writes=["txw"])
                    yield
                    inproj_chunk(13, (pmx, "pmx"))
                    yield
                    P.op("act", lambda e: e.activation(out=sxg[:, 0, :], in_=pmx[:], func=AF.Sigmoid), reads=["pmx"], writes=["sxg"])
                    yield
                    inproj_chunk(14, (pmx, "pmx"))
                    yield
                    P.op("act", lambda e: e.activation(out=sxg[0:32, 1, :], in_=pmx[0:32, :], func=AF.Sigmoid),
                         reads=["pmx", "sxg"], writes=["sxg"])
                    yield

                    def zmm(e):
                        e.matmul(psM[0][:, 0:512], lhsT=txw[0:64, :], rhs=w2b[0:64, :], start=True, stop=False)
                        return e.matmul(psM[0][:, 0:512], lhsT=onesb[0:1, :], rhs=w0row[0:1, :], start=False, stop=True)

                    P.op("pe", zmm, reads=["txw", "w2b", "onesb", "w0row"], writes=["psM0"])
                    yield
                    P.op("act", lambda e: e.activation(out=etok[:], in_=psM[0][:, 0:512], func=AF.Sigmoid), reads=["psM0"], writes=["etok"])
                    yield

                    def cmm(e):
                        ins = None
                        for p in range(4):
                            ins = e.matmul(psD[:, 512 + p * 256:512 + (p + 1) * 256], lhsT=etok[:, p * 128:(p + 1) * 128], rhs=Tri2[:],
                                           start=True, stop=True)
                        return ins

                    P.op("pe", cmm, reads=["etok", "Tri2"], writes=["psD1", "psD2"])
                    yield
                    cum = psD[:, 512:1536].rearrange("p (a c) -> p a c", a=4)
                    P.op("act", lambda e: e.activation(out=Dt[:], in_=cum[:, :, 0:128], func=AF.Exp, scale=-1.0),
                         reads=["psD1", "psD2"], writes=["Dt"])
                    yield
                    P.op("act", lambda e: e.activation(out=Dinv[:], in_=cum[:, :, 0:128], func=AF.Exp, scale=1.0),
                         reads=["psD1", "psD2"], writes=["Dinv"])
                    yield
                    P.op("act", lambda e: e.activation(out=Dprev[:], in_=cum[:, :, 128:256], func=AF.Exp, scale=-1.0),
                         reads=["psD1", "psD2"], writes=["Dprev"])
                    yield


                def pair_gen(p):
                    X, Xk, Y, Yk = PX[p], PXk[p], PY[p], PYk[p]
                    rf, kf, vf = pmr[p], pmk[p], pmv[p]
                    rk_, kk_, vk_ = f"pmr{p}", f"pmk{p}", f"pmv{p}"
                    as_, tA_, tB_, tC_, t16_, v16_ = asig[p], tA[p], tB[p], tC[p], t16[p], v16[p]
                    ak, tAk, tBk, tCk, t16k, v16k = f"asig{p}", f"tA{p}", f"tB{p}", f"tC{p}", f"t16{p}", f"v16{p}"
                    cs = slice(p * 128, (p + 1) * 128)
                    for (c, dst, dkey, bank, bkey) in ((p, rf, rk_, X, Xk), (4 + p, kf, kk_, Y, Yk), (8 + p, vf, vk_, X, Xk)):
                        yield from inproj_chunk_g(c, dst, dkey, bank, bkey, ltm[p], f"ltm{p}")
                    P.op("pe", lambda e: e.matmul(Y[:, 0:128], lhsT=a2b[64:128, cs], rhs=txw[64:128, :], start=True, stop=True),
                         reads=["a2b", "txw"], writes=[Yk])
                    yield
                    P.op("act", lambda e: e.activation(out=as_[:], in_=Y[:, 0:128], func=AF.Sigmoid, bias=colap(C_A0 + p)),
                         reads=[Yk, "cols"], writes=[ak])
                    yield

                    def gmm(e):
                        e.matmul(X[:, 0:128], lhsT=g2b[:, 0, cs], rhs=sxg[:, 0, :], start=True, stop=False)
                        return e.matmul(X[:, 0:128], lhsT=g2b[0:32, 1, cs], rhs=sxg[0:32, 1, :], start=False, stop=True)

                    P.op("pe", gmm, reads=["g2b", "sxg"], writes=[Xk])
                    yield
                    P.op("act", lambda e: e.activation(out=gT[p][:], in_=X[:, 0:128], func=AF.Copy), reads=[Xk], writes=[f"gT{p}"])
                    yield
                    P.op("dve", lambda e: e.tensor_scalar(out=tA_[:], in0=kf[:], scalar1=colap(C_KK + p), scalar2=None, op0=ALU.mult),
                         reads=[kk_, "cols"], writes=[tAk])
                    yield
                    P.op("pool", lambda e: e.tensor_tensor(out=t16_[:], in0=tA_[:], in1=tA_[:], op=ALU.mult), reads=[tAk], writes=[t16k])
                    yield
                    P.op("pe", lambda e: e.matmul(Y[:, 0:128], lhsT=BDb[:], rhs=t16_[:], start=True, stop=True), reads=["BDb", t16k], writes=[Yk])
                    yield
                    P.op("act", lambda e: e.activation(out=tB_[:], in_=Y[:, 0:128], func=AF.Sqrt), reads=[Yk], writes=[tBk])
                    yield
                    P.op("dve", lambda e: e.tensor_scalar(out=tB_[:], in0=tB_[:], scalar1=1e-12, scalar2=None, op0=ALU.max), reads=[tBk], writes=[tBk])
                    P.op("dve", lambda e: e.reciprocal(out=tB_[:], in_=tB_[:]), reads=[tBk], writes=[tBk])
                    yield
                    P.op("pool", lambda e: e.tensor_tensor(out=tA_[:], in0=tA_[:], in1=tB_[:], op=ALU.mult), reads=[tAk, tBk], writes=[tAk])
                    yield
                    P.op("dve", lambda e: e.tensor_scalar(out=tC_[:], in0=as_[:], scalar1=colap(C_KA + p), scalar2=oka[:, p:p + 1],
                                                          op0=ALU.mult, op1=ALU.add), reads=[ak, "cols", "oka"], writes=[tCk])
                    yield
                    P.op("pool", lambda e: e.tensor_tensor(out=kf[:], in0=kf[:], in1=tC_[:], op=ALU.mult), reads=[kk_, tCk], writes=[kk_])
                    yield
                    P.op("dve", lambda e: e.scalar_tensor_tensor(out=AR[p][:, 0, :], in0=tA_[:], scalar=-1.0, in1=Dprev[:, p, :],
                                                                 op0=ALU.mult, op1=ALU.mult), reads=[tAk, "Dprev"], writes=[f"AR{p}"])
                    P.op("pool", lambda e: e.tensor_tensor(out=AR[p][:, 1, :], in0=rf[:], in1=Dt[:, p, :], op=ALU.mult),
                         reads=[rk_, "Dt", f"AR{p}"], writes=[f"AR{p}"])
                    yield
                    P.op("pool", lambda e: e.tensor_tensor(out=tB_[:], in0=tA_[:], in1=as_[:], op=ALU.mult), reads=[tAk, ak, tBk], writes=[tBk])
                    yield
                    P.op("dve", lambda e: e.tensor_tensor(out=BT[p][:], in0=tB_[:], in1=Dinv[:, p, :], op=ALU.mult), reads=[tBk, "Dinv"], writes=[f"BT{p}"])
                    P.op("dve", lambda e: e.tensor_tensor(out=KT[p][:], in0=kf[:], in1=Dinv[:, p, :], op=ALU.mult), reads=[kk_, "Dinv"], writes=[f"KT{p}"])
                    yield
                    P.op("dve", lambda e: e.scalar_tensor_tensor(out=t16_[:], in0=rf[:], scalar=colap(C_RK + p), in1=kf[:],
                                                                 op0=ALU.mult, op1=ALU.mult), reads=[rk_, kk_, "cols", t16k], writes=[t16k])
                    yield
                    P.op("pe", lambda e: e.matmul(Y[:, 0:128], lhsT=BDb[:], rhs=t16_[:], start=True, stop=True), reads=["BDb", t16k], writes=[Yk])
                    yield
                    P.op("dve", lambda e: e.tensor_tensor(out=bon[p][:], in0=Y[:, 0:128], in1=vf[:], op=ALU.mult), reads=[Yk, vk_], writes=[f"bon{p}"])
                    P.op("dve", lambda e: e.tensor_scalar(out=bon[p][:], in0=bon[p][:], scalar1=colap(C_LB + p), scalar2=None, op0=ALU.add),
                         reads=[f"bon{p}", "cols"], writes=[f"bon{p}"])
                    yield
                    P.op("act", lambda e: e.activation(out=v16_[:], in_=vf[:], func=AF.Copy), reads=[vk_], writes=[v16k])
                    yield
                    pT = X[:, 0:256].bitcast(BF16)

                    def tr3(e):
                        e.transpose(out=pT[:, 0:128], in_=v16_[:], identity=ident[:])
                        e.transpose(out=pT[:, 128:256], in_=BT[p][:], identity=ident[:])
                        return e.transpose(out=pT[:, 256:384], in_=KT[p][:], identity=ident[:])

                    P.op("pe", tr3, reads=[v16k, f"BT{p}", f"KT{p}", "ident"], writes=[Xk])
                    yield
                    P.op("act", lambda e: e.activation(out=TOK[p][:].rearrange("p a t -> p (a t)"), in_=pT[:, 0:384], func=AF.Copy),
                         reads=[Xk], writes=[f"TOK{p}"])
                    yield

                    def amm(e):
                        ins = None
                        for h, bank in ((0, X), (1, Y)):
                            hr = slice(64 * h, 64 * h + 64)
                            e.matmul(bank[:, 0:256], lhsT=BT[p][hr, :], rhs=AR[p][hr, :, :].rearrange("p a t -> p (a t)"), start=True, stop=True)
                            ins = e.matmul(bank[:, 256:512], lhsT=KT[p][hr, :], rhs=AR[p][hr, :, :].rearrange("p a t -> p (a t)"),
                                           start=True, stop=True)
                        return ins

                    P.op("pe", amm, reads=[f"BT{p}", f"KT{p}", f"AR{p}"], writes=[Xk, Yk])
                    yield
                    P.op("dve", lambda e: e.tensor_tensor(out=AM[p][:, 0, :], in0=X[:, 0:512], in1=maskA[:, 0, :], op=ALU.mult),
                         reads=[Xk, "maskA"], writes=[f"AM{p}"])
                    P.op("dve", lambda e: e.tensor_tensor(out=AM[p][:, 1, :], in0=Y[:, 0:512], in1=maskA[:, 1, :], op=ALU.mult),
                         reads=[Yk, "maskA", f"AM{p}"], writes=[f"AM{p}"])
                    yield
                    pTL = Y[:, 0:128].bitcast(BF16)

                    def lmm(e):
                        e.transpose(out=pTL[:, 0:128], in_=AM[p][:, 0, 0:128], identity=ident[:])
                        return e.transpose(out=pTL[:, 128:256], in_=AM[p][:, 1, 0:128], identity=ident[:])

                    P.op("pe", lmm, reads=[f"AM{p}", "ident"], writes=[Yk])
                    for h in range(2):
                        P.op("pool", lambda e, h=h: e.tensor_tensor(out=MT[p][0][:, h, 128:256], in0=AM[p][:, h, 0:128], in1=ident[:], op=ALU.add),
                             reads=[f"AM{p}", "ident", f"MT{p}_0"], writes=[f"MT{p}_0"])
                    yield
                    P.op("dve", lambda e: e.tensor_copy(out=L0[p][:].rearrange("p h c -> p (h c)"), in_=pTL), reads=[Yk], writes=[f"L0{p}"])
                    yield
                    Xm = X[:, 0:512].rearrange("p (h c) -> p h c", h=2)
                    Yl = Y[:, 0:256].rearrange("p (h c) -> p h c", h=2)

                    def r1(e):
                        ins = None
                        for h in range(2):
                            e.matmul(X[:, h * 256:h * 256 + 128], lhsT=L0[p][:, h, :], rhs=AM[p][:, h, 0:128], start=True, stop=True)
                            ins = e.matmul(Y[:, h * 128:(h + 1) * 128], lhsT=AM[p][:, h, 0:128], rhs=L0[p][:, h, :], start=True, stop=True)
                        return ins

                    P.op("pe", r1, reads=[f"L0{p}", f"AM{p}"], writes=[Xk, Yk])
                    yield
                    P.op("act", lambda e: e.activation(out=MT[p][0][:, :, 0:128], in_=Xm[:, :, 0:128], func=AF.Copy),
                         reads=[Xk, f"MT{p}_0"], writes=[f"MT{p}_0"])
                    P.op("dve", lambda e: e.tensor_copy(out=LK[p][0][:], in_=Yl), reads=[Yk], writes=[f"LK{p}_0"])
                    yield
                    for rnd in range(2, 8):
                        src = rnd % 2
                        dst = 1 - src
                        last = (rnd == 7)
                        mts, mtd, lks, lkd = MT[p][src], MT[p][dst], LK[p][src], LK[p][dst]

                        def rk(e, mts=mts, lks=lks, last=last):
                            ins = None
                            for h in range(2):
                                if last:
                                    e.matmul(X[:, h * 256 + 128:h * 256 + 256], lhsT=lks[:, h, :], rhs=mts[:, h, 128:256], start=True, stop=False)
                                    ins = e.matmul(X[:, h * 256 + 128:h * 256 + 256], lhsT=ident[:], rhs=mts[:, h, 128:256], start=False, stop=True)
                                else:
                                    e.matmul(X[:, h * 256:h * 256 + 256], lhsT=lks[:, h, :], rhs=mts[:, h, :], start=True, stop=False)
                                    e.matmul(X[:, h * 256 + 128:h * 256 + 256], lhsT=ident[:], rhs=mts[:, h, 128:256], start=False, stop=True)
                            if not last:
                                for h in range(2):
                                    ins = e.matmul(Y[:, h * 128:(h + 1) * 128], lhsT=mts[:, h, 0:128], rhs=lks[:, h, :], start=True, stop=True)
                            return ins

                        P.op("pe", rk, reads=[f"MT{p}_{src}", f"LK{p}_{src}", "ident"], writes=[Xk] if last else [Xk, Yk])
                        yield
                        if last:
                            P.op("act", lambda e, mtd=mtd: e.activation(out=mtd[:, :, 128:256], in_=Xm[:, :, 128:256], func=AF.Copy),
                                 reads=[Xk, f"MT{p}_{dst}"], writes=[f"MT{p}_{dst}"])
                        else:
                            P.op("act", lambda e, mtd=mtd: e.activation(out=mtd[:], in_=Xm, func=AF.Copy), reads=[Xk], writes=[f"MT{p}_{dst}"])
                            P.op("dve", lambda e, lkd=lkd: e.tensor_copy(out=lkd[:], in_=Yl), reads=[Yk], writes=[f"LK{p}_{dst}"])
                        yield
                    P.op("pool", lambda e: e.tensor_copy(out=DC[:, p:p + 1], in_=Dt[:, p, 127:128]), reads=["Dt", "DC"], writes=["DC"])
                    yield


                def tail():
                    for p in range(4):
                        bank, bk = serial_ps[p], serial_key[p]

                        def xmm(e, p=p, bank=bank):
                            e.matmul(bank[:, 0:64], lhsT=AM[p][:, 0, 256:384], rhs=TOK[p][:, 0, 0:64], start=True, stop=False)
                            e.matmul(bank[:, 64:128], lhsT=AM[p][:, 1, 256:384], rhs=TOK[p][:, 0, 64:128], start=False, stop=False)
                            return e.matmul(bank[:, 0:128], lhsT=AR[p][:, 0, :], rhs=Sb[p][:], start=False, stop=True)

                        P.op("pe", xmm, reads=[f"AM{p}", f"TOK{p}", f"AR{p}", f"Sb{p}"], writes=[bk])
                    for p in range(4):
                        bank, bk = serial_ps[p], serial_key[p]
                        P.op("act", lambda e, p=p, bank=bank: e.activation(out=Xb[p][:], in_=bank[:, 0:128], func=AF.Copy),
                             reads=[bk], writes=[f"Xb{p}"])
                    for p in range(4):
                        bank, bk = serial_ps[p], serial_key[p]

                        def umm(e, p=p, bank=bank):
                            e.matmul(bank[:, 128:192], lhsT=MT[p][0][:, 0, 128:256], rhs=Xb[p][:, 0:64], start=True, stop=True)
                            return e.matmul(bank[:, 192:256], lhsT=MT[p][0][:, 1, 128:256], rhs=Xb[p][:, 64:128], start=True, stop=True)

                        P.op("pe", umm, reads=[f"MT{p}_0", f"Xb{p}"], writes=[bk])
                    for p in range(4):
                        bank, bk = serial_ps[p], serial_key[p]
                        P.op("dve", lambda e, p=p, bank=bank: e.tensor_copy(out=Ub[p][:], in_=bank[:, 128:256]), reads=[bk], writes=[f"Ub{p}"])
                    for p in range(4):
                        bank, bk = serial_ps[p], serial_key[p]

                        def ymm(e, p=p, bank=bank):
                            e.matmul(bank[:, 256:384], lhsT=Sb[p][:], rhs=AR[p][:, 1, :], start=True, stop=False)
                            ins = None
                            for h in range(2):
                                hs = slice(64 * h, 64 * h + 64)
                                e.matmul(bank[hs, 256:384], lhsT=Ub[p][:, hs], rhs=AM[p][:, h, 128:256], start=False, stop=False)
                                ins = e.matmul(bank[hs, 256:384], lhsT=TOK[p][:, 0, hs], rhs=AM[p][:, h, 384:512], start=False, stop=True)
                            return ins

                        P.op("pe", ymm, reads=[f"Sb{p}", f"AR{p}", f"Ub{p}", f"AM{p}", f"TOK{p}"], writes=[bk])

                        def smm(e, p=p, bank=bank):
                            e.matmul(bank[:, 384:512], lhsT=TOK[p][:, 2, :], rhs=TOK[p][:, 0, :], start=True, stop=False)
                            return e.matmul(bank[:, 384:512], lhsT=TOK[p][:, 1, :], rhs=Ub[p][:], start=False, stop=True)

                        P.op("pe", smm, reads=[f"TOK{p}", f"Ub{p}"], writes=[bk])
                    for p in range(4):
                        bank, bk = serial_ps[p], serial_key[p]
                        P.op("act", lambda e, p=p, bank=bank: e.activation(out=yT[:, p, :], in_=bank[:, 256:384], func=AF.Copy),
                             reads=[bk, "yT"], writes=["yT"])
                        P.op("dve", lambda e, p=p, bank=bank: e.tensor_tensor(out=tS[:], in0=bank[:, 384:512], in1=Sw[p][:], op=ALU.add),
                             reads=[bk, f"Sw{p}", "tS"], writes=["tS"])
                        P.op("dve", lambda e, p=p: e.scalar_tensor_tensor(out=Sw[p][:], in0=tS[:], scalar=DC[:, p:p + 1], in1=BDf[:],
                                                                          op0=ALU.mult, op1=ALU.mult),
                             reads=["tS", f"Sw{p}", "DC", "BDf"], writes=[f"Sw{p}"])
                        P.op("act", lambda e, p=p: e.activation(out=Sb[p][:], in_=Sw[p][:], func=AF.Copy), reads=[f"Sw{p}"], writes=[f"Sb{p}"])

                    yTf = yT[:].rearrange("p a t -> p (a t)")
                    P.op("pe", lambda e: e.matmul(psD[:, 0:512], lhsT=BD64[:], rhs=yTf, start=True, stop=True), reads=["BD64", "yT"], writes=["psD0"])
                    yield
                    P.op("dve", lambda e: e.tensor_tensor(out=yc[:], in0=yTf, in1=psD[:, 0:512], op=ALU.subtract), reads=["yT", "psD0"], writes=["yc"])
                    yield
                    P.op("act", lambda e: e.activation(out=ysq[:], in_=yc[:], func=AF.Square), reads=["yc"], writes=["ysq"])
                    yield
                    P.op("pe", lambda e: e.matmul(psD[:, 0:512], lhsT=BD64[:], rhs=ysq[:], start=True, stop=True), reads=["BD64", "ysq"], writes=["psD0"])
                    yield
                    P.op("act", lambda e: e.activation(out=yrs[:], in_=psD[:, 0:512], func=AF.Sqrt, bias=LNX_EPS), reads=["psD0"], writes=["yrs"])
                    yield
                    P.op("dve", lambda e: e.reciprocal(out=yrs[:], in_=yrs[:]), reads=["yrs"], writes=["yrs"])
                    yield
                    P.op("pool", lambda e: e.tensor_tensor(out=yc[:], in0=yc[:], in1=yrs[:], op=ALU.mult), reads=["yc", "yrs"], writes=["yc"])
                    yield
                    for p in range(4):
                        P.op("dve", lambda e, p=p: e.scalar_tensor_tensor(out=y3[:], in0=yc[:, p * 128:(p + 1) * 128], scalar=colap(C_LG + p),
                                                                          in1=bon[p][:], op0=ALU.mult, op1=ALU.add),
                             reads=["yc", "cols", f"bon{p}"], writes=["y3"])
                        P.op("pool", lambda e, p=p: e.tensor_tensor(out=ygT[:, p, :], in0=y3[:], in1=gT[p][:], op=ALU.mult),
                             reads=["y3", f"gT{p}", ygk], writes=[ygk])
                    P.dma("sp", lambda e, tl=tl, ygT=ygT: e.dma_start(out=yg_d[tl], in_=ygT[:].rearrange("p a t -> p (a t)")), reads=[ygk], writes=["yg_d"])
                    yield


                return front, pair_gen, tail

            class KeyProxy:
                PAT = re.compile(r'^(AM|AR|TOK|MT|bon|gT)\d')

                def __init__(self, par):
                    self.par = par

                def km(self, k):
                    if k in ('Dt', 'Dinv', 'Dprev', 'txw', 'sxg', 'DC'):
                        return f'{k}@{self.par}'
                    return k

                def op(self, eng, fn, reads=(), writes=()):
                    return P.op(eng, fn, [self.km(k) for k in reads], [self.km(k) for k in writes])

                def dma(self, q, fn, reads=(), writes=()):
                    return P.dma(q, fn, [self.km(k) for k in reads], [self.km(k) for k in writes])

            def run_rr(gens):
                gens = list(gens)
                while gens:
                    for g in list(gens):
                        try:
                            next(g)
                        except StopIteration:
                            gens.remove(g)

            parts = [tile_parts(tl, KeyProxy(tl % 2)) for tl in range(NT)]
            run_rr([parts[0][0]()])
            for tl in range(NT):
                run_rr([parts[tl][1](p) for p in range(4)])
                gens = [parts[tl][2]()]
                if tl + 1 < NT:
                    gens.append(parts[tl + 1][0]())
                run_rr(gens)

            P.barrier()

        with ExitStack() as st:
            P.scope = "M2"
            w_inB = sbuf(st, "w_inB", [128, 8, 3072], BF16)
            wsTf = sbuf(st, "wsTf", [128, 4, 128], F32)
            wsTb = sbuf(st, "wsTb", [128, 4, 128], BF16)
            bsrow = sbuf(st, "bsrow", [1, 512], BF16)
            lrow = sbuf(st, "lrow", [1, 2, 512], F32)
            lnvg = sbuf(st, "lnvg", [128, 512], F32)
            lnvb = sbuf(st, "lnvb", [128, 512], F32)
            for j in range(3):
                for k in range(8):
                    P.dma("pool", lambda e, k=k, j=j: e.dma_start(out=w_inB[:, k, j * 1024:(j + 1) * 1024],
                                                                  in_=w_in_d[k * 128:(k + 1) * 128, A_COLS + j * 1024:A_COLS + (j + 1) * 1024]),
                          writes=[f"w_inB{j}"])
            P.dma("sp", lambda e: e.dma_start(out=wsTf[:], in_=wsT_d.rearrange("g s t -> s g t")), writes=["wsTf"])
            P.dma("pool", lambda e: e.dma_start(out=bsrow[:], in_=bs_d), writes=["bsrow"])
            P.dma("sp", lambda e: e.dma_start(out=lrow[:, 0, :], in_=lnvg_d), writes=["lrow"])
            P.dma("sp", lambda e: e.dma_start(out=lrow[:, 1, :], in_=lnvb_d), reads=["lrow"], writes=["lrow"])
            for g in range(4):
                P.op("dve", lambda e, g=g: e.tensor_tensor(out=wsTb[:, g, :], in0=wsTf[:, g, :], in1=mIU[:], op=ALU.mult),
                     reads=["wsTf", "mIU", "wsTb"], writes=["wsTb"])
            for i, (dst, dk) in enumerate([(lnvg, "lnvg"), (lnvb, "lnvb")]):
                P.op("pe", lambda e, i=i: e.matmul(psM[0][:, 0:512], lhsT=onesf[0:1, :], rhs=lrow[0:1, i, :], start=True, stop=True),
                     reads=["onesf", "lrow"], writes=["psM0"])
                P.op("act", lambda e, dst=dst: e.activation(out=dst[:], in_=psM[0][:, 0:512], func=AF.Copy), reads=["psM0"], writes=[dk])

            TB = min(4, NT)
            BN = TB * 128
            NBLK = NT // TB
            hTB = [sbuf(st, f"hTB{i}", [128, 8, BN], BF16) for i in range(2)]
            ygB = [sbuf(st, f"ygB{i}", [128, 4, BN], BF16) for i in range(2)]
            uT = sbuf(st, "uTB", [128, 4, BN], BF16)
            mixT = sbuf(st, "mixTB", [128, 4, BN], BF16)
            t1 = sbuf(st, "t1B", [128, 8, BN], BF16)
            zT = sbuf(st, "zTB", [128, 8, BN], BF16)
            gsT = [sbuf(st, f"gsT{i}", [128, BN], BF16) for i in range(2)]
            t2T = [sbuf(st, f"t2T{i}", [128, BN], BF16) for i in range(2)]
            xinL = [sbuf(st, f"xinb{i}", [128, D], F32) for i in range(4)]
            vgL = [sbuf(st, f"vg{i}", [128, 512], F32) for i in range(2)]
            vlnL = [sbuf(st, f"vln{i}", [128, 512], BF16) for i in range(2)]
            xsbL = [sbuf(st, f"xsb2{i}", [128, D], BF16) for i in range(2)]
            h2tL = [sbuf(st, f"h2t{i}", [128, 8, 128], BF16) for i in range(4)]
            sm = [{n: sbuf(st, f"{n}{i}", [128, w], F32) for n, w in (("bst", 6), ("mv", 2), ("lsd", 1), ("lrs", 1), ("lnm", 1), ("ss2", 1), ("sd2", 1), ("rs2", 1))}
                  for i in range(2)]
            RING = [(psM[0], "psM0"), (psM[1], "psM1"), (psS, "psS"), (psD[:, 0:512], "psD0"), (psD[:, 512:1024], "psD1"), (psD[:, 1024:1536], "psD2"),
                    (psA[:, 0:512], "psA"), (psA[:, 512:1024], "psA2")]
            ring_i = [0]

            def ring():
                r = RING[ring_i[0] % len(RING)]
                ring_i[0] += 1
                return r

            wrb = sbuf(st, "wrb", [128, 8, 36], BF16)
            brrow = sbuf(st, "brrow", [1, 36], BF16)
            P.dma("pool", lambda e: e.dma_start(out=wrb[:], in_=wr_d.rearrange("(k p) n -> p k n", p=128)), writes=["wrb"])
            P.dma("pool", lambda e: e.dma_start(out=brrow[:], in_=br_d), writes=["brrow"])
            NRS = 4
            rsc = []
            for i in range(NRS):
                d = {"Lg": sbuf(st, f"Lg{i}", [128, 36], F32), "c": sbuf(st, f"rc{i}", [128, 10], F32), "ohg": sbuf(st, f"ohg{i}", [128, 4], F32),
                     "ex4": sbuf(st, f"ex4{i}", [128, 4], F32), "esel": sbuf(st, f"esel{i}", [128, 8], F32), "e2": sbuf(st, f"e2{i}", [128, 8], F32),
                     "mk1": sbuf(st, f"mk1{i}", [128, 8], F32), "mk2": sbuf(st, f"mk2{i}", [128, 8], F32), "wg8": sbuf(st, f"wg8{i}", [128, 8], F32)}
                rsc.append(d)

            def router_rest(tl):
                sl = tl % NRS
                d = rsc[sl]
                Lg, ohg, ex4, esel, e2, mk1, mk2, wg8 = d["Lg"], d["ohg"], d["ex4"], d["esel"], d["e2"], d["mk1"], d["mk2"], d["wg8"]
                cc = d["c"]
                gmax, ngmax, se, gp, m1, m2, dd, w1, w2 = [cc[:, i:i + 1] for i in range(9)]
                rk = f"rt{sl}"
                steps = [
                    ("dve", lambda e: e.tensor_reduce(out=gmax, in_=Lg[:, 0:4], axis=AX.X, op=ALU.max)),
                    ("dve", lambda e: e.tensor_scalar(out=ohg[:], in0=Lg[:, 0:4], scalar1=gmax, scalar2=None, op0=ALU.is_ge)),
                    ("dve", lambda e: e.tensor_scalar(out=ngmax, in0=gmax, scalar1=-1.0, scalar2=None, op0=ALU.mult)),
                    ("act", lambda e: e.activation(out=ex4[:], in_=Lg[:, 0:4], func=AF.Exp, bias=ngmax)),
                    ("dve", lambda e: e.tensor_reduce(out=se, in_=ex4[:], axis=AX.X, op=ALU.add)),
                    ("dve", lambda e: e.reciprocal(out=gp, in_=se)),
                    ("dve", lambda e: e.tensor_scalar(out=esel[:], in0=Lg[:, 4:12], scalar1=ohg[:, 0:1], scalar2=None, op0=ALU.mult)),
                ]
                for g in range(1, 4):
                    steps.append(("dve", lambda e, g=g: e.scalar_tensor_tensor(out=esel[:], in0=Lg[:, 4 + 8 * g:12 + 8 * g], scalar=ohg[:, g:g + 1],
                                                                               in1=esel[:], op0=ALU.mult, op1=ALU.add)))
                steps += [
                    ("dve", lambda e: e.tensor_reduce(out=m1, in_=esel[:], axis=AX.X, op=ALU.max)),
                    ("dve", lambda e: e.tensor_scalar(out=mk1[:], in0=esel[:], scalar1=m1, scalar2=None, op0=ALU.is_ge)),
                    ("dve", lambda e: e.scalar_tensor_tensor(out=e2[:], in0=mk1[:], scalar=-1e30, in1=esel[:], op0=ALU.mult, op1=ALU.add)),
                    ("dve", lambda e: e.tensor_reduce(out=m2, in_=e2[:], axis=AX.X, op=ALU.max)),
                    ("dve", lambda e: e.tensor_scalar(out=mk2[:], in0=e2[:], scalar1=m2, scalar2=None, op0=ALU.is_ge)),
                    ("dve", lambda e: e.tensor_tensor(out=dd, in0=m2, in1=m1, op=ALU.subtract)),
                    ("act", lambda e: e.activation(out=w2, in_=dd, func=AF.Sigmoid)),
                    ("act", lambda e: e.activation(out=w1, in_=dd, func=AF.Sigmoid, scale=-1.0)),
                    ("dve", lambda e: e.tensor_tensor(out=w1, in0=w1, in1=gp, op=ALU.mult)),
                    ("dve", lambda e: e.tensor_tensor(out=w2, in0=w2, in1=gp, op=ALU.mult)),
                    ("dve", lambda e: e.tensor_scalar(out=wg8[:], in0=mk1[:], scalar1=w1, scalar2=None, op0=ALU.mult)),
                    ("dve", lambda e: e.scalar_tensor_tensor(out=wg8[:], in0=mk2[:], scalar=w2, in1=wg8[:], op0=ALU.mult, op1=ALU.add)),
                ]
                for eng, fn in steps:
                    P.op(eng, fn, reads=[rk], writes=[rk])
                    yield
                for g in range(4):
                    P.op("dve", lambda e, g=g: e.tensor_scalar(out=comb[:, tl, g * 8:(g + 1) * 8], in0=wg8[:], scalar1=ohg[:, g:g + 1],
                                                               scalar2=None, op0=ALU.mult), reads=[rk, f"comb{tl}"], writes=[f"comb{tl}"])
                yield


            def run_rr2(gens):
                gens = list(gens)
                while gens:
                    for g in list(gens):
                        try:
                            next(g)
                        except StopIteration:
                            gens.remove(g)

            def load_block(b):
                hB, yB = hTB[b % 2], ygB[b % 2]
                for i in range(TB):
                    tl = b * TB + i
                    P.dma("sp", lambda e, tl=tl, i=i: e.dma_start(out=hB[:, :, i * 128:(i + 1) * 128], in_=hT_d[tl].rearrange("p (k t) -> p k t", k=8)),
                          reads=["hT_d"], writes=[f"hTB{b % 2}"])
                    P.dma("sp", lambda e, tl=tl, i=i: e.dma_start(out=yB[:, :, i * 128:(i + 1) * 128], in_=yg_d[tl].rearrange("p (a t) -> p a t", a=4)),
                          reads=["yg_d"], writes=[f"ygB{b % 2}"])

            load_block(0)
            pending = []
            for b in range(NBLK):
                hB, yB, hk, yk = hTB[b % 2], ygB[b % 2], f"hTB{b % 2}", f"ygB{b % 2}"
                if b + 1 < NBLK:
                    load_block(b + 1)

                def U(c):
                    bank, bk = ring()

                    def umm2(e, c=c, bank=bank):
                        ins = None
                        for k in range(8):
                            ins = e.matmul(bank[:, 0:BN], lhsT=w_inB[:, k, c * 128:(c + 1) * 128], rhs=hB[:, k, :], start=(k == 0), stop=(k == 7))
                        return ins

                    P.op("pe", umm2, reads=["w_inB0", hk], writes=[bk])
                    P.op("act", lambda e, c=c, bank=bank: e.activation(out=uT[:, c, :], in_=bank[:, 0:BN], func=AF.Gelu), reads=[bk, "uTB"], writes=["uTB"])

                def g1(m):
                    bank, bk = ring()
                    c0 = 1024 + m * 128

                    def gm(e):
                        ins = None
                        for k in range(8):
                            ins = e.matmul(bank[:, 0:BN], lhsT=w_inB[:, k, c0:c0 + 128], rhs=hB[:, k, :], start=(k == 0), stop=(k == 7))
                        return ins

                    gs = gsT[m % 2]
                    P.op("pe", gm, reads=["w_inB1", hk], writes=[bk])
                    P.op("act", lambda e: e.activation(out=gs[:], in_=bank[:, 0:BN], func=AF.Sigmoid, bias=colap(C_BG + m)), reads=[bk, "cols"], writes=[f"gsT{m % 2}"])
                    bank2, bk2 = ring()

                    def ym(e):
                        ins = None
                        for c in range(4):
                            ins = e.matmul(bank2[:, 0:BN], lhsT=woA[:, c, m * 128:(m + 1) * 128], rhs=yB[:, c, :], start=(c == 0), stop=(c == 3))
                        return ins

                    P.op("pe", ym, reads=["woA", yk], writes=[bk2])
                    P.op("dve", lambda e: e.tensor_tensor(out=t1[:, m, :], in0=bank2[:, 0:BN], in1=gs[:], op=ALU.mult), reads=[bk2, f"gsT{m % 2}", "t1B"], writes=["t1B"])

                def V(i):
                    tl = b * TB + i
                    ts_ = slice(i * 128, (i + 1) * 128)
                    vg, vln, d_ = vgL[i % 2], vlnL[i % 2], sm[i % 2]
                    q = i % 2
                    bank, bk = ring()

                    def vmm(e, bank=bank, ts_=ts_):
                        ins = None
                        for k in range(8):
                            ins = e.matmul(bank[:, 0:512], lhsT=hB[:, k, ts_], rhs=w_inB[:, k, 512:1024], start=(k == 0), stop=(k == 7))
                        return ins

                    P.op("pe", vmm, reads=["w_inB0", hk], writes=[bk])
                    P.op("act", lambda e, bank=bank, vg=vg: e.activation(out=vg[:], in_=bank[:, 0:512], func=AF.Gelu), reads=[bk], writes=[f"vg{q}"])
                    P.op("dve", lambda e, vg=vg, d_=d_: e.bn_stats(out=d_["bst"][:], in_=vg[:]), reads=[f"vg{q}"], writes=[f"sm{q}"])
                    P.op("dve", lambda e, d_=d_: e.bn_aggr(out=d_["mv"][:], in_=d_["bst"][:]), reads=[f"sm{q}"], writes=[f"sm{q}"])
                    P.op("act", lambda e, d_=d_: e.activation(out=d_["lsd"][:], in_=d_["mv"][:, 1:2], func=AF.Sqrt, bias=LN_EPS), reads=[f"sm{q}"], writes=[f"sm{q}"])
                    P.op("dve", lambda e, d_=d_: e.reciprocal(out=d_["lrs"][:], in_=d_["lsd"][:]), reads=[f"sm{q}"], writes=[f"sm{q}"])
                    P.op("dve", lambda e, d_=d_: e.tensor_scalar(out=d_["lnm"][:], in0=d_["mv"][:, 0:1], scalar1=-1.0, scalar2=d_["lrs"][:], op0=ALU.mult, op1=ALU.mult),
                         reads=[f"sm{q}"], writes=[f"sm{q}"])
                    P.op("dve", lambda e, vg=vg, d_=d_: e.tensor_scalar(out=vg[:], in0=vg[:], scalar1=d_["lrs"][:], scalar2=d_["lnm"][:], op0=ALU.mult, op1=ALU.add),
                         reads=[f"vg{q}", f"sm{q}"], writes=[f"vg{q}"])
                    P.op("pool", lambda e, vg=vg: e.tensor_tensor(out=vg[:], in0=vg[:], in1=lnvg[:], op=ALU.mult), reads=[f"vg{q}", "lnvg"], writes=[f"vg{q}"])
                    P.op("pool", lambda e, vg=vg, vln=vln: e.tensor_tensor(out=vln[:], in0=vg[:], in1=lnvb[:], op=ALU.add), reads=[f"vg{q}", "lnvb"], writes=[f"vln{q}"])

                def SV(i):
                    ts_ = slice(i * 128, (i + 1) * 128)
                    vln = vlnL[i % 2]
                    q = i % 2
                    bank, bk = ring()

                    def svmm(e, bank=bank, vln=vln):
                        ins = None
                        for g in range(4):
                            e.matmul(bank[:, g * 128:(g + 1) * 128], lhsT=vln[:, g * 128:(g + 1) * 128], rhs=wsTb[:, g, :], start=True, stop=False)
                            ins = e.matmul(bank[:, g * 128:(g + 1) * 128], lhsT=onesb[0:1, :], rhs=bsrow[0:1, g * 128:(g + 1) * 128], start=False, stop=True)
                        return ins

                    P.op("pe", svmm, reads=[f"vln{q}", "wsTb", "onesb", "bsrow"], writes=[bk])
                    P.op("dve", lambda e, bank=bank, ts_=ts_: e.tensor_tensor(out=mixT[:, :, ts_], in0=bank[:, 0:512].rearrange("p (g t) -> p g t", g=4),
                                                                           in1=uT[:, :, ts_], op=ALU.mult), reads=[bk, "uTB", "mixTB"], writes=["mixTB"])
                def G2(m):
                    bank, bk = ring()
                    c0 = 2048 + m * 128

                    def gm(e, bank=bank, c0=c0):
                        ins = None
                        for k in range(8):
                            ins = e.matmul(bank[:, 0:BN], lhsT=w_inB[:, k, c0:c0 + 128], rhs=hB[:, k, :], start=(k == 0), stop=(k == 7))
                        return ins

                    gs, t2 = gsT[m % 2], t2T[m % 2]
                    P.op("pe", gm, reads=["w_inB2", hk], writes=[bk])
                    P.op("act", lambda e, bank=bank, gs=gs, m=m: e.activation(out=gs[:], in_=bank[:, 0:BN], func=AF.Sigmoid, bias=colap(C_BG + 8 + m)),
                         reads=[bk, "cols"], writes=[f"gsT{m % 2}"])
                    bank2, bk2 = ring()

                    def ym(e, bank2=bank2, m=m):
                        ins = None
                        for c in range(4):
                            ins = e.matmul(bank2[:, 0:BN], lhsT=woB[:, c, m * 128:(m + 1) * 128], rhs=mixT[:, c, :], start=(c == 0), stop=(c == 3))
                        return ins

                    P.op("pe", ym, reads=["woB", "mixTB"], writes=[bk2])
                    P.op("dve", lambda e, bank2=bank2, gs=gs, t2=t2: e.tensor_tensor(out=t2[:], in0=bank2[:, 0:BN], in1=gs[:], op=ALU.mult),
                         reads=[bk2, f"gsT{m % 2}"], writes=[f"t2T{m % 2}"])
                    P.op("pool", lambda e, m=m, t2=t2: e.tensor_tensor(out=zT[:, m, :], in0=t1[:, m, :], in1=t2[:], op=ALU.add),
                         reads=["t1B", f"t2T{m % 2}", "zTB"], writes=["zTB"])

                def O(i):
                    tl = b * TB + i
                    ts_ = slice(i * 128, (i + 1) * 128)
                    q = i % 2
                    xb_, xsb, d_ = xinL[i], xsbL[q], sm[q]
                    for hf in range(2):
                        bank, bk = ring()

                        def omm(e, bank=bank, hf=hf, ts_=ts_):
                            ins = None
                            for m in range(8):
                                ins = e.matmul(bank[:, 0:512], lhsT=zT[:, m, ts_], rhs=wout[:, m, hf * 512:(hf + 1) * 512], start=(m == 0), stop=(m == 7))
                            return ins

                        P.op("pe", omm, reads=["zTB", "wout"], writes=[bk])
                        P.op("dve", lambda e, bank=bank, hf=hf, xb_=xb_: e.tensor_tensor(out=xb_[:, hf * 512:(hf + 1) * 512], in0=bank[:, 0:512],
                                                                                     in1=xb_[:, hf * 512:(hf + 1) * 512], op=ALU.add),
                             reads=[bk, f"xinb{i}"], writes=[f"xinb{i}"])
                    P.dma("sp", lambda e, xb_=xb_, tl=tl: e.dma_start(out=x1_d[tl * 128:(tl + 1) * 128, :], in_=xb_[:]), reads=[f"xinb{i}"], writes=["x1_d"])
                    P.op("act", lambda e, xb_=xb_, xsb=xsb, d_=d_: e.activation(out=xsb[:], in_=xb_[:], func=AF.Square, accum_out=d_["ss2"][:]),
                         reads=[f"xinb{i}"], writes=[f"xsb2{q}", f"sm{q}"])
                    P.op("act", lambda e, d_=d_: e.activation(out=d_["sd2"][:], in_=d_["ss2"][:], func=AF.Sqrt, bias=NORM_EPS, scale=1.0 / D), reads=[f"sm{q}"], writes=[f"sm{q}"])
                    P.op("dve", lambda e, d_=d_: e.reciprocal(out=d_["rs2"][:], in_=d_["sd2"][:]), reads=[f"sm{q}"], writes=[f"sm{q}"])
                    P.op("dve", lambda e, xb_=xb_, xsb=xsb, d_=d_: e.tensor_scalar(out=xsb[:], in0=xb_[:], scalar1=d_["rs2"][:], scalar2=None, op0=ALU.mult),
                         reads=[f"xinb{i}", f"sm{q}", f"xsb2{q}"], writes=[f"xsb2{q}"])

                def TR(i):
                    tl = b * TB + i
                    tc = slice(tl * 128, (tl + 1) * 128)
                    q = i % 2
                    xsb, h2t = xsbL[q], h2tL[i]
                    bank, bk = ring()
                    pT = bank[:].bitcast(BF16)

                    def tr(e, pT=pT, xsb=xsb):
                        ins = None
                        for k in range(8):
                            ins = e.transpose(out=pT[:, k * 128:(k + 1) * 128], in_=xsb[:, k * 128:(k + 1) * 128], identity=ident[:])
                        return ins

                    P.op("pe", tr, reads=[f"xsb2{q}", "ident"], writes=[bk])
                    P.op("dve", lambda e, pT=pT, h2t=h2t: e.tensor_tensor(out=h2t[:], in0=pT.rearrange("p (k t) -> p k t", k=8), in1=g2bc[:], op=ALU.mult),
                         reads=[bk, "g2bc"], writes=[f"h2t{i}"])
                    P.dma("sp", lambda e, h2t=h2t, tc=tc: e.dma_start(out=h2_d[:, :, tc], in_=h2t[:]), reads=[f"h2t{i}"], writes=["h2_d"])

                def RM(i):
                    h2t = h2tL[i]
                    bank, bk = ring()
                    Lg = rsc[i % NRS]["Lg"]

                    def rmm(e, bank=bank, h2t=h2t):
                        for k in range(8):
                            e.matmul(bank[:, 0:36], lhsT=h2t[:, k, :], rhs=wrb[:, k, :], start=(k == 0), stop=False)
                        return e.matmul(bank[:, 0:36], lhsT=onesb[0:1, :], rhs=brrow[0:1, :], start=False, stop=True)

                    P.op("pe", rmm, reads=[f"h2t{i}", "wrb", "onesb", "brrow"], writes=[bk])
                    P.op("dve", lambda e, bank=bank, Lg=Lg: e.tensor_copy(out=Lg[:], in_=bank[:, 0:36]), reads=[bk, f"rt{i % NRS}"], writes=[f"rt{i % NRS}"])
                def drain(nsteps):
                    for _ in range(nsteps):
                        for g in list(pending):
                            try:
                                next(g)
                            except StopIteration:
                                pending.remove(g)

                for i in range(TB):
                    P.dma("sp", lambda e, i=i: e.dma_start(out=xinL[i][:], in_=x_d[(b * TB + i) * 128:(b * TB + i + 1) * 128, :]), writes=[f"xinb{i}"])
                for c in range(4):
                    U(c)
                for i in range(TB):
                    V(i)
                    for m in (2 * i, 2 * i + 1):
                        if m < 8:
                            g1(m)
                    if i >= 1:
                        SV(i - 1)
                for m in range(2 * TB, 8):
                    g1(m)
                SV(TB - 1)
                for m in range(8):
                    G2(m)
                    drain(4)
                for i in range(TB):
                    O(i)
                    if i >= 1:
                        TR(i - 1)
                    if i >= 2:
                        RM(i - 2)
                TR(TB - 1)
                if TB >= 2:
                    RM(TB - 2)
                RM(TB - 1)
                drain(1000)
                pending.extend(router_rest(b * TB + i) for i in range(TB))
            drain(1000)

            P.barrier()
        stP.close()

        with ExitStack() as st:
            P.scope = "router"
            h2 = sbuf(st, "h2", [128, 8, T], BF16)
            yacc = sbuf(st, "yacc", [128, NT, D], F32)
            fgrow = sbuf(st, "fgrow", [1, D], F32)
            fgbc = sbuf(st, "fgbc", [128, D], F32)
            P.dma("sp", lambda e: e.dma_start(out=h2[:], in_=h2_d), reads=["h2_d"], writes=["h2"])
            for tl in range(NT):
                P.dma("sp", lambda e, tl=tl: e.dma_start(out=yacc[:, tl, :], in_=x1_d[tl * 128:(tl + 1) * 128, :]), reads=["x1_d"], writes=[f"yacc{tl}"])
            P.dma("sp", lambda e: e.dma_start(out=fgrow[:], in_=fg_d), writes=["fgrow"])
            for hf in range(2):
                P.op("pe", lambda e, hf=hf: e.matmul(psM[0][:, 0:512], lhsT=onesf[0:1, :], rhs=fgrow[0:1, hf * 512:(hf + 1) * 512], start=True, stop=True),
                     reads=["onesf", "fgrow"], writes=["psM0"])
                P.op("act", lambda e, hf=hf: e.activation(out=fgbc[:, hf * 512:(hf + 1) * 512], in_=psM[0][:, 0:512], func=AF.Copy),
                     reads=["psM0", "fgbc"], writes=["fgbc"])

            P.scope = "experts"
            Wg = [sbuf(st, f"Wg{i}", [128, 8, 256], BF16) for i in range(2)]
            Wu = [sbuf(st, f"Wu{i}", [128, 8, 256], BF16) for i in range(2)]
            Wd = [sbuf(st, f"Wd{i}", [128, 2, D], BF16) for i in range(2)]
            GN_ = min(512, T)
            NG = T // GN_
            sg = [sbuf(st, f"sg{i}", [128, GN_], F32) for i in range(2)]
            actT = sbuf(st, "actT", [128, 2, GN_], BF16)
            gu_ps = [psM[0], psM[1], psS, psD[:, 0:512]]
            gu_k = ["psM0", "psM1", "psS", "psD0"]
            d_ps = [psA, psD[:, 512:1536]]
            d_k = [["psA", "psA2"], ["psD1", "psD2"]]
            actT2 = [actT, sbuf(st, "actTb", [128, 2, GN_], BF16)]
            TPG = GN_ // 128

            def load_expert(ex):
                b = ex % 2
                P.dma("pool", lambda e: e.dma_start(out=Wg[b][:], in_=weg_d[ex].rearrange("(k p) n -> p k n", p=128)), writes=[f"Wg{b}"])
                P.dma("pool", lambda e: e.dma_start(out=Wu[b][:], in_=weu_d[ex].rearrange("(k p) n -> p k n", p=128)), writes=[f"Wu{b}"])
                P.dma("pool", lambda e: e.dma_start(out=Wd[b][:], in_=wed_d[ex].rearrange("(k p) n -> p k n", p=128)), writes=[f"Wd{b}"])

            def G(u, f):
                ex, gi = divmod(u, NG)
                b = ex % 2
                gc = slice(gi * GN_, (gi + 1) * GN_)
                aT = actT2[u % 2]
                ak = f"actT{u % 2}_{f}"

                def gumm(e):
                    ins = None
                    for k in range(8):
                        e.matmul(gu_ps[2 * f][:, 0:GN_], lhsT=Wg[b][:, k, f * 128:(f + 1) * 128], rhs=h2[:, k, gc], start=(k == 0), stop=(k == 7))
                    for k in range(8):
                        ins = e.matmul(gu_ps[2 * f + 1][:, 0:GN_], lhsT=Wu[b][:, k, f * 128:(f + 1) * 128], rhs=h2[:, k, gc], start=(k == 0), stop=(k == 7))
                    return ins

                P.op("pe", gumm, reads=[f"Wg{b}", f"Wu{b}", "h2"], writes=[gu_k[2 * f], gu_k[2 * f + 1]])
                P.op("act", lambda e: e.activation(out=sg[f][:], in_=gu_ps[2 * f][:, 0:GN_], func=AF.Silu), reads=[gu_k[2 * f]], writes=[f"sg{f}"])
                P.op("dve", lambda e: e.tensor_tensor(out=aT[:, f, :], in0=gu_ps[2 * f + 1][:, 0:GN_], in1=sg[f][:], op=ALU.mult),
                     reads=[gu_k[2 * f + 1], f"sg{f}"], writes=[ak])

            dstate = [0]

            def Dn(u, ti):
                ex, gi = divmod(u, NG)
                b = ex % 2
                tl = gi * TPG + ti
                aT = actT2[u % 2]
                dps = d_ps[dstate[0] % 2]
                dk = d_k[dstate[0] % 2]
                dstate[0] += 1

                def dmm(e):
                    ins = None
                    for hf in range(2):
                        for f in range(2):
                            ins = e.matmul(dps[:, hf * 512:(hf + 1) * 512], lhsT=aT[:, f, ti * 128:(ti + 1) * 128],
                                           rhs=Wd[b][:, f, hf * 512:(hf + 1) * 512], start=(f == 0), stop=(f == 1))
                    return ins

                P.op("pe", dmm, reads=[f"actT{u % 2}_0", f"actT{u % 2}_1", f"Wd{b}"], writes=dk)
                P.op("dve", lambda e: e.scalar_tensor_tensor(out=yacc[:, tl, :], in0=dps[:, 0:1024], scalar=comb[:, tl, ex:ex + 1],
                                                             in1=yacc[:, tl, :], op0=ALU.mult, op1=ALU.add),
                     reads=dk + [f"comb{tl}", f"yacc{tl}"], writes=[f"yacc{tl}"])

            junk = sbuf(st, "junk3", [128, D], BF16)
            ss = sbuf(st, "ss3", [128, 1], F32)
            sd = sbuf(st, "sd3", [128, 1], F32)
            rs = sbuf(st, "rs3", [128, 1], F32)
            ob = [sbuf(st, f"ob{i}", [128, D], F32) for i in range(2)]

            def final_tile(tl):
                o = ob[tl % 2]
                ok = f"ob{tl % 2}"
                P.op("act", lambda e: e.activation(out=junk[:], in_=yacc[:, tl, :], func=AF.Square, accum_out=ss[:]),
                     reads=[f"yacc{tl}"], writes=["junk3", "ss3"])
                P.op("act", lambda e: e.activation(out=sd[:], in_=ss[:], func=AF.Sqrt, bias=NORM_EPS, scale=1.0 / D), reads=["ss3"], writes=["sd3"])
                P.op("dve", lambda e: e.reciprocal(out=rs[:], in_=sd[:]), reads=["sd3"], writes=["rs3"])
                P.op("dve", lambda e: e.scalar_tensor_tensor(out=o[:], in0=yacc[:, tl, :], scalar=rs[:], in1=fgbc[:], op0=ALU.mult, op1=ALU.mult),
                     reads=[f"yacc{tl}", "rs3", "fgbc"], writes=[ok])
                P.dma("sp", lambda e: e.dma_start(out=out_d[tl * 128:(tl + 1) * 128, :], in_=o[:]), reads=[ok], writes=["out"])

            NU = NE * NG
            load_expert(0)
            if NE > 1:
                load_expert(1)
            G(0, 0)
            G(0, 1)
            for u in range(NU):
                ex, gi = divmod(u, NG)
                P.scope = "experts" if ex != 5 else "ex5"
                half = (TPG + 1) // 2
                if u + 1 < NU:
                    G(u + 1, 0)
                for ti in range(0, half):
                    Dn(u, ti)
                    if ex == NE - 1:
                        final_tile(gi * TPG + ti)
                if u + 1 < NU:
                    G(u + 1, 1)
                for ti in range(half, TPG):
                    Dn(u, ti)
                    if ex == NE - 1:
                        final_tile(gi * TPG + ti)
                if gi == NG - 1 and ex + 2 < NE:
                    load_expert(ex + 2)

            P.final_wait("sp")
            P.emit()
    return nc


def host_layout(inp, b, T):
    f = lambda a: np.ascontiguousarray(np.asarray(a, dtype=np.float32))

    def colpack(v, n):
        v = np.asarray(v, np.float32).reshape(-1)
        pad = np.zeros(n * 128, np.float32)
        pad[:v.size] = v
        return pad.reshape(n, 128).T

    cols = np.concatenate([
        colpack(inp["tmix_mu"][0], 15), colpack(inp["k_k"][0], 4), colpack(inp["k_a"][0], 4), colpack(inp["r_k"][0], 4),
        colpack(inp["a0"][0], 4), colpack(inp["lnx_g"][0], 4), colpack(inp["lnx_b"][0], 4), colpack(inp["b_gate"][0], 16),
        colpack(inp["norm1_g"][0], 8), colpack(inp["norm2_g"][0], 8)], axis=1)
    w_re = np.asarray(inp["w_re"][0], np.float32)
    w_r = np.concatenate([np.asarray(inp["w_rg"][0], np.float32), w_re.transpose(1, 0, 2).reshape(D, 32)], axis=1)
    b_r = np.concatenate([np.asarray(inp["b_rg"][0], np.float32).reshape(-1), np.asarray(inp["b_re"][0], np.float32).reshape(-1)])
    return {
        "x": f(inp["x"][b, :T]), "w_in": f(inp["w_in"][0]), "cols": f(cols), "w0": f(inp["w0"][0]).reshape(1, 512),
        "w2": f(inp["w2"][0]), "a2": f(inp["a2"][0]), "g2": f(inp["g2"][0]), "w_oA": f(inp["w_oA"][0]), "w_oB": f(inp["w_oB"][0]),
        "w_out": f(inp["w_out"][0]), "lnv_g": f(inp["lnv_g"][0]).reshape(1, 512), "lnv_b": f(inp["lnv_b"][0]).reshape(1, 512),
        "wsT": f(np.asarray(inp["w_s"][0], np.float32).transpose(0, 2, 1)), "b_s": f(inp["b_s"][0]).reshape(1, 512),
        "w_r": f(w_r), "b_r": f(b_r).reshape(1, 36), "w_e_gate": f(inp["w_e_gate"][0]), "w_e_up": f(inp["w_e_up"][0]),
        "w_e_down": f(inp["w_e_down"][0]), "final_g": f(inp["final_g"]).reshape(1, D),
    }


def kernel(**inputs):
    T = 2048
    nc = build(T)
    in_maps = [host_layout(inputs, b, T) for b in range(8)]
    res = run_bass_kernel_spmd(nc, in_maps, core_ids=list(range(8)))
    return np.stack([np.asarray(r["out"], dtype=np.float32) for r in res.results], axis=0)
```

```python
import re
import numpy as np
from contextlib import ExitStack
import concourse.bass as bass
import concourse.mybir as mybir
from concourse.bass_utils import run_bass_kernel_spmd

F32 = mybir.dt.float32
BF16 = mybir.dt.bfloat16
AF = mybir.ActivationFunctionType
ALU = mybir.AluOpType
AX = mybir.AxisListType
ENGS = ["pe", "dve", "act", "pool", "sp"]
DMA_RING = {"sp": 6, "pool": 4}

D = 1024
IN_COLS = 4896
A_COLS = 1824
NCHA = 15
LNX_EPS = 64e-5
LN_EPS = 1e-5
NORM_EPS = 1e-6
EXPM05 = float(np.exp(-0.5))
PROFILE_SCOPES = False

C_MU, C_KK, C_KA, C_RK, C_A0, C_LG, C_LB, C_BG, C_G1, C_G2 = 0, 15, 19, 23, 27, 31, 35, 39, 55, 63
NCOL = 71


class Rec:
    def __init__(self):
        self.calls = []

    def __getattr__(self, name):
        def f(*a, **k):
            self.calls.append((name, a, k))
            return self
        return f


def _record(fn):
    r = Rec()
    fn(r)
    assert r.calls
    return r.calls


class Prog:
    def __init__(self, nc, stack):
        self.nc = nc
        self.lists = {e: [] for e in ENGS}
        self.count = {e: 0 for e in ENGS}
        self.sems = {}
        for e in ENGS:
            self.sems[("e", e)] = stack.enter_context(nc.semaphore(f"s_{e}"))
        for q, r in DMA_RING.items():
            for i in range(r):
                self.sems[("d", q, i)] = stack.enter_context(nc.semaphore(f"d_{q}{i}"))
        self.dma_n = {q: 0 for q in DMA_RING}
        self.waited = {e: {} for e in ENGS}
        self.last_w = {}
        self.readers = {}
        self.scope = None

    def _waits(self, eng, reads, writes, extra=()):
        need = {}

        def add(s, v):
            if need.get(s, 0) < v:
                need[s] = v

        for k in reads:
            if k in self.last_w:
                add(*self.last_w[k])
        for k in writes:
            if k in self.last_w:
                add(*self.last_w[k])
            for s, v in self.readers.get(k, {}).items():
                add(s, v)
        for s, v in extra:
            add(s, v)
        out = []
        wd = self.waited[eng]
        for s, v in need.items():
            if wd.get(s, 0) < v:
                wd[s] = v
                out.append((s, v))
        return out

    def _commit(self, tok, reads, writes):
        for k in writes:
            self.last_w[k] = tok
            self.readers[k] = {}
        for k in reads:
            d = self.readers.setdefault(k, {})
            if d.get(tok[0], 0) < tok[1]:
                d[tok[0]] = tok[1]

    LIMIT = None
    NOPS = 0

    def op(self, eng, fn, reads=(), writes=()):
        Prog.NOPS += 1
        if Prog.LIMIT is not None and Prog.NOPS > Prog.LIMIT:
            return None
        writes = list(writes) + [k for k in reads if k.startswith("ps")]
        waits = self._waits(eng, reads, writes)
        self.count[eng] += 1
        tok = (("e", eng), self.count[eng])
        self.lists[eng].append((waits, _record(fn), (("e", eng), 1), self.scope))
        self._commit(tok, reads, writes)
        return tok

    def dma(self, q, fn, reads=(), writes=()):
        Prog.NOPS += 1
        if Prog.LIMIT is not None and Prog.NOPS > Prog.LIMIT:
            return None
        r = DMA_RING[q]
        i = self.dma_n[q]
        self.dma_n[q] += 1
        slot = i % r
        skey = ("d", q, slot)
        extra = [(skey, 16 * (i // r))] if i >= r else []
        waits = self._waits(q, reads, writes, extra)
        tok = (skey, 16 * (i // r + 1))
        self.lists[q].append((waits, _record(fn), (skey, 16), self.scope))
        self._commit(tok, reads, writes)
        return tok

    def _all_tokens(self):
        toks = []
        for q, r in DMA_RING.items():
            n = self.dma_n[q]
            for slot in range(min(r, n)):
                toks.append((("d", q, slot), 16 * ((n - 1 - slot) // r + 1)))
        for e in ENGS:
            if self.count[e]:
                toks.append((("e", e), self.count[e]))
        return toks

    def barrier(self):
        toks = self._all_tokens()
        for e in ENGS:
            waits = self._waits(e, (), (), toks)
            if waits:
                self.lists[e].append((waits, None, None, None))

    def final_wait(self, eng):
        waits = self._waits(eng, (), (), self._all_tokens())
        self.lists[eng].append((waits, None, None, None))

    def emit(self):
        engmap = {"pe": "tensor", "dve": "vector", "act": "scalar", "pool": "gpsimd", "sp": "sync"}
        with self.nc.Block() as block:
            for e in ENGS:
                lst = self.lists[e]
                if not lst:
                    continue

                def body(engine, lst=lst):
                    cur, cm = None, None
                    for waits, fn, inc, scope in lst:
                        if PROFILE_SCOPES and scope != cur:
                            if cm is not None:
                                cm.__exit__(None, None, None)
                                cm = None
                            if scope is not None:
                                cm = self.nc.named_scope(scope)
                                cm.__enter__()
                            cur = scope
                        for s, v in waits:
                            engine.wait_ge(self.sems[s], v)
                        if fn is not None:
                            ins = None
                            for name, a, k in fn:
                                ins = getattr(engine, name)(*a, **k)
                            ins.then_inc(self.sems[inc[0]], inc[1])
                    if cm is not None:
                        cm.__exit__(None, None, None)

                getattr(block, engmap[e])(body)


def build(T, NE=32, dbg=None):
    NT = T // 128
    nc = bass.Bass("TRN2", target_bir_lowering=False)

    def din(name, shape, dt=F32):
        return nc.dram_tensor(name, list(shape), dt, kind="ExternalInput").ap()

    x_d = din("x", [T, D])
    w_in_d = din("w_in", [D, IN_COLS])
    cols_d = din("cols", [128, NCOL])
    w0_d = din("w0", [1, 512])
    w2_d = din("w2", [64, 512])
    a2_d = din("a2", [64, 512])
    g2_d = din("g2", [160, 512])
    woA_d = din("w_oA", [512, D])
    woB_d = din("w_oB", [512, D])
    wout_d = din("w_out", [D, D])
    lnvg_d = din("lnv_g", [1, 512])
    lnvb_d = din("lnv_b", [1, 512])
    wsT_d = din("wsT", [4, 128, 128])
    bs_d = din("b_s", [1, 512])
    wr_d = din("w_r", [D, 36])
    br_d = din("b_r", [1, 36])
    weg_d = din("w_e_gate", [32, D, 256])
    weu_d = din("w_e_up", [32, D, 256])
    wed_d = din("w_e_down", [32, 256, D])
    fg_d = din("final_g", [1, D])
    out_d = nc.dram_tensor("out", [T, D], F32, kind="ExternalOutput").ap()
    x1_d = nc.dram_tensor("x1_scr", [T, D], F32, kind="Internal").ap()
    h2_d = nc.dram_tensor("h2_scr", [128, 8, T], BF16, kind="Internal").ap()
    hT_d = nc.dram_tensor("hT_scr", [T // 128, 128, 1024], BF16, kind="Internal").ap()
    yg_d = nc.dram_tensor("yg_scr", [T // 128, 128, 512], BF16, kind="Internal").ap()
    dbg_out = {}
    if dbg:
        for name, shape in dbg.items():
            dbg_out[name] = nc.dram_tensor("dbg_" + name, list(shape), F32, kind="ExternalOutput").ap()

    with ExitStack() as st0:
        P = Prog(nc, st0)

        def sbuf(st, name, shape, dt):
            return st.enter_context(nc.sbuf_tensor("sb_" + name, list(shape), dt))

        psM = [st0.enter_context(nc.psum_tensor(f"psM{i}", [128, 512], F32)) for i in range(2)]
        psA = st0.enter_context(nc.psum_tensor("psA", [128, 1024], F32))
        psD = st0.enter_context(nc.psum_tensor("psD", [128, 1536], F32))
        psS = st0.enter_context(nc.psum_tensor("psS", [128, 512], F32))

        identf = sbuf(st0, "identf", [128, 128], F32)
        ident = sbuf(st0, "ident", [128, 128], BF16)
        mSU = sbuf(st0, "mSU", [128, 128], F32)
        mIU = sbuf(st0, "mIU", [128, 128], F32)
        maskA = sbuf(st0, "maskA", [128, 2, 512], F32)
        maskL = sbuf(st0, "maskL", [128, 2, 128], F32)
        BDf = sbuf(st0, "BDf", [128, 128], F32)
        BDb = sbuf(st0, "BDb", [128, 128], BF16)
        BD64 = sbuf(st0, "BD64", [128, 128], F32)
        Tri2 = sbuf(st0, "Tri2", [128, 256], F32)
        onesb = sbuf(st0, "onesb", [1, 128], BF16)
        onesf = sbuf(st0, "onesf", [128, 128], F32)
        cols = sbuf(st0, "cols", [128, NCOL], F32)
        omu = sbuf(st0, "omu", [128, NCHA], F32)
        oka = sbuf(st0, "oka", [128, 4], F32)
        g1bc = sbuf(st0, "g1bc", [128, 8, 128], F32)
        g2bc = sbuf(st0, "g2bc", [128, 8, 128], F32)

        def colap(c):
            return cols[:, c:c + 1]

        P.dma("sp", lambda e: e.dma_start(out=cols[:], in_=cols_d), writes=["cols"])
        P.op("pool", lambda e: e.memset(onesf[:], 1.0), writes=["onesf"])
        P.op("pool", lambda e: e.memset(identf[:], 0.0), writes=["identf"])
        P.op("pool", lambda e: e.affine_select(out=identf[:], in_=identf[:], pattern=[[-1, 128]], compare_op=ALU.not_equal,
                                               fill=1.0, base=0, channel_multiplier=1), reads=["identf"], writes=["identf"])
        P.op("dve", lambda e: e.tensor_copy(out=ident[:], in_=identf[:]), reads=["identf"], writes=["ident"])
        P.op("pool", lambda e: e.affine_select(out=mSU[:], in_=onesf[:], pattern=[[1, 128]], compare_op=ALU.is_gt,
                                               fill=0.0, base=0, channel_multiplier=-1), reads=["onesf"], writes=["mSU"])
        P.op("pool", lambda e: e.affine_select(out=mIU[:], in_=onesf[:], pattern=[[1, 128]], compare_op=ALU.is_ge,
                                               fill=0.0, base=0, channel_multiplier=-1), reads=["onesf"], writes=["mIU"])
        for h in range(2):
            for q in range(4):
                src = mSU if q % 2 == 0 else mIU
                P.op("dve", lambda e, h=h, q=q, src=src: e.tensor_copy(out=maskA[:, h, q * 128:(q + 1) * 128], in_=src[:]),
                     reads=["mSU", "mIU"], writes=["maskA"])
            P.op("pool", lambda e, h=h: e.affine_select(out=maskL[:, h, :], in_=onesf[:], pattern=[[-1, 128]], compare_op=ALU.is_gt,
                                                        fill=0.0, base=0, channel_multiplier=1), reads=["onesf"], writes=["maskL"])
        P.op("pool", lambda e: e.memset(BDf[:], 0.0), writes=["BDf"])
        P.op("pool", lambda e: e.memset(BDf[0:64, 0:64], 1.0), reads=["BDf"], writes=["BDf"])
        P.op("pool", lambda e: e.memset(BDf[64:128, 64:128], 1.0), reads=["BDf"], writes=["BDf"])
        P.op("dve", lambda e: e.tensor_copy(out=BDb[:], in_=BDf[:]), reads=["BDf"], writes=["BDb"])
        P.op("dve", lambda e: e.tensor_scalar(out=BD64[:], in0=BDf[:], scalar1=1.0 / 64.0, scalar2=None, op0=ALU.mult),
             reads=["BDf"], writes=["BD64"])
        P.op("dve", lambda e: e.tensor_scalar(out=Tri2[:, 0:128], in0=mIU[:], scalar1=EXPM05, scalar2=None, op0=ALU.mult),
             reads=["mIU"], writes=["Tri2"])
        P.op("dve", lambda e: e.tensor_scalar(out=Tri2[:, 128:256], in0=mSU[:], scalar1=EXPM05, scalar2=None, op0=ALU.mult),
             reads=["mSU", "Tri2"], writes=["Tri2"])
        P.op("dve", lambda e: e.tensor_copy(out=onesb[:], in_=onesf[0:1, :]), reads=["onesf"], writes=["onesb"])
        P.op("dve", lambda e: e.tensor_scalar(out=omu[:], in0=cols[:, C_MU:C_MU + NCHA], scalar1=-1.0, scalar2=1.0,
                                              op0=ALU.mult, op1=ALU.add), reads=["cols"], writes=["omu"])
        P.op("dve", lambda e: e.tensor_scalar(out=oka[:], in0=cols[:, C_KA:C_KA + 4], scalar1=-1.0, scalar2=1.0,
                                              op0=ALU.mult, op1=ALU.add), reads=["cols"], writes=["oka"])
        for k in range(8):
            P.op("dve", lambda e, k=k: e.tensor_scalar(out=g1bc[:, k, :], in0=onesf[:], scalar1=colap(C_G1 + k), scalar2=None,
                                                       op0=ALU.mult), reads=["cols", "onesf"], writes=["g1bc"])
            P.op("dve", lambda e, k=k: e.tensor_scalar(out=g2bc[:, k, :], in0=onesf[:], scalar1=colap(C_G2 + k), scalar2=None,
                                                       op0=ALU.mult), reads=["cols", "onesf"], writes=["g2bc"])

        def rms_to_T(st_keys, xin, xin_key, gbc, gbc_key, dst_ap, dst_key, tmp, tag):
            junk, ss, sd, rs, xsb = tmp
            P.op("act", lambda e: e.activation(out=junk[:], in_=xin, func=AF.Square, accum_out=ss[:]),
                 reads=[xin_key], writes=["junk" + tag, "ss" + tag])
            P.op("act", lambda e: e.activation(out=sd[:], in_=ss[:], func=AF.Sqrt, bias=NORM_EPS, scale=1.0 / D),
                 reads=["ss" + tag], writes=["sd" + tag])
            P.op("dve", lambda e: e.reciprocal(out=rs[:], in_=sd[:]), reads=["sd" + tag], writes=["rs" + tag])
            P.op("dve", lambda e: e.tensor_scalar(out=xsb[:], in0=xin, scalar1=rs[:], scalar2=None, op0=ALU.mult),
                 reads=[xin_key, "rs" + tag], writes=["xsb" + tag])
            pT = psM[0][:].bitcast(BF16)

            def tr(e):
                ins = None
                for k in range(8):
                    ins = e.transpose(out=pT[:, k * 128:(k + 1) * 128], in_=xsb[:, k * 128:(k + 1) * 128], identity=ident[:])
                return ins

            P.op("pe", tr, reads=["xsb" + tag, "ident"], writes=["psM0"])
            P.op("dve", lambda e: e.tensor_tensor(out=dst_ap, in0=pT.rearrange("p (k t) -> p k t", k=8), in1=gbc[:], op=ALU.mult),
                 reads=["psM0", gbc_key], writes=[dst_key])

        P.scope = "M1"
        stP = ExitStack()
        comb = sbuf(st0, "comb", [128, NT, 32], F32)
        woA = sbuf(stP, "woA", [128, 4, D], BF16)
        woB = sbuf(stP, "woB", [128, 4, D], BF16)
        wout = sbuf(stP, "wout", [128, 8, D], BF16)

        with ExitStack() as st:
            hTb = [sbuf(st, f"hTt{i}", [128, 8, 128], BF16) for i in range(2)]
            ygb = [sbuf(st, f"ygt{i}", [128, 4, 128], BF16) for i in range(2)]
            w_inA = sbuf(st, "w_inA", [128, 8, A_COLS], BF16)
            w2b = sbuf(st, "w2b", [64, 512], BF16)
            a2b = sbuf(st, "a2b", [128, 512], BF16)
            g2b = sbuf(st, "g2b", [128, 2, 512], BF16)
            w0row = sbuf(st, "w0row", [1, 512], BF16)
            for k in range(8):
                P.dma("pool", lambda e, k=k: e.dma_start(out=w_inA[:, k, :], in_=w_in_d[k * 128:(k + 1) * 128, 0:A_COLS]),
                      writes=["w_inA"])
            P.dma("pool", lambda e: e.dma_start(out=w2b[:], in_=w2_d), writes=["w2b"])
            P.dma("pool", lambda e: e.dma_start(out=a2b[64:128, :], in_=a2_d), writes=["a2b"])
            P.dma("pool", lambda e: e.dma_start(out=g2b[:, 0, :], in_=g2_d[0:128, :]), writes=["g2b"])
            P.dma("pool", lambda e: e.dma_start(out=g2b[0:32, 1, :], in_=g2_d[128:160, :]), reads=["g2b"], writes=["g2b"])
            P.dma("pool", lambda e: e.dma_start(out=w0row[:], in_=w0_d), writes=["w0row"])
            P.dma("pool", lambda e: e.dma_start(out=woA[:], in_=woA_d.rearrange("(k p) n -> p k n", p=128)), writes=["woA"])
            P.dma("pool", lambda e: e.dma_start(out=woB[:], in_=woB_d.rearrange("(k p) n -> p k n", p=128)), writes=["woB"])
            P.dma("pool", lambda e: e.dma_start(out=wout[:], in_=wout_d.rearrange("(k p) n -> p k n", p=128)), writes=["wout"])

            xin = [sbuf(st, f"xin{i}", [128, D], F32) for i in range(2)]
            junk = sbuf(st, "junk", [128, D], BF16)
            ss = sbuf(st, "ss", [128, 1], F32)
            sd = sbuf(st, "sd", [128, 1], F32)
            rs = sbuf(st, "rs", [128, 1], F32)
            xsb = sbuf(st, "xsb", [128, D], BF16)
            carry = sbuf(st, "carry", [128, NCHA], F32)
            ltmp = sbuf(st, "ltmp", [128, 129], F32)
            pmx = sbuf(st, "pmx", [128, 128], F32)
            pmxF = [sbuf(st, f"pmxF{j}", [128, 128], F32) for j in range(3)]
            ltF = [sbuf(st, f"ltF{j}", [128, 129], F32) for j in range(3)]
            txw2 = [sbuf(st, f"txw_{i}", [128, 128], BF16) for i in range(2)]
            sxg2 = [sbuf(st, f"sxg_{i}", [128, 2, 128], BF16) for i in range(2)]
            DC2 = [sbuf(st, f"DC_{i}", [128, 4], F32) for i in range(2)]
            etok = sbuf(st, "etok", [128, 512], F32)
            Dt2 = [sbuf(st, f"Dt_{i}", [128, 4, 128], F32) for i in range(2)]
            Dinv2 = [sbuf(st, f"Dinv_{i}", [128, 4, 128], F32) for i in range(2)]
            Dprev2 = [sbuf(st, f"Dprev_{i}", [128, 4, 128], F32) for i in range(2)]
            asig = [sbuf(st, f"asig{p}", [128, 128], F32) for p in range(4)]
            tA = [sbuf(st, f"tA{p}", [128, 128], F32) for p in range(4)]
            tB = [sbuf(st, f"tB{p}", [128, 128], F32) for p in range(4)]
            tC = [sbuf(st, f"tC{p}", [128, 128], F32) for p in range(4)]
            t16 = [sbuf(st, f"t16{p}", [128, 128], BF16) for p in range(4)]
            v16 = [sbuf(st, f"v16{p}", [128, 128], BF16) for p in range(4)]
            pmr = [sbuf(st, f"pmr{p}", [128, 128], F32) for p in range(4)]
            pmk = [sbuf(st, f"pmk{p}", [128, 128], F32) for p in range(4)]
            pmv = [sbuf(st, f"pmv{p}", [128, 128], F32) for p in range(4)]
            ltm = [sbuf(st, f"ltm{p}", [128, 129], F32) for p in range(4)]
            PX = [psM[0], psM[1], psA[:, 0:512], psA[:, 512:1024]]
            PXk = ["psM0", "psM1", "psA", "psA2"]
            PY = [psD[:, 0:512], psD[:, 512:1024], psD[:, 1024:1536], psS]
            PYk = ["psD0", "psD1", "psD2", "psS"]
            AR2 = [[sbuf(st, f"AR{p}_{i}", [128, 2, 128], BF16) for p in range(4)] for i in range(1)] * 2
            BT = [sbuf(st, f"BT{p}", [128, 128], BF16) for p in range(4)]
            KT = [sbuf(st, f"KT{p}", [128, 128], BF16) for p in range(4)]
            TOK2 = [[sbuf(st, f"TOK{p}_{i}", [128, 3, 128], BF16) for p in range(4)] for i in range(1)] * 2
            bon2 = [[sbuf(st, f"bon{p}_{i}", [128, 128], F32) for p in range(4)] for i in range(1)] * 2
            gT2 = [[sbuf(st, f"gT{p}_{i}", [128, 128], F32) for p in range(4)] for i in range(1)] * 2
            AM2 = [[sbuf(st, f"AM{p}_{i}", [128, 2, 512], BF16) for p in range(4)] for i in range(1)] * 2
            L0 = [sbuf(st, f"L0{p}", [128, 2, 128], BF16) for p in range(4)]
            MT2 = [[[sbuf(st, f"MT{p}_{i}_{j}", [128, 2, 256], BF16) for i in range(2)] for p in range(4)] for j in range(1)] * 2
            LK = [[sbuf(st, f"LK{p}_{i}", [128, 2, 128], BF16) for i in range(2)] for p in range(4)]
            Sw = [sbuf(st, f"Sw{p}", [128, 128], F32) for p in range(4)]
            Sb = [sbuf(st, f"Sb{p}", [128, 128], BF16) for p in range(4)]
            Xb = [sbuf(st, f"Xb{p}", [128, 128], BF16) for p in range(4)]
            Ub = [sbuf(st, f"Ub{p}", [128, 128], BF16) for p in range(4)]
            yT = sbuf(st, "yT", [128, 4, 128], F32)
            yc = sbuf(st, "yc", [128, 512], F32)
            ysq = sbuf(st, "ysq", [128, 512], F32)
            yrs = sbuf(st, "yrs", [128, 512], F32)
            y3p = [sbuf(st, f"y3_{p}", [128, 128], F32) for p in range(4)]
            tSp = [sbuf(st, f"tS_{p}", [128, 128], F32) for p in range(4)]

            P.op("pool", lambda e: e.memset(carry[:], 0.0), writes=[f"carry{c}" for c in range(NCHA)])
            for p in range(4):
                P.op("pool", lambda e, p=p: e.memset(Sw[p][:], 0.0), writes=[f"Sw{p}"])
                P.op("pool", lambda e, p=p: e.memset(Sb[p][:], 0.0), writes=[f"Sb{p}"])

            def LV(p):
                return psD[:, 1024:1280] if p % 2 == 0 else psS[:, 0:256]

            def DK(p):
                return ["psD0", "psD2"] if p % 2 == 0 else ["psD1", "psS"]

            serial_ps = [psS, psM[1], psA[:, 0:512], psA[:, 512:1024]]
            serial_key = ["psS", "psM1", "psA", "psA2"]

            def tile_parts(tl, P):
                par = tl % 2
                AM, AR, TOK, MT, bon, gT = AM2[par], AR2[par], TOK2[par], MT2[par], bon2[par], gT2[par]
                Dt, Dinv, Dprev, txw, sxg, DC = Dt2[par], Dinv2[par], Dprev2[par], txw2[par], sxg2[par], DC2[par]
                tc = slice(tl * 128, (tl + 1) * 128)
                xb_, xk = xin[tl % 2], f"xin{tl % 2}"
                hT, hTk = hTb[tl % 2], f"hTt{tl % 2}"
                ygT, ygk = ygb[tl % 2], f"ygt{tl % 2}"

                def inproj_chunk(c, dst_fn):
                    rows = 128 if c < 14 else 32
                    bank, bkey = (psD[:, 512:1024], "psD1") if c == 13 else (psM[0], "psM0")

                    def mmf(e, c=c, rows=rows, bank=bank):
                        ins = None
                        for k in range(8):
                            ins = e.matmul(bank[0:rows, 0:128], lhsT=w_inA[:, k, c * 128:c * 128 + rows], rhs=hT[:, k, :],
                                           start=(k == 0), stop=(k == 7))
                        return ins

                    P.op("pe", mmf, reads=["w_inA", hTk], writes=[bkey])
                    P.op("act", lambda e: e.activation(out=ltmp[0:rows, 1:129], in_=bank[0:rows, 0:128], func=AF.Copy,
                                                       scale=cols[0:rows, C_MU + c:C_MU + c + 1]),
                         reads=[bkey, "cols"], writes=["ltmp"])
                    P.op("pool", lambda e: e.tensor_copy(out=ltmp[0:rows, 0:1], in_=carry[0:rows, c:c + 1]),
                         reads=[f"carry{c}", "ltmp"], writes=["ltmp"])
                    P.op("pool", lambda e: e.tensor_copy(out=carry[0:rows, c:c + 1], in_=ltmp[0:rows, 128:129]),
                         reads=["ltmp", f"carry{c}"], writes=[f"carry{c}"])
                    dst, dkey = dst_fn
                    P.op("dve", lambda e: e.scalar_tensor_tensor(out=dst[0:rows, :], in0=bank[0:rows, 0:128],
                                                                 scalar=omu[0:rows, c:c + 1], in1=ltmp[0:rows, 0:128],
                                                                 op0=ALU.mult, op1=ALU.add),
                         reads=[bkey, "omu", "ltmp"], writes=[dkey])

                def inproj_chunk_g(c, dst, dkey, bank, bkey, lt, ltk):
                    rows = 128

                    def mmf(e):
                        ins = None
                        for k in range(8):
                            ins = e.matmul(bank[0:rows, 0:128], lhsT=w_inA[:, k, c * 128:c * 128 + rows], rhs=hT[:, k, :],
                                           start=(k == 0), stop=(k == 7))
                        return ins

                    P.op("pe", mmf, reads=["w_inA", hTk], writes=[bkey])
                    yield
                    P.op("act", lambda e: e.activation(out=lt[:, 1:129], in_=bank[:, 0:128], func=AF.Copy, scale=cols[:, C_MU + c:C_MU + c + 1]),
                         reads=[bkey, "cols"], writes=[ltk])
                    yield
                    P.op("pool", lambda e: e.tensor_copy(out=lt[:, 0:1], in_=carry[:, c:c + 1]), reads=[f"carry{c}", ltk], writes=[ltk])
                    P.op("pool", lambda e: e.tensor_copy(out=carry[:, c:c + 1], in_=lt[:, 128:129]), reads=[ltk, f"carry{c}"], writes=[f"carry{c}"])
                    yield
                    P.op("dve", lambda e: e.scalar_tensor_tensor(out=dst[:], in0=bank[:, 0:128], scalar=omu[:, c:c + 1], in1=lt[:, 0:128],
                                                                 op0=ALU.mult, op1=ALU.add), reads=[bkey, "omu", ltk], writes=[dkey])
                    yield


                def front():
                    xb_ = xin[tl % 2]
                    xk = f"xin{tl % 2}"
                    P.dma("sp", lambda e, xb_=xb_, tl=tl: e.dma_start(out=xb_[:], in_=x_d[tl * 128:(tl + 1) * 128, :]), writes=[xk])
                    yield
                    hT, hTk = hTb[tl % 2], f"hTt{tl % 2}"
                    ygT, ygk = ygb[tl % 2], f"ygt{tl % 2}"
                    rms_to_T(None, xb_[:], xk, g1bc, "g1bc", hT[:], hTk, (junk, ss, sd, rs, xsb), "1")
                    yield
                    P.dma("sp", lambda e, tl=tl, hT=hT: e.dma_start(out=hT_d[tl], in_=hT[:].rearrange("p k t -> p (k t)")), reads=[hTk], writes=["hT_d"])
                    yield

                    FC = [(12, 128, psM[0], "psM0"), (13, 128, psD[:, 512:1024], "psD1"), (14, 32, psD[:, 1024:1536], "psD2")]
                    for j, (c, rows, bank, bkey) in enumerate(FC):
                        def mmf(e, c=c, rows=rows, bank=bank):
                            ins = None
                            for k in range(8):
                                ins = e.matmul(bank[0:rows, 0:128], lhsT=w_inA[:, k, c * 128:c * 128 + rows], rhs=hT[:, k, :], start=(k == 0), stop=(k == 7))
                            return ins

                        P.op("pe", mmf, reads=["w_inA", hTk], writes=[bkey])
                    yield
                    for j, (c, rows, bank, bkey) in enumerate(FC):
                        P.op("act", lambda e, j=j, c=c, rows=rows, bank=bank: e.activation(out=ltF[j][0:rows, 1:129], in_=bank[0:rows, 0:128], func=AF.Copy,
                                                                                           scale=cols[0:rows, C_MU + c:C_MU + c + 1]),
                             reads=[bkey, "cols"], writes=[f"ltF{j}"])
                    yield
                    for j, (c, rows, bank, bkey) in enumerate(FC):
                        P.op("pool", lambda e, j=j, c=c, rows=rows: e.tensor_copy(out=ltF[j][0:rows, 0:1], in_=carry[0:rows, c:c + 1]),
                             reads=[f"carry{c}", f"ltF{j}"], writes=[f"ltF{j}"])
                        P.op("pool", lambda e, j=j, c=c, rows=rows: e.tensor_copy(out=carry[0:rows, c:c + 1], in_=ltF[j][0:rows, 128:129]),
                             reads=[f"ltF{j}", f"carry{c}"], writes=[f"carry{c}"])
                    yield
                    for j, (c, rows, bank, bkey) in enumerate(FC):
                        P.op("dve", lambda e, j=j, c=c, rows=rows, bank=bank: e.scalar_tensor_tensor(out=pmxF[j][0:rows, :], in0=bank[0:rows, 0:128],
                                                                                                    scalar=omu[0:rows, c:c + 1], in1=ltF[j][0:rows, 0:128],
                                                                                                    op0=ALU.mult, op1=ALU.add),
                             reads=[bkey, "omu", f"ltF{j}"], writes=[f"pmxF{j}"])
                    yield
                    P.op("act", lambda e: e.activation(out=txw[0:64, :], in_=pmxF[0][0:64, :], func=AF.Tanh), reads=["pmxF0"], writes=["txw"])
                    P.op("dve", lambda e: e.tensor_copy(out=txw[64:128, :], in_=pmxF[0][64:128, :]), reads=["pmxF0", "txw"], writes=["txw"])
                    P.op("act", lambda e: e.activation(out=sxg[:, 0, :], in_=pmxF[1][:], func=AF.Sigmoid), reads=["pmxF1"], writes=["sxg"])
                    P.op("act", lambda e: e.activation(out=sxg[0:32, 1, :], in_=pmxF[2][0:32, :], func=AF.Sigmoid), reads=["pmxF2", "sxg"], writes=["sxg"])
                    yield

                    def zmm(e):
                        e.matmul(psM[0][:, 0:512], lhsT=txw[0:64, :], rhs=w2b[0:64, :], start=True, stop=False)
                        return e.matmul(psM[0][:, 0:512], lhsT=onesb[0:1, :], rhs=w0row[0:1, :], start=False, stop=True)

                    P.op("pe", zmm, reads=["txw", "w2b", "onesb", "w0row"], writes=["psM0"])
                    yield
                    P.op("act", lambda e: e.activation(out=etok[:], in_=psM[0][:, 0:512], func=AF.Sigmoid), reads=["psM0"], writes=["etok"])
                    yield

                    def cmm(e):
                        ins = None
                        for p in range(4):
                            ins = e.matmul(psD[:, 512 + p * 256:512 + (p + 1) * 256], lhsT=etok[:, p * 128:(p + 1) * 128], rhs=Tri2[:],
                                           start=True, stop=True)
                        return ins

                    P.op("pe", cmm, reads=["etok", "Tri2"], writes=["psD1", "psD2"])
                    yield
                    cum = psD[:, 512:1536].rearrange("p (a c) -> p a c", a=4)
                    P.op("act", lambda e: e.activation(out=Dt[:], in_=cum[:, :, 0:128], func=AF.Exp, scale=-1.0),
                         reads=["psD1", "psD2"], writes=["Dt"])
                    yield
                    P.op("act", lambda e: e.activation(out=Dinv[:], in_=cum[:, :, 0:128], func=AF.Exp, scale=1.0),
                         reads=["psD1", "psD2"], writes=["Dinv"])
                    yield
                    P.op("act", lambda e: e.activation(out=Dprev[:], in_=cum[:, :, 128:256], func=AF.Exp, scale=-1.0),
                         reads=["psD1", "psD2"], writes=["Dprev"])
                    yield


                def pair_gen(p):
                    X, Xk, Y, Yk = PX[p], PXk[p], PY[p], PYk[p]
                    rf, kf, vf = pmr[p], pmk[p], pmv[p]
                    rk_, kk_, vk_ = f"pmr{p}", f"pmk{p}", f"pmv{p}"
                    as_, tA_, tB_, tC_, t16_, v16_ = asig[p], tA[p], tB[p], tC[p], t16[p], v16[p]
                    ak, tAk, tBk, tCk, t16k, v16k = f"asig{p}", f"tA{p}", f"tB{p}", f"tC{p}", f"t16{p}", f"v16{p}"
                    cs = slice(p * 128, (p + 1) * 128)
                    for (c, dst, dkey, bank, bkey) in ((p, rf, rk_, X, Xk), (4 + p, kf, kk_, Y, Yk), (8 + p, vf, vk_, X, Xk)):
                        yield from inproj_chunk_g(c, dst, dkey, bank, bkey, ltm[p], f"ltm{p}")
                    P.op("pe", lambda e: e.matmul(Y[:, 0:128], lhsT=a2b[64:128, cs], rhs=txw[64:128, :], start=True, stop=True),
                         reads=["a2b", "txw"], writes=[Yk])
                    yield
                    P.op("act", lambda e: e.activation(out=as_[:], in_=Y[:, 0:128], func=AF.Sigmoid, bias=colap(C_A0 + p)),
                         reads=[Yk, "cols"], writes=[ak])
                    yield

                    def gmm(e):
                        e.matmul(X[:, 0:128], lhsT=g2b[:, 0, cs], rhs=sxg[:, 0, :], start=True, stop=False)
                        return e.matmul(X[:, 0:128], lhsT=g2b[0:32, 1, cs], rhs=sxg[0:32, 1, :], start=False, stop=True)

                    P.op("pe", gmm, reads=["g2b", "sxg"], writes=[Xk])
                    yield
                    P.op("act", lambda e: e.activation(out=gT[p][:], in_=X[:, 0:128], func=AF.Copy), reads=[Xk], writes=[f"gT{p}"])
                    yield
                    P.op("dve", lambda e: e.tensor_scalar(out=tA_[:], in0=kf[:], scalar1=colap(C_KK + p), scalar2=None, op0=ALU.mult),
                         reads=[kk_, "cols"], writes=[tAk])
                    yield
                    P.op("pool", lambda e: e.tensor_tensor(out=t16_[:], in0=tA_[:], in1=tA_[:], op=ALU.mult), reads=[tAk], writes=[t16k])
                    yield
                    P.op("pe", lambda e: e.matmul(Y[:, 0:128], lhsT=BDb[:], rhs=t16_[:], start=True, stop=True), reads=["BDb", t16k], writes=[Yk])
                    yield
                    P.op("act", lambda e: e.activation(out=tB_[:], in_=Y[:, 0:128], func=AF.Sqrt), reads=[Yk], writes=[tBk])
                    yield
                    P.op("dve", lambda e: e.tensor_scalar(out=tB_[:], in0=tB_[:], scalar1=1e-12, scalar2=None, op0=ALU.max), reads=[tBk], writes=[tBk])
                    P.op("dve", lambda e: e.reciprocal(out=tB_[:], in_=tB_[:]), reads=[tBk], writes=[tBk])
                    yield
                    P.op("pool", lambda e: e.tensor_tensor(out=tA_[:], in0=tA_[:], in1=tB_[:], op=ALU.mult), reads=[tAk, tBk], writes=[tAk])
                    yield
                    P.op("dve", lambda e: e.tensor_scalar(out=tC_[:], in0=as_[:], scalar1=colap(C_KA + p), scalar2=oka[:, p:p + 1],
                                                          op0=ALU.mult, op1=ALU.add), reads=[ak, "cols", "oka"], writes=[tCk])
                    yield
                    P.op("pool", lambda e: e.tensor_tensor(out=kf[:], in0=kf[:], in1=tC_[:], op=ALU.mult), reads=[kk_, tCk], writes=[kk_])
                    yield
                    P.op("dve", lambda e: e.scalar_tensor_tensor(out=AR[p][:, 0, :], in0=tA_[:], scalar=-1.0, in1=Dprev[:, p, :],
                                                                 op0=ALU.mult, op1=ALU.mult), reads=[tAk, "Dprev"], writes=[f"AR{p}"])
                    P.op("pool", lambda e: e.tensor_tensor(out=AR[p][:, 1, :], in0=rf[:], in1=Dt[:, p, :], op=ALU.mult),
                         reads=[rk_, "Dt", f"AR{p}"], writes=[f"AR{p}"])
                    yield
                    P.op("pool", lambda e: e.tensor_tensor(out=tB_[:], in0=tA_[:], in1=as_[:], op=ALU.mult), reads=[tAk, ak, tBk], writes=[tBk])
                    yield
                    P.op("dve", lambda e: e.tensor_tensor(out=BT[p][:], in0=tB_[:], in1=Dinv[:, p, :], op=ALU.mult), reads=[tBk, "Dinv"], writes=[f"BT{p}"])
                    P.op("dve", lambda e: e.tensor_tensor(out=KT[p][:], in0=kf[:], in1=Dinv[:, p, :], op=ALU.mult), reads=[kk_, "Dinv"], writes=[f"KT{p}"])
                    yield
                    P.op("dve", lambda e: e.scalar_tensor_tensor(out=t16_[:], in0=rf[:], scalar=colap(C_RK + p), in1=kf[:],
                                                                 op0=ALU.mult, op1=ALU.mult), reads=[rk_, kk_, "cols", t16k], writes=[t16k])
                    yield
                    P.op("pe", lambda e: e.matmul(Y[:, 0:128], lhsT=BDb[:], rhs=t16_[:], start=True, stop=True), reads=["BDb", t16k], writes=[Yk])
                    yield
                    P.op("dve", lambda e: e.tensor_tensor(out=bon[p][:], in0=Y[:, 0:128], in1=vf[:], op=ALU.mult), reads=[Yk, vk_], writes=[f"bon{p}"])
                    P.op("dve", lambda e: e.tensor_scalar(out=bon[p][:], in0=bon[p][:], scalar1=colap(C_LB + p), scalar2=None, op0=ALU.add),
                         reads=[f"bon{p}", "cols"], writes=[f"bon{p}"])
                    yield
                    P.op("act", lambda e: e.activation(out=v16_[:], in_=vf[:], func=AF.Copy), reads=[vk_], writes=[v16k])
                    yield
                    pT = X[:, 0:256].bitcast(BF16)

                    def tr3(e):
                        e.transpose(out=pT[:, 0:128], in_=v16_[:], identity=ident[:])
                        e.transpose(out=pT[:, 128:256], in_=BT[p][:], identity=ident[:])
                        return e.transpose(out=pT[:, 256:384], in_=KT[p][:], identity=ident[:])

                    P.op("pe", tr3, reads=[v16k, f"BT{p}", f"KT{p}", "ident"], writes=[Xk])
                    yield
                    P.op("act", lambda e: e.activation(out=TOK[p][:].rearrange("p a t -> p (a t)"), in_=pT[:, 0:384], func=AF.Copy),
                         reads=[Xk], writes=[f"TOK{p}"])
                    yield

                    def amm(e):
                        ins = None
                        for h, bank in ((0, X), (1, Y)):
                            hr = slice(64 * h, 64 * h + 64)
                            e.matmul(bank[:, 0:256], lhsT=BT[p][hr, :], rhs=AR[p][hr, :, :].rearrange("p a t -> p (a t)"), start=True, stop=True)
                            ins = e.matmul(bank[:, 256:512], lhsT=KT[p][hr, :], rhs=AR[p][hr, :, :].rearrange("p a t -> p (a t)"),
                                           start=True, stop=True)
                        return ins

                    P.op("pe", amm, reads=[f"BT{p}", f"KT{p}", f"AR{p}"], writes=[Xk, Yk])
                    yield
                    P.op("dve", lambda e: e.tensor_tensor(out=AM[p][:, 0, :], in0=X[:, 0:512], in1=maskA[:, 0, :], op=ALU.mult),
                         reads=[Xk, "maskA"], writes=[f"AM{p}"])
                    P.op("dve", lambda e: e.tensor_tensor(out=AM[p][:, 1, :], in0=Y[:, 0:512], in1=maskA[:, 1, :], op=ALU.mult),
                         reads=[Yk, "maskA", f"AM{p}"], writes=[f"AM{p}"])
                    yield
                    pTL = Y[:, 0:128].bitcast(BF16)

                    def lmm(e):
                        e.transpose(out=pTL[:, 0:128], in_=AM[p][:, 0, 0:128], identity=ident[:])
                        return e.transpose(out=pTL[:, 128:256], in_=AM[p][:, 1, 0:128], identity=ident[:])

                    P.op("pe", lmm, reads=[f"AM{p}", "ident"], writes=[Yk])
                    for h in range(2):
                        P.op("pool", lambda e, h=h: e.tensor_tensor(out=MT[p][0][:, h, 128:256], in0=AM[p][:, h, 0:128], in1=ident[:], op=ALU.add),
                             reads=[f"AM{p}", "ident", f"MT{p}_0"], writes=[f"MT{p}_0"])
                    yield
                    P.op("dve", lambda e: e.tensor_copy(out=L0[p][:].rearrange("p h c -> p (h c)"), in_=pTL), reads=[Yk], writes=[f"L0{p}"])
                    yield
                    Xm = X[:, 0:512].rearrange("p (h c) -> p h c", h=2)
                    Yl = Y[:, 0:256].rearrange("p (h c) -> p h c", h=2)

                    def r1(e):
                        ins = None
                        for h in range(2):
                            e.matmul(X[:, h * 256:h * 256 + 128], lhsT=L0[p][:, h, :], rhs=AM[p][:, h, 0:128], start=True, stop=True)
                            ins = e.matmul(Y[:, h * 128:(h + 1) * 128], lhsT=AM[p][:, h, 0:128], rhs=L0[p][:, h, :], start=True, stop=True)
                        return ins

                    P.op("pe", r1, reads=[f"L0{p}", f"AM{p}"], writes=[Xk, Yk])
                    yield
                    P.op("act", lambda e: e.activation(out=MT[p][0][:, :, 0:128], in_=Xm[:, :, 0:128], func=AF.Copy),
                         reads=[Xk, f"MT{p}_0"], writes=[f"MT{p}_0"])
                    P.op("dve", lambda e: e.tensor_copy(out=LK[p][0][:], in_=Yl), reads=[Yk], writes=[f"LK{p}_0"])
                    yield
                    for rnd in range(2, 8):
                        src = rnd % 2
                        dst = 1 - src
                        last = (rnd == 7)
                        mts, mtd, lks, lkd = MT[p][src], MT[p][dst], LK[p][src], LK[p][dst]

                        def rk(e, mts=mts, lks=lks, last=last):
                            ins = None
                            for h in range(2):
                                if last:
                                    e.matmul(X[:, h * 256 + 128:h * 256 + 256], lhsT=lks[:, h, :], rhs=mts[:, h, 128:256], start=True, stop=False)
                                    ins = e.matmul(X[:, h * 256 + 128:h * 256 + 256], lhsT=ident[:], rhs=mts[:, h, 128:256], start=False, stop=True)
                                else:
                                    e.matmul(X[:, h * 256:h * 256 + 256], lhsT=lks[:, h, :], rhs=mts[:, h, :], start=True, stop=False)
                                    e.matmul(X[:, h * 256 + 128:h * 256 + 256], lhsT=ident[:], rhs=mts[:, h, 128:256], start=False, stop=True)
                            if not last:
                                for h in range(2):
                                    ins = e.matmul(Y[:, h * 128:(h + 1) * 128], lhsT=mts[:, h, 0:128], rhs=lks[:, h, :], start=True, stop=True)
                            return ins

                        P.op("pe", rk, reads=[f"MT{p}_{src}", f"LK{p}_{src}", "ident"], writes=[Xk] if last else [Xk, Yk])
                        yield
                        if last:
                            P.op("act", lambda e, mtd=mtd: e.activation(out=mtd[:, :, 128:256], in_=Xm[:, :, 128:256], func=AF.Copy),
                                 reads=[Xk, f"MT{p}_{dst}"], writes=[f"MT{p}_{dst}"])
                        else:
                            P.op("act", lambda e, mtd=mtd: e.activation(out=mtd[:], in_=Xm, func=AF.Copy), reads=[Xk], writes=[f"MT{p}_{dst}"])
                            P.op("dve", lambda e, lkd=lkd: e.tensor_copy(out=lkd[:], in_=Yl), reads=[Yk], writes=[f"LK{p}_{dst}"])
                        yield
                    P.op("pool", lambda e: e.tensor_copy(out=DC[:, p:p + 1], in_=Dt[:, p, 127:128]), reads=["Dt", "DC"], writes=["DC"])
                    yield


                def tail_pair(p):
                    bank, bk = serial_ps[p], serial_key[p]
                    yTp, ycp, ysp, yrp = yT[:, p, :], yc[:, p * 128:(p + 1) * 128], ysq[:, p * 128:(p + 1) * 128], yrs[:, p * 128:(p + 1) * 128]

                    def xmm(e):
                        e.matmul(bank[:, 0:64], lhsT=AM[p][:, 0, 256:384], rhs=TOK[p][:, 0, 0:64], start=True, stop=False)
                        e.matmul(bank[:, 64:128], lhsT=AM[p][:, 1, 256:384], rhs=TOK[p][:, 0, 64:128], start=False, stop=False)
                        return e.matmul(bank[:, 0:128], lhsT=AR[p][:, 0, :], rhs=Sb[p][:], start=False, stop=True)

                    P.op("pe", xmm, reads=[f"AM{p}", f"TOK{p}", f"AR{p}", f"Sb{p}"], writes=[bk])
                    yield
                    P.op("act", lambda e: e.activation(out=Xb[p][:], in_=bank[:, 0:128], func=AF.Copy), reads=[bk], writes=[f"Xb{p}"])
                    yield

                    def umm(e):
                        e.matmul(bank[:, 128:192], lhsT=MT[p][0][:, 0, 128:256], rhs=Xb[p][:, 0:64], start=True, stop=True)
                        return e.matmul(bank[:, 192:256], lhsT=MT[p][0][:, 1, 128:256], rhs=Xb[p][:, 64:128], start=True, stop=True)

                    P.op("pe", umm, reads=[f"MT{p}_0", f"Xb{p}"], writes=[bk])
                    yield
                    P.op("dve", lambda e: e.tensor_copy(out=Ub[p][:], in_=bank[:, 128:256]), reads=[bk], writes=[f"Ub{p}"])
                    yield

                    def ymm(e):
                        e.matmul(bank[:, 256:384], lhsT=Sb[p][:], rhs=AR[p][:, 1, :], start=True, stop=False)
                        ins = None
                        for h in range(2):
                            hs = slice(64 * h, 64 * h + 64)
                            e.matmul(bank[hs, 256:384], lhsT=Ub[p][:, hs], rhs=AM[p][:, h, 128:256], start=False, stop=False)
                            ins = e.matmul(bank[hs, 256:384], lhsT=TOK[p][:, 0, hs], rhs=AM[p][:, h, 384:512], start=False, stop=True)
                        return ins

                    P.op("pe", ymm, reads=[f"Sb{p}", f"AR{p}", f"Ub{p}", f"AM{p}", f"TOK{p}"], writes=[bk])

                    def smm(e):
                        e.matmul(bank[:, 384:512], lhsT=TOK[p][:, 2, :], rhs=TOK[p][:, 0, :], start=True, stop=False)
                        return e.matmul(bank[:, 384:512], lhsT=TOK[p][:, 1, :], rhs=Ub[p][:], start=False, stop=True)

                    P.op("pe", smm, reads=[f"TOK{p}", f"Ub{p}"], writes=[bk])
                    yield
                    P.op("act", lambda e: e.activation(out=yTp, in_=bank[:, 256:384], func=AF.Copy), reads=[bk], writes=[f"yT{p}"])
                    P.op("dve", lambda e: e.tensor_tensor(out=tSp[p][:], in0=bank[:, 384:512], in1=Sw[p][:], op=ALU.add),
                         reads=[bk, f"Sw{p}"], writes=[f"tS{p}"])
                    yield
                    P.op("dve", lambda e: e.scalar_tensor_tensor(out=Sw[p][:], in0=tSp[p][:], scalar=DC[:, p:p + 1], in1=BDf[:], op0=ALU.mult, op1=ALU.mult),
                         reads=[f"tS{p}", f"Sw{p}", "DC", "BDf"], writes=[f"Sw{p}"])
                    P.op("pe", lambda e: e.matmul(bank[:, 0:128], lhsT=BD64[:], rhs=yTp, start=True, stop=True), reads=["BD64", f"yT{p}"], writes=[bk])
                    yield
                    P.op("act", lambda e: e.activation(out=Sb[p][:], in_=Sw[p][:], func=AF.Copy), reads=[f"Sw{p}"], writes=[f"Sb{p}"])
                    P.op("dve", lambda e: e.tensor_tensor(out=ycp, in0=yTp, in1=bank[:, 0:128], op=ALU.subtract), reads=[f"yT{p}", bk], writes=[f"yc{p}"])
                    yield
                    P.op("act", lambda e: e.activation(out=ysp, in_=ycp, func=AF.Square), reads=[f"yc{p}"], writes=[f"ysq{p}"])
                    yield
                    P.op("pe", lambda e: e.matmul(bank[:, 0:128], lhsT=BD64[:], rhs=ysp, start=True, stop=True), reads=["BD64", f"ysq{p}"], writes=[bk])
                    yield
                    P.op("act", lambda e: e.activation(out=yrp, in_=bank[:, 0:128], func=AF.Sqrt, bias=LNX_EPS), reads=[bk], writes=[f"yrs{p}"])
                    yield
                    P.op("dve", lambda e: e.reciprocal(out=yrp, in_=yrp), reads=[f"yrs{p}"], writes=[f"yrs{p}"])
                    yield
                    P.op("pool", lambda e: e.tensor_tensor(out=ycp, in0=ycp, in1=yrp, op=ALU.mult), reads=[f"yc{p}", f"yrs{p}"], writes=[f"yc{p}"])
                    yield
                    P.op("dve", lambda e: e.scalar_tensor_tensor(out=y3p[p][:], in0=ycp, scalar=colap(C_LG + p), in1=bon[p][:], op0=ALU.mult, op1=ALU.add),
                         reads=[f"yc{p}", "cols", f"bon{p}"], writes=[f"y3{p}"])
                    yield
                    P.op("pool", lambda e: e.tensor_tensor(out=ygT[:, p, :], in0=y3p[p][:], in1=gT[p][:], op=ALU.mult),
                         reads=[f"y3{p}", f"gT{p}"], writes=[f"{ygk}_{p}"])
                    yield

                def tail():
                    gens = [tail_pair(p) for p in range(4)]
                    while gens:
                        for g in list(gens):
                            try:
                                next(g)
                            except StopIteration:
                                gens.remove(g)
                        yield
                    P.dma("sp", lambda e: e.dma_start(out=yg_d[tl], in_=ygT[:].rearrange("p a t -> p (a t)")),
                          reads=[f"{ygk}_{p}" for p in range(4)], writes=["yg_d"] + [f"{ygk}_{p}" for p in range(4)])
                    yield

                return front, pair_gen, tail

            class KeyProxy:
                PAT = re.compile(r'^(AM|AR|TOK|MT|bon|gT)\d')

                def __init__(self, par):
                    self.par = par

                def km(self, k):
                    if k in ('Dt', 'Dinv', 'Dprev', 'txw', 'sxg', 'DC'):
                        return f'{k}@{self.par}'
                    return k

                def op(self, eng, fn, reads=(), writes=()):
                    return P.op(eng, fn, [self.km(k) for k in reads], [self.km(k) for k in writes])

                def dma(self, q, fn, reads=(), writes=()):
                    return P.dma(q, fn, [self.km(k) for k in reads], [self.km(k) for k in writes])

            def run_rr(gens):
                gens = list(gens)
                while gens:
                    for g in list(gens):
                        try:
                            next(g)
                        except StopIteration:
                            gens.remove(g)

            parts = [tile_parts(tl, KeyProxy(tl % 2)) for tl in range(NT)]
            run_rr([parts[0][0]()])
            for tl in range(NT):
                run_rr([parts[tl][1](p) for p in range(4)])
                gens = [parts[tl][2]()]
                if tl + 1 < NT:
                    gens.append(parts[tl + 1][0]())
                run_rr(gens)

            P.barrier()

        with ExitStack() as st:
            P.scope = "M2"
            w_inB = sbuf(st, "w_inB", [128, 8, 3072], BF16)
            wsTf = sbuf(st, "wsTf", [128, 4, 128], F32)
            wsTb = sbuf(st, "wsTb", [128, 4, 128], BF16)
            bsrow = sbuf(st, "bsrow", [1, 512], BF16)
            lrow = sbuf(st, "lrow", [1, 2, 512], F32)
            lnvg = sbuf(st, "lnvg", [128, 512], F32)
            lnvb = sbuf(st, "lnvb", [128, 512], F32)
            for j in range(3):
                for k in range(8):
                    P.dma("pool", lambda e, k=k, j=j: e.dma_start(out=w_inB[:, k, j * 1024:(j + 1) * 1024],
                                                                  in_=w_in_d[k * 128:(k + 1) * 128, A_COLS + j * 1024:A_COLS + (j + 1) * 1024]),
                          writes=[f"w_inB{j}"])
            P.dma("sp", lambda e: e.dma_start(out=wsTf[:], in_=wsT_d.rearrange("g s t -> s g t")), writes=["wsTf"])
            P.dma("pool", lambda e: e.dma_start(out=bsrow[:], in_=bs_d), writes=["bsrow"])
            P.dma("sp", lambda e: e.dma_start(out=lrow[:, 0, :], in_=lnvg_d), writes=["lrow"])
            P.dma("sp", lambda e: e.dma_start(out=lrow[:, 1, :], in_=lnvb_d), reads=["lrow"], writes=["lrow"])
            for g in range(4):
                P.op("dve", lambda e, g=g: e.tensor_tensor(out=wsTb[:, g, :], in0=wsTf[:, g, :], in1=mIU[:], op=ALU.mult),
                     reads=["wsTf", "mIU", "wsTb"], writes=["wsTb"])
            for i, (dst, dk) in enumerate([(lnvg, "lnvg"), (lnvb, "lnvb")]):
                P.op("pe", lambda e, i=i: e.matmul(psM[0][:, 0:512], lhsT=onesf[0:1, :], rhs=lrow[0:1, i, :], start=True, stop=True),
                     reads=["onesf", "lrow"], writes=["psM0"])
                P.op("act", lambda e, dst=dst: e.activation(out=dst[:], in_=psM[0][:, 0:512], func=AF.Copy), reads=["psM0"], writes=[dk])

            TB = min(4, NT)
            BN = TB * 128
            NBLK = NT // TB
            hTB = [sbuf(st, f"hTB{i}", [128, 8, BN], BF16) for i in range(2)]
            ygB = [sbuf(st, f"ygB{i}", [128, 4, BN], BF16) for i in range(2)]
            uT = sbuf(st, "uTB", [128, 4, BN], BF16)
            mixT = sbuf(st, "mixTB", [128, 4, BN], BF16)
            t1 = sbuf(st, "t1B", [128, 8, BN], BF16)
            zT = sbuf(st, "zTB", [128, 8, BN], BF16)
            gsT = [sbuf(st, f"gsT{i}", [128, BN], BF16) for i in range(2)]
            t2T = [sbuf(st, f"t2T{i}", [128, BN], BF16) for i in range(2)]
            xinL = [sbuf(st, f"xinb{i}", [128, D], F32) for i in range(4)]
            vgL = [sbuf(st, f"vg{i}", [128, 512], F32) for i in range(2)]
            vlnL = [sbuf(st, f"vln{i}", [128, 512], BF16) for i in range(2)]
            xsbL = [sbuf(st, f"xsb2{i}", [128, D], BF16) for i in range(2)]
            h2tL = [sbuf(st, f"h2t{i}", [128, 8, 128], BF16) for i in range(4)]
            sm = [{n: sbuf(st, f"{n}{i}", [128, w], F32) for n, w in (("bst", 6), ("mv", 2), ("lsd", 1), ("lrs", 1), ("lnm", 1), ("ss2", 1), ("sd2", 1), ("rs2", 1))}
                  for i in range(2)]
            RING = [(psM[0], "psM0"), (psM[1], "psM1"), (psS, "psS"), (psD[:, 0:512], "psD0"), (psD[:, 512:1024], "psD1"), (psD[:, 1024:1536], "psD2"),
                    (psA[:, 0:512], "psA"), (psA[:, 512:1024], "psA2")]
            ring_i = [0]

            def ring():
                r = RING[ring_i[0] % len(RING)]
                ring_i[0] += 1
                return r

            wrb = sbuf(st, "wrb", [128, 8, 36], BF16)
            brrow = sbuf(st, "brrow", [1, 36], BF16)
            P.dma("pool", lambda e: e.dma_start(out=wrb[:], in_=wr_d.rearrange("(k p) n -> p k n", p=128)), writes=["wrb"])
            P.dma("pool", lambda e: e.dma_start(out=brrow[:], in_=br_d), writes=["brrow"])
            NRS = 4
            rsc = []
            for i in range(NRS):
                d = {"Lg": sbuf(st, f"Lg{i}", [128, 36], F32), "c": sbuf(st, f"rc{i}", [128, 10], F32), "ohg": sbuf(st, f"ohg{i}", [128, 4], F32),
                     "ex4": sbuf(st, f"ex4{i}", [128, 4], F32), "esel": sbuf(st, f"esel{i}", [128, 8], F32), "e2": sbuf(st, f"e2{i}", [128, 8], F32),
                     "mk1": sbuf(st, f"mk1{i}", [128, 8], F32), "mk2": sbuf(st, f"mk2{i}", [128, 8], F32), "wg8": sbuf(st, f"wg8{i}", [128, 8], F32)}
                rsc.append(d)

            def router_rest(tl):
                sl = tl % NRS
                d = rsc[sl]
                Lg, ohg, ex4, esel, e2, mk1, mk2, wg8 = d["Lg"], d["ohg"], d["ex4"], d["esel"], d["e2"], d["mk1"], d["mk2"], d["wg8"]
                cc = d["c"]
                gmax, ngmax, se, gp, m1, m2, dd, w1, w2 = [cc[:, i:i + 1] for i in range(9)]
                rk = f"rt{sl}"
                steps = [
                    ("dve", lambda e: e.tensor_reduce(out=gmax, in_=Lg[:, 0:4], axis=AX.X, op=ALU.max)),
                    ("dve", lambda e: e.tensor_scalar(out=ohg[:], in0=Lg[:, 0:4], scalar1=gmax, scalar2=None, op0=ALU.is_ge)),
                    ("dve", lambda e: e.tensor_scalar(out=ngmax, in0=gmax, scalar1=-1.0, scalar2=None, op0=ALU.mult)),
                    ("act", lambda e: e.activation(out=ex4[:], in_=Lg[:, 0:4], func=AF.Exp, bias=ngmax)),
                    ("dve", lambda e: e.tensor_reduce(out=se, in_=ex4[:], axis=AX.X, op=ALU.add)),
                    ("dve", lambda e: e.reciprocal(out=gp, in_=se)),
                    ("dve", lambda e: e.tensor_scalar(out=esel[:], in0=Lg[:, 4:12], scalar1=ohg[:, 0:1], scalar2=None, op0=ALU.mult)),
                ]
                for g in range(1, 4):
                    steps.append(("dve", lambda e, g=g: e.scalar_tensor_tensor(out=esel[:], in0=Lg[:, 4 + 8 * g:12 + 8 * g], scalar=ohg[:, g:g + 1],
                                                                               in1=esel[:], op0=ALU.mult, op1=ALU.add)))
                steps += [
                    ("dve", lambda e: e.tensor_reduce(out=m1, in_=esel[:], axis=AX.X, op=ALU.max)),
                    ("dve", lambda e: e.tensor_scalar(out=mk1[:], in0=esel[:], scalar1=m1, scalar2=None, op0=ALU.is_ge)),
                    ("dve", lambda e: e.scalar_tensor_tensor(out=e2[:], in0=mk1[:], scalar=-1e30, in1=esel[:], op0=ALU.mult, op1=ALU.add)),
                    ("dve", lambda e: e.tensor_reduce(out=m2, in_=e2[:], axis=AX.X, op=ALU.max)),
                    ("dve", lambda e: e.tensor_scalar(out=mk2[:], in0=e2[:], scalar1=m2, scalar2=None, op0=ALU.is_ge)),
                    ("dve", lambda e: e.tensor_tensor(out=dd, in0=m2, in1=m1, op=ALU.subtract)),
                    ("act", lambda e: e.activation(out=w2, in_=dd, func=AF.Sigmoid)),
                    ("act", lambda e: e.activation(out=w1, in_=dd, func=AF.Sigmoid, scale=-1.0)),
                    ("dve", lambda e: e.tensor_tensor(out=w1, in0=w1, in1=gp, op=ALU.mult)),
                    ("dve", lambda e: e.tensor_tensor(out=w2, in0=w2, in1=gp, op=ALU.mult)),
                    ("dve", lambda e: e.tensor_scalar(out=wg8[:], in0=mk1[:], scalar1=w1, scalar2=None, op0=ALU.mult)),
                    ("dve", lambda e: e.scalar_tensor_tensor(out=wg8[:], in0=mk2[:], scalar=w2, in1=wg8[:], op0=ALU.mult, op1=ALU.add)),
                ]
                for eng, fn in steps:
                    P.op(eng, fn, reads=[rk], writes=[rk])
                    yield
                for g in range(4):
                    P.op("dve", lambda e, g=g: e.tensor_scalar(out=comb[:, tl, g * 8:(g + 1) * 8], in0=wg8[:], scalar1=ohg[:, g:g + 1],
                                                               scalar2=None, op0=ALU.mult), reads=[rk, f"comb{tl}"], writes=[f"comb{tl}"])
                yield


            def run_rr2(gens):
                gens = list(gens)
                while gens:
                    for g in list(gens):
                        try:
                            next(g)
                        except StopIteration:
                            gens.remove(g)

            def load_block(b):
                hB, yB = hTB[b % 2], ygB[b % 2]
                for i in range(TB):
                    tl = b * TB + i
                    P.dma("sp", lambda e, tl=tl, i=i: e.dma_start(out=hB[:, :, i * 128:(i + 1) * 128], in_=hT_d[tl].rearrange("p (k t) -> p k t", k=8)),
                          reads=["hT_d"], writes=[f"hTB{b % 2}"])
                    P.dma("sp", lambda e, tl=tl, i=i: e.dma_start(out=yB[:, :, i * 128:(i + 1) * 128], in_=yg_d[tl].rearrange("p (a t) -> p a t", a=4)),
                          reads=["yg_d"], writes=[f"ygB{b % 2}"])

            load_block(0)
            pending = []
            for b in range(NBLK):
                hB, yB, hk, yk = hTB[b % 2], ygB[b % 2], f"hTB{b % 2}", f"ygB{b % 2}"
                if b + 1 < NBLK:
                    load_block(b + 1)

                def U(c):
                    bank, bk = ring()

                    def umm2(e, c=c, bank=bank):
                        ins = None
                        for k in range(8):
                            ins = e.matmul(bank[:, 0:BN], lhsT=w_inB[:, k, c * 128:(c + 1) * 128], rhs=hB[:, k, :], start=(k == 0), stop=(k == 7))
                        return ins

                    P.op("pe", umm2, reads=["w_inB0", hk], writes=[bk])
                    P.op("act", lambda e, c=c, bank=bank: e.activation(out=uT[:, c, :], in_=bank[:, 0:BN], func=AF.Gelu), reads=[bk, "uTB"], writes=["uTB"])

                def g1(m):
                    bank, bk = ring()
                    c0 = 1024 + m * 128

                    def gm(e):
                        ins = None
                        for k in range(8):
                            ins = e.matmul(bank[:, 0:BN], lhsT=w_inB[:, k, c0:c0 + 128], rhs=hB[:, k, :], start=(k == 0), stop=(k == 7))
                        return ins

                    gs = gsT[m % 2]
                    P.op("pe", gm, reads=["w_inB1", hk], writes=[bk])
                    P.op("act", lambda e: e.activation(out=gs[:], in_=bank[:, 0:BN], func=AF.Sigmoid, bias=colap(C_BG + m)), reads=[bk, "cols"], writes=[f"gsT{m % 2}"])
                    bank2, bk2 = ring()

                    def ym(e):
                        ins = None
                        for c in range(4):
                            ins = e.matmul(bank2[:, 0:BN], lhsT=woA[:, c, m * 128:(m + 1) * 128], rhs=yB[:, c, :], start=(c == 0), stop=(c == 3))
                        return ins

                    P.op("pe", ym, reads=["woA", yk], writes=[bk2])
                    P.op("dve", lambda e: e.tensor_tensor(out=t1[:, m, :], in0=bank2[:, 0:BN], in1=gs[:], op=ALU.mult), reads=[bk2, f"gsT{m % 2}", "t1B"], writes=["t1B"])

                def V(i):
                    tl = b * TB + i
                    ts_ = slice(i * 128, (i + 1) * 128)
                    vg, vln, d_ = vgL[i % 2], vlnL[i % 2], sm[i % 2]
                    q = i % 2
                    bank, bk = ring()

                    def vmm(e, bank=bank, ts_=ts_):
                        ins = None
                        for k in range(8):
                            ins = e.matmul(bank[:, 0:512], lhsT=hB[:, k, ts_], rhs=w_inB[:, k, 512:1024], start=(k == 0), stop=(k == 7))
                        return ins

                    P.op("pe", vmm, reads=["w_inB0", hk], writes=[bk])
                    P.op("act", lambda e, bank=bank, vg=vg: e.activation(out=vg[:], in_=bank[:, 0:512], func=AF.Gelu), reads=[bk], writes=[f"vg{q}"])
                    P.op("dve", lambda e, vg=vg, d_=d_: e.bn_stats(out=d_["bst"][:], in_=vg[:]), reads=[f"vg{q}"], writes=[f"sm{q}"])
                    P.op("dve", lambda e, d_=d_: e.bn_aggr(out=d_["mv"][:], in_=d_["bst"][:]), reads=[f"sm{q}"], writes=[f"sm{q}"])
                    P.op("act", lambda e, d_=d_: e.activation(out=d_["lsd"][:], in_=d_["mv"][:, 1:2], func=AF.Sqrt, bias=LN_EPS), reads=[f"sm{q}"], writes=[f"sm{q}"])
                    P.op("dve", lambda e, d_=d_: e.reciprocal(out=d_["lrs"][:], in_=d_["lsd"][:]), reads=[f"sm{q}"], writes=[f"sm{q}"])
                    P.op("dve", lambda e, d_=d_: e.tensor_scalar(out=d_["lnm"][:], in0=d_["mv"][:, 0:1], scalar1=-1.0, scalar2=d_["lrs"][:], op0=ALU.mult, op1=ALU.mult),
                         reads=[f"sm{q}"], writes=[f"sm{q}"])
                    P.op("dve", lambda e, vg=vg, d_=d_: e.tensor_scalar(out=vg[:], in0=vg[:], scalar1=d_["lrs"][:], scalar2=d_["lnm"][:], op0=ALU.mult, op1=ALU.add),
                         reads=[f"vg{q}", f"sm{q}"], writes=[f"vg{q}"])
                    P.op("pool", lambda e, vg=vg: e.tensor_tensor(out=vg[:], in0=vg[:], in1=lnvg[:], op=ALU.mult), reads=[f"vg{q}", "lnvg"], writes=[f"vg{q}"])
                    P.op("pool", lambda e, vg=vg, vln=vln: e.tensor_tensor(out=vln[:], in0=vg[:], in1=lnvb[:], op=ALU.add), reads=[f"vg{q}", "lnvb"], writes=[f"vln{q}"])

                def SV(i):
                    ts_ = slice(i * 128, (i + 1) * 128)
                    vln = vlnL[i % 2]
                    q = i % 2
                    bank, bk = ring()

                    def svmm(e, bank=bank, vln=vln):
                        ins = None
                        for g in range(4):
                            e.matmul(bank[:, g * 128:(g + 1) * 128], lhsT=vln[:, g * 128:(g + 1) * 128], rhs=wsTb[:, g, :], start=True, stop=False)
                            ins = e.matmul(bank[:, g * 128:(g + 1) * 128], lhsT=onesb[0:1, :], rhs=bsrow[0:1, g * 128:(g + 1) * 128], start=False, stop=True)
                        return ins

                    P.op("pe", svmm, reads=[f"vln{q}", "wsTb", "onesb", "bsrow"], writes=[bk])
                    P.op("dve", lambda e, bank=bank, ts_=ts_: e.tensor_tensor(out=mixT[:, :, ts_], in0=bank[:, 0:512].rearrange("p (g t) -> p g t", g=4),
                                                                           in1=uT[:, :, ts_], op=ALU.mult), reads=[bk, "uTB", "mixTB"], writes=["mixTB"])
                def G2(m):
                    bank, bk = ring()
                    c0 = 2048 + m * 128

                    def gm(e, bank=bank, c0=c0):
                        ins = None
                        for k in range(8):
                            ins = e.matmul(bank[:, 0:BN], lhsT=w_inB[:, k, c0:c0 + 128], rhs=hB[:, k, :], start=(k == 0), stop=(k == 7))
                        return ins

                    gs, t2 = gsT[m % 2], t2T[m % 2]
                    P.op("pe", gm, reads=["w_inB2", hk], writes=[bk])
                    P.op("act", lambda e, bank=bank, gs=gs, m=m: e.activation(out=gs[:], in_=bank[:, 0:BN], func=AF.Sigmoid, bias=colap(C_BG + 8 + m)),
                         reads=[bk, "cols"], writes=[f"gsT{m % 2}"])
                    bank2, bk2 = ring()

                    def ym(e, bank2=bank2, m=m):
                        ins = None
                        for c in range(4):
                            ins = e.matmul(bank2[:, 0:BN], lhsT=woB[:, c, m * 128:(m + 1) * 128], rhs=mixT[:, c, :], start=(c == 0), stop=(c == 3))
                        return ins

                    P.op("pe", ym, reads=["woB", "mixTB"], writes=[bk2])
                    P.op("dve", lambda e, bank2=bank2, gs=gs, t2=t2: e.tensor_tensor(out=t2[:], in0=bank2[:, 0:BN], in1=gs[:], op=ALU.mult),
                         reads=[bk2, f"gsT{m % 2}"], writes=[f"t2T{m % 2}"])
                    P.op("pool", lambda e, m=m, t2=t2: e.tensor_tensor(out=zT[:, m, :], in0=t1[:, m, :], in1=t2[:], op=ALU.add),
                         reads=["t1B", f"t2T{m % 2}", "zTB"], writes=["zTB"])

                def O(i):
                    tl = b * TB + i
                    ts_ = slice(i * 128, (i + 1) * 128)
                    q = i % 2
                    xb_, xsb, d_ = xinL[i], xsbL[q], sm[q]
                    for hf in range(2):
                        bank, bk = ring()

                        def omm(e, bank=bank, hf=hf, ts_=ts_):
                            ins = None
                            for m in range(8):
                                ins = e.matmul(bank[:, 0:512], lhsT=zT[:, m, ts_], rhs=wout[:, m, hf * 512:(hf + 1) * 512], start=(m == 0), stop=(m == 7))
                            return ins

                        P.op("pe", omm, reads=["zTB", "wout"], writes=[bk])
                        P.op("dve", lambda e, bank=bank, hf=hf, xb_=xb_: e.tensor_tensor(out=xb_[:, hf * 512:(hf + 1) * 512], in0=bank[:, 0:512],
                                                                                     in1=xb_[:, hf * 512:(hf + 1) * 512], op=ALU.add),
                             reads=[bk, f"xinb{i}"], writes=[f"xinb{i}"])
                    P.dma("sp", lambda e, xb_=xb_, tl=tl: e.dma_start(out=x1_d[tl * 128:(tl + 1) * 128, :], in_=xb_[:]), reads=[f"xinb{i}"], writes=["x1_d"])
                    P.op("act", lambda e, xb_=xb_, xsb=xsb, d_=d_: e.activation(out=xsb[:], in_=xb_[:], func=AF.Square, accum_out=d_["ss2"][:]),
                         reads=[f"xinb{i}"], writes=[f"xsb2{q}", f"sm{q}"])
                    P.op("act", lambda e, d_=d_: e.activation(out=d_["sd2"][:], in_=d_["ss2"][:], func=AF.Sqrt, bias=NORM_EPS, scale=1.0 / D), reads=[f"sm{q}"], writes=[f"sm{q}"])
                    P.op("dve", lambda e, d_=d_: e.reciprocal(out=d_["rs2"][:], in_=d_["sd2"][:]), reads=[f"sm{q}"], writes=[f"sm{q}"])
                    P.op("dve", lambda e, xb_=xb_, xsb=xsb, d_=d_: e.tensor_scalar(out=xsb[:], in0=xb_[:], scalar1=d_["rs2"][:], scalar2=None, op0=ALU.mult),
                         reads=[f"xinb{i}", f"sm{q}", f"xsb2{q}"], writes=[f"xsb2{q}"])

                def TR(i):
                    tl = b * TB + i
                    tc = slice(tl * 128, (tl + 1) * 128)
                    q = i % 2
                    xsb, h2t = xsbL[q], h2tL[i]
                    bank, bk = ring()
                    pT = bank[:].bitcast(BF16)

                    def tr(e, pT=pT, xsb=xsb):
                        ins = None
                        for k in range(8):
                            ins = e.transpose(out=pT[:, k * 128:(k + 1) * 128], in_=xsb[:, k * 128:(k + 1) * 128], identity=ident[:])
                        return ins

                    P.op("pe", tr, reads=[f"xsb2{q}", "ident"], writes=[bk])
                    P.op("dve", lambda e, pT=pT, h2t=h2t: e.tensor_tensor(out=h2t[:], in0=pT.rearrange("p (k t) -> p k t", k=8), in1=g2bc[:], op=ALU.mult),
                         reads=[bk, "g2bc"], writes=[f"h2t{i}"])
                    P.dma("sp", lambda e, h2t=h2t, tc=tc: e.dma_start(out=h2_d[:, :, tc], in_=h2t[:]), reads=[f"h2t{i}"], writes=["h2_d"])

                def RM(i):
                    h2t = h2tL[i]
                    bank, bk = ring()
                    Lg = rsc[i % NRS]["Lg"]

                    def rmm(e, bank=bank, h2t=h2t):
                        for k in range(8):
                            e.matmul(bank[:, 0:36], lhsT=h2t[:, k, :], rhs=wrb[:, k, :], start=(k == 0), stop=False)
                        return e.matmul(bank[:, 0:36], lhsT=onesb[0:1, :], rhs=brrow[0:1, :], start=False, stop=True)

                    P.op("pe", rmm, reads=[f"h2t{i}", "wrb", "onesb", "brrow"], writes=[bk])
                    P.op("dve", lambda e, bank=bank, Lg=Lg: e.tensor_copy(out=Lg[:], in_=bank[:, 0:36]), reads=[bk, f"rt{i % NRS}"], writes=[f"rt{i % NRS}"])
                def drain(nsteps):
                    for _ in range(nsteps):
                        for g in list(pending):
                            try:
                                next(g)
                            except StopIteration:
                                pending.remove(g)

                for i in range(TB):
                    P.dma("sp", lambda e, i=i: e.dma_start(out=xinL[i][:], in_=x_d[(b * TB + i) * 128:(b * TB + i + 1) * 128, :]), writes=[f"xinb{i}"])
                for c in range(4):
                    U(c)
                for i in range(TB):
                    V(i)
                    for m in (2 * i, 2 * i + 1):
                        if m < 8:
                            g1(m)
                    if i >= 1:
                        SV(i - 1)
                for m in range(2 * TB, 8):
                    g1(m)
                SV(TB - 1)
                for m in range(8):
                    G2(m)
                    drain(4)
                for i in range(TB):
                    O(i)
                    if i >= 1:
                        TR(i - 1)
                    if i >= 2:
                        RM(i - 2)
                TR(TB - 1)
                if TB >= 2:
                    RM(TB - 2)
                RM(TB - 1)
                drain(1000)
                pending.extend(router_rest(b * TB + i) for i in range(TB))
            drain(1000)

            P.barrier()
        stP.close()

        with ExitStack() as st:
            P.scope = "router"
            h2 = sbuf(st, "h2", [128, 8, T], BF16)
            yacc = sbuf(st, "yacc", [128, NT, D], F32)
            fgrow = sbuf(st, "fgrow", [1, D], F32)
            fgbc = sbuf(st, "fgbc", [128, D], F32)
            P.dma("sp", lambda e: e.dma_start(out=h2[:], in_=h2_d), reads=["h2_d"], writes=["h2"])
            for tl in range(NT):
                P.dma("sp", lambda e, tl=tl: e.dma_start(out=yacc[:, tl, :], in_=x1_d[tl * 128:(tl + 1) * 128, :]), reads=["x1_d"], writes=[f"yacc{tl}"])
            P.dma("sp", lambda e: e.dma_start(out=fgrow[:], in_=fg_d), writes=["fgrow"])
            for hf in range(2):
                P.op("pe", lambda e, hf=hf: e.matmul(psM[0][:, 0:512], lhsT=onesf[0:1, :], rhs=fgrow[0:1, hf * 512:(hf + 1) * 512], start=True, stop=True),
                     reads=["onesf", "fgrow"], writes=["psM0"])
                P.op("act", lambda e, hf=hf: e.activation(out=fgbc[:, hf * 512:(hf + 1) * 512], in_=psM[0][:, 0:512], func=AF.Copy),
                     reads=["psM0", "fgbc"], writes=["fgbc"])

            P.scope = "experts"
            Wg = [sbuf(st, f"Wg{i}", [128, 8, 256], BF16) for i in range(2)]
            Wu = [sbuf(st, f"Wu{i}", [128, 8, 256], BF16) for i in range(2)]
            Wd = [sbuf(st, f"Wd{i}", [128, 2, D], BF16) for i in range(2)]
            GN_ = min(512, T)
            NG = T // GN_
            sg = [sbuf(st, f"sg{i}", [128, GN_], F32) for i in range(2)]
            actT = sbuf(st, "actT", [128, 2, GN_], BF16)
            gu_ps = [psM[0], psM[1], psS, psD[:, 0:512]]
            gu_k = ["psM0", "psM1", "psS", "psD0"]
            d_ps = [psA, psD[:, 512:1536]]
            d_k = [["psA", "psA2"], ["psD1", "psD2"]]
            actT2 = [actT, sbuf(st, "actTb", [128, 2, GN_], BF16)]
            TPG = GN_ // 128

            def load_expert(ex):
                b = ex % 2
                P.dma("pool", lambda e: e.dma_start(out=Wg[b][:], in_=weg_d[ex].rearrange("(k p) n -> p k n", p=128)), writes=[f"Wg{b}"])
                P.dma("pool", lambda e: e.dma_start(out=Wu[b][:], in_=weu_d[ex].rearrange("(k p) n -> p k n", p=128)), writes=[f"Wu{b}"])
                P.dma("pool", lambda e: e.dma_start(out=Wd[b][:], in_=wed_d[ex].rearrange("(k p) n -> p k n", p=128)), writes=[f"Wd{b}"])

            def G(u, f):
                ex, gi = divmod(u, NG)
                b = ex % 2
                gc = slice(gi * GN_, (gi + 1) * GN_)
                aT = actT2[u % 2]
                ak = f"actT{u % 2}_{f}"

                def gumm(e):
                    ins = None
                    for k in range(8):
                        e.matmul(gu_ps[2 * f][:, 0:GN_], lhsT=Wg[b][:, k, f * 128:(f + 1) * 128], rhs=h2[:, k, gc], start=(k == 0), stop=(k == 7))
                    for k in range(8):
                        ins = e.matmul(gu_ps[2 * f + 1][:, 0:GN_], lhsT=Wu[b][:, k, f * 128:(f + 1) * 128], rhs=h2[:, k, gc], start=(k == 0), stop=(k == 7))
                    return ins

                P.op("pe", gumm, reads=[f"Wg{b}", f"Wu{b}", "h2"], writes=[gu_k[2 * f], gu_k[2 * f + 1]])
                P.op("act", lambda e: e.activation(out=sg[f][:], in_=gu_ps[2 * f][:, 0:GN_], func=AF.Silu), reads=[gu_k[2 * f]], writes=[f"sg{f}"])
                P.op("dve", lambda e: e.tensor_tensor(out=aT[:, f, :], in0=gu_ps[2 * f + 1][:, 0:GN_], in1=sg[f][:], op=ALU.mult),
                     reads=[gu_k[2 * f + 1], f"sg{f}"], writes=[ak])

            dstate = [0]

            def Dn(u, ti):
                ex, gi = divmod(u, NG)
                b = ex % 2
                tl = gi * TPG + ti
                aT = actT2[u % 2]
                dps = d_ps[dstate[0] % 2]
                dk = d_k[dstate[0] % 2]
                dstate[0] += 1

                def dmm(e):
                    ins = None
                    for hf in range(2):
                        for f in range(2):
                            ins = e.matmul(dps[:, hf * 512:(hf + 1) * 512], lhsT=aT[:, f, ti * 128:(ti + 1) * 128],
                                           rhs=Wd[b][:, f, hf * 512:(hf + 1) * 512], start=(f == 0), stop=(f == 1))
                    return ins

                P.op("pe", dmm, reads=[f"actT{u % 2}_0", f"actT{u % 2}_1", f"Wd{b}"], writes=dk)
                P.op("dve", lambda e: e.scalar_tensor_tensor(out=yacc[:, tl, :], in0=dps[:, 0:1024], scalar=comb[:, tl, ex:ex + 1],
                                                             in1=yacc[:, tl, :], op0=ALU.mult, op1=ALU.add),
                     reads=dk + [f"comb{tl}", f"yacc{tl}"], writes=[f"yacc{tl}"])

            junk = sbuf(st, "junk3", [128, D], BF16)
            ss = sbuf(st, "ss3", [128, 1], F32)
            sd = sbuf(st, "sd3", [128, 1], F32)
            rs = sbuf(st, "rs3", [128, 1], F32)
            ob = [sbuf(st, f"ob{i}", [128, D], F32) for i in range(2)]

            def final_tile(tl):
                o = ob[tl % 2]
                ok = f"ob{tl % 2}"
                P.op("act", lambda e: e.activation(out=junk[:], in_=yacc[:, tl, :], func=AF.Square, accum_out=ss[:]),
                     reads=[f"yacc{tl}"], writes=["junk3", "ss3"])
                P.op("act", lambda e: e.activation(out=sd[:], in_=ss[:], func=AF.Sqrt, bias=NORM_EPS, scale=1.0 / D), reads=["ss3"], writes=["sd3"])
                P.op("dve", lambda e: e.reciprocal(out=rs[:], in_=sd[:]), reads=["sd3"], writes=["rs3"])
                P.op("dve", lambda e: e.scalar_tensor_tensor(out=o[:], in0=yacc[:, tl, :], scalar=rs[:], in1=fgbc[:], op0=ALU.mult, op1=ALU.mult),
                     reads=[f"yacc{tl}", "rs3", "fgbc"], writes=[ok])
                P.dma("sp", lambda e: e.dma_start(out=out_d[tl * 128:(tl + 1) * 128, :], in_=o[:]), reads=[ok], writes=["out"])

            NU = NE * NG
            load_expert(0)
            if NE > 1:
                load_expert(1)
            G(0, 0)
            G(0, 1)
            for u in range(NU):
                ex, gi = divmod(u, NG)
                P.scope = "experts" if ex != 5 else "ex5"
                half = (TPG + 1) // 2
                if u + 1 < NU:
                    G(u + 1, 0)
                for ti in range(0, half):
                    Dn(u, ti)
                    if ex == NE - 1:
                        final_tile(gi * TPG + ti)
                if u + 1 < NU:
                    G(u + 1, 1)
                for ti in range(half, TPG):
                    Dn(u, ti)
                    if ex == NE - 1:
                        final_tile(gi * TPG + ti)
                if gi == NG - 1 and ex + 2 < NE:
                    load_expert(ex + 2)

            P.final_wait("sp")
            P.emit()
    return nc


def host_layout(inp, b, T):
    f = lambda a: np.ascontiguousarray(np.asarray(a, dtype=np.float32))

    def colpack(v, n):
        v = np.asarray(v, np.float32).reshape(-1)
        pad = np.zeros(n * 128, np.float32)
        pad[:v.size] = v
        return pad.reshape(n, 128).T

    cols = np.concatenate([
        colpack(inp["tmix_mu"][0], 15), colpack(inp["k_k"][0], 4), colpack(inp["k_a"][0], 4), colpack(inp["r_k"][0], 4),
        colpack(inp["a0"][0], 4), colpack(inp["lnx_g"][0], 4), colpack(inp["lnx_b"][0], 4), colpack(inp["b_gate"][0], 16),
        colpack(inp["norm1_g"][0], 8), colpack(inp["norm2_g"][0], 8)], axis=1)
    w_re = np.asarray(inp["w_re"][0], np.float32)
    w_r = np.concatenate([np.asarray(inp["w_rg"][0], np.float32), w_re.transpose(1, 0, 2).reshape(D, 32)], axis=1)
    b_r = np.concatenate([np.asarray(inp["b_rg"][0], np.float32).reshape(-1), np.asarray(inp["b_re"][0], np.float32).reshape(-1)])
    return {
        "x": f(inp["x"][b, :T]), "w_in": f(inp["w_in"][0]), "cols": f(cols), "w0": f(inp["w0"][0]).reshape(1, 512),
        "w2": f(inp["w2"][0]), "a2": f(inp["a2"][0]), "g2": f(inp["g2"][0]), "w_oA": f(inp["w_oA"][0]), "w_oB": f(inp["w_oB"][0]),
        "w_out": f(inp["w_out"][0]), "lnv_g": f(inp["lnv_g"][0]).reshape(1, 512), "lnv_b": f(inp["lnv_b"][0]).reshape(1, 512),
        "wsT": f(np.asarray(inp["w_s"][0], np.float32).transpose(0, 2, 1)), "b_s": f(inp["b_s"][0]).reshape(1, 512),
        "w_r": f(w_r), "b_r": f(b_r).reshape(1, 36), "w_e_gate": f(inp["w_e_gate"][0]), "w_e_up": f(inp["w_e_up"][0]),
        "w_e_down": f(inp["w_e_down"][0]), "final_g": f(inp["final_g"]).reshape(1, D),
    }


def kernel(**inputs):
    T = 2048
    nc = build(T)
    in_maps = [host_layout(inputs, b, T) for b in range(8)]
    res = run_bass_kernel_spmd(nc, in_maps, core_ids=list(range(8)))
    return np.stack([np.asarray(r["out"], dtype=np.float32) for r in res.results], axis=0)
```

```python
import re
import numpy as np
from contextlib import ExitStack
import concourse.bass as bass
import concourse.mybir as mybir
from concourse.bass_utils import run_bass_kernel_spmd

F32 = mybir.dt.float32
BF16 = mybir.dt.bfloat16
AF = mybir.ActivationFunctionType
ALU = mybir.AluOpType
AX = mybir.AxisListType
ENGS = ["pe", "dve", "act", "pool", "sp"]
DMA_RING = {"sp": 6, "pool": 4}

D = 1024
IN_COLS = 4896
A_COLS = 1824
NCHA = 15
LNX_EPS = 64e-5
LN_EPS = 1e-5
NORM_EPS = 1e-6
EXPM05 = float(np.exp(-0.5))
PROFILE_SCOPES = False

C_MU, C_KK, C_KA, C_RK, C_A0, C_LG, C_LB, C_BG, C_G1, C_G2 = 0, 15, 19, 23, 27, 31, 35, 39, 55, 63
NCOL = 71


class Rec:
    def __init__(self):
        self.calls = []

    def __getattr__(self, name):
        def f(*a, **k):
            self.calls.append((name, a, k))
            return self
        return f


def _record(fn):
    r = Rec()
    fn(r)
    assert r.calls
    return r.calls


class Prog:
    def __init__(self, nc, stack):
        self.nc = nc
        self.lists = {e: [] for e in ENGS}
        self.count = {e: 0 for e in ENGS}
        self.sems = {}
        for e in ENGS:
            self.sems[("e", e)] = stack.enter_context(nc.semaphore(f"s_{e}"))
        for q, r in DMA_RING.items():
            for i in range(r):
                self.sems[("d", q, i)] = stack.enter_context(nc.semaphore(f"d_{q}{i}"))
        self.dma_n = {q: 0 for q in DMA_RING}
        self.waited = {e: {} for e in ENGS}
        self.last_w = {}
        self.readers = {}
        self.scope = None

    def _waits(self, eng, reads, writes, extra=()):
        need = {}

        def add(s, v):
            if need.get(s, 0) < v:
                need[s] = v

        for k in reads:
            if k in self.last_w:
                add(*self.last_w[k])
        for k in writes:
            if k in self.last_w:
                add(*self.last_w[k])
            for s, v in self.readers.get(k, {}).items():
                add(s, v)
        for s, v in extra:
            add(s, v)
        out = []
        wd = self.waited[eng]
        for s, v in need.items():
            if wd.get(s, 0) < v:
                wd[s] = v
                out.append((s, v))
        return out

    def _commit(self, tok, reads, writes):
        for k in writes:
            self.last_w[k] = tok
            self.readers[k] = {}
        for k in reads:
            d = self.readers.setdefault(k, {})
            if d.get(tok[0], 0) < tok[1]:
                d[tok[0]] = tok[1]

    LIMIT = None
    NOPS = 0

    def op(self, eng, fn, reads=(), writes=()):
        Prog.NOPS += 1
        if Prog.LIMIT is not None and Prog.NOPS > Prog.LIMIT:
            return None
        writes = list(writes) + [k for k in reads if k.startswith("ps")]
        waits = self._waits(eng, reads, writes)
        self.count[eng] += 1
        tok = (("e", eng), self.count[eng])
        self.lists[eng].append((waits, _record(fn), (("e", eng), 1), self.scope))
        self._commit(tok, reads, writes)
        return tok

    def dma(self, q, fn, reads=(), writes=()):
        Prog.NOPS += 1
        if Prog.LIMIT is not None and Prog.NOPS > Prog.LIMIT:
            return None
        r = DMA_RING[q]
        i = self.dma_n[q]
        self.dma_n[q] += 1
        slot = i % r
        skey = ("d", q, slot)
        extra = [(skey, 16 * (i // r))] if i >= r else []
        waits = self._waits(q, reads, writes, extra)
        tok = (skey, 16 * (i // r + 1))
        self.lists[q].append((waits, _record(fn), (skey, 16), self.scope))
        self._commit(tok, reads, writes)
        return tok

    def _all_tokens(self):
        toks = []
        for q, r in DMA_RING.items():
            n = self.dma_n[q]
            for slot in range(min(r, n)):
                toks.append((("d", q, slot), 16 * ((n - 1 - slot) // r + 1)))
        for e in ENGS:
            if self.count[e]:
                toks.append((("e", e), self.count[e]))
        return toks

    def barrier(self):
        toks = self._all_tokens()
        for e in ENGS:
            waits = self._waits(e, (), (), toks)
            if waits:
                self.lists[e].append((waits, None, None, None))

    def final_wait(self, eng):
        waits = self._waits(eng, (), (), self._all_tokens())
        self.lists[eng].append((waits, None, None, None))

    def emit(self):
        engmap = {"pe": "tensor", "dve": "vector", "act": "scalar", "pool": "gpsimd", "sp": "sync"}
        with self.nc.Block() as block:
            for e in ENGS:
                lst = self.lists[e]
                if not lst:
                    continue

                def body(engine, lst=lst):
                    cur, cm = None, None
                    for waits, fn, inc, scope in lst:
                        if PROFILE_SCOPES and scope != cur:
                            if cm is not None:
                                cm.__exit__(None, None, None)
                                cm = None
                            if scope is not None:
                                cm = self.nc.named_scope(scope)
                                cm.__enter__()
                            cur = scope
                        for s, v in waits:
                            engine.wait_ge(self.sems[s], v)
                        if fn is not None:
                            ins = None
                            for name, a, k in fn:
                                ins = getattr(engine, name)(*a, **k)
                            ins.then_inc(self.sems[inc[0]], inc[1])
                    if cm is not None:
                        cm.__exit__(None, None, None)

                getattr(block, engmap[e])(body)


def build(T, NE=32, dbg=None):
    NT = T // 128
    nc = bass.Bass("TRN2", target_bir_lowering=False)

    def din(name, shape, dt=F32):
        return nc.dram_tensor(name, list(shape), dt, kind="ExternalInput").ap()

    x_d = din("x", [T, D])
    w_in_d = din("w_in", [D, IN_COLS])
    cols_d = din("cols", [128, NCOL])
    w0_d = din("w0", [1, 512])
    w2_d = din("w2", [64, 512])
    a2_d = din("a2", [64, 512])
    g2_d = din("g2", [160, 512])
    woA_d = din("w_oA", [512, D])
    woB_d = din("w_oB", [512, D])
    wout_d = din("w_out", [D, D])
    lnvg_d = din("lnv_g", [1, 512])
    lnvb_d = din("lnv_b", [1, 512])
    wsT_d = din("wsT", [4, 128, 128])
    bs_d = din("b_s", [1, 512])
    wr_d = din("w_r", [D, 36])
    br_d = din("b_r", [1, 36])
    weg_d = din("w_e_gate", [32, D, 256])
    weu_d = din("w_e_up", [32, D, 256])
    wed_d = din("w_e_down", [32, 256, D])
    fg_d = din("final_g", [1, D])
    out_d = nc.dram_tensor("out", [T, D], F32, kind="ExternalOutput").ap()
    x1_d = nc.dram_tensor("x1_scr", [T, D], F32, kind="Internal").ap()
    h2_d = nc.dram_tensor("h2_scr", [128, 8, T], BF16, kind="Internal").ap()
    hT_d = nc.dram_tensor("hT_scr", [T // 128, 128, 1024], BF16, kind="Internal").ap()
    yg_d = nc.dram_tensor("yg_scr", [T // 128, 128, 512], BF16, kind="Internal").ap()
    dbg_out = {}
    if dbg:
        for name, shape in dbg.items():
            dbg_out[name] = nc.dram_tensor("dbg_" + name, list(shape), F32, kind="ExternalOutput").ap()

    with ExitStack() as st0:
        P = Prog(nc, st0)

        def sbuf(st, name, shape, dt):
            return st.enter_context(nc.sbuf_tensor("sb_" + name, list(shape), dt))

        psM = [st0.enter_context(nc.psum_tensor(f"psM{i}", [128, 512], F32)) for i in range(2)]
        psA = st0.enter_context(nc.psum_tensor("psA", [128, 1024], F32))
        psD = st0.enter_context(nc.psum_tensor("psD", [128, 1536], F32))
        psS = st0.enter_context(nc.psum_tensor("psS", [128, 512], F32))

        identf = sbuf(st0, "identf", [128, 128], F32)
        ident = sbuf(st0, "ident", [128, 128], BF16)
        mSU = sbuf(st0, "mSU", [128, 128], F32)
        mIU = sbuf(st0, "mIU", [128, 128], F32)
        maskA = sbuf(st0, "maskA", [128, 2, 512], F32)
        maskL = sbuf(st0, "maskL", [128, 2, 128], F32)
        BDf = sbuf(st0, "BDf", [128, 128], F32)
        BDb = sbuf(st0, "BDb", [128, 128], BF16)
        BD64 = sbuf(st0, "BD64", [128, 128], F32)
        Tri2 = sbuf(st0, "Tri2", [128, 256], F32)
        onesb = sbuf(st0, "onesb", [1, 128], BF16)
        onesf = sbuf(st0, "onesf", [128, 128], F32)
        cols = sbuf(st0, "cols", [128, NCOL], F32)
        omu = sbuf(st0, "omu", [128, NCHA], F32)
        oka = sbuf(st0, "oka", [128, 4], F32)
        g1bc = sbuf(st0, "g1bc", [128, 8, 128], F32)
        g2bc = sbuf(st0, "g2bc", [128, 8, 128], F32)

        def colap(c):
            return cols[:, c:c + 1]

        P.dma("sp", lambda e: e.dma_start(out=cols[:], in_=cols_d), writes=["cols"])
        P.op("pool", lambda e: e.memset(onesf[:], 1.0), writes=["onesf"])
        P.op("pool", lambda e: e.memset(identf[:], 0.0), writes=["identf"])
        P.op("pool", lambda e: e.affine_select(out=identf[:], in_=identf[:], pattern=[[-1, 128]], compare_op=ALU.not_equal,
                                               fill=1.0, base=0, channel_multiplier=1), reads=["identf"], writes=["identf"])
        P.op("dve", lambda e: e.tensor_copy(out=ident[:], in_=identf[:]), reads=["identf"], writes=["ident"])
        P.op("pool", lambda e: e.affine_select(out=mSU[:], in_=onesf[:], pattern=[[1, 128]], compare_op=ALU.is_gt,
                                               fill=0.0, base=0, channel_multiplier=-1), reads=["onesf"], writes=["mSU"])
        P.op("pool", lambda e: e.affine_select(out=mIU[:], in_=onesf[:], pattern=[[1, 128]], compare_op=ALU.is_ge,
                                               fill=0.0, base=0, channel_multiplier=-1), reads=["onesf"], writes=["mIU"])
        for h in range(2):
            for q in range(4):
                src = mSU if q % 2 == 0 else mIU
                P.op("dve", lambda e, h=h, q=q, src=src: e.tensor_copy(out=maskA[:, h, q * 128:(q + 1) * 128], in_=src[:]),
                     reads=["mSU", "mIU"], writes=["maskA"])
            P.op("pool", lambda e, h=h: e.affine_select(out=maskL[:, h, :], in_=onesf[:], pattern=[[-1, 128]], compare_op=ALU.is_gt,
                                                        fill=0.0, base=0, channel_multiplier=1), reads=["onesf"], writes=["maskL"])
        P.op("pool", lambda e: e.memset(BDf[:], 0.0), writes=["BDf"])
        P.op("pool", lambda e: e.memset(BDf[0:64, 0:64], 1.0), reads=["BDf"], writes=["BDf"])
        P.op("pool", lambda e: e.memset(BDf[64:128, 64:128], 1.0), reads=["BDf"], writes=["BDf"])
        P.op("dve", lambda e: e.tensor_copy(out=BDb[:], in_=BDf[:]), reads=["BDf"], writes=["BDb"])
        P.op("dve", lambda e: e.tensor_scalar(out=BD64[:], in0=BDf[:], scalar1=1.0 / 64.0, scalar2=None, op0=ALU.mult),
             reads=["BDf"], writes=["BD64"])
        P.op("dve", lambda e: e.tensor_scalar(out=Tri2[:, 0:128], in0=mIU[:], scalar1=EXPM05, scalar2=None, op0=ALU.mult),
             reads=["mIU"], writes=["Tri2"])
        P.op("dve", lambda e: e.tensor_scalar(out=Tri2[:, 128:256], in0=mSU[:], scalar1=EXPM05, scalar2=None, op0=ALU.mult),
             reads=["mSU", "Tri2"], writes=["Tri2"])
        P.op("dve", lambda e: e.tensor_copy(out=onesb[:], in_=onesf[0:1, :]), reads=["onesf"], writes=["onesb"])
        P.op("dve", lambda e: e.tensor_scalar(out=omu[:], in0=cols[:, C_MU:C_MU + NCHA], scalar1=-1.0, scalar2=1.0,
                                              op0=ALU.mult, op1=ALU.add), reads=["cols"], writes=["omu"])
        P.op("dve", lambda e: e.tensor_scalar(out=oka[:], in0=cols[:, C_KA:C_KA + 4], scalar1=-1.0, scalar2=1.0,
                                              op0=ALU.mult, op1=ALU.add), reads=["cols"], writes=["oka"])
        for k in range(8):
            P.op("dve", lambda e, k=k: e.tensor_scalar(out=g1bc[:, k, :], in0=onesf[:], scalar1=colap(C_G1 + k), scalar2=None,
                                                       op0=ALU.mult), reads=["cols", "onesf"], writes=["g1bc"])
            P.op("dve", lambda e, k=k: e.tensor_scalar(out=g2bc[:, k, :], in0=onesf[:], scalar1=colap(C_G2 + k), scalar2=None,
                                                       op0=ALU.mult), reads=["cols", "onesf"], writes=["g2bc"])

        def rms_to_T(st_keys, xin, xin_key, gbc, gbc_key, dst_ap, dst_key, tmp, tag):
            junk, ss, sd, rs, xsb = tmp
            P.op("act", lambda e: e.activation(out=junk[:], in_=xin, func=AF.Square, accum_out=ss[:]),
                 reads=[xin_key], writes=["junk" + tag, "ss" + tag])
            P.op("act", lambda e: e.activation(out=sd[:], in_=ss[:], func=AF.Sqrt, bias=NORM_EPS, scale=1.0 / D),
                 reads=["ss" + tag], writes=["sd" + tag])
            P.op("dve", lambda e: e.reciprocal(out=rs[:], in_=sd[:]), reads=["sd" + tag], writes=["rs" + tag])
            P.op("dve", lambda e: e.tensor_scalar(out=xsb[:], in0=xin, scalar1=rs[:], scalar2=None, op0=ALU.mult),
                 reads=[xin_key, "rs" + tag], writes=["xsb" + tag])
            pT = psM[0][:].bitcast(BF16)

            def tr(e):
                ins = None
                for k in range(8):
                    ins = e.transpose(out=pT[:, k * 128:(k + 1) * 128], in_=xsb[:, k * 128:(k + 1) * 128], identity=ident[:])
                return ins

            P.op("pe", tr, reads=["xsb" + tag, "ident"], writes=["psM0"])
            P.op("dve", lambda e: e.tensor_tensor(out=dst_ap, in0=pT.rearrange("p (k t) -> p k t", k=8), in1=gbc[:], op=ALU.mult),
                 reads=["psM0", gbc_key], writes=[dst_key])

        P.scope = "M1"
        stP = ExitStack()
        comb = sbuf(st0, "comb", [128, NT, 32], F32)
        woA = sbuf(stP, "woA", [128, 4, D], BF16)
        woB = sbuf(stP, "woB", [128, 4, D], BF16)
        wout = sbuf(stP, "wout", [128, 8, D], BF16)

        with ExitStack() as st:
            hTb = [sbuf(st, f"hTt{i}", [128, 8, 128], BF16) for i in range(2)]
            ygb = [sbuf(st, f"ygt{i}", [128, 4, 128], BF16) for i in range(2)]
            w_inA = sbuf(st, "w_inA", [128, 8, A_COLS], BF16)
            w2b = sbuf(st, "w2b", [64, 512], BF16)
            a2b = sbuf(st, "a2b", [128, 512], BF16)
            g2b = sbuf(st, "g2b", [128, 2, 512], BF16)
            w0row = sbuf(st, "w0row", [1, 512], BF16)
            for k in range(8):
                P.dma("pool", lambda e, k=k: e.dma_start(out=w_inA[:, k, :], in_=w_in_d[k * 128:(k + 1) * 128, 0:A_COLS]),
                      writes=["w_inA"])
            P.dma("pool", lambda e: e.dma_start(out=w2b[:], in_=w2_d), writes=["w2b"])
            P.dma("pool", lambda e: e.dma_start(out=a2b[64:128, :], in_=a2_d), writes=["a2b"])
            P.dma("pool", lambda e: e.dma_start(out=g2b[:, 0, :], in_=g2_d[0:128, :]), writes=["g2b"])
            P.dma("pool", lambda e: e.dma_start(out=g2b[0:32, 1, :], in_=g2_d[128:160, :]), reads=["g2b"], writes=["g2b"])
            P.dma("pool", lambda e: e.dma_start(out=w0row[:], in_=w0_d), writes=["w0row"])
            P.dma("pool", lambda e: e.dma_start(out=woA[:], in_=woA_d.rearrange("(k p) n -> p k n", p=128)), writes=["woA"])
            P.dma("pool", lambda e: e.dma_start(out=woB[:], in_=woB_d.rearrange("(k p) n -> p k n", p=128)), writes=["woB"])
            P.dma("pool", lambda e: e.dma_start(out=wout[:], in_=wout_d.rearrange("(k p) n -> p k n", p=128)), writes=["wout"])

            xin = [sbuf(st, f"xin{i}", [128, D], F32) for i in range(2)]
            junk = sbuf(st, "junk", [128, D], BF16)
            ss = sbuf(st, "ss", [128, 1], F32)
            sd = sbuf(st, "sd", [128, 1], F32)
            rs = sbuf(st, "rs", [128, 1], F32)
            xsb = sbuf(st, "xsb", [128, D], BF16)
            carry = sbuf(st, "carry", [128, NCHA], F32)
            ltmp = sbuf(st, "ltmp", [128, 129], F32)
            pmx = sbuf(st, "pmx", [128, 128], F32)
            pmxF = [sbuf(st, f"pmxF{j}", [128, 128], F32) for j in range(3)]
            ltF = [sbuf(st, f"ltF{j}", [128, 129], F32) for j in range(3)]
            txw2 = [sbuf(st, f"txw_{i}", [128, 128], BF16) for i in range(2)]
            sxg2 = [sbuf(st, f"sxg_{i}", [128, 2, 128], BF16) for i in range(2)]
            DC2 = [sbuf(st, f"DC_{i}", [128, 4], F32) for i in range(2)]
            etok = sbuf(st, "etok", [128, 512], F32)
            Dt2 = [sbuf(st, f"Dt_{i}", [128, 4, 128], F32) for i in range(2)]
            Dinv2 = [sbuf(st, f"Dinv_{i}", [128, 4, 128], F32) for i in range(2)]
            Dprev2 = [sbuf(st, f"Dprev_{i}", [128, 4, 128], F32) for i in range(2)]
            asig = [sbuf(st, f"asig{p}", [128, 128], F32) for p in range(4)]
            tA = [sbuf(st, f"tA{p}", [128, 128], F32) for p in range(4)]
            tB = [sbuf(st, f"tB{p}", [128, 128], F32) for p in range(4)]
            tC = [sbuf(st, f"tC{p}", [128, 128], F32) for p in range(4)]
            t16 = [sbuf(st, f"t16{p}", [128, 128], BF16) for p in range(4)]
            v16 = [sbuf(st, f"v16{p}", [128, 128], BF16) for p in range(4)]
            pmr = [sbuf(st, f"pmr{p}", [128, 128], F32) for p in range(4)]
            pmk = [sbuf(st, f"pmk{p}", [128, 128], F32) for p in range(4)]
            pmv = [sbuf(st, f"pmv{p}", [128, 128], F32) for p in range(4)]
            ltm = [sbuf(st, f"ltm{p}", [128, 129], F32) for p in range(4)]
            PX = [psM[0], psM[1], psA[:, 0:512], psA[:, 512:1024]]
            PXk = ["psM0", "psM1", "psA", "psA2"]
            PY = [psD[:, 0:512], psD[:, 512:1024], psD[:, 1024:1536], psS]
            PYk = ["psD0", "psD1", "psD2", "psS"]
            AR2 = [[sbuf(st, f"AR{p}_{i}", [128, 2, 128], BF16) for p in range(4)] for i in range(1)] * 2
            BT = [sbuf(st, f"BT{p}", [128, 128], BF16) for p in range(4)]
            KT = [sbuf(st, f"KT{p}", [128, 128], BF16) for p in range(4)]
            TOK2 = [[sbuf(st, f"TOK{p}_{i}", [128, 3, 128], BF16) for p in range(4)] for i in range(1)] * 2
            bon2 = [[sbuf(st, f"bon{p}_{i}", [128, 128], F32) for p in range(4)] for i in range(1)] * 2
            gT2 = [[sbuf(st, f"gT{p}_{i}", [128, 128], F32) for p in range(4)] for i in range(1)] * 2
            AM2 = [[sbuf(st, f"AM{p}_{i}", [128, 2, 512], BF16) for p in range(4)] for i in range(1)] * 2
            L0 = [sbuf(st, f"L0{p}", [128, 2, 128], BF16) for p in range(4)]
            MT2 = [[[sbuf(st, f"MT{p}_{i}_{j}", [128, 2, 256], BF16) for i in range(2)] for p in range(4)] for j in range(1)] * 2
            LK = [[sbuf(st, f"LK{p}_{i}", [128, 2, 128], BF16) for i in range(2)] for p in range(4)]
            Sw = [sbuf(st, f"Sw{p}", [128, 128], F32) for p in range(4)]
            Sb = [sbuf(st, f"Sb{p}", [128, 128], BF16) for p in range(4)]
            Xb = [sbuf(st, f"Xb{p}", [128, 128], BF16) for p in range(4)]
            Ub = [sbuf(st, f"Ub{p}", [128, 128], BF16) for p in range(4)]
            yT = sbuf(st, "yT", [128, 4, 128], F32)
            yc = sbuf(st, "yc", [128, 512], F32)
            ysq = sbuf(st, "ysq", [128, 512], F32)
            yrs = sbuf(st, "yrs", [128, 512], F32)
            y3p = [sbuf(st, f"y3_{p}", [128, 128], F32) for p in range(4)]
            tSp = [sbuf(st, f"tS_{p}", [128, 128], F32) for p in range(4)]

            P.op("pool", lambda e: e.memset(carry[:], 0.0), writes=[f"carry{c}" for c in range(NCHA)])
            for p in range(4):
                P.op("pool", lambda e, p=p: e.memset(Sw[p][:], 0.0), writes=[f"Sw{p}"])
                P.op("pool", lambda e, p=p: e.memset(Sb[p][:], 0.0), writes=[f"Sb{p}"])

            def LV(p):
                return psD[:, 1024:1280] if p % 2 == 0 else psS[:, 0:256]

            def DK(p):
                return ["psD0", "psD2"] if p % 2 == 0 else ["psD1", "psS"]

            serial_ps = [psS, psM[1], psA[:, 0:512], psA[:, 512:1024]]
            serial_key = ["psS", "psM1", "psA", "psA2"]

            def tile_parts(tl, P):
                par = tl % 2
                AM, AR, TOK, MT, bon, gT = AM2[par], AR2[par], TOK2[par], MT2[par], bon2[par], gT2[par]
                Dt, Dinv, Dprev, txw, sxg, DC = Dt2[par], Dinv2[par], Dprev2[par], txw2[par], sxg2[par], DC2[par]
                tc = slice(tl * 128, (tl + 1) * 128)
                xb_, xk = xin[tl % 2], f"xin{tl % 2}"
                hT, hTk = hTb[tl % 2], f"hTt{tl % 2}"
                ygT, ygk = ygb[tl % 2], f"ygt{tl % 2}"

                def inproj_chunk(c, dst_fn):
                    rows = 128 if c < 14 else 32
                    bank, bkey = (psD[:, 512:1024], "psD1") if c == 13 else (psM[0], "psM0")

                    def mmf(e, c=c, rows=rows, bank=bank):
                        ins = None
                        for k in range(8):
                            ins = e.matmul(bank[0:rows, 0:128], lhsT=w_inA[:, k, c * 128:c * 128 + rows], rhs=hT[:, k, :],
                                           start=(k == 0), stop=(k == 7))
                        return ins

                    P.op("pe", mmf, reads=["w_inA", hTk], writes=[bkey])
                    P.op("act", lambda e: e.activation(out=ltmp[0:rows, 1:129], in_=bank[0:rows, 0:128], func=AF.Copy,
                                                       scale=cols[0:rows, C_MU + c:C_MU + c + 1]),
                         reads=[bkey, "cols"], writes=["ltmp"])
                    P.op("pool", lambda e: e.tensor_copy(out=ltmp[0:rows, 0:1], in_=carry[0:rows, c:c + 1]),
                         reads=[f"carry{c}", "ltmp"], writes=["ltmp"])
                    P.op("pool", lambda e: e.tensor_copy(out=carry[0:rows, c:c + 1], in_=ltmp[0:rows, 128:129]),
                         reads=["ltmp", f"carry{c}"], writes=[f"carry{c}"])
                    dst, dkey = dst_fn
                    P.op("dve", lambda e: e.scalar_tensor_tensor(out=dst[0:rows, :], in0=bank[0:rows, 0:128],
                                                                 scalar=omu[0:rows, c:c + 1], in1=ltmp[0:rows, 0:128],
                                                                 op0=ALU.mult, op1=ALU.add),
                         reads=[bkey, "omu", "ltmp"], writes=[dkey])

                def inproj_chunk_g(c, dst, dkey, bank, bkey, lt, ltk):
                    rows = 128

                    def mmf(e):
                        ins = None
                        for k in range(8):
                            ins = e.matmul(bank[0:rows, 0:128], lhsT=w_inA[:, k, c * 128:c * 128 + rows], rhs=hT[:, k, :],
                                           start=(k == 0), stop=(k == 7))
                        return ins

                    P.op("pe", mmf, reads=["w_inA", hTk], writes=[bkey])
                    yield
                    P.op("act", lambda e: e.activation(out=lt[:, 1:129], in_=bank[:, 0:128], func=AF.Copy, scale=cols[:, C_MU + c:C_MU + c + 1]),
                         reads=[bkey, "cols"], writes=[ltk])
                    yield
                    P.op("pool", lambda e: e.tensor_copy(out=lt[:, 0:1], in_=carry[:, c:c + 1]), reads=[f"carry{c}", ltk], writes=[ltk])
                    P.op("pool", lambda e: e.tensor_copy(out=carry[:, c:c + 1], in_=lt[:, 128:129]), reads=[ltk, f"carry{c}"], writes=[f"carry{c}"])
                    yield
                    P.op("dve", lambda e: e.scalar_tensor_tensor(out=dst[:], in0=bank[:, 0:128], scalar=omu[:, c:c + 1], in1=lt[:, 0:128],
                                                                 op0=ALU.mult, op1=ALU.add), reads=[bkey, "omu", ltk], writes=[dkey])
                    yield


                def front():
                    xb_ = xin[tl % 2]
                    xk = f"xin{tl % 2}"
                    P.dma("sp", lambda e, xb_=xb_, tl=tl: e.dma_start(out=xb_[:], in_=x_d[tl * 128:(tl + 1) * 128, :]), writes=[xk])
                    yield
                    hT, hTk = hTb[tl % 2], f"hTt{tl % 2}"
                    ygT, ygk = ygb[tl % 2], f"ygt{tl % 2}"
                    rms_to_T(None, xb_[:], xk, g1bc, "g1bc", hT[:], hTk, (junk, ss, sd, rs, xsb), "1")
                    yield
                    P.dma("sp", lambda e, tl=tl, hT=hT: e.dma_start(out=hT_d[tl], in_=hT[:].rearrange("p k t -> p (k t)")), reads=[hTk], writes=["hT_d"])
                    yield

                    FC = [(12, 128, psM[0], "psM0"), (13, 128, psD[:, 512:1024], "psD1"), (14, 32, psD[:, 1024:1536], "psD2")]
                    for j, (c, rows, bank, bkey) in enumerate(FC):
                        def mmf(e, c=c, rows=rows, bank=bank):
                            ins = None
                            for k in range(8):
                                ins = e.matmul(bank[0:rows, 0:128], lhsT=w_inA[:, k, c * 128:c * 128 + rows], rhs=hT[:, k, :], start=(k == 0), stop=(k == 7))
                            return ins

                        P.op("pe", mmf, reads=["w_inA", hTk], writes=[bkey])
                    yield
                    for j, (c, rows, bank, bkey) in enumerate(FC):
                        P.op("act", lambda e, j=j, c=c, rows=rows, bank=bank: e.activation(out=ltF[j][0:rows, 1:129], in_=bank[0:rows, 0:128], func=AF.Copy,
                                                                                           scale=cols[0:rows, C_MU + c:C_MU + c + 1]),
                             reads=[bkey, "cols"], writes=[f"ltF{j}"])
                    yield
                    for j, (c, rows, bank, bkey) in enumerate(FC):
                        P.op("pool", lambda e, j=j, c=c, rows=rows: e.tensor_copy(out=ltF[j][0:rows, 0:1], in_=carry[0:rows, c:c + 1]),
                             reads=[f"carry{c}", f"ltF{j}"], writes=[f"ltF{j}"])
                        P.op("pool", lambda e, j=j, c=c, rows=rows: e.tensor_copy(out=carry[0:rows, c:c + 1], in_=ltF[j][0:rows, 128:129]),
                             reads=[f"ltF{j}", f"carry{c}"], writes=[f"carry{c}"])
                    yield
                    for j, (c, rows, bank, bkey) in enumerate(FC):
                        P.op("dve", lambda e, j=j, c=c, rows=rows, bank=bank: e.scalar_tensor_tensor(out=pmxF[j][0:rows, :], in0=bank[0:rows, 0:128],
                                                                                                    scalar=omu[0:rows, c:c + 1], in1=ltF[j][0:rows, 0:128],
                                                                                                    op0=ALU.mult, op1=ALU.add),
                             reads=[bkey, "omu", f"ltF{j}"], writes=[f"pmxF{j}"])
                    yield
                    P.op("act", lambda e: e.activation(out=txw[0:64, :], in_=pmxF[0][0:64, :], func=AF.Tanh), reads=["pmxF0"], writes=["txw"])
                    P.op("dve", lambda e: e.tensor_copy(out=txw[64:128, :], in_=pmxF[0][64:128, :]), reads=["pmxF0", "txw"], writes=["txw"])
                    P.op("act", lambda e: e.activation(out=sxg[:, 0, :], in_=pmxF[1][:], func=AF.Sigmoid), reads=["pmxF1"], writes=["sxg"])
                    P.op("act", lambda e: e.activation(out=sxg[0:32, 1, :], in_=pmxF[2][0:32, :], func=AF.Sigmoid), reads=["pmxF2", "sxg"], writes=["sxg"])
                    yield

                    def zmm(e):
                        e.matmul(psM[0][:, 0:512], lhsT=txw[0:64, :], rhs=w2b[0:64, :], start=True, stop=False)
                        return e.matmul(psM[0][:, 0:512], lhsT=onesb[0:1, :], rhs=w0row[0:1, :], start=False, stop=True)

                    P.op("pe", zmm, reads=["txw", "w2b", "onesb", "w0row"], writes=["psM0"])
                    yield
                    P.op("act", lambda e: e.activation(out=etok[:], in_=psM[0][:, 0:512], func=AF.Sigmoid), reads=["psM0"], writes=["etok"])
                    yield

                    def cmm(e):
                        ins = None
                        for p in range(4):
                            ins = e.matmul(psD[:, 512 + p * 256:512 + (p + 1) * 256], lhsT=etok[:, p * 128:(p + 1) * 128], rhs=Tri2[:],
                                           start=True, stop=True)
                        return ins

                    P.op("pe", cmm, reads=["etok", "Tri2"], writes=["psD1", "psD2"])
                    yield
                    cum = psD[:, 512:1536].rearrange("p (a c) -> p a c", a=4)
                    P.op("act", lambda e: e.activation(out=Dt[:], in_=cum[:, :, 0:128], func=AF.Exp, scale=-1.0),
                         reads=["psD1", "psD2"], writes=["Dt"])
                    yield
                    P.op("act", lambda e: e.activation(out=Dinv[:], in_=cum[:, :, 0:128], func=AF.Exp, scale=1.0),
                         reads=["psD1", "psD2"], writes=["Dinv"])
                    yield
                    P.op("act", lambda e: e.activation(out=Dprev[:], in_=cum[:, :, 128:256], func=AF.Exp, scale=-1.0),
                         reads=["psD1", "psD2"], writes=["Dprev"])
                    yield


                def pair_gen(p):
                    X, Xk, Y, Yk = PX[p], PXk[p], PY[p], PYk[p]
                    rf, kf, vf = pmr[p], pmk[p], pmv[p]
                    rk_, kk_, vk_ = f"pmr{p}", f"pmk{p}", f"pmv{p}"
                    as_, tA_, tB_, tC_, t16_, v16_ = asig[p], tA[p], tB[p], tC[p], t16[p], v16[p]
                    ak, tAk, tBk, tCk, t16k, v16k = f"asig{p}", f"tA{p}", f"tB{p}", f"tC{p}", f"t16{p}", f"v16{p}"
                    cs = slice(p * 128, (p + 1) * 128)
                    for (c, dst, dkey, bank, bkey) in ((p, rf, rk_, X, Xk), (4 + p, kf, kk_, Y, Yk), (8 + p, vf, vk_, X, Xk)):
                        yield from inproj_chunk_g(c, dst, dkey, bank, bkey, ltm[p], f"ltm{p}")
                    P.op("pe", lambda e: e.matmul(Y[:, 0:128], lhsT=a2b[64:128, cs], rhs=txw[64:128, :], start=True, stop=True),
                         reads=["a2b", "txw"], writes=[Yk])
                    yield
                    P.op("act", lambda e: e.activation(out=as_[:], in_=Y[:, 0:128], func=AF.Sigmoid, bias=colap(C_A0 + p)),
                         reads=[Yk, "cols"], writes=[ak])
                    yield

                    def gmm(e):
                        e.matmul(X[:, 0:128], lhsT=g2b[:, 0, cs], rhs=sxg[:, 0, :], start=True, stop=False)
                        return e.matmul(X[:, 0:128], lhsT=g2b[0:32, 1, cs], rhs=sxg[0:32, 1, :], start=False, stop=True)

                    P.op("pe", gmm, reads=["g2b", "sxg"], writes=[Xk])
                    yield
                    P.op("act", lambda e: e.activation(out=gT[p][:], in_=X[:, 0:128], func=AF.Copy), reads=[Xk], writes=[f"gT{p}"])
                    yield
                    P.op("dve", lambda e: e.tensor_scalar(out=tA_[:], in0=kf[:], scalar1=colap(C_KK + p), scalar2=None, op0=ALU.mult),
                         reads=[kk_, "cols"], writes=[tAk])
                    yield
                    P.op("pool", lambda e: e.tensor_tensor(out=t16_[:], in0=tA_[:], in1=tA_[:], op=ALU.mult), reads=[tAk], writes=[t16k])
                    yield
                    P.op("pe", lambda e: e.matmul(Y[:, 0:128], lhsT=BDb[:], rhs=t16_[:], start=True, stop=True), reads=["BDb", t16k], writes=[Yk])
                    yield
                    P.op("act", lambda e: e.activation(out=tB_[:], in_=Y[:, 0:128], func=AF.Sqrt), reads=[Yk], writes=[tBk])
                    yield
                    P.op("dve", lambda e: e.tensor_scalar(out=tB_[:], in0=tB_[:], scalar1=1e-12, scalar2=None, op0=ALU.max), reads=[tBk], writes=[tBk])
                    P.op("dve", lambda e: e.reciprocal(out=tB_[:], in_=tB_[:]), reads=[tBk], writes=[tBk])
                    yield
                    P.op("pool", lambda e: e.tensor_tensor(out=tA_[:], in0=tA_[:], in1=tB_[:], op=ALU.mult), reads=[tAk, tBk], writes=[tAk])
                    yield
                    P.op("dve", lambda e: e.tensor_scalar(out=tC_[:], in0=as_[:], scalar1=colap(C_KA + p), scalar2=oka[:, p:p + 1],
                                                          op0=ALU.mult, op1=ALU.add), reads=[ak, "cols", "oka"], writes=[tCk])
                    yield
                    P.op("pool", lambda e: e.tensor_tensor(out=kf[:], in0=kf[:], in1=tC_[:], op=ALU.mult), reads=[kk_, tCk], writes=[kk_])
                    yield
                    P.op("dve", lambda e: e.scalar_tensor_tensor(out=AR[p][:, 0, :], in0=tA_[:], scalar=-1.0, in1=Dprev[:, p, :],
                                                                 op0=ALU.mult, op1=ALU.mult), reads=[tAk, "Dprev"], writes=[f"AR{p}"])
                    P.op("pool", lambda e: e.tensor_tensor(out=AR[p][:, 1, :], in0=rf[:], in1=Dt[:, p, :], op=ALU.mult),
                         reads=[rk_, "Dt", f"AR{p}"], writes=[f"AR{p}"])
                    yield
                    P.op("pool", lambda e: e.tensor_tensor(out=tB_[:], in0=tA_[:], in1=as_[:], op=ALU.mult), reads=[tAk, ak, tBk], writes=[tBk])
                    yield
                    P.op("dve", lambda e: e.tensor_tensor(out=BT[p][:], in0=tB_[:], in1=Dinv[:, p, :], op=ALU.mult), reads=[tBk, "Dinv"], writes=[f"BT{p}"])
                    P.op("dve", lambda e: e.tensor_tensor(out=KT[p][:], in0=kf[:], in1=Dinv[:, p, :], op=ALU.mult), reads=[kk_, "Dinv"], writes=[f"KT{p}"])
                    yield
                    P.op("dve", lambda e: e.scalar_tensor_tensor(out=t16_[:], in0=rf[:], scalar=colap(C_RK + p), in1=kf[:],
                                                                 op0=ALU.mult, op1=ALU.mult), reads=[rk_, kk_, "cols", t16k], writes=[t16k])
                    yield
                    P.op("pe", lambda e: e.matmul(Y[:, 0:128], lhsT=BDb[:], rhs=t16_[:], start=True, stop=True), reads=["BDb", t16k], writes=[Yk])
                    yield
                    P.op("dve", lambda e: e.tensor_tensor(out=bon[p][:], in0=Y[:, 0:128], in1=vf[:], op=ALU.mult), reads=[Yk, vk_], writes=[f"bon{p}"])
                    P.op("dve", lambda e: e.tensor_scalar(out=bon[p][:], in0=bon[p][:], scalar1=colap(C_LB + p), scalar2=None, op0=ALU.add),
                         reads=[f"bon{p}", "cols"], writes=[f"bon{p}"])
                    yield
                    P.op("act", lambda e: e.activation(out=v16_[:], in_=vf[:], func=AF.Copy), reads=[vk_], writes=[v16k])
                    yield
                    pT = X[:, 0:256].bitcast(BF16)

                    def tr3(e):
                        e.transpose(out=pT[:, 0:128], in_=v16_[:], identity=ident[:])
                        e.transpose(out=pT[:, 128:256], in_=BT[p][:], identity=ident[:])
                        return e.transpose(out=pT[:, 256:384], in_=KT[p][:], identity=ident[:])

                    P.op("pe", tr3, reads=[v16k, f"BT{p}", f"KT{p}", "ident"], writes=[Xk])
                    yield
                    P.op("act", lambda e: e.activation(out=TOK[p][:].rearrange("p a t -> p (a t)"), in_=pT[:, 0:384], func=AF.Copy),
                         reads=[Xk], writes=[f"TOK{p}"])
                    yield

                    def amm(e):
                        ins = None
                        for h, bank in ((0, X), (1, Y)):
                            hr = slice(64 * h, 64 * h + 64)
                            e.matmul(bank[:, 0:256], lhsT=BT[p][hr, :], rhs=AR[p][hr, :, :].rearrange("p a t -> p (a t)"), start=True, stop=True)
                            ins = e.matmul(bank[:, 256:512], lhsT=KT[p][hr, :], rhs=AR[p][hr, :, :].rearrange("p a t -> p (a t)"),
                                           start=True, stop=True)
                        return ins

                    P.op("pe", amm, reads=[f"BT{p}", f"KT{p}", f"AR{p}"], writes=[Xk, Yk])
                    yield
                    P.op("dve", lambda e: e.tensor_tensor(out=AM[p][:, 0, :], in0=X[:, 0:512], in1=maskA[:, 0, :], op=ALU.mult),
                         reads=[Xk, "maskA"], writes=[f"AM{p}"])
                    P.op("dve", lambda e: e.tensor_tensor(out=AM[p][:, 1, :], in0=Y[:, 0:512], in1=maskA[:, 1, :], op=ALU.mult),
                         reads=[Yk, "maskA", f"AM{p}"], writes=[f"AM{p}"])
                    yield
                    pTL = Y[:, 0:128].bitcast(BF16)

                    def lmm(e):
                        e.transpose(out=pTL[:, 0:128], in_=AM[p][:, 0, 0:128], identity=ident[:])
                        return e.transpose(out=pTL[:, 128:256], in_=AM[p][:, 1, 0:128], identity=ident[:])

                    P.op("pe", lmm, reads=[f"AM{p}", "ident"], writes=[Yk])
                    for h in range(2):
                        P.op("pool", lambda e, h=h: e.tensor_tensor(out=MT[p][0][:, h, 128:256], in0=AM[p][:, h, 0:128], in1=ident[:], op=ALU.add),
                             reads=[f"AM{p}", "ident", f"MT{p}_0"], writes=[f"MT{p}_0"])
                    yield
                    P.op("dve", lambda e: e.tensor_copy(out=L0[p][:].rearrange("p h c -> p (h c)"), in_=pTL), reads=[Yk], writes=[f"L0{p}"])
                    yield
                    Xm = X[:, 0:512].rearrange("p (h c) -> p h c", h=2)
                    Yl = Y[:, 0:256].rearrange("p (h c) -> p h c", h=2)

                    def r1(e):
                        ins = None
                        for h in range(2):
                            e.matmul(X[:, h * 256:h * 256 + 128], lhsT=L0[p][:, h, :], rhs=AM[p][:, h, 0:128], start=True, stop=True)
                            ins = e.matmul(Y[:, h * 128:(h + 1) * 128], lhsT=AM[p][:, h, 0:128], rhs=L0[p][:, h, :], start=True, stop=True)
                        return ins

                    P.op("pe", r1, reads=[f"L0{p}", f"AM{p}"], writes=[Xk, Yk])
                    yield
                    P.op("act", lambda e: e.activation(out=MT[p][0][:, :, 0:128], in_=Xm[:, :, 0:128], func=AF.Copy),
                         reads=[Xk, f"MT{p}_0"], writes=[f"MT{p}_0"])
                    P.op("dve", lambda e: e.tensor_copy(out=LK[p][0][:], in_=Yl), reads=[Yk], writes=[f"LK{p}_0"])
                    yield
                    for rnd in range(2, 8):
                        src = rnd % 2
                        dst = 1 - src
                        last = (rnd == 7)
                        mts, mtd, lks, lkd = MT[p][src], MT[p][dst], LK[p][src], LK[p][dst]

                        def rk(e, mts=mts, lks=lks, last=last):
                            ins = None
                            for h in range(2):
                                if last:
                                    e.matmul(X[:, h * 256 + 128:h * 256 + 256], lhsT=lks[:, h, :], rhs=mts[:, h, 128:256], start=True, stop=False)
                                    ins = e.matmul(X[:, h * 256 + 128:h * 256 + 256], lhsT=ident[:], rhs=mts[:, h, 128:256], start=False, stop=True)
                                else:
                                    e.matmul(X[:, h * 256:h * 256 + 256], lhsT=lks[:, h, :], rhs=mts[:, h, :], start=True, stop=False)
                                    e.matmul(X[:, h * 256 + 128:h * 256 + 256], lhsT=ident[:], rhs=mts[:, h, 128:256], start=False, stop=True)
                            if not last:
                                for h in range(2):
                                    ins = e.matmul(Y[:, h * 128:(h + 1) * 128], lhsT=mts[:, h, 0:128], rhs=lks[:, h, :], start=True, stop=True)
                            return ins

                        P.op("pe", rk, reads=[f"MT{p}_{src}", f"LK{p}_{src}", "ident"], writes=[Xk] if last else [Xk, Yk])
                        yield
                        if last:
                            P.op("act", lambda e, mtd=mtd: e.activation(out=mtd[:, :, 128:256], in_=Xm[:, :, 128:256], func=AF.Copy),
                                 reads=[Xk, f"MT{p}_{dst}"], writes=[f"MT{p}_{dst}"])
                        else:
                            P.op("act", lambda e, mtd=mtd: e.activation(out=mtd[:], in_=Xm, func=AF.Copy), reads=[Xk], writes=[f"MT{p}_{dst}"])
                            P.op("dve", lambda e, lkd=lkd: e.tensor_copy(out=lkd[:], in_=Yl), reads=[Yk], writes=[f"LK{p}_{dst}"])
                        yield
                    P.op("pool", lambda e: e.tensor_copy(out=DC[:, p:p + 1], in_=Dt[:, p, 127:128]), reads=["Dt", "DC"], writes=["DC"])
                    yield


                def tail_pair(p):
                    bank, bk = serial_ps[p], serial_key[p]
                    yTp, ycp, ysp, yrp = yT[:, p, :], yc[:, p * 128:(p + 1) * 128], ysq[:, p * 128:(p + 1) * 128], yrs[:, p * 128:(p + 1) * 128]

                    def xmm(e):
                        e.matmul(bank[:, 0:64], lhsT=AM[p][:, 0, 256:384], rhs=TOK[p][:, 0, 0:64], start=True, stop=False)
                        e.matmul(bank[:, 64:128], lhsT=AM[p][:, 1, 256:384], rhs=TOK[p][:, 0, 64:128], start=False, stop=False)
                        return e.matmul(bank[:, 0:128], lhsT=AR[p][:, 0, :], rhs=Sb[p][:], start=False, stop=True)

                    P.op("pe", xmm, reads=[f"AM{p}", f"TOK{p}", f"AR{p}", f"Sb{p}"], writes=[bk])
                    yield
                    P.op("act", lambda e: e.activation(out=Xb[p][:], in_=bank[:, 0:128], func=AF.Copy), reads=[bk], writes=[f"Xb{p}"])
                    yield

                    def umm(e):
                        e.matmul(bank[:, 128:192], lhsT=MT[p][0][:, 0, 128:256], rhs=Xb[p][:, 0:64], start=True, stop=True)
                        return e.matmul(bank[:, 192:256], lhsT=MT[p][0][:, 1, 128:256], rhs=Xb[p][:, 64:128], start=True, stop=True)

                    P.op("pe", umm, reads=[f"MT{p}_0", f"Xb{p}"], writes=[bk])
                    yield
                    P.op("dve", lambda e: e.tensor_copy(out=Ub[p][:], in_=bank[:, 128:256]), reads=[bk], writes=[f"Ub{p}"])
                    yield

                    def ymm(e):
                        e.matmul(bank[:, 256:384], lhsT=Sb[p][:], rhs=AR[p][:, 1, :], start=True, stop=False)
                        ins = None
                        for h in range(2):
                            hs = slice(64 * h, 64 * h + 64)
                            e.matmul(bank[hs, 256:384], lhsT=Ub[p][:, hs], rhs=AM[p][:, h, 128:256], start=False, stop=False)
                            ins = e.matmul(bank[hs, 256:384], lhsT=TOK[p][:, 0, hs], rhs=AM[p][:, h, 384:512], start=False, stop=True)
                        return ins

                    P.op("pe", ymm, reads=[f"Sb{p}", f"AR{p}", f"Ub{p}", f"AM{p}", f"TOK{p}"], writes=[bk])

                    def smm(e):
                        e.matmul(bank[:, 384:512], lhsT=TOK[p][:, 2, :], rhs=TOK[p][:, 0, :], start=True, stop=False)
                        return e.matmul(bank[:, 384:512], lhsT=TOK[p][:, 1, :], rhs=Ub[p][:], start=False, stop=True)

                    P.op("pe", smm, reads=[f"TOK{p}", f"Ub{p}"], writes=[bk])
                    yield
                    P.op("act", lambda e: e.activation(out=yTp, in_=bank[:, 256:384], func=AF.Copy), reads=[bk], writes=[f"yT{p}"])
                    P.op("dve", lambda e: e.tensor_tensor(out=tSp[p][:], in0=bank[:, 384:512], in1=Sw[p][:], op=ALU.add),
                         reads=[bk, f"Sw{p}"], writes=[f"tS{p}"])
                    yield
                    P.op("dve", lambda e: e.scalar_tensor_tensor(out=Sw[p][:], in0=tSp[p][:], scalar=DC[:, p:p + 1], in1=BDf[:], op0=ALU.mult, op1=ALU.mult),
                         reads=[f"tS{p}", f"Sw{p}", "DC", "BDf"], writes=[f"Sw{p}"])
                    P.op("pe", lambda e: e.matmul(bank[:, 0:128], lhsT=BD64[:], rhs=yTp, start=True, stop=True), reads=["BD64", f"yT{p}"], writes=[bk])
                    yield
                    P.op("act", lambda e: e.activation(out=Sb[p][:], in_=Sw[p][:], func=AF.Copy), reads=[f"Sw{p}"], writes=[f"Sb{p}"])
                    P.op("dve", lambda e: e.tensor_tensor(out=ycp, in0=yTp, in1=bank[:, 0:128], op=ALU.subtract), reads=[f"yT{p}", bk], writes=[f"yc{p}"])
                    yield
                    P.op("act", lambda e: e.activation(out=ysp, in_=ycp, func=AF.Square), reads=[f"yc{p}"], writes=[f"ysq{p}"])
                    yield
                    P.op("pe", lambda e: e.matmul(bank[:, 0:128], lhsT=BD64[:], rhs=ysp, start=True, stop=True), reads=["BD64", f"ysq{p}"], writes=[bk])
                    yield
                    P.op("act", lambda e: e.activation(out=yrp, in_=bank[:, 0:128], func=AF.Sqrt, bias=LNX_EPS), reads=[bk], writes=[f"yrs{p}"])
                    yield
                    P.op("dve", lambda e: e.reciprocal(out=yrp, in_=yrp), reads=[f"yrs{p}"], writes=[f"yrs{p}"])
                    yield
                    P.op("pool", lambda e: e.tensor_tensor(out=ycp, in0=ycp, in1=yrp, op=ALU.mult), reads=[f"yc{p}", f"yrs{p}"], writes=[f"yc{p}"])
                    yield
                    P.op("dve", lambda e: e.scalar_tensor_tensor(out=y3p[p][:], in0=ycp, scalar=colap(C_LG + p), in1=bon[p][:], op0=ALU.mult, op1=ALU.add),
                         reads=[f"yc{p}", "cols", f"bon{p}"], writes=[f"y3{p}"])
                    yield
                    P.op("pool", lambda e: e.tensor_tensor(out=ygT[:, p, :], in0=y3p[p][:], in1=gT[p][:], op=ALU.mult),
                         reads=[f"y3{p}", f"gT{p}"], writes=[f"{ygk}_{p}"])
                    yield

                def tail():
                    gens = [tail_pair(p) for p in range(4)]
                    while gens:
                        for g in list(gens):
                            try:
                                next(g)
                            except StopIteration:
                                gens.remove(g)
                        yield
                    P.dma("sp", lambda e: e.dma_start(out=yg_d[tl], in_=ygT[:].rearrange("p a t -> p (a t)")),
                          reads=[f"{ygk}_{p}" for p in range(4)], writes=["yg_d"] + [f"{ygk}_{p}" for p in range(4)])
                    yield

                return front, pair_gen, tail

            class KeyProxy:
                PAT = re.compile(r'^(AM|AR|TOK|MT|bon|gT)\d')

                def __init__(self, par):
                    self.par = par

                def km(self, k):
                    if k in ('Dt', 'Dinv', 'Dprev', 'txw', 'sxg', 'DC'):
                        return f'{k}@{self.par}'
                    return k

                def op(self, eng, fn, reads=(), writes=()):
                    return P.op(eng, fn, [self.km(k) for k in reads], [self.km(k) for k in writes])

                def dma(self, q, fn, reads=(), writes=()):
                    return P.dma(q, fn, [self.km(k) for k in reads], [self.km(k) for k in writes])

            def run_rr(gens):
                gens = list(gens)
                while gens:
                    for g in list(gens):
                        try:
                            next(g)
                        except StopIteration:
                            gens.remove(g)

            parts = [tile_parts(tl, KeyProxy(tl % 2)) for tl in range(NT)]
            run_rr([parts[0][0]()])
            for tl in range(NT):
                run_rr([parts[tl][1](p) for p in range(4)])
                gens = [parts[tl][2]()]
                if tl + 1 < NT:
                    gens.append(parts[tl + 1][0]())
                run_rr(gens)

            P.barrier()

        with ExitStack() as st:
            P.scope = "M2"
            w_inB = sbuf(st, "w_inB", [128, 8, 3072], BF16)
            wsTf = sbuf(st, "wsTf", [128, 4, 128], F32)
            wsTb = sbuf(st, "wsTb", [128, 4, 128], BF16)
            bsrow = sbuf(st, "bsrow", [1, 512], BF16)
            lrow = sbuf(st, "lrow", [1, 2, 512], F32)
            lnvg = sbuf(st, "lnvg", [128, 512], F32)
            lnvb = sbuf(st, "lnvb", [128, 512], F32)
            for j in range(3):
                for k in range(8):
                    P.dma("pool", lambda e, k=k, j=j: e.dma_start(out=w_inB[:, k, j * 1024:(j + 1) * 1024],
                                                                  in_=w_in_d[k * 128:(k + 1) * 128, A_COLS + j * 1024:A_COLS + (j + 1) * 1024]),
                          writes=[f"w_inB{j}"])
            P.dma("sp", lambda e: e.dma_start(out=wsTf[:], in_=wsT_d.rearrange("g s t -> s g t")), writes=["wsTf"])
            P.dma("pool", lambda e: e.dma_start(out=bsrow[:], in_=bs_d), writes=["bsrow"])
            P.dma("sp", lambda e: e.dma_start(out=lrow[:, 0, :], in_=lnvg_d), writes=["lrow"])
            P.dma("sp", lambda e: e.dma_start(out=lrow[:, 1, :], in_=lnvb_d), reads=["lrow"], writes=["lrow"])
            for g in range(4):
                P.op("dve", lambda e, g=g: e.tensor_tensor(out=wsTb[:, g, :], in0=wsTf[:, g, :], in1=mIU[:], op=ALU.mult),
                     reads=["wsTf", "mIU", "wsTb"], writes=["wsTb"])
            for i, (dst, dk) in enumerate([(lnvg, "lnvg"), (lnvb, "lnvb")]):
                P.op("pe", lambda e, i=i: e.matmul(psM[0][:, 0:512], lhsT=onesf[0:1, :], rhs=lrow[0:1, i, :], start=True, stop=True),
                     reads=["onesf", "lrow"], writes=["psM0"])
                P.op("act", lambda e, dst=dst: e.activation(out=dst[:], in_=psM[0][:, 0:512], func=AF.Copy), reads=["psM0"], writes=[dk])

            TB = min(4, NT)
            BN = TB * 128
            NBLK = NT // TB
            hTB = [sbuf(st, f"hTB{i}", [128, 8, BN], BF16) for i in range(2)]
            ygB = [sbuf(st, f"ygB{i}", [128, 4, BN], BF16) for i in range(2)]
            uT = sbuf(st, "uTB", [128, 4, BN], BF16)
            mixT = sbuf(st, "mixTB", [128, 4, BN], BF16)
            t1 = sbuf(st, "t1B", [128, 8, BN], BF16)
            zT = sbuf(st, "zTB", [128, 8, BN], BF16)
            gsT = [sbuf(st, f"gsT{i}", [128, BN], BF16) for i in range(2)]
            t2T = [sbuf(st, f"t2T{i}", [128, BN], BF16) for i in range(2)]
            xinL = [sbuf(st, f"xinb{i}", [128, D], F32) for i in range(4)]
            vgL = [sbuf(st, f"vg{i}", [128, 512], F32) for i in range(2)]
            vlnL = [sbuf(st, f"vln{i}", [128, 512], BF16) for i in range(2)]
            xsbL = [sbuf(st, f"xsb2{i}", [128, D], BF16) for i in range(2)]
            h2tL = [sbuf(st, f"h2t{i}", [128, 8, 128], BF16) for i in range(4)]
            sm = [{n: sbuf(st, f"{n}{i}", [128, w], F32) for n, w in (("bst", 6), ("mv", 2), ("lsd", 1), ("lrs", 1), ("lnm", 1), ("ss2", 1), ("sd2", 1), ("rs2", 1))}
                  for i in range(2)]
            RING = [(psM[0], "psM0"), (psM[1], "psM1"), (psS, "psS"), (psD[:, 0:512], "psD0"), (psD[:, 512:1024], "psD1"), (psD[:, 1024:1536], "psD2"),
                    (psA[:, 0:512], "psA"), (psA[:, 512:1024], "psA2")]
            ring_i = [0]

            def ring():
                r = RING[ring_i[0] % len(RING)]
                ring_i[0] += 1
                return r

            wrb = sbuf(st, "wrb", [128, 8, 36], BF16)
            brrow = sbuf(st, "brrow", [1, 36], BF16)
            P.dma("pool", lambda e: e.dma_start(out=wrb[:], in_=wr_d.rearrange("(k p) n -> p k n", p=128)), writes=["wrb"])
            P.dma("pool", lambda e: e.dma_start(out=brrow[:], in_=br_d), writes=["brrow"])
            NRS = 4
            rsc = []
            for i in range(NRS):
                d = {"Lg": sbuf(st, f"Lg{i}", [128, 36], F32), "c": sbuf(st, f"rc{i}", [128, 10], F32), "ohg": sbuf(st, f"ohg{i}", [128, 4], F32),
                     "ex4": sbuf(st, f"ex4{i}", [128, 4], F32), "esel": sbuf(st, f"esel{i}", [128, 8], F32), "e2": sbuf(st, f"e2{i}", [128, 8], F32),
                     "mk1": sbuf(st, f"mk1{i}", [128, 8], F32), "mk2": sbuf(st, f"mk2{i}", [128, 8], F32), "wg8": sbuf(st, f"wg8{i}", [128, 8], F32)}
                rsc.append(d)

            def router_rest(tl):
                sl = tl % NRS
                d = rsc[sl]
                Lg, ohg, ex4, esel, e2, mk1, mk2, wg8 = d["Lg"], d["ohg"], d["ex4"], d["esel"], d["e2"], d["mk1"], d["mk2"], d["wg8"]
                cc = d["c"]
                gmax, ngmax, se, gp, m1, m2, dd, w1, w2 = [cc[:, i:i + 1] for i in range(9)]
                rk = f"rt{sl}"
                steps = [
                    ("dve", lambda e: e.tensor_reduce(out=gmax, in_=Lg[:, 0:4], axis=AX.X, op=ALU.max)),
                    ("dve", lambda e: e.tensor_scalar(out=ohg[:], in0=Lg[:, 0:4], scalar1=gmax, scalar2=None, op0=ALU.is_ge)),
                    ("dve", lambda e: e.tensor_scalar(out=ngmax, in0=gmax, scalar1=-1.0, scalar2=None, op0=ALU.mult)),
                    ("act", lambda e: e.activation(out=ex4[:], in_=Lg[:, 0:4], func=AF.Exp, bias=ngmax)),
                    ("dve", lambda e: e.tensor_reduce(out=se, in_=ex4[:], axis=AX.X, op=ALU.add)),
                    ("dve", lambda e: e.reciprocal(out=gp, in_=se)),
                    ("dve", lambda e: e.tensor_scalar(out=esel[:], in0=Lg[:, 4:12], scalar1=ohg[:, 0:1], scalar2=None, op0=ALU.mult)),
                ]
                for g in range(1, 4):
                    steps.append(("dve", lambda e, g=g: e.scalar_tensor_tensor(out=esel[:], in0=Lg[:, 4 + 8 * g:12 + 8 * g], scalar=ohg[:, g:g + 1],
                                                                               in1=esel[:], op0=ALU.mult, op1=ALU.add)))
                steps += [
                    ("dve", lambda e: e.tensor_reduce(out=m1, in_=esel[:], axis=AX.X, op=ALU.max)),
                    ("dve", lambda e: e.tensor_scalar(out=mk1[:], in0=esel[:], scalar1=m1, scalar2=None, op0=ALU.is_ge)),
                    ("dve", lambda e: e.scalar_tensor_tensor(out=e2[:], in0=mk1[:], scalar=-1e30, in1=esel[:], op0=ALU.mult, op1=ALU.add)),
                    ("dve", lambda e: e.tensor_reduce(out=m2, in_=e2[:], axis=AX.X, op=ALU.max)),
                    ("dve", lambda e: e.tensor_scalar(out=mk2[:], in0=e2[:], scalar1=m2, scalar2=None, op0=ALU.is_ge)),
                    ("dve", lambda e: e.tensor_tensor(out=dd, in0=m2, in1=m1, op=ALU.subtract)),
                    ("act", lambda e: e.activation(out=w2, in_=dd, func=AF.Sigmoid)),
                    ("act", lambda e: e.activation(out=w1, in_=dd, func=AF.Sigmoid, scale=-1.0)),
                    ("dve", lambda e: e.tensor_tensor(out=w1, in0=w1, in1=gp, op=ALU.mult)),
                    ("dve", lambda e: e.tensor_tensor(out=w2, in0=w2, in1=gp, op=ALU.mult)),
                    ("dve", lambda e: e.tensor_scalar(out=wg8[:], in0=mk1[:], scalar1=w1, scalar2=None, op0=ALU.mult)),
                    ("dve", lambda e: e.scalar_tensor_tensor(out=wg8[:], in0=mk2[:], scalar=w2, in1=wg8[:], op0=ALU.mult, op1=ALU.add)),
                ]
                for eng, fn in steps:
                    P.op(eng, fn, reads=[rk], writes=[rk])
                    yield
                for g in range(4):
                    P.op("dve", lambda e, g=g: e.tensor_scalar(out=comb[:, tl, g * 8:(g + 1) * 8], in0=wg8[:], scalar1=ohg[:, g:g + 1],
                                                               scalar2=None, op0=ALU.mult), reads=[rk, f"comb{tl}"], writes=[f"comb{tl}"])
                yield


            def run_rr2(gens):
                gens = list(gens)
                while gens:
                    for g in list(gens):
                        try:
                            next(g)
                        except StopIteration:
                            gens.remove(g)

            def load_block(b):
                hB, yB = hTB[b % 2], ygB[b % 2]
                for i in range(TB):
                    tl = b * TB + i
                    P.dma("sp", lambda e, tl=tl, i=i: e.dma_start(out=hB[:, :, i * 128:(i + 1) * 128], in_=hT_d[tl].rearrange("p (k t) -> p k t", k=8)),
                          reads=["hT_d"], writes=[f"hTB{b % 2}"])
                    P.dma("sp", lambda e, tl=tl, i=i: e.dma_start(out=yB[:, :, i * 128:(i + 1) * 128], in_=yg_d[tl].rearrange("p (a t) -> p a t", a=4)),
                          reads=["yg_d"], writes=[f"ygB{b % 2}"])

            pending = []

            def drain(nsteps):
                for _ in range(nsteps):
                    for g in list(pending):
                        try:
                            next(g)
                        except StopIteration:
                            pending.remove(g)

            def make_block(b):
                hB, yB, hk, yk = hTB[b % 2], ygB[b % 2], f"hTB{b % 2}", f"ygB{b % 2}"

                def U(c):
                    bank, bk = ring()

                    def umm2(e, c=c, bank=bank):
                        ins = None
                        for k in range(8):
                            ins = e.matmul(bank[:, 0:BN], lhsT=w_inB[:, k, c * 128:(c + 1) * 128], rhs=hB[:, k, :], start=(k == 0), stop=(k == 7))
                        return ins

                    P.op("pe", umm2, reads=["w_inB0", hk], writes=[bk])
                    P.op("act", lambda e, c=c, bank=bank: e.activation(out=uT[:, c, :], in_=bank[:, 0:BN], func=AF.Gelu), reads=[bk, "uTB"], writes=["uTB"])

                def g1(m):
                    bank, bk = ring()
                    c0 = 1024 + m * 128

                    def gm(e):
                        ins = None
                        for k in range(8):
                            ins = e.matmul(bank[:, 0:BN], lhsT=w_inB[:, k, c0:c0 + 128], rhs=hB[:, k, :], start=(k == 0), stop=(k == 7))
                        return ins

                    gs = gsT[m % 2]
                    P.op("pe", gm, reads=["w_inB1", hk], writes=[bk])
                    P.op("act", lambda e: e.activation(out=gs[:], in_=bank[:, 0:BN], func=AF.Sigmoid, bias=colap(C_BG + m)), reads=[bk, "cols"], writes=[f"gsT{m % 2}"])
                    bank2, bk2 = ring()

                    def ym(e):
                        ins = None
                        for c in range(4):
                            ins = e.matmul(bank2[:, 0:BN], lhsT=woA[:, c, m * 128:(m + 1) * 128], rhs=yB[:, c, :], start=(c == 0), stop=(c == 3))
                        return ins

                    P.op("pe", ym, reads=["woA", yk], writes=[bk2])
                    P.op("dve", lambda e: e.tensor_tensor(out=t1[:, m, :], in0=bank2[:, 0:BN], in1=gs[:], op=ALU.mult), reads=[bk2, f"gsT{m % 2}", "t1B"], writes=["t1B"])

                def V(i):
                    tl = b * TB + i
                    ts_ = slice(i * 128, (i + 1) * 128)
                    vg, vln, d_ = vgL[i % 2], vlnL[i % 2], sm[i % 2]
                    q = i % 2
                    bank, bk = ring()

                    def vmm(e, bank=bank, ts_=ts_):
                        ins = None
                        for k in range(8):
                            ins = e.matmul(bank[:, 0:512], lhsT=hB[:, k, ts_], rhs=w_inB[:, k, 512:1024], start=(k == 0), stop=(k == 7))
                        return ins

                    P.op("pe", vmm, reads=["w_inB0", hk], writes=[bk])
                    P.op("act", lambda e, bank=bank, vg=vg: e.activation(out=vg[:], in_=bank[:, 0:512], func=AF.Gelu), reads=[bk], writes=[f"vg{q}"])
                    P.op("dve", lambda e, vg=vg, d_=d_: e.bn_stats(out=d_["bst"][:], in_=vg[:]), reads=[f"vg{q}"], writes=[f"sm{q}"])
                    P.op("dve", lambda e, d_=d_: e.bn_aggr(out=d_["mv"][:], in_=d_["bst"][:]), reads=[f"sm{q}"], writes=[f"sm{q}"])
                    P.op("act", lambda e, d_=d_: e.activation(out=d_["lsd"][:], in_=d_["mv"][:, 1:2], func=AF.Sqrt, bias=LN_EPS), reads=[f"sm{q}"], writes=[f"sm{q}"])
                    P.op("dve", lambda e, d_=d_: e.reciprocal(out=d_["lrs"][:], in_=d_["lsd"][:]), reads=[f"sm{q}"], writes=[f"sm{q}"])
                    P.op("dve", lambda e, d_=d_: e.tensor_scalar(out=d_["lnm"][:], in0=d_["mv"][:, 0:1], scalar1=-1.0, scalar2=d_["lrs"][:], op0=ALU.mult, op1=ALU.mult),
                         reads=[f"sm{q}"], writes=[f"sm{q}"])
                    P.op("dve", lambda e, vg=vg, d_=d_: e.tensor_scalar(out=vg[:], in0=vg[:], scalar1=d_["lrs"][:], scalar2=d_["lnm"][:], op0=ALU.mult, op1=ALU.add),
                         reads=[f"vg{q}", f"sm{q}"], writes=[f"vg{q}"])
                    P.op("pool", lambda e, vg=vg: e.tensor_tensor(out=vg[:], in0=vg[:], in1=lnvg[:], op=ALU.mult), reads=[f"vg{q}", "lnvg"], writes=[f"vg{q}"])
                    P.op("pool", lambda e, vg=vg, vln=vln: e.tensor_tensor(out=vln[:], in0=vg[:], in1=lnvb[:], op=ALU.add), reads=[f"vg{q}", "lnvb"], writes=[f"vln{q}"])

                def SV(i):
                    ts_ = slice(i * 128, (i + 1) * 128)
                    vln = vlnL[i % 2]
                    q = i % 2
                    bank, bk = ring()

                    def svmm(e, bank=bank, vln=vln):
                        ins = None
                        for g in range(4):
                            e.matmul(bank[:, g * 128:(g + 1) * 128], lhsT=vln[:, g * 128:(g + 1) * 128], rhs=wsTb[:, g, :], start=True, stop=False)
                            ins = e.matmul(bank[:, g * 128:(g + 1) * 128], lhsT=onesb[0:1, :], rhs=bsrow[0:1, g * 128:(g + 1) * 128], start=False, stop=True)
                        return ins

                    P.op("pe", svmm, reads=[f"vln{q}", "wsTb", "onesb", "bsrow"], writes=[bk])
                    P.op("dve", lambda e, bank=bank, ts_=ts_: e.tensor_tensor(out=mixT[:, :, ts_], in0=bank[:, 0:512].rearrange("p (g t) -> p g t", g=4),
                                                                           in1=uT[:, :, ts_], op=ALU.mult), reads=[bk, "uTB", "mixTB"], writes=["mixTB"])
                def G2(m):
                    bank, bk = ring()
                    c0 = 2048 + m * 128

                    def gm(e, bank=bank, c0=c0):
                        ins = None
                        for k in range(8):
                            ins = e.matmul(bank[:, 0:BN], lhsT=w_inB[:, k, c0:c0 + 128], rhs=hB[:, k, :], start=(k == 0), stop=(k == 7))
                        return ins

                    gs, t2 = gsT[m % 2], t2T[m % 2]
                    P.op("pe", gm, reads=["w_inB2", hk], writes=[bk])
                    P.op("act", lambda e, bank=bank, gs=gs, m=m: e.activation(out=gs[:], in_=bank[:, 0:BN], func=AF.Sigmoid, bias=colap(C_BG + 8 + m)),
                         reads=[bk, "cols"], writes=[f"gsT{m % 2}"])
                    bank2, bk2 = ring()

                    def ym(e, bank2=bank2, m=m):
                        ins = None
                        for c in range(4):
                            ins = e.matmul(bank2[:, 0:BN], lhsT=woB[:, c, m * 128:(m + 1) * 128], rhs=mixT[:, c, :], start=(c == 0), stop=(c == 3))
                        return ins

                    P.op("pe", ym, reads=["woB", "mixTB"], writes=[bk2])
                    P.op("dve", lambda e, bank2=bank2, gs=gs, t2=t2: e.tensor_tensor(out=t2[:], in0=bank2[:, 0:BN], in1=gs[:], op=ALU.mult),
                         reads=[bk2, f"gsT{m % 2}"], writes=[f"t2T{m % 2}"])
                    P.op("pool", lambda e, m=m, t2=t2: e.tensor_tensor(out=zT[:, m, :], in0=t1[:, m, :], in1=t2[:], op=ALU.add),
                         reads=["t1B", f"t2T{m % 2}", "zTB"], writes=["zTB"])

                def O(i):
                    tl = b * TB + i
                    ts_ = slice(i * 128, (i + 1) * 128)
                    q = i % 2
                    xb_, xsb, d_ = xinL[i], xsbL[q], sm[q]
                    for hf in range(2):
                        bank, bk = ring()

                        def omm(e, bank=bank, hf=hf, ts_=ts_):
                            ins = None
                            for m in range(8):
                                ins = e.matmul(bank[:, 0:512], lhsT=zT[:, m, ts_], rhs=wout[:, m, hf * 512:(hf + 1) * 512], start=(m == 0), stop=(m == 7))
                            return ins

                        P.op("pe", omm, reads=["zTB", "wout"], writes=[bk])
                        P.op("dve", lambda e, bank=bank, hf=hf, xb_=xb_: e.tensor_tensor(out=xb_[:, hf * 512:(hf + 1) * 512], in0=bank[:, 0:512],
                                                                                     in1=xb_[:, hf * 512:(hf + 1) * 512], op=ALU.add),
                             reads=[bk, f"xinb{i}"], writes=[f"xinb{i}"])
                    P.dma("sp", lambda e, xb_=xb_, tl=tl: e.dma_start(out=x1_d[tl * 128:(tl + 1) * 128, :], in_=xb_[:]), reads=[f"xinb{i}"], writes=["x1_d"])
                    P.op("act", lambda e, xb_=xb_, xsb=xsb, d_=d_: e.activation(out=xsb[:], in_=xb_[:], func=AF.Square, accum_out=d_["ss2"][:]),
                         reads=[f"xinb{i}"], writes=[f"xsb2{q}", f"sm{q}"])
                    P.op("act", lambda e, d_=d_: e.activation(out=d_["sd2"][:], in_=d_["ss2"][:], func=AF.Sqrt, bias=NORM_EPS, scale=1.0 / D), reads=[f"sm{q}"], writes=[f"sm{q}"])
                    P.op("dve", lambda e, d_=d_: e.reciprocal(out=d_["rs2"][:], in_=d_["sd2"][:]), reads=[f"sm{q}"], writes=[f"sm{q}"])
                    P.op("dve", lambda e, xb_=xb_, xsb=xsb, d_=d_: e.tensor_scalar(out=xsb[:], in0=xb_[:], scalar1=d_["rs2"][:], scalar2=None, op0=ALU.mult),
                         reads=[f"xinb{i}", f"sm{q}", f"xsb2{q}"], writes=[f"xsb2{q}"])

                def TR(i):
                    tl = b * TB + i
                    tc = slice(tl * 128, (tl + 1) * 128)
                    q = i % 2
                    xsb, h2t = xsbL[q], h2tL[i]
                    bank, bk = ring()
                    pT = bank[:].bitcast(BF16)

                    def tr(e, pT=pT, xsb=xsb):
                        ins = None
                        for k in range(8):
                            ins = e.transpose(out=pT[:, k * 128:(k + 1) * 128], in_=xsb[:, k * 128:(k + 1) * 128], identity=ident[:])
                        return ins

                    P.op("pe", tr, reads=[f"xsb2{q}", "ident"], writes=[bk])
                    P.op("dve", lambda e, pT=pT, h2t=h2t: e.tensor_tensor(out=h2t[:], in0=pT.rearrange("p (k t) -> p k t", k=8), in1=g2bc[:], op=ALU.mult),
                         reads=[bk, "g2bc"], writes=[f"h2t{i}"])
                    P.dma("sp", lambda e, h2t=h2t, tc=tc: e.dma_start(out=h2_d[:, :, tc], in_=h2t[:]), reads=[f"h2t{i}"], writes=["h2_d"])

                def RM(i):
                    h2t = h2tL[i]
                    bank, bk = ring()
                    Lg = rsc[i % NRS]["Lg"]

                    def rmm(e, bank=bank, h2t=h2t):
                        for k in range(8):
                            e.matmul(bank[:, 0:36], lhsT=h2t[:, k, :], rhs=wrb[:, k, :], start=(k == 0), stop=False)
                        return e.matmul(bank[:, 0:36], lhsT=onesb[0:1, :], rhs=brrow[0:1, :], start=False, stop=True)

                    P.op("pe", rmm, reads=[f"h2t{i}", "wrb", "onesb", "brrow"], writes=[bk])
                    P.op("dve", lambda e, bank=bank, Lg=Lg: e.tensor_copy(out=Lg[:], in_=bank[:, 0:36]), reads=[bk, f"rt{i % NRS}"], writes=[f"rt{i % NRS}"])
                def xload(i):
                    P.dma("sp", lambda e: e.dma_start(out=xinL[i][:], in_=x_d[(b * TB + i) * 128:(b * TB + i + 1) * 128, :]), writes=[f"xinb{i}"])

                return dict(U=U, g1=g1, V=V, SV=SV, G2=G2, O=O, TR=TR, RM=RM, xload=xload)

            BL = [make_block(b) for b in range(NBLK)]

            def phase1_items(b, with_x):
                B = BL[b]
                items = []
                first = []
                if with_x:
                    first += [lambda i=i: B["xload"](i) for i in range(TB)]
                first += [lambda c=c: B["U"](c) for c in range(4)]
                items.append(first)
                for i in range(TB):
                    it = [lambda i=i: B["V"](i)]
                    for m in (2 * i, 2 * i + 1):
                        if m < 8:
                            it.append(lambda m=m: B["g1"](m))
                    if i >= 1:
                        it.append(lambda i=i: B["SV"](i - 1))
                    items.append(it)
                last = [lambda m=m: B["g1"](m) for m in range(2 * TB, 8)]
                last.append(lambda: B["SV"](TB - 1))
                items.append(last)
                return items

            def phase3_items(b, nxt):
                B = BL[b]
                items = []
                for i in range(TB + 2):
                    it = []
                    if i < TB:
                        it.append(lambda i=i: B["O"](i))
                        if nxt is not None:
                            it.append(lambda i=i: BL[nxt]["xload"](i))
                    if 1 <= i <= TB:
                        it.append(lambda i=i: B["TR"](i - 1))
                    if 2 <= i <= TB + 1:
                        it.append(lambda i=i: B["RM"](i - 2))
                    items.append(it)
                return items

            def run_items(*lists):
                n = max(len(l) for l in lists)
                for j in range(n):
                    for l in lists:
                        if j < len(l):
                            for f in l[j]:
                                f()

            load_block(0)
            run_items(phase1_items(0, True))
            for b in range(NBLK):
                if b + 1 < NBLK:
                    load_block(b + 1)
                for m in range(8):
                    BL[b]["G2"](m)
                    drain(4)
                drain(1000)
                if b + 1 < NBLK:
                    run_items(phase3_items(b, b + 1), phase1_items(b + 1, False))
                else:
                    run_items(phase3_items(b, None))
                pending.extend(router_rest(b * TB + i) for i in range(TB))
            drain(1000)

            P.barrier()
        stP.close()

        with ExitStack() as st:
            P.scope = "router"
            h2 = sbuf(st, "h2", [128, 8, T], BF16)
            yacc = sbuf(st, "yacc", [128, NT, D], F32)
            fgrow = sbuf(st, "fgrow", [1, D], F32)
            fgbc = sbuf(st, "fgbc", [128, D], F32)
            P.dma("sp", lambda e: e.dma_start(out=h2[:], in_=h2_d), reads=["h2_d"], writes=["h2"])
            for tl in range(NT):
                P.dma("sp", lambda e, tl=tl: e.dma_start(out=yacc[:, tl, :], in_=x1_d[tl * 128:(tl + 1) * 128, :]), reads=["x1_d"], writes=[f"yacc{tl}"])
            P.dma("sp", lambda e: e.dma_start(out=fgrow[:], in_=fg_d), writes=["fgrow"])
            for hf in range(2):
                P.op("pe", lambda e, hf=hf: e.matmul(psM[0][:, 0:512], lhsT=onesf[0:1, :], rhs=fgrow[0:1, hf * 512:(hf + 1) * 512], start=True, stop=True),
                     reads=["onesf", "fgrow"], writes=["psM0"])
                P.op("act", lambda e, hf=hf: e.activation(out=fgbc[:, hf * 512:(hf + 1) * 512], in_=psM[0][:, 0:512], func=AF.Copy),
                     reads=["psM0", "fgbc"], writes=["fgbc"])

            P.scope = "experts"
            Wg = [sbuf(st, f"Wg{i}", [128, 8, 256], BF16) for i in range(2)]
            Wu = [sbuf(st, f"Wu{i}", [128, 8, 256], BF16) for i in range(2)]
            Wd = [sbuf(st, f"Wd{i}", [128, 2, D], BF16) for i in range(2)]
            GN_ = min(512, T)
            NG = T // GN_
            sg = [sbuf(st, f"sg{i}", [128, GN_], F32) for i in range(2)]
            actT = sbuf(st, "actT", [128, 2, GN_], BF16)
            gu_ps = [psM[0], psM[1], psS, psD[:, 0:512]]
            gu_k = ["psM0", "psM1", "psS", "psD0"]
            d_ps = [psA, psD[:, 512:1536]]
            d_k = [["psA", "psA2"], ["psD1", "psD2"]]
            actT2 = [actT, sbuf(st, "actTb", [128, 2, GN_], BF16)]
            TPG = GN_ // 128

            def load_expert(ex):
                b = ex % 2
                P.dma("pool", lambda e: e.dma_start(out=Wg[b][:], in_=weg_d[ex].rearrange("(k p) n -> p k n", p=128)), writes=[f"Wg{b}"])
                P.dma("pool", lambda e: e.dma_start(out=Wu[b][:], in_=weu_d[ex].rearrange("(k p) n -> p k n", p=128)), writes=[f"Wu{b}"])
                P.dma("pool", lambda e: e.dma_start(out=Wd[b][:], in_=wed_d[ex].rearrange("(k p) n -> p k n", p=128)), writes=[f"Wd{b}"])

            def G(u, f):
                ex, gi = divmod(u, NG)
                b = ex % 2
                gc = slice(gi * GN_, (gi + 1) * GN_)
                aT = actT2[u % 2]
                ak = f"actT{u % 2}_{f}"

                def gumm(e):
                    ins = None
                    for k in range(8):
                        e.matmul(gu_ps[2 * f][:, 0:GN_], lhsT=Wg[b][:, k, f * 128:(f + 1) * 128], rhs=h2[:, k, gc], start=(k == 0), stop=(k == 7))
                    for k in range(8):
                        ins = e.matmul(gu_ps[2 * f + 1][:, 0:GN_], lhsT=Wu[b][:, k, f * 128:(f + 1) * 128], rhs=h2[:, k, gc], start=(k == 0), stop=(k == 7))
                    return ins

                P.op("pe", gumm, reads=[f"Wg{b}", f"Wu{b}", "h2"], writes=[gu_k[2 * f], gu_k[2 * f + 1]])
                P.op("act", lambda e: e.activation(out=sg[f][:], in_=gu_ps[2 * f][:, 0:GN_], func=AF.Silu), reads=[gu_k[2 * f]], writes=[f"sg{f}"])
                P.op("dve", lambda e: e.tensor_tensor(out=aT[:, f, :], in0=gu_ps[2 * f + 1][:, 0:GN_], in1=sg[f][:], op=ALU.mult),
                     reads=[gu_k[2 * f + 1], f"sg{f}"], writes=[ak])

            dstate = [0]

            def Dn(u, ti):
                ex, gi = divmod(u, NG)
                b = ex % 2
                tl = gi * TPG + ti
                aT = actT2[u % 2]
                dps = d_ps[dstate[0] % 2]
                dk = d_k[dstate[0] % 2]
                dstate[0] += 1

                def dmm(e):
                    ins = None
                    for hf in range(2):
                        for f in range(2):
                            ins = e.matmul(dps[:, hf * 512:(hf + 1) * 512], lhsT=aT[:, f, ti * 128:(ti + 1) * 128],
                                           rhs=Wd[b][:, f, hf * 512:(hf + 1) * 512], start=(f == 0), stop=(f == 1))
                    return ins

                P.op("pe", dmm, reads=[f"actT{u % 2}_0", f"actT{u % 2}_1", f"Wd{b}"], writes=dk)
                P.op("dve", lambda e: e.scalar_tensor_tensor(out=yacc[:, tl, :], in0=dps[:, 0:1024], scalar=comb[:, tl, ex:ex + 1],
                                                             in1=yacc[:, tl, :], op0=ALU.mult, op1=ALU.add),
                     reads=dk + [f"comb{tl}", f"yacc{tl}"], writes=[f"yacc{tl}"])

            junk = sbuf(st, "junk3", [128, D], BF16)
            ss = sbuf(st, "ss3", [128, 1], F32)
            sd = sbuf(st, "sd3", [128, 1], F32)
            rs = sbuf(st, "rs3", [128, 1], F32)
            ob = [sbuf(st, f"ob{i}", [128, D], F32) for i in range(2)]

            def final_tile(tl):
                o = ob[tl % 2]
                ok = f"ob{tl % 2}"
                P.op("act", lambda e: e.activation(out=junk[:], in_=yacc[:, tl, :], func=AF.Square, accum_out=ss[:]),
                     reads=[f"yacc{tl}"], writes=["junk3", "ss3"])
                P.op("act", lambda e: e.activation(out=sd[:], in_=ss[:], func=AF.Sqrt, bias=NORM_EPS, scale=1.0 / D), reads=["ss3"], writes=["sd3"])
                P.op("dve", lambda e: e.reciprocal(out=rs[:], in_=sd[:]), reads=["sd3"], writes=["rs3"])
                P.op("dve", lambda e: e.scalar_tensor_tensor(out=o[:], in0=yacc[:, tl, :], scalar=rs[:], in1=fgbc[:], op0=ALU.mult, op1=ALU.mult),
                     reads=[f"yacc{tl}", "rs3", "fgbc"], writes=[ok])
                P.dma("sp", lambda e: e.dma_start(out=out_d[tl * 128:(tl + 1) * 128, :], in_=o[:]), reads=[ok], writes=["out"])

            NU = NE * NG
            load_expert(0)
            if NE > 1:
                load_expert(1)
            G(0, 0)
            G(0, 1)
            for u in range(NU):
                ex, gi = divmod(u, NG)
                P.scope = "experts" if ex != 5 else "ex5"
                half = (TPG + 1) // 2
                if u + 1 < NU:
                    G(u + 1, 0)
                for ti in range(0, half):
                    Dn(u, ti)
                    if ex == NE - 1:
                        final_tile(gi * TPG + ti)
                if u + 1 < NU:
                    G(u + 1, 1)
                for ti in range(half, TPG):
                    Dn(u, ti)
                    if ex == NE - 1:
                        final_tile(gi * TPG + ti)
                if gi == NG - 1 and ex + 2 < NE:
                    load_expert(ex + 2)

            P.final_wait("sp")
            P.emit()
    return nc


def host_layout(inp, b, T):
    f = lambda a: np.ascontiguousarray(np.asarray(a, dtype=np.float32))

    def colpack(v, n):
        v = np.asarray(v, np.float32).reshape(-1)
        pad = np.zeros(n * 128, np.float32)
        pad[:v.size] = v
        return pad.reshape(n, 128).T

    cols = np.concatenate([
        colpack(inp["tmix_mu"][0], 15), colpack(inp["k_k"][0], 4), colpack(inp["k_a"][0], 4), colpack(inp["r_k"][0], 4),
        colpack(inp["a0"][0], 4), colpack(inp["lnx_g"][0], 4), colpack(inp["lnx_b"][0], 4), colpack(inp["b_gate"][0], 16),
        colpack(inp["norm1_g"][0], 8), colpack(inp["norm2_g"][0], 8)], axis=1)
    w_re = np.asarray(inp["w_re"][0], np.float32)
    w_r = np.concatenate([np.asarray(inp["w_rg"][0], np.float32), w_re.transpose(1, 0, 2).reshape(D, 32)], axis=1)
    b_r = np.concatenate([np.asarray(inp["b_rg"][0], np.float32).reshape(-1), np.asarray(inp["b_re"][0], np.float32).reshape(-1)])
    return {
        "x": f(inp["x"][b, :T]), "w_in": f(inp["w_in"][0]), "cols": f(cols), "w0": f(inp["w0"][0]).reshape(1, 512),
        "w2": f(inp["w2"][0]), "a2": f(inp["a2"][0]), "g2": f(inp["g2"][0]), "w_oA": f(inp["w_oA"][0]), "w_oB": f(inp["w_oB"][0]),
        "w_out": f(inp["w_out"][0]), "lnv_g": f(inp["lnv_g"][0]).reshape(1, 512), "lnv_b": f(inp["lnv_b"][0]).reshape(1, 512),
        "wsT": f(np.asarray(inp["w_s"][0], np.float32).transpose(0, 2, 1)), "b_s": f(inp["b_s"][0]).reshape(1, 512),
        "w_r": f(w_r), "b_r": f(b_r).reshape(1, 36), "w_e_gate": f(inp["w_e_gate"][0]), "w_e_up": f(inp["w_e_up"][0]),
        "w_e_down": f(inp["w_e_down"][0]), "final_g": f(inp["final_g"]).reshape(1, D),
    }


def kernel(**inputs):
    T = 2048
    nc = build(T)
    in_maps = [host_layout(inputs, b, T) for b in range(8)]
    res = run_bass_kernel_spmd(nc, in_maps, core_ids=list(range(8)))
    return np.stack([np.asarray(r["out"], dtype=np.float32) for r in res.results], axis=0)
```
